# Optimizing a Trainium2 kernel written in Bass

```python
import math
import functools
import jax
import jax.numpy as jnp
from jax import lax
import numpy as np

D_MODEL = 1024
BATCH = 8
SEQ = 2048
DEPTH = 2

GRID_W = 64
CTX_LEN = 256
HEAD_DIM = 64
ROPE_BASE = 10000.0
EPS = 1e-6

NA_HEADS = 8
NA_WIN_ROWS = 8
NA_WIN_COLS = 16
NA_QCOLS = 16
NA_KCOLS = 32
NA_WIDTH = NA_HEADS * HEAD_DIM

SSM_HEADS = 16
SSM_HEADDIM = 64
SSM_D_INNER = SSM_HEADS * SSM_HEADDIM
SSM_STATE = 128
SSM_GROUPS = 2
SSM_CONV = 5
SSM_CONV_CH = SSM_D_INNER + 2 * SSM_GROUPS * SSM_STATE
SCAN_CHUNK = 128

SWA_HEADS = 8
SWA_KV_HEADS = 2
SWA_WINDOW = 128
SWA_BLOCK = 128
SWA_Q = SWA_HEADS * HEAD_DIM
SWA_KV = SWA_KV_HEADS * HEAD_DIM

RET_HEADS = 8
RET_QK_DIM = 64
RET_V_DIM = 128
RET_QK = RET_HEADS * RET_QK_DIM
RET_V = RET_HEADS * RET_V_DIM

EVEN_IN = 3 * NA_WIDTH + SSM_D_INNER + SSM_CONV_CH + 2 * SSM_HEADS
EVEN_OUT = NA_WIDTH + SSM_D_INNER
ODD_IN = SWA_Q + 2 * SWA_KV + 2 * RET_QK + 2 * RET_V
ODD_OUT = SWA_Q + RET_V

D_FF = 2816
N_EXPERTS = 8
TOP_K = 2
D_FF_EXPERT = 3584

kernel_name = "hybrid_diffusion_prefix_backbone"


def split_cols(t, sizes):
    return jnp.split(t, np.cumsum(sizes)[:-1].tolist(), axis=-1)


def to_heads(t, n_heads):
    return t.reshape(t.shape[:-1] + (n_heads, t.shape[-1] // n_heads))


def rmsnorm(x, g):
    xf = x.astype(jnp.float32)
    y = xf * lax.rsqrt(jnp.mean(jnp.square(xf), axis=-1, keepdims=True) + EPS)
    return (y * g.astype(jnp.float32)).astype(x.dtype)


def modulate(h, shift, scale):
    return h * (1 + scale) + shift


def axial_rope(n_tokens, dim):
    t = jnp.arange(n_tokens)
    row = (t // GRID_W).astype(jnp.float32)
    col = (t % GRID_W).astype(jnp.float32)
    n_freq = dim // 4
    inv = ROPE_BASE ** (-jnp.arange(n_freq, dtype=jnp.float32) / n_freq)
    ang = jnp.concatenate([row[:, None] * inv, col[:, None] * inv], axis=-1)
    return jnp.cos(ang)[:, None, :], jnp.sin(ang)[:, None, :]


def apply_rope(x, cos, sin):
    x1, x2 = jnp.split(x.astype(jnp.float32), 2, axis=-1)
    return jnp.concatenate([x1 * cos - x2 * sin, x2 * cos + x1 * sin], axis=-1).astype(x.dtype)


def centred_depthwise_conv(x, w, bias):
    pad = (w.shape[0] - 1) // 2
    y = lax.conv_general_dilated(x, w, window_strides=(1,), padding=[(pad, pad)],
                                 dimension_numbers=('NWC', 'WIO', 'NWC'),
                                 feature_group_count=x.shape[-1])
    return y + bias


def chunked_scan(x, a, Bm, Cm, h0, need_y):
    b, L, H, P = x.shape
    G, N = Bm.shape[2], Bm.shape[3]
    hg = H // G
    Q = SCAN_CHUNK
    nc = L // Q
    xr = x.reshape(b, nc, Q, G, hg, P)
    Br = Bm.reshape(b, nc, Q, G, N)
    Cr = Cm.reshape(b, nc, Q, G, N)
    a_cum = jnp.cumsum(a.astype(jnp.float32).reshape(b, nc, Q, G, hg), axis=2)
    a_tot = a_cum[:, :, -1]
    w_state = jnp.exp(a_tot[:, :, None] - a_cum).astype(x.dtype)
    states = jnp.einsum('bcjgn,bcjgh,bcjghp->bcghpn', Br, w_state, xr)

    def step(h, inp):
        decay, s = inp
        return jnp.exp(decay)[..., None, None].astype(h.dtype) * h + s, h

    h_final, h_prev = lax.scan(step, h0.reshape(b, G, hg, P, N),
                               (jnp.moveaxis(a_tot, 1, 0), jnp.moveaxis(states, 1, 0)))
    h_final = h_final.reshape(b, H, P, N)
    if not need_y:
        return None, h_final
    h_prev = jnp.moveaxis(h_prev, 0, 1)
    diff = a_cum[:, :, :, None] - a_cum[:, :, None, :]
    causal = np.tril(np.ones((Q, Q), dtype=bool))[:, :, None, None]
    decay_mat = jnp.exp(jnp.where(causal, diff, -jnp.inf)).astype(x.dtype)
    cb = jnp.einsum('bcign,bcjgn->bcijg', Cr, Br)
    y_diag = jnp.einsum('bcijg,bcijgh,bcjghp->bcighp', cb, decay_mat, xr)
    y_off = jnp.einsum('bcign,bcghpn,bcigh->bcighp', Cr, h_prev, jnp.exp(a_cum).astype(x.dtype))
    return (y_diag + y_off).reshape(b, L, H, P), h_final


def scan_from_context(xc, ac, Bc, Cc, xl, al, Bl, Cl, reverse, need_ctx):
    if reverse:
        xc, ac, Bc, Cc, xl, al, Bl, Cl = [jnp.flip(t, axis=1) for t in (xc, ac, Bc, Cc, xl, al, Bl, Cl)]
    b, _, H, P = xc.shape
    h0 = jnp.zeros((b, H, P, Bc.shape[-1]), xc.dtype)
    yc, hc = chunked_scan(xc, ac, Bc, Cc, h0, need_ctx)
    yl, _ = chunked_scan(xl, al, Bl, Cl, hc, True)
    if reverse:
        yl = jnp.flip(yl, axis=1)
        yc = jnp.flip(yc, axis=1) if need_ctx else None
    return yc, yl


def context_attention(q, k, v, sink):
    b, lc, H, D = q.shape
    kvh = k.shape[2]
    g = H // kvh
    s = jnp.einsum('bqkgd,bskd->bkgqs', q.reshape(b, lc, kvh, g, D), k) * D ** -0.5
    if sink is not None:
        s_sink = jnp.broadcast_to(sink.reshape(1, kvh, g, 1, 1).astype(s.dtype), (b, kvh, g, lc, 1))
        s = jnp.concatenate([s, s_sink], axis=-1)
    p = jax.nn.softmax(s.astype(jnp.float32), axis=-1).astype(v.dtype)
    o = jnp.einsum('bkgqs,bskd->bqkgd', p[..., :lc], v)
    return o.reshape(b, lc, H * D)


def neighbourhood_attention(q, k, v, k_ctx, v_ctx, rpb):
    b, L, H, D = q.shape
    rows = L // GRID_W
    kr = min(NA_WIN_ROWS, rows)
    ncb = GRID_W // NA_QCOLS
    scale = D ** -0.5
    qcol = np.arange(GRID_W).reshape(ncb, NA_QCOLS)
    kstart = np.clip(np.arange(ncb) * NA_QCOLS - NA_WIN_COLS // 2, 0, GRID_W - NA_KCOLS)
    kcol = kstart[:, None] + np.arange(NA_KCOLS)[None, :]
    c0 = np.clip(qcol - NA_WIN_COLS // 2, 0, GRID_W - NA_WIN_COLS)
    col_ok = (kcol[:, None, :] >= c0[:, :, None]) & (kcol[:, None, :] < c0[:, :, None] + NA_WIN_COLS)
    col_idx = np.clip(kcol[:, None, :] - qcol[:, :, None] + NA_WIN_COLS - 1, 0, 2 * NA_WIN_COLS - 2)
    qg = q.reshape(b, rows, ncb, NA_QCOLS, H, D)
    kg = k.reshape(b, rows, GRID_W, H, D)
    vg = v.reshape(b, rows, GRID_W, H, D)
    n_nb = kr * NA_KCOLS

    def one_row(r):
        r0 = jnp.clip(r - kr // 2, 0, rows - kr)
        q_r = lax.dynamic_index_in_dim(qg, r, axis=1, keepdims=False)
        k_blk = lax.dynamic_slice_in_dim(kg, r0, kr, axis=1)[:, :, kcol]
        v_blk = lax.dynamic_slice_in_dim(vg, r0, kr, axis=1)[:, :, kcol]
        drow = r0 + jnp.arange(kr) - r + NA_WIN_ROWS - 1
        bias = jnp.moveaxis(rpb[:, drow[:, None, None, None], col_idx[None]], 1, 3)
        s_nb = jnp.einsum('bjqhd,brjkhd->bhjqrk', q_r, k_blk) * scale + bias[None]
        s_nb = jnp.where(col_ok[:, :, None, :], s_nb, -jnp.inf)
        s_ctx = jnp.einsum('bjqhd,bkhd->bhjqk', q_r, k_ctx) * scale
        s = jnp.concatenate([s_nb.reshape(b, H, ncb, NA_QCOLS, n_nb), s_ctx.astype(s_nb.dtype)], axis=-1)
        p = jax.nn.softmax(s.astype(jnp.float32), axis=-1).astype(v.dtype)
        p_nb = p[..., :n_nb].reshape(b, H, ncb, NA_QCOLS, kr, NA_KCOLS)
        o = (jnp.einsum('bhjqrk,brjkhd->bjqhd', p_nb, v_blk)
             + jnp.einsum('bhjqk,bkhd->bjqhd', p[..., n_nb:], v_ctx))
        return o.reshape(b, GRID_W, H * D)

    out = lax.map(one_row, jnp.arange(rows))
    return jnp.moveaxis(out, 0, 1).reshape(b, L, H * D)


def sliding_window_attention(q, k, v, k_ctx, v_ctx, sink):
    b, L, H, D = q.shape
    kvh = k.shape[2]
    g = H // kvh
    blk = SWA_BLOCK
    nb = L // blk
    lc = k_ctx.shape[1]
    nw = 3 * blk
    scale = D ** -0.5
    qb = q.reshape(b, nb, blk, kvh, g, D)
    pad = ((0, 0), (blk, blk), (0, 0), (0, 0))
    kp = jnp.pad(k, pad).reshape(b, nb + 2, blk, kvh, D)
    vp = jnp.pad(v, pad).reshape(b, nb + 2, blk, kvh, D)
    k_win = jnp.concatenate([kp[:, :-2], kp[:, 1:-1], kp[:, 2:]], axis=2)
    v_win = jnp.concatenate([vp[:, :-2], vp[:, 1:-1], vp[:, 2:]], axis=2)
    qpos = np.arange(L).reshape(nb, blk)
    kpos = np.arange(nb)[:, None] * blk - blk + np.arange(nw)[None, :]
    ok = ((kpos[:, None, :] >= 0) & (kpos[:, None, :] < L)
          & (np.abs(kpos[:, None, :] - qpos[:, :, None]) <= SWA_WINDOW))
    s_loc = jnp.where(ok, jnp.einsum('bnqkgd,bnskd->bkgnqs', qb, k_win) * scale, -jnp.inf)
    s_ctx = jnp.einsum('bnqkgd,bskd->bkgnqs', qb, k_ctx) * scale
    s_sink = jnp.broadcast_to(sink.reshape(1, kvh, g, 1, 1, 1).astype(s_ctx.dtype), (b, kvh, g, nb, blk, 1))
    s = jnp.concatenate([s_loc, s_ctx, s_sink], axis=-1)
    p = jax.nn.softmax(s.astype(jnp.float32), axis=-1).astype(v.dtype)
    o = (jnp.einsum('bkgnqs,bnskd->bnqkgd', p[..., :nw], v_win)
         + jnp.einsum('bkgnqs,bskd->bnqkgd', p[..., nw:nw + lc], v_ctx))
    return o.reshape(b, L, H * D)


def ssm_inputs(xbc, dt_raw, conv_w, conv_b, dt_bias, a_log):
    b, L, _ = xbc.shape
    xbc = jax.nn.silu(centred_depthwise_conv(xbc, conv_w, conv_b))
    xs, Bm, Cm = split_cols(xbc, [SSM_D_INNER, SSM_GROUPS * SSM_STATE, SSM_GROUPS * SSM_STATE])
    xs = xs.reshape(b, L, SSM_HEADS, SSM_HEADDIM)
    Bm = Bm.reshape(b, L, SSM_GROUPS, SSM_STATE)
    Cm = Cm.reshape(b, L, SSM_GROUPS, SSM_STATE)
    dt = jax.nn.softplus(dt_raw.reshape(b, L, 2, SSM_HEADS) + dt_bias)
    a = dt * (-jnp.exp(a_log))
    xdt = xs[:, :, None] * dt[..., None]
    return xs, Bm, Cm, xdt, a


def ssm_output(y, xs, z, d_skip, g):
    b, L = y.shape[:2]
    y = (y + xs * d_skip[:, None]).reshape(b, L, SSM_D_INNER)
    yz = (y * jax.nn.silu(z)).reshape(b, L, SSM_GROUPS, SSM_D_INNER // SSM_GROUPS).astype(jnp.float32)
    yz = yz * lax.rsqrt(jnp.mean(jnp.square(yz), axis=-1, keepdims=True) + EPS)
    return (yz.reshape(b, L, SSM_D_INNER) * g.astype(jnp.float32)).astype(y.dtype)


def head_layernorm(y, g, bias):
    b, L, H, P = y.shape
    yf = y.astype(jnp.float32)
    mu = jnp.mean(yf, axis=-1, keepdims=True)
    var = jnp.mean(jnp.square(yf - mu), axis=-1, keepdims=True)
    yn = ((yf - mu) * lax.rsqrt(var + EPS)).reshape(b, L, H * P)
    return (yn * g.astype(jnp.float32) + bias.astype(jnp.float32)).astype(y.dtype)


def even_mixer(hc, hl, w_in, w_out, rpb, conv_w, conv_b, dt_bias, a_log, d_skip, gn_g, need_ctx):
    sizes = [NA_WIDTH, NA_WIDTH, NA_WIDTH, SSM_D_INNER, SSM_CONV_CH, 2 * SSM_HEADS]
    qc, kc, vc, zc, xbcc, dtc = split_cols(hc @ w_in, sizes)
    ql, kl, vl, zl, xbcl, dtl = split_cols(hl @ w_in, sizes)
    kc = to_heads(kc, NA_HEADS)
    vc = to_heads(vc, NA_HEADS)
    ya_l = neighbourhood_attention(to_heads(ql, NA_HEADS), to_heads(kl, NA_HEADS), to_heads(vl, NA_HEADS), kc, vc, rpb)
    xs_c, Bc, Cc, xdt_c, a_c = ssm_inputs(xbcc, dtc, conv_w, conv_b, dt_bias, a_log)
    xs_l, Bl, Cl, xdt_l, a_l = ssm_inputs(xbcl, dtl, conv_w, conv_b, dt_bias, a_log)
    yf_c, yf_l = scan_from_context(xdt_c[:, :, 0], a_c[:, :, 0], Bc, Cc,
                                   xdt_l[:, :, 0], a_l[:, :, 0], Bl, Cl, False, need_ctx)
    yb_c, yb_l = scan_from_context(xdt_c[:, :, 1], a_c[:, :, 1], Bc, Cc,
                                   xdt_l[:, :, 1], a_l[:, :, 1], Bl, Cl, True, need_ctx)
    ys_l = ssm_output(yf_l + yb_l, xs_l, zl, d_skip, gn_g)
    y_l = jnp.concatenate([ya_l, ys_l], axis=-1) @ w_out
    if not need_ctx:
        return None, y_l
    ya_c = context_attention(to_heads(qc, NA_HEADS), kc, vc, None)
    ys_c = ssm_output(yf_c + yb_c, xs_c, zc, d_skip, gn_g)
    y_c = jnp.concatenate([ya_c, ys_c], axis=-1) @ w_out
    return y_c, y_l


def odd_mixer(hc, hl, w_in, w_out, sink, log_decay, gn_g, gn_b, need_ctx):
    sizes = [SWA_Q, SWA_KV, SWA_KV, RET_QK, RET_QK, RET_V, RET_V]
    qc, kc, vc, rqc, rkc, rvc, rgc = split_cols(hc @ w_in, sizes)
    ql, kl, vl, rql, rkl, rvl, rgl = split_cols(hl @ w_in, sizes)
    b, L, _ = hl.shape
    lc = hc.shape[1]
    cos, sin = axial_rope(L, HEAD_DIM)
    kc = to_heads(kc, SWA_KV_HEADS)
    vc = to_heads(vc, SWA_KV_HEADS)
    yw_l = sliding_window_attention(apply_rope(to_heads(ql, SWA_HEADS), cos, sin),
                                    apply_rope(to_heads(kl, SWA_KV_HEADS), cos, sin),
                                    to_heads(vl, SWA_KV_HEADS), kc, vc, sink)
    kscale = RET_QK_DIM ** -0.5
    rq_l = apply_rope(to_heads(rql, RET_HEADS), cos, sin)
    rk_l = apply_rope(to_heads(rkl, RET_HEADS), cos, sin) * kscale
    rv_l = to_heads(rvl, RET_HEADS)
    rq_c = to_heads(rqc, RET_HEADS)
    rk_c = to_heads(rkc, RET_HEADS) * kscale
    rv_c = to_heads(rvc, RET_HEADS)
    log_gamma = jnp.log1p(-jnp.exp(log_decay.astype(jnp.float32)))
    ga_c = [jnp.broadcast_to(log_gamma[d], (b, lc, RET_HEADS)) for d in (0, 1)]
    ga_l = [jnp.broadcast_to(log_gamma[d], (b, L, RET_HEADS)) for d in (0, 1)]
    rf_c, rf_l = scan_from_context(rv_c, ga_c[0], rk_c, rq_c, rv_l, ga_l[0], rk_l, rq_l, False, need_ctx)
    rb_c, rb_l = scan_from_context(rv_c, ga_c[1], rk_c, rq_c, rv_l, ga_l[1], rk_l, rq_l, True, need_ctx)
    yr_l = jax.nn.silu(rgl) * head_layernorm(rf_l + rb_l, gn_g, gn_b)
    y_l = jnp.concatenate([yw_l, yr_l], axis=-1) @ w_out
    if not need_ctx:
        return None, y_l
    yw_c = context_attention(to_heads(qc, SWA_HEADS), kc, vc, sink)
    yr_c = jax.nn.silu(rgc) * head_layernorm(rf_c + rb_c, gn_g, gn_b)
    y_c = jnp.concatenate([yw_c, yr_c], axis=-1) @ w_out
    return y_c, y_l


def swiglu(h, w1, w3, w2):
    return (jax.nn.silu(h @ w1) * (h @ w3)) @ w2


def moe_swiglu(h, router, w1, w3, w2):
    logits = (h @ router).astype(jnp.float32)
    top_v, top_i = lax.top_k(logits, TOP_K)
    gates = jax.nn.softmax(top_v, axis=-1)
    dense_gate = jnp.sum(jax.nn.one_hot(top_i, N_EXPERTS, dtype=jnp.float32) * gates[..., None], axis=-2).astype(h.dtype)
    out = jnp.zeros_like(h)
    for e in range(N_EXPERTS):
        out = out + dense_gate[..., e:e + 1] * swiglu(h, w1[e], w3[e], w2[e])
    return out


def setup_inputs(seed: int = 0) -> dict:
    key = jax.random.key(seed)
    keys = iter(jax.random.split(key, 48))
    f32 = jnp.float32
    n_even = (DEPTH + 1) // 2
    n_odd = DEPTH // 2
    D = D_MODEL

    def normal(shape, scale):
        return jax.random.normal(next(keys), shape, f32) * scale

    def gain(shape):
        return 1.0 + normal(shape, 0.05)

    a_log = jnp.log(jax.random.uniform(next(keys), (n_even, 2, SSM_HEADS), f32, 1.0, 16.0))
    dt0 = jnp.exp(jax.random.uniform(next(keys), (n_even, 2, SSM_HEADS), f32, math.log(1e-3), math.log(1e-1)))
    dt_bias = dt0 + jnp.log(-jnp.expm1(-dt0))
    base_decay = (-5.0 - jnp.arange(RET_HEADS, dtype=f32)) * math.log(2.0)
    return {
        'x': normal((BATCH, SEQ, D), 1.0),
        'c': normal((BATCH, D), 1.0),
        'ctx': normal((BATCH, CTX_LEN, D), 1.0),
        'c_ctx': normal((D,), 1.0),
        'ada_w': normal((DEPTH, D, 6 * D), D ** -0.5),
        'ada_b': normal((DEPTH, 6 * D), 0.02),
        'norm_attn_g': gain((DEPTH, D)),
        'norm_ffn_g': gain((DEPTH, D)),
        'ev_w_in': normal((n_even, D, EVEN_IN), D ** -0.5),
        'ev_w_out': normal((n_even, EVEN_OUT, D), EVEN_OUT ** -0.5),
        'na_rpb': normal((n_even, NA_HEADS, 2 * NA_WIN_ROWS - 1, 2 * NA_WIN_COLS - 1), 0.02),
        'ssm_conv_w': normal((n_even, SSM_CONV, 1, SSM_CONV_CH), SSM_CONV ** -0.5),
        'ssm_conv_b': normal((n_even, SSM_CONV_CH), 0.02),
        'ssm_dt_bias': dt_bias,
        'ssm_a_log': a_log,
        'ssm_d': gain((n_even, SSM_HEADS)),
        'ssm_norm_g': gain((n_even, SSM_D_INNER)),
        'ffn_w1': normal((n_even, D, D_FF), D ** -0.5),
        'ffn_w3': normal((n_even, D, D_FF), D ** -0.5),
        'ffn_w2': normal((n_even, D_FF, D), D_FF ** -0.5),
        'od_w_in': normal((n_odd, D, ODD_IN), D ** -0.5),
        'od_w_out': normal((n_odd, ODD_OUT, D), ODD_OUT ** -0.5),
        'swa_sink': normal((n_odd, SWA_HEADS), 1.0),
        'ret_log_decay': base_decay + normal((n_odd, 2, RET_HEADS), 0.05),
        'ret_gn_g': gain((n_odd, RET_V)),
        'ret_gn_b': normal((n_odd, RET_V), 0.02),
        'moe_router': normal((n_odd, D, N_EXPERTS), D ** -0.5),
        'moe_w1': normal((n_odd, N_EXPERTS, D, D_FF_EXPERT), D ** -0.5),
        'moe_w3': normal((n_odd, N_EXPERTS, D, D_FF_EXPERT), D ** -0.5),
        'moe_w2': normal((n_odd, N_EXPERTS, D_FF_EXPERT, D), D_FF_EXPERT ** -0.5),
        'final_g': gain((D,)),
    }


def reference(x, c, ctx, c_ctx, ada_w, ada_b, norm_attn_g, norm_ffn_g,
              ev_w_in, ev_w_out, na_rpb, ssm_conv_w, ssm_conv_b, ssm_dt_bias, ssm_a_log, ssm_d, ssm_norm_g,
              ffn_w1, ffn_w3, ffn_w2,
              od_w_in, od_w_out, swa_sink, ret_log_decay, ret_gn_g, ret_gn_b,
              moe_router, moe_w1, moe_w3, moe_w2, final_g):
    xl, xc = x, ctx
    for l in range(DEPTH):
        i = l // 2
        need_ctx = l < DEPTH - 1
        mod_l = (jax.nn.silu(c) @ ada_w[l] + ada_b[l])[:, None, :]
        mod_c = (jax.nn.silu(c_ctx) @ ada_w[l] + ada_b[l])[None, None, :]
        sh1_l, sc1_l, g1_l, sh2_l, sc2_l, g2_l = jnp.split(mod_l, 6, axis=-1)
        sh1_c, sc1_c, g1_c, sh2_c, sc2_c, g2_c = jnp.split(mod_c, 6, axis=-1)
        hl = modulate(rmsnorm(xl, norm_attn_g[l]), sh1_l, sc1_l)
        hc = modulate(rmsnorm(xc, norm_attn_g[l]), sh1_c, sc1_c)
        if l % 2 == 0:
            yc, yl = even_mixer(hc, hl, ev_w_in[i], ev_w_out[i], na_rpb[i], ssm_conv_w[i], ssm_conv_b[i],
                                ssm_dt_bias[i], ssm_a_log[i], ssm_d[i], ssm_norm_g[i], need_ctx)
            ffn = functools.partial(swiglu, w1=ffn_w1[i], w3=ffn_w3[i], w2=ffn_w2[i])
        else:
            yc, yl = odd_mixer(hc, hl, od_w_in[i], od_w_out[i], swa_sink[i], ret_log_decay[i],
                               ret_gn_g[i], ret_gn_b[i], need_ctx)
            ffn = functools.partial(moe_swiglu, router=moe_router[i], w1=moe_w1[i], w3=moe_w3[i], w2=moe_w2[i])
        xl = xl + g1_l * yl
        xl = xl + g2_l * ffn(modulate(rmsnorm(xl, norm_ffn_g[l]), sh2_l, sc2_l))
        if need_ctx:
            xc = xc + g1_c * yc
            xc = xc + g2_c * ffn(modulate(rmsnorm(xc, norm_ffn_g[l]), sh2_c, sc2_c))
    return rmsnorm(xl, final_g)
```

```python
import math
from contextlib import ExitStack
import numpy as np
import concourse.bass as bass
import concourse.mybir as mybir
from concourse.bass_utils import run_bass_kernel_spmd

F32 = mybir.dt.float32
BF16 = mybir.dt.bfloat16
AF = mybir.ActivationFunctionType
ALU = mybir.AluOpType
AX = mybir.AxisListType

ENGS = ("pe", "act", "dve", "pool", "sp")
NDMA_SEMS = 24


class Ctx:
    def __init__(self, nc, es):
        self.nc = nc
        self.eng_sem = {e: es.enter_context(nc.semaphore("sem_" + e)) for e in ENGS if e != "sp"}
        self.eng_cnt = {e: 0 for e in self.eng_sem}
        self.dma_sems = [es.enter_context(nc.semaphore("dsem%d" % i)) for i in range(NDMA_SEMS)]
        self.dma_cnt = [0] * NDMA_SEMS
        self.dma_rr = 0
        self.eng_obj = {"pe": nc.tensor, "act": nc.scalar, "dve": nc.vector, "pool": nc.gpsimd, "sp": nc.sync}


class Phase:
    def __init__(self, ctx, name="ph"):
        self.ctx = ctx
        self.name = name
        self.ops = []
        self.state = {}
        self.rr = 0

    def _st(self, key):
        if isinstance(key, tuple):
            nm, sub = key[0], tuple(key[1:])
        else:
            nm, sub = key, ()
        d = self.state.setdefault(nm, {})
        return d, sub

    @staticmethod
    def _overlap(a, b):
        n = min(len(a), len(b))
        return a[:n] == b[:n]

    def op(self, eng, fn, reads=(), writes=(), dma=False, pe_acc=False):
        oid = len(self.ops)
        deps = set()
        for key in reads:
            d, sub = self._st(key)
            for s2, st in d.items():
                if self._overlap(sub, s2) and st["w"] is not None:
                    deps.add(st["w"])
            d.setdefault(sub, {"w": None, "r": []})["r"].append(oid)
        for key in writes:
            d, sub = self._st(key)
            for s2 in list(d.keys()):
                if self._overlap(sub, s2):
                    st = d[s2]
                    if st["w"] is not None:
                        deps.add(st["w"])
                    deps.update(st["r"])
                    if len(s2) > len(sub):
                        del d[s2]
            st = d.setdefault(sub, {"w": None, "r": []})
            st["w"] = oid
            st["r"] = []
        deps.discard(oid)
        o = {"eng": eng, "fn": fn, "deps": deps, "dma": dma, "pe_acc": pe_acc}
        if dma:
            c = self.ctx
            k = c.dma_rr
            c.dma_rr = (c.dma_rr + 1) % NDMA_SEMS
            prev = getattr(self, "_dma_prev", {}).get(k)
            if prev is not None:
                deps.add(prev)
            self.__dict__.setdefault("_dma_prev", {})[k] = oid
            c.dma_cnt[k] += 16
            o["dsem"] = k
            o["dval"] = c.dma_cnt[k]
        self.ops.append(o)
        return oid

    def pe(self, fn, reads=(), writes=(), acc=False):
        return self.op("pe", fn, reads, writes, pe_acc=acc)

    def act(self, fn, reads=(), writes=()):
        return self.op("act", fn, reads, writes)

    def dve(self, fn, reads=(), writes=()):
        return self.op("dve", fn, reads, writes)

    def pool(self, fn, reads=(), writes=()):
        return self.op("pool", fn, reads, writes)

    def any2(self, fn, reads=(), writes=()):
        self.rr += 1
        return self.op("dve" if self.rr % 2 else "pool", fn, reads, writes)

    def dma(self, q, out, in_, reads=(), writes=()):
        return self.op(q, lambda e: e.dma_start(out=out, in_=in_), reads, writes, dma=True)

    def emit(self):
        c = self.ctx
        ops = self.ops
        for o in ops:
            best = {}
            pd = []
            for d in o["deps"]:
                po = ops[d]
                if po["dma"]:
                    pd.append(d)
                    continue
                if po["eng"] == "pe" and o["eng"] == "pe" and not o["dma"]:
                    continue
                if d > best.get(po["eng"], -1):
                    best[po["eng"]] = d
            o["deps"] = set(pd) | set(best.values())
        needed = set()
        for o in ops:
            for d in o["deps"]:
                po = ops[d]
                if po["dma"]:
                    continue
                needed.add(d)
        last_of = {}
        for i, o in enumerate(ops):
            if not o["dma"]:
                last_of[o["eng"]] = i
        for i in last_of.values():
            needed.add(i)
        for i, o in enumerate(ops):
            if o["dma"]:
                continue
            if i in needed:
                c.eng_cnt[o["eng"]] += 1
                o["inc"] = True
            o["cnt"] = c.eng_cnt[o["eng"]] if i in needed else None
        per_eng = {e: [] for e in ENGS}
        waited = {e: {} for e in ENGS}
        for i, o in enumerate(ops):
            w = {}
            for d in o["deps"]:
                po = ops[d]
                if po["dma"]:
                    key = ("d", po["dsem"])
                    val = po["dval"]
                else:
                    if po["eng"] == "pe" and o["eng"] == "pe" and not o["dma"]:
                        continue
                    key = ("e", po["eng"])
                    val = po["cnt"]
                if val > w.get(key, 0):
                    w[key] = val
            wl = []
            for key, val in w.items():
                if waited[o["eng"]].get(key, 0) >= val:
                    continue
                waited[o["eng"]][key] = val
                wl.append((key, val))
            o["waits"] = wl
            per_eng[o["eng"]].append(o)
        fin = []
        for e, i in last_of.items():
            fin.append((("e", e), ops[i]["cnt"]))
        for k in range(NDMA_SEMS):
            if c.dma_cnt[k] > 0:
                fin.append((("d", k), c.dma_cnt[k]))

        def semof(key):
            return c.eng_sem[key[1]] if key[0] == "e" else c.dma_sems[key[1]]

        def run(engname):
            def body(eng):
                for o in per_eng[engname]:
                    for key, val in o["waits"]:
                        eng.wait_ge(semof(key), val)
                    ins = o["fn"](eng)
                    if o["dma"]:
                        ins.then_inc(c.dma_sems[o["dsem"]], 16)
                    elif o.get("inc"):
                        ins.then_inc(c.eng_sem[o["eng"]], 1)
                if engname == "sp":
                    for key, val in fin:
                        eng.wait_ge(semof(key), val)
            return body

        with c.nc.Block() as block:
            block.tensor(run("pe"))
            block.scalar(run("act"))
            block.vector(run("dve"))
            block.gpsimd(run("pool"))
            block.sync(run("sp"))
        self.ops = []
        self.state = {}
        self._dma_prev = {}


D = 1024
L = 2048
LC = 256
T = L + LC
NT = T // 128
KC = D // 128
EPS = 1e-6
BLKS = [(0, 512), (512, 512), (1024, 512), (1536, 512), (2048, 256)]

VR = {}
_r = 0
for _nm, _n in (("c", 8), ("c_ctx", 8), ("ada_b0", 48), ("ada_b1", 48), ("g_attn0", 8), ("g_attn1", 8),
                ("g_ffn0", 8), ("g_ffn1", 8), ("final_g", 8), ("conv_w", 60), ("conv_b", 12), ("ssm_g", 8)):
    VR[_nm] = _r
    _r += _n
NVR = _r

NPAIRS = 4
NGROUPS = 2
ATT_LAG = 2
RPERM = [0, 2, 1, 3]
NEXPERTS = 8
STOP_AFTER = 99
RUN_RET = True
RET_SCAN = True
DBG_NOPOST = False
DBG_NOP2 = False
DBG_SKIP = set()
SKIP_L0 = False


def _na_tiles():
    out = []
    for t in range(16):
        qr = np.arange(t * 128, (t + 1) * 128) // 64
        r0 = np.clip(qr - 4, 0, 24)
        out.append(list(range(int(r0.min()) // 2, (int(r0.max()) + 7) // 2 + 1)))
    return out


NA_KT = _na_tiles()


def na_bias_table(rpb):
    out = np.full((8, 16, 128, 640), -30000.0, np.float32)
    for t in range(16):
        qpos = np.arange(t * 128, (t + 1) * 128)
        qr, qc = qpos // 64, qpos % 64
        r0 = np.clip(qr - 4, 0, 24)
        c0 = np.clip(qc - 8, 0, 48)
        for j, kt in enumerate(NA_KT[t]):
            kpos = np.arange(kt * 128, (kt + 1) * 128)
            kr, kc = kpos // 64, kpos % 64
            ok = ((kr[:, None] >= r0[None, :]) & (kr[:, None] < r0[None, :] + 8)
                  & (kc[:, None] >= c0[None, :]) & (kc[:, None] < c0[None, :] + 16))
            dr = np.clip(kr[:, None] - qr[None, :] + 7, 0, 14)
            dc = np.clip(kc[:, None] - qc[None, :] + 15, 0, 30)
            vals = rpb[:, dr, dc]
            out[:, t, :, j * 128:(j + 1) * 128] = np.where(ok[None], vals, np.float32(-30000.0))
    return out


def swa_bias_table():
    out = np.full((16, 128, 384), -30000.0, np.float32)
    for t in range(16):
        kts = [kt for kt in (t - 1, t, t + 1) if 0 <= kt < 16]
        qpos = np.arange(t * 128, (t + 1) * 128)
        for j, kt in enumerate(kts):
            kpos = np.arange(kt * 128, (kt + 1) * 128)
            ok = np.abs(kpos[:, None] - qpos[None, :]) <= 128
            out[t, :, j * 128:(j + 1) * 128] = np.where(ok, np.float32(0.0), np.float32(-30000.0))
    return out


def build_program(dbg=None):
    nc = bass.Bass("TRN2", target_bir_lowering=False)
    _uid = [0]

    def SBT(name, shape, dtype):
        _uid[0] += 1
        return nc.sbuf_tensor("%s_%d" % (name, _uid[0]), shape, dtype)

    def PST(name, shape, dtype):
        _uid[0] += 1
        return nc.psum_tensor("%s_%d" % (name, _uid[0]), shape, dtype)
    dt = nc.dram_tensor
    x_d = dt("x", [L, D], F32, kind="ExternalInput").ap()
    ctx_d = dt("ctx", [LC, D], F32, kind="ExternalInput").ap()
    vecs_d = dt("vecs", [256, 128], F32, kind="ExternalInput").ap()
    cst_d = dt("cst", [128, 1024], F32, kind="ExternalInput").ap()
    ada_w_d = dt("ada_w", [2, D, 6 * D], F32, kind="ExternalInput").ap()
    out_d = dt("out", [L, D], F32, kind="ExternalOutput").ap()
    ev_w_in_d = dt("ev_w_in", [D, 4128], F32, kind="ExternalInput").ap()
    od_w_in_d = dt("od_w_in", [D, 3840], F32, kind="ExternalInput").ap()
    nab_d = dt("nab", [8, 16, 128, 640], F32, kind="ExternalInput").ap()
    swab_d = dt("swab", [16, 128, 384], F32, kind="ExternalInput").ap()
    sink_d = dt("sink", [1, 8], F32, kind="ExternalInput").ap()
    dtba_d = dt("dtba", [2, 32], F32, kind="ExternalInput").ap()
    ev_w_out_d = dt("ev_w_out", [1536, D], F32, kind="ExternalInput").ap()
    od_w_out_d = dt("od_w_out", [1536, D], F32, kind="ExternalInput").ap()
    ffn_w1_d = dt("ffn_w1", [D, 2816], F32, kind="ExternalInput").ap()
    ffn_w3_d = dt("ffn_w3", [D, 2816], F32, kind="ExternalInput").ap()
    ffn_w2_d = dt("ffn_w2", [2816, D], F32, kind="ExternalInput").ap()
    if STOP_AFTER >= 4:
        moe_w1_t = dt("moe_w1", [8, D, 3584], F32, kind="ExternalInput").ap()
        moe_w3_t = dt("moe_w3", [8, D, 3584], F32, kind="ExternalInput").ap()
        moe_w2_t = dt("moe_w2", [8, 3584, D], F32, kind="ExternalInput").ap()
        moe_w1_d = [moe_w1_t[e] for e in range(8)]
        moe_w3_d = [moe_w3_t[e] for e in range(8)]
        moe_w2_d = [moe_w2_t[e] for e in range(8)]
    sel_d = dt("sel", [8, 1024], F32, kind="ExternalInput").ap()
    rope_d = dt("rope", [2, 128, L], F32, kind="ExternalInput").ap()
    retld_d = dt("retld", [2, 8], F32, kind="ExternalInput").ap()
    retg_d = dt("retg", [1, 1024], F32, kind="ExternalInput").ap()
    retb_d = dt("retb", [1, 1024], F32, kind="ExternalInput").ap()
    router_d = dt("router", [D, 8], F32, kind="ExternalInput").ap()
    ssmd_d = dt("ssmd", [1, 16], F32, kind="ExternalInput").ap()
    ssmg_d = dt("ssmg", [1, 1024], F32, kind="ExternalInput").ap()
    dbg_d = None
    if dbg:
        dbg_d = {k: dt("dbg_" + k, list(shp), F32, kind="ExternalOutput").ap() for k, shp in dbg.items()}

    with ExitStack() as es:
        ctx = Ctx(nc, es)
        sb = lambda name, shape, dtype: es.enter_context(SBT(name, shape, dtype))
        XT = sb("XT", [128, KC, T], F32)
        HT = sb("HT", [128, KC, T], BF16)
        VT = sb("VT", [128, 256], F32)
        CF = sb("CF", [128, 1024], F32)
        CB = sb("CB", [128, 1024], BF16)
        MOD = sb("MOD", [128, 2, 2, 48], F32)
        AB = sb("AB", [128, 2, 2, 6, 8], F32)
        IDF = CF[:, 0:128]
        ONESF = CF[:, 128:256]
        IDB = CB[:, 0:128]
        ONESB = CB[:, 128:256]

        with ExitStack() as ps_es:
            ph = Phase(ctx, "p0")
            PS = [ps_es.enter_context(PST("ps%d" % i, [128, 512], F32)) for i in range(8)]
            XIN = [ps_es.enter_context(SBT("xin%d" % i, [128, D], F32)) for i in range(3)]
            VIN = ps_es.enter_context(SBT("vin", [128, 2, 128], F32))
            SC = ps_es.enter_context(SBT("sc", [128, KC, 2], BF16))
            AW = [ps_es.enter_context(SBT("aw%d" % i, [128, KC, 768], BF16)) for i in range(2)]
            ph.dma("sp", CF[:], cst_d[:, :], writes=["CF"])
            ph.dma("pool", CB[:], cst_d[:, :], writes=["CB"])
            ph.dma("sp", VIN[:], vecs_d.rearrange("(a p) n -> p a n", p=128), writes=["VIN"])
            for a in range(2):
                ph.pe(lambda e, a=a: e.transpose(PS[0][:, a * 128:(a + 1) * 128], VIN[:, a, :], IDF),
                      reads=["CF", "VIN"], writes=[("ps", 0)])
            ph.dve(lambda e: e.tensor_copy(out=VT[:], in_=PS[0][:, 0:256]), reads=[("ps", 0)], writes=["VT"])
            pi = 1
            for t in range(NT):
                xin = XIN[t % 3]
                src = x_d[t * 128:(t + 1) * 128, :] if t < 16 else ctx_d[(t - 16) * 128:(t - 15) * 128, :]
                ph.dma("sp", xin[:], src, writes=[("xin", t % 3)])
                for half in range(2):
                    b = 1 + (pi % 7)
                    pi += 1
                    for kk in range(4):
                        k = half * 4 + kk
                        ph.pe(lambda e, b=b, kk=kk, k=k, xin=xin: e.transpose(
                            PS[b][:, kk * 128:(kk + 1) * 128], xin[:, k * 128:(k + 1) * 128], IDF),
                            reads=["CF", ("xin", t % 3)], writes=[("ps", b)])
                    dst = XT[:, half * 4:half * 4 + 4, t * 128:(t + 1) * 128]
                    srcp = PS[b][:].rearrange("p (a n) -> p a n", a=4)
                    if (t + half) % 2 == 0:
                        ph.dve(lambda e, dst=dst, srcp=srcp: e.tensor_copy(out=dst, in_=srcp),
                               reads=[("ps", b)], writes=[("XT", t)])
                    else:
                        ph.act(lambda e, dst=dst, srcp=srcp: e.copy(out=dst, in_=srcp),
                               reads=[("ps", b)], writes=[("XT", t)])
            for j, nm in enumerate(("c", "c_ctx")):
                ph.act(lambda e, j=j, nm=nm: e.activation(out=SC[:, :, j], in_=VT[:, VR[nm]:VR[nm] + 8], func=AF.Silu),
                       reads=["VT"], writes=["SC"])
            si = 0
            for l in range(2):
                for s in range(8):
                    aw = AW[si % 2]
                    ph.dma("pool", aw[:], ada_w_d[l, :, s * 768:(s + 1) * 768].rearrange("(k p) n -> p k n", p=128),
                           writes=[("aw", si % 2)])
                    for c6 in range(6):
                        cc = s * 6 + c6
                        for k in range(KC):
                            ph.pe(lambda e, l=l, cc=cc, k=k, c6=c6, aw=aw: e.matmul(
                                PS[0][:, l * 96 + cc * 2:l * 96 + cc * 2 + 2], aw[:, k, c6 * 128:(c6 + 1) * 128],
                                SC[:, k, :], start=(k == 0), stop=(k == KC - 1)),
                                reads=[("aw", si % 2), "SC"], writes=[("ps", 0)])
                    si += 1
            for l in range(2):
                for j in range(2):
                    r0 = VR["ada_b%d" % l]
                    ph.dve(lambda e, l=l, j=j, r0=r0: e.tensor_tensor(
                        out=MOD[:, l, j, :], in0=PS[0][:, l * 96:(l + 1) * 96].rearrange("p (c j) -> p c j", j=2)[:, :, j],
                        in1=VT[:, r0:r0 + 48], op=ALU.add), reads=[("ps", 0), "VT"], writes=["MOD"])
                    for (ai, sci, gname) in ((0, 1, "g_attn%d" % l), (3, 4, "g_ffn%d" % l)):
                        g0 = VR[gname]
                        ph.dve(lambda e, l=l, j=j, ai=ai, sci=sci, g0=g0: e.scalar_tensor_tensor(
                            out=AB[:, l, j, ai, :], in0=MOD[:, l, j, sci * 8:(sci + 1) * 8], scalar=1.0,
                            in1=VT[:, g0:g0 + 8], op0=ALU.add, op1=ALU.mult), reads=["MOD", "VT"], writes=["AB"])
                    for (bi, shi) in ((1, 0), (2, 2), (4, 3), (5, 5)):
                        ph.dve(lambda e, l=l, j=j, bi=bi, shi=shi: e.tensor_copy(
                            out=AB[:, l, j, bi, :], in_=MOD[:, l, j, shi * 8:(shi + 1) * 8]), reads=["MOD"], writes=["AB"])
            ph.emit()

        def norm_mod(l, which, LG=None):
            ai, bi = (0, 1) if which == 1 else (3, 4)
            with ExitStack() as pes:
                ph = Phase(ctx, "nm")
                PS = [pes.enter_context(PST("nps%d" % i, [128, 512], F32)) for i in range(4)]
                SQ = [pes.enter_context(SBT("sq%d" % i, [128, 512], BF16)) for i in range(3)]
                RS = [pes.enter_context(SBT("rs%d" % i, [128, 512], F32)) for i in range(2)]
                TMP = [pes.enter_context(SBT("tmp%d" % i, [128, 512], F32)) for i in range(3)]
                if LG is not None:
                    PSR = [pes.enter_context(PST("npr%d" % i, [128, 512], F32)) for i in range(2)]
                    H2F = pes.enter_context(SBT("h2f", [128, KC, 512], F32))
                    RWF = pes.enter_context(SBT("rwf", [128, KC, 8], F32))
                    ph.dma("sp", RWF[:], router_d.rearrange("(k p) n -> p k n", p=128), writes=["rwf"])
                qi = 0
                ti = 0
                for bi_, (t0, tn) in enumerate(BLKS):
                    j = 0 if t0 < L else 1
                    pb = bi_ % 4
                    for k in range(KC):
                        sq = SQ[qi % 3]
                        ph.act(lambda e, sq=sq, k=k, t0=t0, tn=tn: e.activation(out=sq[:, :tn], in_=XT[:, k, t0:t0 + tn], func=AF.Square),
                               reads=[], writes=[("sq", qi % 3)])
                        ph.pe(lambda e, sq=sq, k=k, pb=pb, tn=tn: e.matmul(PS[pb][:, :tn], ONESB, sq[:, :tn], start=(k == 0), stop=(k == KC - 1)),
                              reads=[("sq", qi % 3)], writes=[("nps", pb)])
                        qi += 1
                    rs = RS[bi_ % 2]
                    ph.dve(lambda e, rs=rs, pb=pb, tn=tn: e.tensor_scalar(out=rs[:, :tn], in0=PS[pb][:, :tn], scalar1=1.0 / D, scalar2=EPS,
                                                                          op0=ALU.mult, op1=ALU.add), reads=[("nps", pb)], writes=[("rs", bi_ % 2)])
                    ph.act(lambda e, rs=rs, tn=tn: e.sqrt(out=rs[:, :tn], in_=rs[:, :tn]),
                           reads=[("rs", bi_ % 2)], writes=[("rs", bi_ % 2)])
                    ph.dve(lambda e, rs=rs, tn=tn: e.reciprocal(out=rs[:, :tn], in_=rs[:, :tn]),
                           reads=[("rs", bi_ % 2)], writes=[("rs", bi_ % 2)])
                    for k in range(KC):
                        tmp = TMP[ti % 3]
                        ph.any2(lambda e, tmp=tmp, k=k, t0=t0, tn=tn, rs=rs: e.tensor_tensor(out=tmp[:, :tn], in0=XT[:, k, t0:t0 + tn], in1=rs[:, :tn], op=ALU.mult),
                                reads=[("rs", bi_ % 2)], writes=[("tmp", ti % 3)])
                        ph.act(lambda e, tmp=tmp, k=k, t0=t0, tn=tn, j=j: e.activation(out=HT[:, k, t0:t0 + tn], in_=tmp[:, :tn], func=AF.Identity,
                                                                                      scale=AB[:, l, j, ai, k:k + 1], bias=AB[:, l, j, bi, k:k + 1]),
                               reads=[("tmp", ti % 3)], writes=[("HT", k, bi_)])
                        if LG is not None:
                            ph.act(lambda e, tmp=tmp, k=k, tn=tn, j=j: e.activation(out=H2F[:, k, :tn], in_=tmp[:, :tn], func=AF.Identity,
                                                                                scale=AB[:, l, j, ai, k:k + 1], bias=AB[:, l, j, bi, k:k + 1]),
                                   reads=[("tmp", ti % 3)], writes=[("h2f", k)])
                        ti += 1
                    if LG is not None:
                        pr = bi_ % 2
                        for tt in range(tn // 128):
                            for k in range(KC):
                                ph.pe(lambda e, pr=pr, tt=tt, k=k: e.matmul(PSR[pr][:, tt * 8:(tt + 1) * 8], H2F[:, k, tt * 128:(tt + 1) * 128], RWF[:, k, :], start=(k == 0), stop=(k == KC - 1)),
                                      reads=["h2f", "rwf"], writes=[("npr", pr)])
                        ntl = tn // 128
                        ph.dve(lambda e, pr=pr, t0=t0, ntl=ntl: e.tensor_copy(out=LG[:, t0 // 128:t0 // 128 + ntl, :], in_=PSR[pr][:, 0:ntl * 8].rearrange("p (a n) -> p a n", a=ntl)),
                               reads=[("npr", pr)], writes=[("LG", t0)])
                ph.emit()

        YCAT_d = nc.dram_tensor("ycat_scr", [12, 128, T], BF16, kind="Internal").ap()

        def dump(name, src_ap, shape2, reads=()):
            if not dbg_d or name not in dbg_d:
                return
            with ExitStack() as des:
                ph = Phase(ctx, "dump")
                f = des.enter_context(SBT("dbgf_" + name, list(shape2), F32))
                ph.dve(lambda e: e.tensor_copy(out=f[:], in_=src_ap), writes=["f"])
                ph.dma("sp", dbg_d[name], f[:], reads=["f"])
                ph.emit()

        def attention_pair(l, m):
            W_d = ev_w_in_d if l == 0 else od_w_in_d
            with ExitStack() as pes:
                ph = Phase(ctx, "att")
                al = lambda name, shape, dtype: pes.enter_context(SBT(name, shape, dtype))
                PS = [pes.enter_context(PST("aps%d" % i, [128, 512], F32)) for i in range(8)]
                psi = [0]

                def nb():
                    psi[0] = (psi[0] + 1) % 8
                    return psi[0]
                WS = al("ws", [128, KC, 384], BF16)
                QT = al("qt", [128, T], BF16)
                KT = al("kt", [128, T], BF16)
                VK = al("vk", [128, NT, 128], BF16)
                OT = al("ot", [128, T], BF16)
                BI = [al("bi%d" % i, [128, 640], F32) for i in range(3)]
                SS = [al("ss%d" % i, [128, 640], F32) for i in range(2)]
                PT = [al("pt%d" % i, [128, 896], BF16) for i in range(4)]
                RD = [al("rd%d" % i, [128, 128], F32) for i in range(2)]
                Wr = W_d.rearrange("(k p) n -> p k n", p=128)
                if l == 0:
                    cols = [(m * 128, 128), (512 + m * 128, 128), (1024 + m * 128, 128)]
                else:
                    kv = m // 2
                    cols = [(m * 128, 128), (512 + kv * 64, 64), (512 + kv * 64, 64), (640 + kv * 64, 64), (640 + kv * 64, 64)]
                off = 0
                for (c0, cn) in cols:
                    ph.dma("pool", WS[:, :, off:off + cn], Wr[:, :, c0:c0 + cn], writes=[("ws", off)])
                    off += cn
                wsr = [("ws", o) for o in (0, 64, 128, 192, 256, 320)]
                nq = T if l == 0 else L
                for (dst, wo, lim, nm) in ((QT, 0, nq, "qt"), (KT, 128, T, "kt")):
                    for (t0, tn) in BLKS:
                        if t0 >= lim:
                            continue
                        b = nb()
                        for k in range(KC):
                            ph.pe(lambda e, b=b, k=k, wo=wo, t0=t0, tn=tn: e.matmul(PS[b][:, :tn], WS[:, k, wo:wo + 128], HT[:, k, t0:t0 + tn],
                                                                                 start=(k == 0), stop=(k == KC - 1)),
                                  reads=wsr + ["HT"], writes=[("ps", b)])
                        ph.act(lambda e, b=b, dst=dst, t0=t0, tn=tn: e.copy(out=dst[:, t0:t0 + tn], in_=PS[b][:, :tn]),
                               reads=[("ps", b)], writes=[(nm, t0)])
                for t4 in range(0, NT, 4):
                    b = nb()
                    n4 = min(4, NT - t4)
                    for tt in range(n4):
                        t = t4 + tt
                        for k in range(KC):
                            ph.pe(lambda e, b=b, k=k, t=t, tt=tt: e.matmul(PS[b][:, tt * 128:(tt + 1) * 128], HT[:, k, t * 128:(t + 1) * 128], WS[:, k, 256:384],
                                                                         start=(k == 0), stop=(k == KC - 1)),
                                  reads=wsr + ["HT"], writes=[("ps", b)])
                    ph.dve(lambda e, b=b, t4=t4, n4=n4: e.tensor_copy(out=VK[:, t4:t4 + n4, :], in_=PS[b][:, :n4 * 128].rearrange("p (a n) -> p a n", a=n4)),
                           reads=[("ps", b)], writes=[("vk", t4)])
                if l == 1:
                    rope(ph, QT, "qt", nb, PS, al)
                    rope(ph, KT, "kt", nb, PS, al)
                    ES = al("es", [128, 8], F32)
                    ph.dma("sp", ES[:], sink_d[0:1, :].broadcast_to([128, 8]), writes=["es"])
                    ph.act(lambda e: e.activation(out=ES[:], in_=ES[:], func=AF.Exp), reads=["es"], writes=["es"])
                qtiles = list(range(NT)) if l == 0 else list(range(16))
                iters = [(e_, t) for e_ in range(2) for t in qtiles]
                info = {}

                def stage_a(it, e_, t):
                    r0 = 64 * e_
                    h = 2 * m + e_
                    if t >= 16:
                        kts, nbias = [16, 17], 0
                    elif l == 0:
                        kts, nbias = NA_KT[t] + [16, 17], len(NA_KT[t])
                    else:
                        kts = [kt for kt in (t - 1, t, t + 1) if 0 <= kt < 16]
                        nbias = len(kts)
                        kts = kts + [16, 17]
                    nk = len(kts)
                    info[it] = (kts, nk)
                    bi = BI[it % 3]
                    ss = SS[it % 2]
                    pt = PT[it % 4]
                    if nbias:
                        src = nab_d[h, t, :, 0:nbias * 128] if l == 0 else swab_d[t, :, 0:nbias * 128]
                        ph.dma("sp", bi[:, 0:nbias * 128], src, writes=[("bi", it % 3)])
                    banks = [nb(), nb()]
                    for j, kt in enumerate(kts):
                        b = banks[j // 4]
                        ph.pe(lambda e, b=b, j=j, kt=kt, t=t, r0=r0: e.matmul(PS[b][:, (j % 4) * 128:(j % 4 + 1) * 128], KT[r0:r0 + 64, kt * 128:(kt + 1) * 128],
                                                                          QT[r0:r0 + 64, t * 128:(t + 1) * 128], start=True, stop=True),
                              reads=["kt", "qt"], writes=[("ps", b)])
                    for bk in range(2):
                        j0, j1 = bk * 4, min(nk, bk * 4 + 4)
                        if j0 >= j1:
                            continue
                        b = banks[bk]
                        jb = min(j1, max(j0, nbias))
                        if jb > j0:
                            ph.dve(lambda e, b=b, j0=j0, jb=jb, ss=ss, bi=bi: e.scalar_tensor_tensor(
                                out=ss[:, j0 * 128:jb * 128], in0=PS[b][:, (j0 % 4) * 128:(j0 % 4) * 128 + (jb - j0) * 128], scalar=0.125,
                                in1=bi[:, j0 * 128:jb * 128], op0=ALU.mult, op1=ALU.add),
                                reads=[("ps", b), ("bi", it % 3)], writes=[("ss", it % 2, bk)])
                            ph.act(lambda e, j0=j0, jb=jb, ss=ss, pt=pt: e.activation(out=pt[:, j0 * 128:jb * 128], in_=ss[:, j0 * 128:jb * 128], func=AF.Exp),
                                   reads=[("ss", it % 2, bk)], writes=[("pt", it % 4)])
                        if j1 > jb:
                            ph.act(lambda e, b=b, jb=jb, j1=j1, pt=pt: e.activation(out=pt[:, jb * 128:j1 * 128], in_=PS[b][:, (jb % 4) * 128:(jb % 4) * 128 + (j1 - jb) * 128],
                                                                                 func=AF.Exp, scale=0.125),
                                   reads=[("ps", b)], writes=[("pt", it % 4)])

                def stage_b(it, e_, t):
                    r0 = 64 * e_
                    h = 2 * m + e_
                    kts, nk = info[it]
                    pt = PT[it % 4]
                    rd = RD[it % 2]
                    po = nb()
                    for j, kt in enumerate(kts):
                        ph.pe(lambda e, po=po, j=j, kt=kt, pt=pt, nk=nk: e.matmul(PS[po][:, 0:128], VK[:, kt, :], pt[:, j * 128:(j + 1) * 128],
                                                                              start=(j == 0), stop=(j == nk - 1)),
                              reads=["vk", ("pt", it % 4)], writes=[("ps", po)])
                    for j, kt in enumerate(kts):
                        ph.pe(lambda e, po=po, j=j, pt=pt, nk=nk: e.matmul(PS[po][:, 128:256], ONESB, pt[:, j * 128:(j + 1) * 128],
                                                                       start=(j == 0), stop=(j == nk - 1)),
                              reads=[("pt", it % 4)], writes=[("ps", po)])
                    if l == 1:
                        ph.dve(lambda e, po=po, rd=rd, r0=r0, h=h: e.tensor_scalar(out=rd[r0:r0 + 64, :], in0=PS[po][r0:r0 + 64, 128:256], scalar1=ES[r0:r0 + 64, h:h + 1],
                                                                                scalar2=None, op0=ALU.add), reads=[("ps", po), "es"], writes=[("rd", it % 2)])
                        ph.dve(lambda e, rd=rd, r0=r0: e.reciprocal(out=rd[r0:r0 + 64, :], in_=rd[r0:r0 + 64, :]), reads=[("rd", it % 2)], writes=[("rd", it % 2)])
                    else:
                        ph.dve(lambda e, po=po, rd=rd, r0=r0: e.reciprocal(out=rd[r0:r0 + 64, :], in_=PS[po][r0:r0 + 64, 128:256]),
                               reads=[("ps", po)], writes=[("rd", it % 2)])
                    ph.dve(lambda e, po=po, rd=rd, r0=r0, t=t: e.tensor_tensor(out=OT[r0:r0 + 64, t * 128:(t + 1) * 128], in0=PS[po][r0:r0 + 64, 0:128],
                                                                           in1=rd[r0:r0 + 64, :], op=ALU.mult),
                           reads=[("ps", po), ("rd", it % 2)], writes=[("ot", e_, t)])

                LAG = ATT_LAG
                for i in range(len(iters) + LAG):
                    if i < len(iters):
                        stage_a(i, *iters[i])
                    if i >= LAG:
                        stage_b(i - LAG, *iters[i - LAG])
                ph.dma("sp", YCAT_d[m, :, 0:nq], OT[:, 0:nq], reads=["ot"])
                ph.emit()
                if l == 0 and m == 0:
                    dump("OT0", OT[:], (128, T))

        MF = CF[:, 256:384]
        MB = CF[:, 384:512]
        SFm = CF[:, 512:640]
        SBm = CF[:, 640:768]
        SST_d = nc.dram_tensor("sst_scr", [2, NT, 128, 512], BF16, kind="Internal").ap()

        def scan_phase(P, groups, XS, BTOK, BT, CT, A4, DT4, const_decay, post_fn, out_tiles, pre_fn=None, H=8):
            HP = H * P
            nyb = HP // 512
            NTA = 1 if const_decay else NT
            ti = (lambda t: 0) if const_decay else (lambda t: t)
            oes = ExitStack()
            ESC = oes.enter_context(SBT("esc", [128, NTA, 2, H], F32))
            with ExitStack() as pes:
                ph = Phase(ctx, "scan")
                al = lambda name, shape, dtype: pes.enter_context(SBT(name, shape, dtype))
                PS = [pes.enter_context(PST("sps%d" % i, [128, 512], F32)) for i in range(8)]
                psi = [0]

                def nb():
                    psi[0] = (psi[0] + 1) % 8
                    return psi[0]
                if pre_fn is not None:
                    pre_fn(ph, al, PS, nb)
                CUMS = al("cums", [128, NTA, 3, 2 * H], F32)
                EW = al("ew", [128, NTA, 2, H], F32)
                ETOT = al("etot", [128, NTA, 2, H], F32)
                S = [al("st%d" % d, [128, 512], F32) for d in range(2)]
                STMP = al("stmp", [128, 512], F32)
                SBF = [al("sbf%d" % i, [128, 512], BF16) for i in range(3)]
                XW = [al("xw%d" % i, [128, HP], BF16) for i in range(2)]
                for t in range(NTA):
                    b = nb()
                    for ci, lm in enumerate((MF, MB, ONESF)):
                        ph.pe(lambda e, b=b, ci=ci, lm=lm, t=t: e.matmul(PS[b][:, ci * 2 * H:(ci + 1) * 2 * H], lm, A4[:, t].rearrange("p d h -> p (d h)"),
                                                                         start=True, stop=True), reads=["A4", "CF"], writes=[("ps", b)])
                    ph.act(lambda e, b=b, t=t: e.copy(out=CUMS[:, t].rearrange("p c n -> p (c n)"), in_=PS[b][:, 0:6 * H]), reads=[("ps", b)], writes=[("cums", t)])
                ph.act(lambda e: e.activation(out=ETOT[:].rearrange("p t d h -> p t (d h)"), in_=CUMS[:, :, 2, :], func=AF.Exp), reads=["cums"], writes=["etot"])
                for d in range(2):
                    ph.act(lambda e, d=d: e.activation(out=ESC[:, :, d, :], in_=CUMS[:, :, d, d * H:(d + 1) * H], func=AF.Exp), reads=["cums"], writes=[("esc", d)])
                    ph.dve(lambda e, d=d: e.tensor_tensor(out=EW[:, :, d, :], in0=CUMS[:, :, 2, d * H:(d + 1) * H], in1=CUMS[:, :, d, d * H:(d + 1) * H], op=ALU.subtract),
                           reads=["cums"], writes=[("ew", d)])
                    ph.act(lambda e, d=d: e.activation(out=EW[:, :, d, :], in_=EW[:, :, d, :], func=AF.Exp), reads=[("ew", d)], writes=[("ew", d)])
                    if DT4 is not None:
                        ph.dve(lambda e, d=d: e.tensor_tensor(out=EW[:, :, d, :], in0=EW[:, :, d, :], in1=DT4[:, :, d, :], op=ALU.mult),
                               reads=[("ew", d), "DT4"], writes=[("ew", d)])
                order = {0: [16, 17] + list(range(16)), 1: [17, 16] + list(range(15, -1, -1))}
                si = 0
                for d in range(2):
                    ph.dve(lambda e, d=d: e.memset(S[d][:], 0.0), writes=[("st", d)])
                for step in range(NT):
                    for d in range(2):
                        t = order[d][step]
                        sbf = SBF[si % 3]
                        xw = XW[si % 2]
                        ph.act(lambda e, sbf=sbf, d=d: e.copy(out=sbf[:], in_=S[d][:]), reads=[("st", d)], writes=[("sbf", si % 3)])
                        ph.dma("sp", SST_d[d, t], sbf[:], reads=[("sbf", si % 3)], writes=[("sst", d, t)])
                        if step < NT - 1:
                            ph.any2(lambda e, xw=xw, t=t, d=d: e.tensor_tensor(out=xw[:].rearrange("p (h q) -> p h q", h=H), in0=XS[:, t].rearrange("p (h q) -> p h q", h=H),
                                                                          in1=EW[:, ti(t), d, :].unsqueeze(2).broadcast_to([128, H, P]), op=ALU.mult),
                                    reads=["XS", ("ew", d)], writes=[("xw", si % 2)])
                            bks = [nb() for _ in range(nyb)]
                            for gi, g in enumerate(groups):
                                pc0 = g["heads"][0] * P
                                pcn = len(g["heads"]) * P
                                ph.pe(lambda e, g=g, pc0=pc0, pcn=pcn, xw=xw, t=t, bks=bks: e.matmul(
                                    PS[bks[pc0 // 512]][:, pc0 % 512:pc0 % 512 + pcn], BTOK[:, t, g["chunk"] * 128:(g["chunk"] + 1) * 128], xw[:, pc0:pc0 + pcn], start=True, stop=True),
                                    reads=["BTOK", ("xw", si % 2)], writes=[("ps", bks[pc0 // 512])])
                            for gi, g in enumerate(groups):
                                r0, nr, nh = g["row0"], g["nrows"], len(g["heads"])
                                h0 = g["heads"][0]
                                pc0 = h0 * P
                                pcn = nh * P
                                sc0 = g["scol0"]
                                ph.dve(lambda e, r0=r0, nr=nr, nh=nh, h0=h0, sc0=sc0, pcn=pcn, d=d, t=t: e.tensor_tensor(
                                    out=STMP[r0:r0 + nr, sc0:sc0 + pcn].rearrange("p (h q) -> p h q", h=nh), in0=S[d][r0:r0 + nr, sc0:sc0 + pcn].rearrange("p (h q) -> p h q", h=nh),
                                    in1=ETOT[r0:r0 + nr, ti(t), d, h0:h0 + nh].unsqueeze(2).broadcast_to([nr, nh, P]), op=ALU.mult),
                                    reads=[("st", d), "etot", ("sbf", si % 3)], writes=[("stmp", gi)])
                                ph.dve(lambda e, r0=r0, nr=nr, sc0=sc0, pc0=pc0, pcn=pcn, d=d, bks=bks: e.tensor_tensor(
                                    out=S[d][r0:r0 + nr, sc0:sc0 + pcn], in0=PS[bks[pc0 // 512]][r0:r0 + nr, pc0 % 512:pc0 % 512 + pcn], in1=STMP[r0:r0 + nr, sc0:sc0 + pcn], op=ALU.add),
                                    reads=[("stmp", gi), ("ps", bks[pc0 // 512])], writes=[("st", d, gi)])
                        si += 1
                ph.emit()
            with ExitStack() as pes:
                ph = Phase(ctx, "scan2")
                al = lambda name, shape, dtype: pes.enter_context(SBT(name, shape, dtype))
                PS = [pes.enter_context(PST("tps%d" % i, [128, 512], F32)) for i in range(7)]
                PBT = pes.enter_context(PST("tpb", [128, 1024], BF16))
                psi = [0]

                def nb():
                    psi[0] = (psi[0] + 1) % 7
                    return psi[0]
                ng = len(groups)
                GM = [[al("gm%d_%d" % (tp, d), [128, ng, 128], F32) for d in range(2)] for tp in range(2)]
                RH1 = al("rh", [128, H, 128], F32)
                RHS = [RH1, RH1]
                EXPD = [al("expd%d" % d, [128, H, 128], F32) for d in range(2)]
                XD = [al("xd%d" % i, [128, HP], BF16) for i in range(4)] if DT4 is not None else None
                MP = [al("mp%d" % i, [128, H, 128], BF16) for i in range(4)]
                SW = max(g["scol0"] + len(g["heads"]) * P for g in groups)
                SIN = [al("sin%d" % i, [128, SW], BF16) for i in range(4)]
                YT1 = al("yt", [128, HP], F32)
                YT = [YT1, YT1]
                Y = [al("y%d" % i, [128, HP], F32) for i in range(2)]
                post_state = post_fn("init", ph, al, PS, nb, PBT)
                if dbg_d and "SST" in dbg_d:
                    sf = al("dbgsst", [128, 512], F32)
                    ph.dma("sp", SIN[0][:], SST_d[0, 17], writes=[("sin", 0)])
                    ph.dve(lambda e: e.tensor_copy(out=sf[:], in_=SIN[0][:]), reads=[("sin", 0)], writes=["sf"])
                    ph.dma("sp", dbg_d["SST"], sf[:], reads=["sf"])
                masks = (MF, MB)
                u1 = (SFm, SBm)

                def build_expd(t, d):
                    RH = RHS[d]
                    ph.any2(lambda e, t=t, d=d, RH=RH: e.tensor_tensor(out=RH[:], in0=masks[d].unsqueeze(1).broadcast_to([128, H, 128]),
                                                                       in1=A4[:, t, d, :].unsqueeze(2).broadcast_to([128, H, 128]), op=ALU.mult),
                            reads=["A4", "CF"], writes=["rh"])
                    for hh in range(H // 4):
                        b = nb()
                        ph.pe(lambda e, b=b, hh=hh, d=d, RH=RH: e.matmul(PS[b][:], u1[d], RH[:, hh * 4:(hh + 1) * 4, :].rearrange("p h i -> p (h i)"), start=True, stop=True),
                              reads=["rh", "CF"], writes=[("ps", b)])
                        ph.act(lambda e, b=b, hh=hh, d=d: e.activation(out=EXPD[d][:, hh * 4:(hh + 1) * 4, :].rearrange("p h i -> p (h i)"), in_=PS[b][:], func=AF.Exp),
                               reads=[("ps", b)], writes=[("expd", d, hh)])
                if const_decay:
                    for d in range(2):
                        build_expd(0, d)
                it = 0
                def p2_stage_a(ti_, t):
                    tsl = slice(t * 128, (t + 1) * 128)
                    tp = ti_ % 2
                    gbanks = []
                    for gi, g in enumerate(groups):
                        if not gbanks or groups[gbanks[-1][1]]["row0"] != g["row0"] or gbanks[-1][2] == 4:
                            gbanks.append([nb(), gi, 0])
                        bk, g0, n = gbanks[-1]
                        r0, nr, ch = g["row0"], g["nrows"], g["chunk"]
                        ph.pe(lambda e, bk=bk, n=n, r0=r0, nr=nr, ch=ch, tsl=tsl: e.matmul(PS[bk][:, n * 128:(n + 1) * 128], BT[r0:r0 + nr, ch, tsl], CT[r0:r0 + nr, ch, tsl],
                                                                                       start=True, stop=True), reads=["BT", "CT"], writes=[("ps", bk)])
                        gbanks[-1][2] += 1
                    for d in range(2):
                        for (bk, g0, n) in gbanks:
                            ph.dve(lambda e, d=d, bk=bk, g0=g0, n=n, tp=tp: e.tensor_tensor(out=GM[tp][d][:, g0:g0 + n, :], in0=PS[bk][:, 0:n * 128].rearrange("p (g i) -> p g i", g=n),
                                                                                        in1=masks[d].unsqueeze(1).broadcast_to([128, n, 128]), op=ALU.mult),
                                   reads=[("ps", bk), "CF"], writes=[("gm", tp, d, g0)])
                    for d in range(2):
                        slot = tp * 2 + d
                        mp = MP[slot]
                        sin = SIN[slot]
                        ph.dma("sp", sin[:], SST_d[d, t, :, 0:SW], writes=[("sin", slot)])
                        if not const_decay:
                            build_expd(t, d)
                        gmb = GM[tp][d][:] if ng == H else GM[tp][d][:, 0:1, :].broadcast_to([128, H, 128])
                        if DT4 is not None:
                            xd = XD[slot]
                            ph.any2(lambda e, d=d, t=t, xd=xd: e.tensor_tensor(out=xd[:].rearrange("p (h q) -> p h q", h=H), in0=XS[:, t].rearrange("p (h q) -> p h q", h=H),
                                                                              in1=DT4[:, t, d, :].unsqueeze(2).broadcast_to([128, H, P]), op=ALU.mult),
                                    reads=["XS", "DT4"], writes=[("xd", slot)])
                        ph.any2(lambda e, mp=mp, gmb=gmb, d=d: e.tensor_tensor(out=mp[:], in0=EXPD[d][:], in1=gmb, op=ALU.mult),
                                reads=[("expd", d), ("gm", tp, d)], writes=[("mp", slot)])

                def p2_stage_b(ti_, t):
                    tsl = slice(t * 128, (t + 1) * 128)
                    tp = ti_ % 2
                    for d in range(2):
                        slot = tp * 2 + d
                        mp = MP[slot]
                        sin = SIN[slot]
                        yd = [nb() for _ in range(nyb)]
                        for h in range(H):
                            xrhs = XD[slot][:, h * P:(h + 1) * P] if DT4 is not None else XS[:, t, h * P:(h + 1) * P]
                            ph.pe(lambda e, h=h, mp=mp, yd=yd, xrhs=xrhs: e.matmul(PS[yd[h * P // 512]][:, (h * P) % 512:(h * P) % 512 + P], mp[:, h, :], xrhs, start=True, stop=True),
                                  reads=[("mp", slot), "XS", ("xd", slot)], writes=[("ps", yd[h * P // 512])])
                        ybanks = []
                        for gi, g in enumerate(groups):
                            r0, nr, ch = g["row0"], g["nrows"], g["chunk"]
                            pc0 = g["heads"][0] * P
                            pcn = len(g["heads"]) * P
                            sc0 = g["scol0"]
                            if not ybanks or ybanks[-1][5] != r0 or ybanks[-1][2] + pcn > 512:
                                ybanks.append([nb(), pc0, 0, g["heads"][0], 0, r0])
                            bk, used = ybanks[-1][0], ybanks[-1][2]
                            ph.pe(lambda e, r0=r0, nr=nr, ch=ch, pcn=pcn, sc0=sc0, sin=sin, bk=bk, used=used, tsl=tsl: e.matmul(
                                PS[bk][:, used:used + pcn], CT[r0:r0 + nr, ch, tsl], sin[r0:r0 + nr, sc0:sc0 + pcn], start=True, stop=True),
                                reads=["CT", ("sin", slot)], writes=[("ps", bk)])
                            ybanks[-1][2] += pcn
                            ybanks[-1][4] += len(g["heads"])
                        yacc = Y[0] if d == 0 else Y[1]
                        yt = YT[d]
                        for (bk, c0, cn, h0, nh, _) in ybanks:
                            ph.dve(lambda e, bk=bk, c0=c0, cn=cn, h0=h0, nh=nh, t=t, d=d, yt=yt: e.tensor_tensor(
                                out=yt[:, c0:c0 + cn].rearrange("p (h q) -> p h q", h=nh), in0=PS[bk][:, 0:cn].rearrange("p (h q) -> p h q", h=nh),
                                in1=ESC[:, ti(t), d, h0:h0 + nh].unsqueeze(2).broadcast_to([128, nh, P]), op=ALU.mult),
                                reads=[("ps", bk), ("esc", d)], writes=[("yt", c0)])
                        for q in range(nyb):
                            csl = slice(q * 512, (q + 1) * 512)
                            ph.dve(lambda e, q=q, yd=yd, csl=csl, yacc=yacc, yt=yt: e.tensor_tensor(out=yacc[:, csl], in0=PS[yd[q]][:], in1=yt[:, csl], op=ALU.add),
                                   reads=[("ps", yd[q]), "yt"], writes=[("y", d, q)])
                    if not (dbg_d and "Yall" in dbg_d):
                        ph.pool(lambda e: e.tensor_tensor(out=Y[0][:], in0=Y[0][:], in1=Y[1][:], op=ALU.add), reads=[("y", 0), ("y", 1)], writes=[("y", 0)])
                    if dbg_d and "Yall" in dbg_d and HP == 512:
                        ph.dma("sp", dbg_d["Yall"][:, t * 512:(t + 1) * 512], Y[0][:], reads=[("y", 0)])
                        ph.dma("sp", dbg_d["Y1all"][:, t * 512:(t + 1) * 512], Y[1][:], reads=[("y", 1)])
                    post_fn("tile", ph, al, PS, nb, post_state, t, Y[0])

                otl = list(out_tiles)
                for i in range(len(otl) + 1):
                    if i < len(otl):
                        p2_stage_a(i, otl[i])
                    if i >= 1:
                        p2_stage_b(i - 1, otl[i - 1])
                post_fn("fini", ph, al, PS, nb, post_state)
                ph.emit()
            oes.close()

        def ssd_group(g):
            with ExitStack() as ges:
                gal = lambda name, shape, dtype: ges.enter_context(SBT(name, shape, dtype))
                XS = gal("xs", [128, NT, 512], BF16)
                BTOK = gal("btok", [128, NT, 128], BF16)
                BT = gal("bt", [128, 1, T], BF16)
                CT = gal("ct", [128, 1, T], BF16)
                DT4 = gal("dt4", [128, NT, 2, 8], F32)
                A4 = gal("a4", [128, NT, 2, 8], F32)
                WZ = gal("wz", [128, KC, 512], BF16)
                Wr = ev_w_in_d.rearrange("(k p) n -> p k n", p=128)
                with ExitStack() as pes:
                    ph = Phase(ctx, "ssdproj")
                    al = lambda name, shape, dtype: pes.enter_context(SBT(name, shape, dtype))
                    PS = [pes.enter_context(PST("bps%d" % i, [128, 512], F32)) for i in range(6)]
                    PB = [pes.enter_context(PST("bpb%d" % i, [128, 1024], BF16)) for i in range(2)]
                    psi = [0]

                    def nb():
                        psi[0] = (psi[0] + 1) % 6
                        return psi[0]
                    WS = [al("wsl%d" % i, [128, KC, 128], BF16) for i in range(2)]
                    WDT = al("wdt", [128, KC, 16], BF16)
                    DBA = al("dba", [128, 2, 2, 8], F32)
                    XPAD = al("xpad", [128, 2320], F32)
                    ACC = al("acc", [128, T], F32)
                    XST = [al("xst%d" % i, [128, T], BF16) for i in range(2)]
                    ph.dma("pool", WZ[:], Wr[:, :, 1536 + g * 512:1536 + (g + 1) * 512], writes=["wz"])
                    for d in range(2):
                        ph.dma("pool", WDT[:, :, d * 8:(d + 1) * 8], Wr[:, :, 4096 + d * 16 + g * 8:4096 + d * 16 + g * 8 + 8], writes=[("wdt", d)])
                        for w in range(2):
                            ph.dma("sp", DBA[:, w, d, :], dtba_d[w:w + 1, d * 16 + g * 8:d * 16 + g * 8 + 8].broadcast_to([128, 8]), writes=[("dba", w, d)])
                    ph.pool(lambda e: e.memset(XPAD[:], 0.0), writes=["xpad"])
                    b = nb()
                    for t in range(NT):
                        for k in range(KC):
                            ph.pe(lambda e, b=b, t=t, k=k: e.matmul(PS[b][:, t * 16:(t + 1) * 16], HT[:, k, t * 128:(t + 1) * 128], WDT[:, k, :], start=(k == 0), stop=(k == KC - 1)),
                                  reads=["wdt", "HT"], writes=[("ps", b)])
                    dt3 = DT4[:].rearrange("p t d h -> p t (d h)")
                    ph.dve(lambda e, b=b: e.tensor_tensor(out=dt3, in0=PS[b][:, 0:NT * 16].rearrange("p (t n) -> p t n", t=NT),
                                                          in1=DBA[:, 0].rearrange("p d h -> p (d h)").unsqueeze(1).broadcast_to([128, NT, 16]), op=ALU.add),
                           reads=[("ps", b), "dba"], writes=["DT4"])
                    ph.act(lambda e: e.activation(out=dt3, in_=dt3, func=AF.Exp), reads=["DT4"], writes=["DT4"])
                    ph.act(lambda e: e.activation(out=dt3, in_=dt3, func=AF.Ln, bias=1.0), reads=["DT4"], writes=["DT4"])
                    ph.act(lambda e: e.activation(out=DBA[:, 1], in_=DBA[:, 1], func=AF.Exp), reads=["dba"], writes=["dba"])
                    ph.dve(lambda e: e.scalar_tensor_tensor(out=A4[:].rearrange("p t d h -> p t (d h)"), in0=dt3, scalar=-1.0,
                                                            in1=DBA[:, 1].rearrange("p d h -> p (d h)").unsqueeze(1).broadcast_to([128, NT, 16]), op0=ALU.mult, op1=ALU.mult),
                           reads=["DT4", "dba"], writes=["A4"])
                    chunks = [4 * g + i for i in range(4)] + [8 + g, 10 + g]
                    for ci, c in enumerate(chunks):
                        ws = WS[ci % 2]
                        ph.dma("pool", ws[:], Wr[:, :, 2560 + c * 128:2560 + (c + 1) * 128], writes=[("wsl", ci % 2)])
                        for (t0, tn) in BLKS:
                            b = nb()
                            for k in range(KC):
                                ph.pe(lambda e, b=b, k=k, ws=ws, t0=t0, tn=tn: e.matmul(PS[b][:, :tn], ws[:, k, :], HT[:, k, t0:t0 + tn], start=(k == 0), stop=(k == KC - 1)),
                                      reads=[("wsl", ci % 2), "HT"], writes=[("ps", b)])
                            o0 = 2 + t0 if t0 < L else 2054 + (t0 - L)
                            ph.act(lambda e, b=b, o0=o0, tn=tn: e.copy(out=XPAD[:, o0:o0 + tn], in_=PS[b][:, :tn]), reads=[("ps", b)], writes=[("xpad", t0)])
                        eng = "dve"
                        for (o0, a0, n) in ((2, 0, L), (2054, L, LC)):
                            wcol = lambda k, c=c: VT[:, VR["conv_w"] + k * 12 + c:VR["conv_w"] + k * 12 + c + 1]
                            bcol = VT[:, VR["conv_b"] + c:VR["conv_b"] + c + 1]
                            ph.op(eng, lambda e, o0=o0, a0=a0, n=n, wcol=wcol, bcol=bcol: e.tensor_scalar(out=ACC[:, a0:a0 + n], in0=XPAD[:, o0 - 2:o0 - 2 + n], scalar1=wcol(0), scalar2=bcol,
                                                                                                    op0=ALU.mult, op1=ALU.add), reads=["xpad", "VT"], writes=[("acc", a0)])
                            for k in range(1, 5):
                                ph.op(eng, lambda e, o0=o0, a0=a0, n=n, k=k, wcol=wcol: e.scalar_tensor_tensor(out=ACC[:, a0:a0 + n], in0=XPAD[:, o0 - 2 + k:o0 - 2 + k + n], scalar=wcol(k),
                                                                                                      in1=ACC[:, a0:a0 + n], op0=ALU.mult, op1=ALU.add),
                                      reads=["xpad", ("acc", a0)], writes=[("acc", a0)])
                        if ci < 4:
                            dst, dkey = XST[ci % 2][:], ("xst", ci % 2)
                        elif ci == 4:
                            dst, dkey = BT[:, 0, :], "BT"
                        else:
                            dst, dkey = CT[:, 0, :], "CT"
                        ph.act(lambda e, dst=dst: e.activation(out=dst, in_=ACC[:], func=AF.Silu), reads=["acc"], writes=[dkey])
                        if ci <= 4:
                            for t8 in range(0, NT, 8):
                                n8 = min(8, NT - t8)
                                pb = (t8 // 8 + ci) % 2
                                for tt in range(n8):
                                    t = t8 + tt
                                    ph.pe(lambda e, pb=pb, tt=tt, t=t, dst=dst: e.transpose(PB[pb][:, tt * 128:(tt + 1) * 128], dst[:, t * 128:(t + 1) * 128], IDB),
                                          reads=[dkey, "CB"], writes=[("pb", pb)])
                                if ci < 4:
                                    o = XS[:, t8:t8 + n8, ci * 128:(ci + 1) * 128]
                                    okey = ("XS", ci, t8)
                                else:
                                    o = BTOK[:, t8:t8 + n8, :]
                                    okey = ("BTOK", t8)
                                ph.act(lambda e, pb=pb, n8=n8, o=o: e.copy(out=o, in_=PB[pb][:, 0:n8 * 128].rearrange("p (a n) -> p a n", a=n8)),
                                       reads=[("pb", pb)], writes=[okey])
                    ph.emit()
                if g == 0:
                    dump("XS0", XS[:].rearrange("p t n -> p (t n)"), (128, NT * 512))
                    dump("A40", A4[:].rearrange("p t d h -> p (t d h)"), (128, NT * 16))
                    dump("DT40", DT4[:].rearrange("p t d h -> p (t d h)"), (128, NT * 16))
                    dump("CT0", CT[:, 0, :], (128, T))
                    dump("BTOK0", BTOK[:].rearrange("p t n -> p (t n)"), (128, NT * 128))

                def post(stage, ph, al, PS, nb, st=None, t=None, Yt=None):
                    if stage == "init":
                        pbt = st
                        st = {}
                        st["dsk"] = al("dsk", [128, 8], F32)
                        st["gng"] = al("gng", [128, 512], F32)
                        st["sz"] = al("sz", [128, 512], F32)
                        st["yz"] = al("yz", [128, 512], F32)
                        st["sq"] = st["sz"]
                        st["ssq"] = al("ssq", [128, 4], F32)
                        st["yn"] = al("yn", [128, 512], BF16)
                        st["stg"] = [al("stg%d" % i, [128, 4, 128], BF16) for i in range(2)]
                        st["pb"] = pbt
                        ph.dma("sp", st["dsk"][:], ssmd_d[0:1, g * 8:(g + 1) * 8].broadcast_to([128, 8]), writes=["dsk"])
                        ph.dma("sp", st["gng"][:], ssmg_d[0:1, g * 512:(g + 1) * 512].broadcast_to([128, 512]), writes=["gng"])
                        return st
                    if stage == "fini":
                        return
                    dsk, gng, sz, yz, sq, ssq, yn = st["dsk"], st["gng"], st["sz"], st["yz"], st["sq"], st["ssq"], st["yn"]
                    b = nb()
                    for k in range(KC):
                        ph.pe(lambda e, b=b, k=k: e.matmul(PS[b][:], HT[:, k, t * 128:(t + 1) * 128], WZ[:, k, :], start=(k == 0), stop=(k == KC - 1)),
                              reads=["HT", "wz"], writes=[("ps", b)])
                    ph.act(lambda e, b=b: e.activation(out=sz[:], in_=PS[b][:], func=AF.Silu), reads=[("ps", b)], writes=["sz"])
                    ph.dve(lambda e: e.tensor_tensor(out=yz[:].rearrange("p (h q) -> p h q", h=8), in0=XS[:, t].rearrange("p (h q) -> p h q", h=8),
                                                     in1=dsk[:].unsqueeze(2).broadcast_to([128, 8, 64]), op=ALU.mult), reads=["XS", "dsk"], writes=["yz"])
                    ph.dve(lambda e: e.tensor_tensor(out=yz[:], in0=yz[:], in1=Yt[:], op=ALU.add), reads=["yz", ("y", 0)], writes=["yz"])
                    ph.dve(lambda e: e.tensor_tensor(out=yz[:], in0=yz[:], in1=sz[:], op=ALU.mult), reads=["yz", "sz"], writes=["yz"])
                    ph.act(lambda e: e.activation(out=sq[:], in_=yz[:], func=AF.Square, accum_out=ssq[:, 0:1]), reads=["yz"], writes=["sz", "ssq"])
                    ph.dve(lambda e: e.tensor_scalar(out=ssq[:, 1:2], in0=ssq[:, 0:1], scalar1=1.0 / 512, scalar2=EPS, op0=ALU.mult, op1=ALU.add), reads=["ssq"], writes=["ssq"])
                    ph.act(lambda e: e.sqrt(out=ssq[:, 2:3], in_=ssq[:, 1:2]), reads=["ssq"], writes=["ssq"])
                    ph.dve(lambda e: e.reciprocal(out=ssq[:, 3:4], in_=ssq[:, 2:3]), reads=["ssq"], writes=["ssq"])
                    ph.dve(lambda e: e.scalar_tensor_tensor(out=yn[:], in0=yz[:], scalar=ssq[:, 3:4], in1=gng[:], op0=ALU.mult, op1=ALU.mult),
                           reads=["yz", "ssq", "gng"], writes=["yn"])
                    pb = st["pb"]
                    stg = st["stg"][t % 2]
                    for cl in range(4):
                        ph.pe(lambda e, cl=cl: e.transpose(pb[:, cl * 128:(cl + 1) * 128], yn[:, cl * 128:(cl + 1) * 128], IDB), reads=["yn", "CB"], writes=["pbt"])
                    ph.act(lambda e, stg=stg: e.copy(out=stg[:], in_=pb[:, 0:512].rearrange("p (a n) -> p a n", a=4)), reads=["pbt"], writes=[("stg", t % 2)])
                    ph.dma("sp", YCAT_d[4 + 4 * g:8 + 4 * g, :, t * 128:(t + 1) * 128].rearrange("c p t -> p c t"), stg[:], reads=[("stg", t % 2)])

                pes_pb = [None]

                def pre(ph, al, PS, nb):
                    pass
                groups = [dict(chunk=0, row0=0, nrows=128, heads=list(range(8)), scol0=0, sncols=512)]
                scan_phase(64, groups, XS, BTOK, BT, CT, A4, DT4, False, post, list(range(NT)))

        def out_proj(l):
            W_d = ev_w_out_d if l == 0 else od_w_out_d
            with ExitStack() as pes:
                ph = Phase(ctx, "oproj")
                al = lambda name, shape, dtype: pes.enter_context(SBT(name, shape, dtype))
                PS = [pes.enter_context(PST("ops%d" % i, [128, 512], F32)) for i in range(8)]
                WO = al("wo", [128, 12, D], BF16)
                YB = [al("yb%d" % i, [128, 12, 512], BF16) for i in range(2)]
                for c in range(0, 12, 4):
                    ph.dma("pool", WO[:, c:c + 4, :], W_d.rearrange("(c p) n -> p c n", p=128)[:, c:c + 4, :], writes=[("wo", c)])
                pi = 0
                for bi_, (t0, tn) in enumerate(BLKS):
                    if l == 1 and t0 >= L:
                        continue
                    j = 0 if t0 < L else 1
                    yb = YB[bi_ % 2]
                    ph.dma("sp", yb[:, :, :tn], YCAT_d[:, :, t0:t0 + tn].rearrange("c p t -> p c t"), writes=[("yb", bi_ % 2)])
                    for dc in range(KC):
                        b = pi % 8
                        pi += 1
                        for c in range(12):
                            ph.pe(lambda e, b=b, c=c, dc=dc, yb=yb, tn=tn: e.matmul(PS[b][:, :tn], WO[:, c, dc * 128:(dc + 1) * 128], yb[:, c, :tn], start=(c == 0), stop=(c == 11)),
                                  reads=["wo", ("yb", bi_ % 2)], writes=[("ps", b)])
                        ph.dve(lambda e, b=b, dc=dc, t0=t0, tn=tn, j=j: e.scalar_tensor_tensor(out=XT[:, dc, t0:t0 + tn], in0=PS[b][:, :tn], scalar=AB[:, l, j, 2, dc:dc + 1],
                                                                                            in1=XT[:, dc, t0:t0 + tn], op0=ALU.mult, op1=ALU.add),
                               reads=[("ps", b)], writes=[("XT", dc, bi_)])
                ph.emit()

        THIRDS = [[(0, 512), (512, 256)], [(768, 512), (1280, 256)], [(1536, 512), (2048, 256)]]

        def ffn(l, GT=None):
            moe = (l == 1)
            nfc = 28 if moe else 22
            nexp = NEXPERTS if moe else 1
            with ExitStack() as pes:
                ph = Phase(ctx, "ffn")
                al = lambda name, shape, dtype: pes.enter_context(SBT(name, shape, dtype))
                PS = [pes.enter_context(PST("fps%d" % i, [128, 512], F32)) for i in range(8)]
                psi = [0]

                def nb():
                    psi[0] = (psi[0] + 1) % 8
                    return psi[0]
                tgroups = [[(0, 512), (512, 512)], [(1024, 512), (1536, 512)]] if moe else THIRDS
                gmax = 1024 if moe else 768
                fchunks = [list(range(0, 14)), list(range(14, 28))] if moe else [list(range(22))]
                nfl = len(fchunks[0])
                ACTT = al("actt", [128, nfl, gmax], BF16)
                W13 = [al("w13_%d" % i, [128, 2, KC, 128], BF16) for i in range(3)]
                W2S = [al("w2s_%d" % i, [128, nfl, 128], BF16) for i in range(2)]
                SIL = [al("sil%d" % i, [128, 512], F32) for i in range(2)]
                if moe:
                    HG = al("hg", [128, KC, gmax], BF16)
                wi = 0
                w2i = 0
                si = 0
                for th, blks in enumerate(tgroups):
                    tb = blks[0][0]
                    for ex in range(nexp):
                        if moe:
                            w1_d, w3_d, w2_d = moe_w1_d[ex], moe_w3_d[ex], moe_w2_d[ex]
                            for (t0, tn) in blks:
                                b = nb()
                                ph.pe(lambda e, b=b, ex=ex, t0=t0, tn=tn: e.matmul(PS[b][:, :tn], SEL[:, ex * 128:(ex + 1) * 128], GT[:, t0:t0 + tn], start=True, stop=True),
                                      reads=["GT", "SEL"], writes=[("ps", b)])
                                for k in range(KC):
                                    ph.dve(lambda e, b=b, k=k, t0=t0, tn=tn, tb=tb: e.tensor_tensor(out=HG[:, k, t0 - tb:t0 - tb + tn], in0=HT[:, k, t0:t0 + tn], in1=PS[b][:, :tn], op=ALU.mult),
                                           reads=[("ps", b), "HT"], writes=[("hg", k, t0)])
                        else:
                            w1_d, w3_d, w2_d = ffn_w1_d, ffn_w3_d, ffn_w2_d
                        w1r = w1_d.rearrange("(k p) n -> p k n", p=128)
                        w3r = w3_d.rearrange("(k p) n -> p k n", p=128)
                        w2r = w2_d.rearrange("(f p) n -> p f n", p=128)
                        for fcs in fchunks:
                            for fi, fc in enumerate(fcs):
                                w = W13[wi % 3]
                                ph.dma("pool", w[:, 0], w1r[:, :, fc * 128:(fc + 1) * 128], writes=[("w13", wi % 3, 0)])
                                ph.dma("pool", w[:, 1], w3r[:, :, fc * 128:(fc + 1) * 128], writes=[("w13", wi % 3, 1)])
                                for (t0, tn) in blks:
                                    b1, b3 = nb(), nb()
                                    for k in range(KC):
                                        ph.pe(lambda e, b1=b1, k=k, w=w, t0=t0, tn=tn: e.matmul(PS[b1][:, :tn], w[:, 0, k, :], HT[:, k, t0:t0 + tn], start=(k == 0), stop=(k == KC - 1)),
                                              reads=[("w13", wi % 3, 0), "HT"], writes=[("ps", b1)])
                                    for k in range(KC):
                                        rhs = HG[:, k, t0 - tb:t0 - tb + tn] if moe else HT[:, k, t0:t0 + tn]
                                        ph.pe(lambda e, b3=b3, k=k, w=w, rhs=rhs, tn=tn: e.matmul(PS[b3][:, :tn], w[:, 1, k, :], rhs, start=(k == 0), stop=(k == KC - 1)),
                                              reads=[("w13", wi % 3, 1), "HT", "hg"], writes=[("ps", b3)])
                                    sil = SIL[si % 2]
                                    ph.act(lambda e, b1=b1, sil=sil, tn=tn: e.activation(out=sil[:, :tn], in_=PS[b1][:, :tn], func=AF.Silu), reads=[("ps", b1)], writes=[("sil", si % 2)])
                                    ph.dve(lambda e, b3=b3, sil=sil, fi=fi, t0=t0, tn=tn, tb=tb: e.tensor_tensor(out=ACTT[:, fi, t0 - tb:t0 - tb + tn], in0=PS[b3][:, :tn], in1=sil[:, :tn], op=ALU.mult),
                                           reads=[("ps", b3), ("sil", si % 2)], writes=[("actt", fi, t0)])
                                    si += 1
                                wi += 1
                            nf = len(fcs)
                            for dc in range(KC):
                                w2 = W2S[w2i % 2]
                                ph.dma("pool", w2[:, 0:nf, :], w2r[:, fcs[0]:fcs[0] + nf, dc * 128:(dc + 1) * 128], writes=[("w2s", w2i % 2)])
                                for (t0, tn) in blks:
                                    j = 0 if t0 < L else 1
                                    b = nb()
                                    for fi in range(nf):
                                        ph.pe(lambda e, b=b, fi=fi, w2=w2, t0=t0, tn=tn, tb=tb, nf=nf: e.matmul(PS[b][:, :tn], w2[:, fi, :], ACTT[:, fi, t0 - tb:t0 - tb + tn], start=(fi == 0), stop=(fi == nf - 1)),
                                              reads=[("w2s", w2i % 2), "actt"], writes=[("ps", b)])
                                    ph.dve(lambda e, b=b, dc=dc, t0=t0, tn=tn, j=j: e.scalar_tensor_tensor(out=XT[:, dc, t0:t0 + tn], in0=PS[b][:, :tn], scalar=AB[:, l, j, 5, dc:dc + 1],
                                                                                                        in1=XT[:, dc, t0:t0 + tn], op0=ALU.mult, op1=ALU.add),
                                           reads=[("ps", b)], writes=[("XT", dc, t0)])
                                w2i += 1
                ph.emit()

        def final_out():
            with ExitStack() as pes:
                ph = Phase(ctx, "fin")
                al = lambda name, shape, dtype: pes.enter_context(SBT(name, shape, dtype))
                PS = [pes.enter_context(PST("zps%d" % i, [128, 512], F32)) for i in range(8)]
                SQ = [al("fsq%d" % i, [128, 512], BF16) for i in range(3)]
                RS = [al("frs%d" % i, [128, 512], F32) for i in range(2)]
                XN = [al("fxn%d" % i, [128, KC, 512], F32) for i in range(2)]
                OTK = [al("fot%d" % i, [128, D], F32) for i in range(3)]
                qi = 0
                oi = 0
                pi = 0
                g0 = VR["final_g"]
                for bi_, (t0, tn) in enumerate(BLKS[:4]):
                    pb = pi % 8
                    pi += 1
                    for k in range(KC):
                        sq = SQ[qi % 3]
                        ph.act(lambda e, sq=sq, k=k, t0=t0: e.activation(out=sq[:], in_=XT[:, k, t0:t0 + 512], func=AF.Square), reads=["XT"], writes=[("sq", qi % 3)])
                        ph.pe(lambda e, sq=sq, k=k, pb=pb: e.matmul(PS[pb][:], ONESB, sq[:], start=(k == 0), stop=(k == KC - 1)), reads=[("sq", qi % 3)], writes=[("ps", pb)])
                        qi += 1
                    rs = RS[bi_ % 2]
                    xn = XN[bi_ % 2]
                    ph.dve(lambda e, rs=rs, pb=pb: e.tensor_scalar(out=rs[:], in0=PS[pb][:], scalar1=1.0 / D, scalar2=EPS, op0=ALU.mult, op1=ALU.add), reads=[("ps", pb)], writes=[("rs", bi_ % 2)])
                    ph.act(lambda e, rs=rs: e.sqrt(out=rs[:], in_=rs[:]), reads=[("rs", bi_ % 2)], writes=[("rs", bi_ % 2)])
                    ph.dve(lambda e, rs=rs: e.reciprocal(out=rs[:], in_=rs[:]), reads=[("rs", bi_ % 2)], writes=[("rs", bi_ % 2)])
                    for k in range(KC):
                        ph.dve(lambda e, k=k, t0=t0, rs=rs, xn=xn: e.scalar_tensor_tensor(out=xn[:, k, :], in0=XT[:, k, t0:t0 + 512], scalar=VT[:, g0 + k:g0 + k + 1], in1=rs[:], op0=ALU.mult, op1=ALU.mult),
                               reads=["XT", ("rs", bi_ % 2)], writes=[("xn", bi_ % 2, k)])
                    for tt in range(4):
                        otk = OTK[oi % 3]
                        for half in range(2):
                            pb = pi % 8
                            pi += 1
                            for kk in range(4):
                                k = half * 4 + kk
                                ph.pe(lambda e, pb=pb, kk=kk, k=k, tt=tt, xn=xn: e.transpose(PS[pb][:, kk * 128:(kk + 1) * 128], xn[:, k, tt * 128:(tt + 1) * 128], IDF),
                                      reads=[("xn", bi_ % 2), "CF"], writes=[("ps", pb)])
                            if half == 0:
                                ph.act(lambda e, pb=pb, otk=otk: e.copy(out=otk[:, 0:512], in_=PS[pb][:]), reads=[("ps", pb)], writes=[("otk", oi % 3, 0)])
                            else:
                                ph.dve(lambda e, pb=pb, otk=otk: e.tensor_copy(out=otk[:, 512:1024], in_=PS[pb][:]), reads=[("ps", pb)], writes=[("otk", oi % 3, 1)])
                        ph.dma("sp", out_d[t0 + tt * 128:t0 + (tt + 1) * 128, :], otk[:], reads=[("otk", oi % 3)])
                        oi += 1
                ph.emit()

        RM = CB[:, 768:896]

        def rope(ph, X, key, nb, PS, al):
            sid = 0
            store = ph.__dict__.setdefault("_rope_store", {})
            if sid not in store:
                COS = al("cos", [128, L], F32)
                SIN = al("sin", [128, L], F32)
                T1 = [al("rt1_%d" % i, [128, 512], F32) for i in range(2)]
                T2 = [al("rt2_%d" % i, [128, 512], F32) for i in range(2)]
                ph.dma("sp", COS[:], rope_d[0], writes=["cos"])
                ph.dma("sp", SIN[:], rope_d[1], writes=["sin"])
                store[sid] = (COS, SIN, T1, T2, [0])
            COS, SIN, T1, T2, cnt = store[sid]
            for (t0, tn) in BLKS[:4]:
                b = nb()
                i = cnt[0] % 2
                cnt[0] += 1
                ph.pe(lambda e, b=b, t0=t0: e.matmul(PS[b][:], RM, X[:, t0:t0 + 512], start=True, stop=True), reads=[key, "CB"], writes=[("ps", b)])
                ph.dve(lambda e, i=i, t0=t0: e.tensor_tensor(out=T1[i][:], in0=X[:, t0:t0 + 512], in1=COS[:, t0:t0 + 512], op=ALU.mult), reads=[key, "cos"], writes=[("rt1", i)])
                ph.dve(lambda e, i=i, b=b, t0=t0: e.tensor_tensor(out=T2[i][:], in0=PS[b][:], in1=SIN[:, t0:t0 + 512], op=ALU.mult), reads=[("ps", b), "sin"], writes=[("rt2", i)])
                ph.pool(lambda e, i=i, t0=t0: e.tensor_tensor(out=X[:, t0:t0 + 512], in0=T1[i][:], in1=T2[i][:], op=ALU.add), reads=[("rt1", i), ("rt2", i)], writes=[(key, "r", t0) if isinstance(key, str) else key])

        def ret_half(hh):
            Wr = od_w_in_d.rearrange("(k p) n -> p k n", p=128)
            with ExitStack() as ges:
                gal = lambda name, shape, dtype: ges.enter_context(SBT(name, shape, dtype))
                XS = gal("rxs", [128, NT, 512], BF16)
                BTOK = gal("rbtok", [128, NT, 256], BF16)
                BT = gal("rbt", [128, 2, T], BF16)
                CT = gal("rct", [128, 2, T], BF16)
                A4 = gal("ra4", [128, 1, 2, 4], F32)
                WG = gal("rwg", [128, KC, 512], BF16)
                with ExitStack() as pes:
                    ph = Phase(ctx, "retproj")
                    al = lambda name, shape, dtype: pes.enter_context(SBT(name, shape, dtype))
                    PS = [pes.enter_context(PST("rps%d" % i, [128, 512], F32)) for i in range(6)]
                    PB = [pes.enter_context(PST("rpb%d" % i, [128, 1024], BF16)) for i in range(2)]
                    psi = [0]

                    def nb():
                        psi[0] = (psi[0] + 1) % 6
                        return psi[0]
                    WS = [al("rws%d" % i, [128, KC, 128], BF16) for i in range(2)]
                    WV = al("rwv", [128, KC, 512], BF16)
                    for hs in range(4):
                        hl = RPERM[hs]
                        ph.dma("pool", WG[:, :, hs * 128:(hs + 1) * 128], Wr[:, :, 2816 + (4 * hh + hl) * 128:2816 + (4 * hh + hl + 1) * 128], writes=[("wg", hs)])
                        ph.dma("pool", WV[:, :, hs * 128:(hs + 1) * 128], Wr[:, :, 1792 + (4 * hh + hl) * 128:1792 + (4 * hh + hl + 1) * 128], writes=[("wv", hs)])
                        for d in range(2):
                            ph.dma("sp", A4[:, 0, d, hs:hs + 1], retld_d[d:d + 1, 4 * hh + hl:4 * hh + hl + 1].broadcast_to([128, 1]), writes=[("a4", d, hs)])
                    a4f = A4[:].rearrange("p a d h -> p (a d h)")
                    ph.act(lambda e: e.activation(out=a4f, in_=a4f, func=AF.Exp), reads=["a4"], writes=["a4"])
                    ph.act(lambda e: e.activation(out=a4f, in_=a4f, func=AF.Ln, scale=-1.0, bias=1.0), reads=["a4"], writes=["a4"])
                    for t in range(NT):
                        b = nb()
                        for k in range(KC):
                            ph.pe(lambda e, b=b, k=k, t=t: e.matmul(PS[b][:], HT[:, k, t * 128:(t + 1) * 128], WV[:, k, :], start=(k == 0), stop=(k == KC - 1)),
                                  reads=["HT", "wv"], writes=[("ps", b)])
                        if t % 2 == 0:
                            ph.act(lambda e, b=b, t=t: e.copy(out=XS[:, t, :], in_=PS[b][:]), reads=[("ps", b)], writes=[("XS", t)])
                        else:
                            ph.dve(lambda e, b=b, t=t: e.tensor_copy(out=XS[:, t, :], in_=PS[b][:]), reads=[("ps", b)], writes=[("XS", t)])
                    wi = 0
                    for (dst, c0, scale, nm) in ((CT, 768, 1.0, "CT"), (BT, 1280, 0.125, "BT")):
                        for c in range(2):
                            ws = WS[wi % 2]
                            ph.dma("pool", ws[:], Wr[:, :, c0 + (2 * hh + c) * 128:c0 + (2 * hh + c + 1) * 128], writes=[("rws", wi % 2)])
                            for (t0, tn) in BLKS:
                                b = nb()
                                for k in range(KC):
                                    ph.pe(lambda e, b=b, k=k, ws=ws, t0=t0, tn=tn: e.matmul(PS[b][:, :tn], ws[:, k, :], HT[:, k, t0:t0 + tn], start=(k == 0), stop=(k == KC - 1)),
                                          reads=[("rws", wi % 2), "HT"], writes=[("ps", b)])
                                ph.act(lambda e, b=b, dst=dst, c=c, t0=t0, tn=tn, scale=scale: e.activation(out=dst[:, c, t0:t0 + tn], in_=PS[b][:, :tn], func=AF.Copy, scale=scale),
                                       reads=[("ps", b)], writes=[(nm, c, "p", t0)])
                            rope(ph, dst[:, c, :], (nm, c), nb, PS, al)
                            if nm == "BT":
                                for t8 in range(0, NT, 8):
                                    n8 = min(8, NT - t8)
                                    pb = (t8 // 8 + c) % 2
                                    for tt in range(n8):
                                        t = t8 + tt
                                        ph.pe(lambda e, pb=pb, tt=tt, t=t, c=c: e.transpose(PB[pb][:, tt * 128:(tt + 1) * 128], BT[:, c, t * 128:(t + 1) * 128], IDB),
                                              reads=[("BT", c), "CB"], writes=[("pb", pb)])
                                    ph.act(lambda e, pb=pb, n8=n8, t8=t8, c=c: e.copy(out=BTOK[:, t8:t8 + n8, c * 128:(c + 1) * 128], in_=PB[pb][:, 0:n8 * 128].rearrange("p (a n) -> p a n", a=n8)),
                                           reads=[("pb", pb)], writes=[("BTOK", c, t8)])
                            wi += 1
                    ph.emit()
                if hh == 0:
                    dump("RXS", XS[:].rearrange("p t n -> p (t n)"), (128, NT * 512))
                    dump("RCT", CT[:].rearrange("p c t -> p (c t)"), (128, 2 * T))
                    dump("RBTOK", BTOK[:].rearrange("p t n -> p (t n)"), (128, NT * 256))
                    dump("RA4", A4[:].rearrange("p a d h -> p (a d h)"), (128, 8))

                def post(stage, ph, al, PS, nb, st=None, t=None, Yt=None):
                    if stage == "init":
                        pbt = st
                        st = {"pb": pbt}
                        st["gng"] = al("rgng", [128, 512], F32)
                        st["gnb"] = al("rgnb", [128, 512], F32)
                        st["sg"] = al("rsg", [128, 512], F32)
                        st["yc"] = al("ryc", [128, 512], F32)
                        st["stat"] = al("rstat", [128, 4, 4], F32)
                        st["yn"] = al("ryn", [128, 512], BF16)
                        st["stg"] = [al("rstg%d" % i, [128, 4, 128], BF16) for i in range(2)]
                        for hs in range(4):
                            c0 = (4 * hh + RPERM[hs]) * 128
                            ph.dma("sp", st["gng"][:, hs * 128:(hs + 1) * 128], retg_d[0:1, c0:c0 + 128].broadcast_to([128, 128]), writes=[("gng", hs)])
                            ph.dma("sp", st["gnb"][:, hs * 128:(hs + 1) * 128], retb_d[0:1, c0:c0 + 128].broadcast_to([128, 128]), writes=[("gnb", hs)])
                        return st
                    if stage == "fini":
                        return
                    gng, gnb, sg, yc, stat, yn = st["gng"], st["gnb"], st["sg"], st["yc"], st["stat"], st["yn"]
                    b = nb()
                    for k in range(KC):
                        ph.pe(lambda e, b=b, k=k: e.matmul(PS[b][:], HT[:, k, t * 128:(t + 1) * 128], WG[:, k, :], start=(k == 0), stop=(k == KC - 1)),
                              reads=["HT", "wg"], writes=[("ps", b)])
                    ph.act(lambda e, b=b: e.activation(out=sg[:], in_=PS[b][:], func=AF.Silu), reads=[("ps", b)], writes=["sg"])
                    y3 = Yt[:].rearrange("p (h q) -> p h q", h=4)
                    yc3 = yc[:].rearrange("p (h q) -> p h q", h=4)
                    ph.dve(lambda e: e.reduce_sum(out=stat[:, 0, :], in_=y3, axis=AX.X), reads=[("y", 0)], writes=[("stat", 0)])
                    ph.dve(lambda e: e.tensor_scalar(out=stat[:, 1, :], in0=stat[:, 0, :], scalar1=-1.0 / 128, scalar2=None, op0=ALU.mult), reads=[("stat", 0)], writes=[("stat", 1)])
                    ph.dve(lambda e: e.tensor_tensor(out=yc3, in0=y3, in1=stat[:, 1, :].unsqueeze(2).broadcast_to([128, 4, 128]), op=ALU.add), reads=[("y", 0), ("stat", 1)], writes=["yc"])
                    for hq in range(4):
                        ph.act(lambda e, hq=hq: e.activation(out=yn[:, hq * 128:(hq + 1) * 128], in_=yc[:, hq * 128:(hq + 1) * 128], func=AF.Square, accum_out=stat[:, 2, hq:hq + 1]),
                               reads=["yc"], writes=["yn", ("stat", 2, hq)])
                    ph.dve(lambda e: e.tensor_scalar(out=stat[:, 2, :], in0=stat[:, 2, :], scalar1=1.0 / 128, scalar2=EPS, op0=ALU.mult, op1=ALU.add), reads=[("stat", 2)], writes=[("stat", 2)])
                    ph.act(lambda e: e.sqrt(out=stat[:, 2, :], in_=stat[:, 2, :]), reads=[("stat", 2)], writes=[("stat", 2)])
                    ph.dve(lambda e: e.reciprocal(out=stat[:, 3, :], in_=stat[:, 2, :]), reads=[("stat", 2)], writes=[("stat", 3)])
                    ph.dve(lambda e: e.tensor_tensor(out=yc3, in0=yc3, in1=stat[:, 3, :].unsqueeze(2).broadcast_to([128, 4, 128]), op=ALU.mult), reads=["yc", ("stat", 3)], writes=["yc"])
                    ph.pool(lambda e: e.tensor_tensor(out=yc[:], in0=yc[:], in1=gng[:], op=ALU.mult), reads=["yc", "gng"], writes=["yc"])
                    ph.pool(lambda e: e.tensor_tensor(out=yc[:], in0=yc[:], in1=gnb[:], op=ALU.add), reads=["yc", "gnb"], writes=["yc"])
                    ph.dve(lambda e: e.tensor_tensor(out=yn[:], in0=yc[:], in1=sg[:], op=ALU.mult), reads=["yc", "sg"], writes=["yn"])
                    pb = st["pb"]
                    stg = st["stg"][t % 2]
                    for cl in range(4):
                        ph.pe(lambda e, cl=cl: e.transpose(pb[:, RPERM[cl] * 128:(RPERM[cl] + 1) * 128], yn[:, cl * 128:(cl + 1) * 128], IDB), reads=["yn", "CB"], writes=["pbt"])
                    ph.act(lambda e, stg=stg: e.copy(out=stg[:], in_=pb[:, 0:512].rearrange("p (a n) -> p a n", a=4)), reads=["pbt"], writes=[("stg", t % 2)])
                    ph.dma("sp", YCAT_d[4 + 4 * hh:8 + 4 * hh, :, t * 128:(t + 1) * 128].rearrange("c p t -> p c t"), stg[:], reads=[("stg", t % 2)])

                groups = [dict(chunk=hs % 2, row0=(hs // 2) * 64, nrows=64, heads=[hs], scol0=(hs % 2) * 128, sncols=128) for hs in range(4)]
                if RET_SCAN:
                    scan_phase(128, groups, XS, BTOK, BT, CT, A4, None, True, post, list(range(16)), H=4)

        def moe_gate(LG, GT):
            with ExitStack() as pes:
                ph = Phase(ctx, "gate")
                al = lambda name, shape, dtype: pes.enter_context(SBT(name, shape, dtype))
                PS = [pes.enter_context(PST("gps%d" % i, [128, 512], F32)) for i in range(2)]
                M1 = al("m1", [128, NT], F32)
                M2 = al("m2", [128, NT], F32)
                EQ = al("eq", [128, NT, 8], F32)
                L2 = al("l2", [128, NT, 8], F32)
                EXg = al("exg", [128, NT, 8], F32)
                DEN = al("den", [128, NT], F32)
                bc = lambda a: a[:].unsqueeze(2).broadcast_to([128, NT, 8])
                ph.dve(lambda e: e.reduce_max(out=M1[:], in_=LG[:], axis=AX.X), reads=["LG"], writes=["m1"])
                ph.dve(lambda e: e.tensor_tensor(out=EQ[:], in0=LG[:], in1=bc(M1), op=ALU.is_equal), reads=["LG", "m1"], writes=["eq"])
                ph.dve(lambda e: e.scalar_tensor_tensor(out=L2[:], in0=EQ[:], scalar=-1e30, in1=LG[:], op0=ALU.mult, op1=ALU.add), reads=["eq", "LG"], writes=["l2"])
                ph.dve(lambda e: e.reduce_max(out=M2[:], in_=L2[:], axis=AX.X), reads=["l2"], writes=["m2"])
                ph.dve(lambda e: e.tensor_tensor(out=EQ[:], in0=LG[:], in1=bc(M2), op=ALU.is_ge), reads=["LG", "m2"], writes=["eq"])
                ph.dve(lambda e: e.tensor_tensor(out=L2[:], in0=LG[:], in1=bc(M1), op=ALU.subtract), reads=["LG", "m1"], writes=["l2"])
                ph.act(lambda e: e.activation(out=EXg[:], in_=L2[:], func=AF.Exp), reads=["l2"], writes=["exg"])
                ph.dve(lambda e: e.tensor_tensor(out=EXg[:], in0=EXg[:], in1=EQ[:], op=ALU.mult), reads=["exg", "eq"], writes=["exg"])
                ph.dve(lambda e: e.reduce_sum(out=DEN[:], in_=EXg[:], axis=AX.X), reads=["exg"], writes=["den"])
                ph.dve(lambda e: e.reciprocal(out=DEN[:], in_=DEN[:]), reads=["den"], writes=["den"])
                ph.dve(lambda e: e.tensor_tensor(out=EXg[:], in0=EXg[:], in1=bc(DEN), op=ALU.mult), reads=["exg", "den"], writes=["exg"])
                for t4 in range(0, NT, 4):
                    n4 = min(4, NT - t4)
                    b = (t4 // 4) % 2
                    for tt in range(n4):
                        ph.pe(lambda e, b=b, tt=tt, t4=t4: e.transpose(PS[b][0:8, tt * 128:(tt + 1) * 128], EXg[:, t4 + tt, :], IDF), reads=["exg", "CF"], writes=[("ps", b)])
                    ph.act(lambda e, b=b, t4=t4, n4=n4: e.copy(out=GT[:, t4 * 128:(t4 + n4) * 128], in_=PS[b][0:8, 0:n4 * 128]), reads=[("ps", b)], writes=[("GT", t4)])
                ph.emit()

        if not SKIP_L0:
            norm_mod(0, 1)
            for m in range(NPAIRS):
                attention_pair(0, m)
            for g in range(NGROUPS):
                ssd_group(g)
        if STOP_AFTER >= 1 and not SKIP_L0:
            out_proj(0)
            norm_mod(0, 2)
            ffn(0)
        if dbg_d and "XT1" in dbg_d:
            ph = Phase(ctx, "dxt1")
            ph.dma("sp", dbg_d["XT1"].rearrange("p (k t) -> p k t", k=KC), XT[:])
            ph.emit()
        if STOP_AFTER >= 2:
            norm_mod(1, 1)
            for m in range(NPAIRS):
                attention_pair(1, m)
            for hh in range(2 if RUN_RET else 0):
                ret_half(hh)
        if STOP_AFTER >= 3:
            SEL = sb("SEL", [8, 1024], F32)
            LGT = sb("LGT", [128, NT, 8], F32)
            GTT = sb("GTT", [8, T], F32)
            ph = Phase(ctx, "ldsel")
            ph.dma("sp", SEL[:], sel_d[:, :], writes=["SEL"])
            ph.emit()
            out_proj(1)
            norm_mod(1, 2, LG=LGT)
            moe_gate(LGT, GTT)
            dump("GT", GTT[:], (8, T))
        if dbg_d and "XT2" in dbg_d:
            ph = Phase(ctx, "dxt2")
            ph.dma("sp", dbg_d["XT2"].rearrange("p (k t) -> p k t", k=KC), XT[:])
            ph.emit()
        if STOP_AFTER >= 4:
            ffn(1, GT=GTT)
            final_out()
        if dbg_d and "YC" in dbg_d:
            with ExitStack() as des:
                ph = Phase(ctx, "dumpyc")
                yb = des.enter_context(SBT("ycb", [128, T], BF16))
                yf = des.enter_context(SBT("ycf", [128, T], F32))
                for c in range(12):
                    ph.dma("sp", yb[:], YCAT_d[c], writes=["yb"])
                    ph.dve(lambda e: e.tensor_copy(out=yf[:], in_=yb[:]), reads=["yb"], writes=["yf"])
                    ph.dma("sp", dbg_d["YC"][:, c * T:(c + 1) * T], yf[:], reads=["yf"])
                ph.emit()
    return nc


def make_consts():
    c = np.zeros((128, 1024), np.float32)
    c[:, 0:128] = np.eye(128, dtype=np.float32)
    c[:, 128:256] = 1.0
    p = np.arange(128)[:, None]
    i = np.arange(128)[None, :]
    c[:, 256:384] = (p <= i)
    c[:, 384:512] = (p >= i)
    c[:, 512:640] = (p > i)
    c[:, 640:768] = (p < i)
    for f in range(128):
        if f % 64 < 32:
            c[f + 32, 768 + f] = -1.0
        else:
            c[f - 32, 768 + f] = 1.0
    return c


def kernel(**inputs):
    dbg = inputs.pop("_dbg", None)
    inp = {k: np.asarray(v) for k, v in inputs.items()}
    nc = build_program(dbg)
    cst = make_consts()
    nab = na_bias_table(inp["na_rpb"][0])
    swab = swa_bias_table()
    tpos = np.arange(L)
    inv = (10000.0 ** (-np.arange(16, dtype=np.float32) / 16)).astype(np.float32)
    ang = np.concatenate([(tpos // 64).astype(np.float32)[:, None] * inv, (tpos % 64).astype(np.float32)[:, None] * inv], axis=-1)
    fidx = np.arange(128) % 32
    rope_tab = np.stack([np.cos(ang)[:, fidx].T, np.sin(ang)[:, fidx].T], 0).astype(np.float32)
    sel = np.zeros((8, 1024), np.float32)
    for e in range(8):
        sel[e, e * 128:(e + 1) * 128] = 1.0
    in_maps = []
    for b in range(8):
        vecs = np.zeros((256, 128), np.float32)

        def put(nm, arr):
            a = np.ascontiguousarray(arr, dtype=np.float32).reshape(-1, 128)
            vecs[VR[nm]:VR[nm] + a.shape[0]] = a
        put("c", inp["c"][b])
        put("c_ctx", inp["c_ctx"])
        put("ada_b0", inp["ada_b"][0])
        put("ada_b1", inp["ada_b"][1])
        put("g_attn0", inp["norm_attn_g"][0])
        put("g_attn1", inp["norm_attn_g"][1])
        put("g_ffn0", inp["norm_ffn_g"][0])
        put("g_ffn1", inp["norm_ffn_g"][1])
        put("final_g", inp["final_g"])
        put("conv_w", inp["ssm_conv_w"][0].reshape(5, 1536))
        put("conv_b", inp["ssm_conv_b"][0])
        put("ssm_g", inp["ssm_norm_g"][0])
        in_maps.append({
            "x": np.ascontiguousarray(inp["x"][b]),
            "ctx": np.ascontiguousarray(inp["ctx"][b]),
            "vecs": vecs,
            "cst": cst,
            "ada_w": inp["ada_w"],
            "ev_w_in": inp["ev_w_in"][0], "od_w_in": inp["od_w_in"][0], "nab": nab, "swab": swab,
            "sink": inp["swa_sink"],
            "ev_w_out": inp["ev_w_out"][0], "od_w_out": inp["od_w_out"][0],
            "ffn_w1": inp["ffn_w1"][0], "ffn_w3": inp["ffn_w3"][0], "ffn_w2": inp["ffn_w2"][0],
            "sel": sel, "rope": rope_tab, "retld": inp["ret_log_decay"][0], "retg": inp["ret_gn_g"], "retb": inp["ret_gn_b"],
            "router": inp["moe_router"][0],
            "dtba": np.stack([inp["ssm_dt_bias"][0].reshape(32), inp["ssm_a_log"][0].reshape(32)], 0),
            "ssmd": inp["ssm_d"], "ssmg": inp["ssm_norm_g"],
        })
    if STOP_AFTER >= 4:
        for mp in in_maps:
            mp["moe_w1"] = inp["moe_w1"][0]
            mp["moe_w3"] = inp["moe_w3"][0]
            mp["moe_w2"] = inp["moe_w2"][0]
    res = run_bass_kernel_spmd(nc, in_maps, core_ids=list(range(8)))
    if dbg:
        return res
    out = np.stack([r["out"] for r in res.results], axis=0)
    return out
```

```python
import math
from contextlib import ExitStack
import numpy as np
import concourse.bass as bass
import concourse.mybir as mybir
from concourse.bass_utils import run_bass_kernel_spmd

F32 = mybir.dt.float32
BF16 = mybir.dt.bfloat16
AF = mybir.ActivationFunctionType
ALU = mybir.AluOpType
AX = mybir.AxisListType

ENGS = ("pe", "act", "dve", "pool", "sp")
NDMA_SEMS = 24


class Ctx:
    def __init__(self, nc, es):
        self.nc = nc
        self.eng_sem = {e: es.enter_context(nc.semaphore("sem_" + e)) for e in ENGS if e != "sp"}
        self.eng_cnt = {e: 0 for e in self.eng_sem}
        self.dma_sems = [es.enter_context(nc.semaphore("dsem%d" % i)) for i in range(NDMA_SEMS)]
        self.dma_cnt = [0] * NDMA_SEMS
        self.dma_rr = 0
        self.eng_obj = {"pe": nc.tensor, "act": nc.scalar, "dve": nc.vector, "pool": nc.gpsimd, "sp": nc.sync}


class Phase:
    def __init__(self, ctx, name="ph"):
        self.ctx = ctx
        self.name = name
        self.ops = []
        self.state = {}
        self.rr = 0

    def _st(self, key):
        if isinstance(key, tuple):
            nm, sub = key[0], tuple(key[1:])
        else:
            nm, sub = key, ()
        d = self.state.setdefault(nm, {})
        return d, sub

    @staticmethod
    def _overlap(a, b):
        n = min(len(a), len(b))
        return a[:n] == b[:n]

    def op(self, eng, fn, reads=(), writes=(), dma=False, pe_acc=False):
        oid = len(self.ops)
        deps = set()
        for key in reads:
            d, sub = self._st(key)
            for s2, st in d.items():
                if self._overlap(sub, s2) and st["w"] is not None:
                    deps.add(st["w"])
            d.setdefault(sub, {"w": None, "r": []})["r"].append(oid)
        for key in writes:
            d, sub = self._st(key)
            for s2 in list(d.keys()):
                if self._overlap(sub, s2):
                    st = d[s2]
                    if st["w"] is not None:
                        deps.add(st["w"])
                    deps.update(st["r"])
                    if len(s2) > len(sub):
                        del d[s2]
            st = d.setdefault(sub, {"w": None, "r": []})
            st["w"] = oid
            st["r"] = []
        deps.discard(oid)
        o = {"eng": eng, "fn": fn, "deps": deps, "dma": dma, "pe_acc": pe_acc}
        if dma:
            c = self.ctx
            k = c.dma_rr
            c.dma_rr = (c.dma_rr + 1) % NDMA_SEMS
            prev = getattr(self, "_dma_prev", {}).get(k)
            if prev is not None:
                deps.add(prev)
            self.__dict__.setdefault("_dma_prev", {})[k] = oid
            c.dma_cnt[k] += 16
            o["dsem"] = k
            o["dval"] = c.dma_cnt[k]
        self.ops.append(o)
        return oid

    def pe(self, fn, reads=(), writes=(), acc=False):
        return self.op("pe", fn, reads, writes, pe_acc=acc)

    def act(self, fn, reads=(), writes=()):
        return self.op("act", fn, reads, writes)

    def dve(self, fn, reads=(), writes=()):
        return self.op("dve", fn, reads, writes)

    def pool(self, fn, reads=(), writes=()):
        return self.op("pool", fn, reads, writes)

    def any2(self, fn, reads=(), writes=()):
        self.rr += 1
        return self.op("dve" if self.rr % 2 else "pool", fn, reads, writes)

    def dma(self, q, out, in_, reads=(), writes=()):
        return self.op(q, lambda e: e.dma_start(out=out, in_=in_), reads, writes, dma=True)

    def emit(self):
        c = self.ctx
        ops = self.ops
        for o in ops:
            best = {}
            pd = []
            for d in o["deps"]:
                po = ops[d]
                if po["dma"]:
                    pd.append(d)
                    continue
                if po["eng"] == "pe" and o["eng"] == "pe" and not o["dma"]:
                    continue
                if d > best.get(po["eng"], -1):
                    best[po["eng"]] = d
            o["deps"] = set(pd) | set(best.values())
        needed = set()
        for o in ops:
            for d in o["deps"]:
                po = ops[d]
                if po["dma"]:
                    continue
                needed.add(d)
        last_of = {}
        for i, o in enumerate(ops):
            if not o["dma"]:
                last_of[o["eng"]] = i
        for i in last_of.values():
            needed.add(i)
        for i, o in enumerate(ops):
            if o["dma"]:
                continue
            if i in needed:
                c.eng_cnt[o["eng"]] += 1
                o["inc"] = True
            o["cnt"] = c.eng_cnt[o["eng"]] if i in needed else None
        per_eng = {e: [] for e in ENGS}
        waited = {e: {} for e in ENGS}
        for i, o in enumerate(ops):
            w = {}
            for d in o["deps"]:
                po = ops[d]
                if po["dma"]:
                    key = ("d", po["dsem"])
                    val = po["dval"]
                else:
                    if po["eng"] == "pe" and o["eng"] == "pe" and not o["dma"]:
                        continue
                    key = ("e", po["eng"])
                    val = po["cnt"]
                if val > w.get(key, 0):
                    w[key] = val
            wl = []
            for key, val in w.items():
                if waited[o["eng"]].get(key, 0) >= val:
                    continue
                waited[o["eng"]][key] = val
                wl.append((key, val))
            o["waits"] = wl
            per_eng[o["eng"]].append(o)
        fin = []
        for e, i in last_of.items():
            fin.append((("e", e), ops[i]["cnt"]))
        for k in range(NDMA_SEMS):
            if c.dma_cnt[k] > 0:
                fin.append((("d", k), c.dma_cnt[k]))

        def semof(key):
            return c.eng_sem[key[1]] if key[0] == "e" else c.dma_sems[key[1]]

        def run(engname):
            def body(eng):
                for o in per_eng[engname]:
                    for key, val in o["waits"]:
                        eng.wait_ge(semof(key), val)
                    ins = o["fn"](eng)
                    if o["dma"]:
                        ins.then_inc(c.dma_sems[o["dsem"]], 16)
                    elif o.get("inc"):
                        ins.then_inc(c.eng_sem[o["eng"]], 1)
                if engname == "sp":
                    for key, val in fin:
                        eng.wait_ge(semof(key), val)
            return body

        with c.nc.Block() as block:
            block.tensor(run("pe"))
            block.scalar(run("act"))
            block.vector(run("dve"))
            block.gpsimd(run("pool"))
            block.sync(run("sp"))
        self.ops = []
        self.state = {}
        self._dma_prev = {}


D = 1024
L = 2048
LC = 256
T = L + LC
NT = T // 128
KC = D // 128
EPS = 1e-6
BLKS = [(0, 512), (512, 512), (1024, 512), (1536, 512), (2048, 256)]

VR = {}
_r = 0
for _nm, _n in (("c", 8), ("c_ctx", 8), ("ada_b0", 48), ("ada_b1", 48), ("g_attn0", 8), ("g_attn1", 8),
                ("g_ffn0", 8), ("g_ffn1", 8), ("final_g", 8), ("conv_w", 60), ("conv_b", 12), ("ssm_g", 8)):
    VR[_nm] = _r
    _r += _n
NVR = _r

NPAIRS = 4
NGROUPS = 2
ATT_LAG = 2
RPERM = [0, 2, 1, 3]
NEXPERTS = 8
STOP_AFTER = 99
RUN_RET = True
RET_SCAN = True
DBG_NOPOST = False
DBG_NOP2 = False
DBG_SKIP = set()
SKIP_L0 = False


def _na_tiles():
    out = []
    for t in range(16):
        qr = np.arange(t * 128, (t + 1) * 128) // 64
        r0 = np.clip(qr - 4, 0, 24)
        out.append(list(range(int(r0.min()) // 2, (int(r0.max()) + 7) // 2 + 1)))
    return out


NA_KT = _na_tiles()


def na_bias_table(rpb):
    out = np.full((8, 16, 128, 640), -30000.0, np.float32)
    for t in range(16):
        qpos = np.arange(t * 128, (t + 1) * 128)
        qr, qc = qpos // 64, qpos % 64
        r0 = np.clip(qr - 4, 0, 24)
        c0 = np.clip(qc - 8, 0, 48)
        for j, kt in enumerate(NA_KT[t]):
            kpos = np.arange(kt * 128, (kt + 1) * 128)
            kr, kc = kpos // 64, kpos % 64
            ok = ((kr[:, None] >= r0[None, :]) & (kr[:, None] < r0[None, :] + 8)
                  & (kc[:, None] >= c0[None, :]) & (kc[:, None] < c0[None, :] + 16))
            dr = np.clip(kr[:, None] - qr[None, :] + 7, 0, 14)
            dc = np.clip(kc[:, None] - qc[None, :] + 15, 0, 30)
            vals = rpb[:, dr, dc]
            out[:, t, :, j * 128:(j + 1) * 128] = np.where(ok[None], vals, np.float32(-30000.0))
    return out


def swa_bias_table():
    out = np.full((16, 128, 384), -30000.0, np.float32)
    for t in range(16):
        kts = [kt for kt in (t - 1, t, t + 1) if 0 <= kt < 16]
        qpos = np.arange(t * 128, (t + 1) * 128)
        for j, kt in enumerate(kts):
            kpos = np.arange(kt * 128, (kt + 1) * 128)
            ok = np.abs(kpos[:, None] - qpos[None, :]) <= 128
            out[t, :, j * 128:(j + 1) * 128] = np.where(ok, np.float32(0.0), np.float32(-30000.0))
    return out


def build_program(dbg=None):
    nc = bass.Bass("TRN2", target_bir_lowering=False)
    _uid = [0]

    def SBT(name, shape, dtype):
        _uid[0] += 1
        return nc.sbuf_tensor("%s_%d" % (name, _uid[0]), shape, dtype)

    def PST(name, shape, dtype):
        _uid[0] += 1
        return nc.psum_tensor("%s_%d" % (name, _uid[0]), shape, dtype)
    dt = nc.dram_tensor
    x_d = dt("x", [L, D], F32, kind="ExternalInput").ap()
    ctx_d = dt("ctx", [LC, D], F32, kind="ExternalInput").ap()
    vecs_d = dt("vecs", [256, 128], F32, kind="ExternalInput").ap()
    cst_d = dt("cst", [128, 1024], F32, kind="ExternalInput").ap()
    ada_w_d = dt("ada_w", [2, D, 6 * D], F32, kind="ExternalInput").ap()
    out_d = dt("out", [L, D], F32, kind="ExternalOutput").ap()
    ev_w_in_d = dt("ev_w_in", [D, 4128], F32, kind="ExternalInput").ap()
    od_w_in_d = dt("od_w_in", [D, 3840], F32, kind="ExternalInput").ap()
    nab_d = dt("nab", [8, 16, 128, 640], F32, kind="ExternalInput").ap()
    swab_d = dt("swab", [16, 128, 384], F32, kind="ExternalInput").ap()
    sink_d = dt("sink", [1, 8], F32, kind="ExternalInput").ap()
    dtba_d = dt("dtba", [2, 32], F32, kind="ExternalInput").ap()
    ev_w_out_d = dt("ev_w_out", [1536, D], F32, kind="ExternalInput").ap()
    od_w_out_d = dt("od_w_out", [1536, D], F32, kind="ExternalInput").ap()
    ffn_w1_d = dt("ffn_w1", [D, 2816], F32, kind="ExternalInput").ap()
    ffn_w3_d = dt("ffn_w3", [D, 2816], F32, kind="ExternalInput").ap()
    ffn_w2_d = dt("ffn_w2", [2816, D], F32, kind="ExternalInput").ap()
    if STOP_AFTER >= 4:
        moe_w1_t = dt("moe_w1", [8, D, 3584], F32, kind="ExternalInput").ap()
        moe_w3_t = dt("moe_w3", [8, D, 3584], F32, kind="ExternalInput").ap()
        moe_w2_t = dt("moe_w2", [8, 3584, D], F32, kind="ExternalInput").ap()
        moe_w1_d = [moe_w1_t[e] for e in range(8)]
        moe_w3_d = [moe_w3_t[e] for e in range(8)]
        moe_w2_d = [moe_w2_t[e] for e in range(8)]
    sel_d = dt("sel", [8, 1024], F32, kind="ExternalInput").ap()
    rope_d = dt("rope", [2, 128, L], F32, kind="ExternalInput").ap()
    retld_d = dt("retld", [2, 8], F32, kind="ExternalInput").ap()
    retg_d = dt("retg", [1, 1024], F32, kind="ExternalInput").ap()
    retb_d = dt("retb", [1, 1024], F32, kind="ExternalInput").ap()
    router_d = dt("router", [D, 8], F32, kind="ExternalInput").ap()
    ssmd_d = dt("ssmd", [1, 16], F32, kind="ExternalInput").ap()
    ssmg_d = dt("ssmg", [1, 1024], F32, kind="ExternalInput").ap()
    dbg_d = None
    if dbg:
        dbg_d = {k: dt("dbg_" + k, list(shp), F32, kind="ExternalOutput").ap() for k, shp in dbg.items()}

    with ExitStack() as es:
        ctx = Ctx(nc, es)
        sb = lambda name, shape, dtype: es.enter_context(SBT(name, shape, dtype))
        XT = sb("XT", [128, KC, T], F32)
        HT = sb("HT", [128, KC, T], BF16)
        VT = sb("VT", [128, 256], F32)
        CF = sb("CF", [128, 1024], F32)
        CB = sb("CB", [128, 1024], BF16)
        MOD = sb("MOD", [128, 2, 2, 48], F32)
        AB = sb("AB", [128, 2, 2, 6, 8], F32)
        IDF = CF[:, 0:128]
        ONESF = CF[:, 128:256]
        IDB = CB[:, 0:128]
        ONESB = CB[:, 128:256]

        with ExitStack() as ps_es:
            ph = Phase(ctx, "p0")
            PS = [ps_es.enter_context(PST("ps%d" % i, [128, 512], F32)) for i in range(8)]
            XIN = [ps_es.enter_context(SBT("xin%d" % i, [128, D], F32)) for i in range(3)]
            VIN = ps_es.enter_context(SBT("vin", [128, 2, 128], F32))
            SC = ps_es.enter_context(SBT("sc", [128, KC, 2], BF16))
            AW = [ps_es.enter_context(SBT("aw%d" % i, [128, KC, 768], BF16)) for i in range(2)]
            ph.dma("sp", CF[:], cst_d[:, :], writes=["CF"])
            ph.dma("pool", CB[:], cst_d[:, :], writes=["CB"])
            ph.dma("sp", VIN[:], vecs_d.rearrange("(a p) n -> p a n", p=128), writes=["VIN"])
            for a in range(2):
                ph.pe(lambda e, a=a: e.transpose(PS[0][:, a * 128:(a + 1) * 128], VIN[:, a, :], IDF),
                      reads=["CF", "VIN"], writes=[("ps", 0)])
            ph.dve(lambda e: e.tensor_copy(out=VT[:], in_=PS[0][:, 0:256]), reads=[("ps", 0)], writes=["VT"])
            pi = 1
            for t in range(NT):
                xin = XIN[t % 3]
                src = x_d[t * 128:(t + 1) * 128, :] if t < 16 else ctx_d[(t - 16) * 128:(t - 15) * 128, :]
                ph.dma("sp", xin[:], src, writes=[("xin", t % 3)])
                for half in range(2):
                    b = 1 + (pi % 7)
                    pi += 1
                    for kk in range(4):
                        k = half * 4 + kk
                        ph.pe(lambda e, b=b, kk=kk, k=k, xin=xin: e.transpose(
                            PS[b][:, kk * 128:(kk + 1) * 128], xin[:, k * 128:(k + 1) * 128], IDF),
                            reads=["CF", ("xin", t % 3)], writes=[("ps", b)])
                    dst = XT[:, half * 4:half * 4 + 4, t * 128:(t + 1) * 128]
                    srcp = PS[b][:].rearrange("p (a n) -> p a n", a=4)
                    if (t + half) % 2 == 0:
                        ph.dve(lambda e, dst=dst, srcp=srcp: e.tensor_copy(out=dst, in_=srcp),
                               reads=[("ps", b)], writes=[("XT", t)])
                    else:
                        ph.act(lambda e, dst=dst, srcp=srcp: e.copy(out=dst, in_=srcp),
                               reads=[("ps", b)], writes=[("XT", t)])
            for j, nm in enumerate(("c", "c_ctx")):
                ph.act(lambda e, j=j, nm=nm: e.activation(out=SC[:, :, j], in_=VT[:, VR[nm]:VR[nm] + 8], func=AF.Silu),
                       reads=["VT"], writes=["SC"])
            si = 0
            for l in range(2):
                for s in range(8):
                    aw = AW[si % 2]
                    ph.dma("pool", aw[:], ada_w_d[l, :, s * 768:(s + 1) * 768].rearrange("(k p) n -> p k n", p=128),
                           writes=[("aw", si % 2)])
                    for c6 in range(6):
                        cc = s * 6 + c6
                        for k in range(KC):
                            ph.pe(lambda e, l=l, cc=cc, k=k, c6=c6, aw=aw: e.matmul(
                                PS[0][:, l * 96 + cc * 2:l * 96 + cc * 2 + 2], aw[:, k, c6 * 128:(c6 + 1) * 128],
                                SC[:, k, :], start=(k == 0), stop=(k == KC - 1)),
                                reads=[("aw", si % 2), "SC"], writes=[("ps", 0)])
                    si += 1
            for l in range(2):
                for j in range(2):
                    r0 = VR["ada_b%d" % l]
                    ph.dve(lambda e, l=l, j=j, r0=r0: e.tensor_tensor(
                        out=MOD[:, l, j, :], in0=PS[0][:, l * 96:(l + 1) * 96].rearrange("p (c j) -> p c j", j=2)[:, :, j],
                        in1=VT[:, r0:r0 + 48], op=ALU.add), reads=[("ps", 0), "VT"], writes=["MOD"])
                    for (ai, sci, gname) in ((0, 1, "g_attn%d" % l), (3, 4, "g_ffn%d" % l)):
                        g0 = VR[gname]
                        ph.dve(lambda e, l=l, j=j, ai=ai, sci=sci, g0=g0: e.scalar_tensor_tensor(
                            out=AB[:, l, j, ai, :], in0=MOD[:, l, j, sci * 8:(sci + 1) * 8], scalar=1.0,
                            in1=VT[:, g0:g0 + 8], op0=ALU.add, op1=ALU.mult), reads=["MOD", "VT"], writes=["AB"])
                    for (bi, shi) in ((1, 0), (2, 2), (4, 3), (5, 5)):
                        ph.dve(lambda e, l=l, j=j, bi=bi, shi=shi: e.tensor_copy(
                            out=AB[:, l, j, bi, :], in_=MOD[:, l, j, shi * 8:(shi + 1) * 8]), reads=["MOD"], writes=["AB"])
            ph.emit()

        def norm_mod(l, which, LG=None):
            ai, bi = (0, 1) if which == 1 else (3, 4)
            with ExitStack() as pes:
                ph = Phase(ctx, "nm")
                PS = [pes.enter_context(PST("nps%d" % i, [128, 512], F32)) for i in range(4)]
                SQ = [pes.enter_context(SBT("sq%d" % i, [128, 512], BF16)) for i in range(3)]
                RS = [pes.enter_context(SBT("rs%d" % i, [128, 512], F32)) for i in range(2)]
                TMP = [pes.enter_context(SBT("tmp%d" % i, [128, 512], F32)) for i in range(3)]
                if LG is not None:
                    PSR = [pes.enter_context(PST("npr%d" % i, [128, 512], F32)) for i in range(2)]
                    H2F = pes.enter_context(SBT("h2f", [128, KC, 512], F32))
                    RWF = pes.enter_context(SBT("rwf", [128, KC, 8], F32))
                    ph.dma("sp", RWF[:], router_d.rearrange("(k p) n -> p k n", p=128), writes=["rwf"])
                cnt = {"qi": 0, "ti": 0}

                def nm_a(bi_, t0, tn):
                    pb = bi_ % 4
                    for k in range(KC):
                        qi = cnt["qi"]
                        sq = SQ[qi % 3]
                        ph.act(lambda e, sq=sq, k=k, t0=t0, tn=tn: e.activation(out=sq[:, :tn], in_=XT[:, k, t0:t0 + tn], func=AF.Square),
                               reads=[], writes=[("sq", qi % 3)])
                        ph.pe(lambda e, sq=sq, k=k, pb=pb, tn=tn: e.matmul(PS[pb][:, :tn], ONESB, sq[:, :tn], start=(k == 0), stop=(k == KC - 1)),
                              reads=[("sq", qi % 3)], writes=[("nps", pb)])
                        cnt["qi"] += 1
                    rs = RS[bi_ % 2]
                    ph.dve(lambda e, rs=rs, pb=pb, tn=tn: e.tensor_scalar(out=rs[:, :tn], in0=PS[pb][:, :tn], scalar1=1.0 / D, scalar2=EPS,
                                                                          op0=ALU.mult, op1=ALU.add), reads=[("nps", pb)], writes=[("rs", bi_ % 2)])
                    ph.act(lambda e, rs=rs, tn=tn: e.sqrt(out=rs[:, :tn], in_=rs[:, :tn]),
                           reads=[("rs", bi_ % 2)], writes=[("rs", bi_ % 2)])
                    ph.dve(lambda e, rs=rs, tn=tn: e.reciprocal(out=rs[:, :tn], in_=rs[:, :tn]),
                           reads=[("rs", bi_ % 2)], writes=[("rs", bi_ % 2)])

                def nm_b(bi_, t0, tn):
                    j = 0 if t0 < L else 1
                    rs = RS[bi_ % 2]
                    for k in range(KC):
                        ti = cnt["ti"]
                        tmp = TMP[ti % 3]
                        ph.any2(lambda e, tmp=tmp, k=k, t0=t0, tn=tn, rs=rs: e.tensor_tensor(out=tmp[:, :tn], in0=XT[:, k, t0:t0 + tn], in1=rs[:, :tn], op=ALU.mult),
                                reads=[("rs", bi_ % 2)], writes=[("tmp", ti % 3)])
                        ph.act(lambda e, tmp=tmp, k=k, t0=t0, tn=tn, j=j: e.activation(out=HT[:, k, t0:t0 + tn], in_=tmp[:, :tn], func=AF.Identity,
                                                                                      scale=AB[:, l, j, ai, k:k + 1], bias=AB[:, l, j, bi, k:k + 1]),
                               reads=[("tmp", ti % 3)], writes=[("HT", k, bi_)])
                        if LG is not None:
                            ph.act(lambda e, tmp=tmp, k=k, tn=tn, j=j: e.activation(out=H2F[:, k, :tn], in_=tmp[:, :tn], func=AF.Identity,
                                                                                scale=AB[:, l, j, ai, k:k + 1], bias=AB[:, l, j, bi, k:k + 1]),
                                   reads=[("tmp", ti % 3)], writes=[("h2f", k)])
                        cnt["ti"] += 1
                    if LG is not None:
                        pr = bi_ % 2
                        for tt in range(tn // 128):
                            for k in range(KC):
                                ph.pe(lambda e, pr=pr, tt=tt, k=k: e.matmul(PSR[pr][:, tt * 8:(tt + 1) * 8], H2F[:, k, tt * 128:(tt + 1) * 128], RWF[:, k, :], start=(k == 0), stop=(k == KC - 1)),
                                      reads=["h2f", "rwf"], writes=[("npr", pr)])
                        ntl = tn // 128
                        ph.dve(lambda e, pr=pr, t0=t0, ntl=ntl: e.tensor_copy(out=LG[:, t0 // 128:t0 // 128 + ntl, :], in_=PSR[pr][:, 0:ntl * 8].rearrange("p (a n) -> p a n", a=ntl)),
                               reads=[("npr", pr)], writes=[("LG", t0)])

                for i in range(len(BLKS) + 1):
                    if i < len(BLKS):
                        nm_a(i, *BLKS[i])
                    if i >= 1:
                        nm_b(i - 1, *BLKS[i - 1])
                ph.emit()

        YCAT_d = nc.dram_tensor("ycat_scr", [12, 128, T], BF16, kind="Internal").ap()

        def dump(name, src_ap, shape2, reads=()):
            if not dbg_d or name not in dbg_d:
                return
            with ExitStack() as des:
                ph = Phase(ctx, "dump")
                f = des.enter_context(SBT("dbgf_" + name, list(shape2), F32))
                ph.dve(lambda e: e.tensor_copy(out=f[:], in_=src_ap), writes=["f"])
                ph.dma("sp", dbg_d[name], f[:], reads=["f"])
                ph.emit()

        def attention_pair(l, m):
            W_d = ev_w_in_d if l == 0 else od_w_in_d
            with ExitStack() as pes:
                ph = Phase(ctx, "att")
                al = lambda name, shape, dtype: pes.enter_context(SBT(name, shape, dtype))
                PS = [pes.enter_context(PST("aps%d" % i, [128, 512], F32)) for i in range(8)]
                psi = [0]

                def nb():
                    psi[0] = (psi[0] + 1) % 8
                    return psi[0]
                WS = al("ws", [128, KC, 384], BF16)
                QT = al("qt", [128, T], BF16)
                KT = al("kt", [128, T], BF16)
                VK = al("vk", [128, NT, 128], BF16)
                OT = al("ot", [128, T], BF16)
                BI = [al("bi%d" % i, [128, 640], F32) for i in range(3)]
                SS = [al("ss%d" % i, [128, 640], F32) for i in range(2)]
                PT = [al("pt%d" % i, [128, 896], BF16) for i in range(4)]
                RD = [al("rd%d" % i, [128, 128], F32) for i in range(2)]
                Wr = W_d.rearrange("(k p) n -> p k n", p=128)
                if l == 0:
                    cols = [(m * 128, 128), (512 + m * 128, 128), (1024 + m * 128, 128)]
                else:
                    kv = m // 2
                    cols = [(m * 128, 128), (512 + kv * 64, 64), (512 + kv * 64, 64), (640 + kv * 64, 64), (640 + kv * 64, 64)]
                off = 0
                for (c0, cn) in cols:
                    ph.dma("pool", WS[:, :, off:off + cn], Wr[:, :, c0:c0 + cn], writes=[("ws", off)])
                    off += cn
                wsr = [("ws", o) for o in (0, 64, 128, 192, 256, 320)]
                nq = T if l == 0 else L
                for (dst, wo, lim, nm) in ((QT, 0, nq, "qt"), (KT, 128, T, "kt")):
                    for (t0, tn) in BLKS:
                        if t0 >= lim:
                            continue
                        b = nb()
                        for k in range(KC):
                            ph.pe(lambda e, b=b, k=k, wo=wo, t0=t0, tn=tn: e.matmul(PS[b][:, :tn], WS[:, k, wo:wo + 128], HT[:, k, t0:t0 + tn],
                                                                                 start=(k == 0), stop=(k == KC - 1)),
                                  reads=wsr + ["HT"], writes=[("ps", b)])
                        ph.act(lambda e, b=b, dst=dst, t0=t0, tn=tn: e.copy(out=dst[:, t0:t0 + tn], in_=PS[b][:, :tn]),
                               reads=[("ps", b)], writes=[(nm, t0)])
                for t4 in range(0, NT, 4):
                    b = nb()
                    n4 = min(4, NT - t4)
                    for tt in range(n4):
                        t = t4 + tt
                        for k in range(KC):
                            ph.pe(lambda e, b=b, k=k, t=t, tt=tt: e.matmul(PS[b][:, tt * 128:(tt + 1) * 128], HT[:, k, t * 128:(t + 1) * 128], WS[:, k, 256:384],
                                                                         start=(k == 0), stop=(k == KC - 1)),
                                  reads=wsr + ["HT"], writes=[("ps", b)])
                    ph.dve(lambda e, b=b, t4=t4, n4=n4: e.tensor_copy(out=VK[:, t4:t4 + n4, :], in_=PS[b][:, :n4 * 128].rearrange("p (a n) -> p a n", a=n4)),
                           reads=[("ps", b)], writes=[("vk", t4)])
                if l == 1:
                    rope(ph, QT, "qt", nb, PS, al)
                    rope(ph, KT, "kt", nb, PS, al)
                    ES = al("es", [128, 8], F32)
                    ph.dma("sp", ES[:], sink_d[0:1, :].broadcast_to([128, 8]), writes=["es"])
                    ph.act(lambda e: e.activation(out=ES[:], in_=ES[:], func=AF.Exp), reads=["es"], writes=["es"])
                qtiles = list(range(NT)) if l == 0 else list(range(16))
                iters = [(e_, t) for e_ in range(2) for t in qtiles]
                info = {}

                def stage_a(it, e_, t):
                    r0 = 64 * e_
                    h = 2 * m + e_
                    if t >= 16:
                        kts, nbias = [16, 17], 0
                    elif l == 0:
                        kts, nbias = NA_KT[t] + [16, 17], len(NA_KT[t])
                    else:
                        kts = [kt for kt in (t - 1, t, t + 1) if 0 <= kt < 16]
                        nbias = len(kts)
                        kts = kts + [16, 17]
                    nk = len(kts)
                    info[it] = (kts, nk)
                    bi = BI[it % 3]
                    ss = SS[it % 2]
                    pt = PT[it % 4]
                    if nbias:
                        src = nab_d[h, t, :, 0:nbias * 128] if l == 0 else swab_d[t, :, 0:nbias * 128]
                        ph.dma("sp", bi[:, 0:nbias * 128], src, writes=[("bi", it % 3)])
                    banks = [nb(), nb()]
                    for j, kt in enumerate(kts):
                        b = banks[j // 4]
                        ph.pe(lambda e, b=b, j=j, kt=kt, t=t, r0=r0: e.matmul(PS[b][:, (j % 4) * 128:(j % 4 + 1) * 128], KT[r0:r0 + 64, kt * 128:(kt + 1) * 128],
                                                                          QT[r0:r0 + 64, t * 128:(t + 1) * 128], start=True, stop=True),
                              reads=["kt", "qt"], writes=[("ps", b)])
                    for bk in range(2):
                        j0, j1 = bk * 4, min(nk, bk * 4 + 4)
                        if j0 >= j1:
                            continue
                        b = banks[bk]
                        jb = min(j1, max(j0, nbias))
                        if jb > j0:
                            ph.dve(lambda e, b=b, j0=j0, jb=jb, ss=ss, bi=bi: e.scalar_tensor_tensor(
                                out=ss[:, j0 * 128:jb * 128], in0=PS[b][:, (j0 % 4) * 128:(j0 % 4) * 128 + (jb - j0) * 128], scalar=0.125,
                                in1=bi[:, j0 * 128:jb * 128], op0=ALU.mult, op1=ALU.add),
                                reads=[("ps", b), ("bi", it % 3)], writes=[("ss", it % 2, bk)])
                            ph.act(lambda e, j0=j0, jb=jb, ss=ss, pt=pt: e.activation(out=pt[:, j0 * 128:jb * 128], in_=ss[:, j0 * 128:jb * 128], func=AF.Exp),
                                   reads=[("ss", it % 2, bk)], writes=[("pt", it % 4)])
                        if j1 > jb:
                            ph.act(lambda e, b=b, jb=jb, j1=j1, pt=pt: e.activation(out=pt[:, jb * 128:j1 * 128], in_=PS[b][:, (jb % 4) * 128:(jb % 4) * 128 + (j1 - jb) * 128],
                                                                                 func=AF.Exp, scale=0.125),
                                   reads=[("ps", b)], writes=[("pt", it % 4)])

                def stage_b(it, e_, t):
                    r0 = 64 * e_
                    h = 2 * m + e_
                    kts, nk = info[it]
                    pt = PT[it % 4]
                    rd = RD[it % 2]
                    po = nb()
                    for j, kt in enumerate(kts):
                        ph.pe(lambda e, po=po, j=j, kt=kt, pt=pt, nk=nk: e.matmul(PS[po][:, 0:128], VK[:, kt, :], pt[:, j * 128:(j + 1) * 128],
                                                                              start=(j == 0), stop=(j == nk - 1)),
                              reads=["vk", ("pt", it % 4)], writes=[("ps", po)])
                    for j, kt in enumerate(kts):
                        ph.pe(lambda e, po=po, j=j, pt=pt, nk=nk: e.matmul(PS[po][:, 128:256], ONESB, pt[:, j * 128:(j + 1) * 128],
                                                                       start=(j == 0), stop=(j == nk - 1)),
                              reads=[("pt", it % 4)], writes=[("ps", po)])
                    if l == 1:
                        ph.dve(lambda e, po=po, rd=rd, r0=r0, h=h: e.tensor_scalar(out=rd[r0:r0 + 64, :], in0=PS[po][r0:r0 + 64, 128:256], scalar1=ES[r0:r0 + 64, h:h + 1],
                                                                                scalar2=None, op0=ALU.add), reads=[("ps", po), "es"], writes=[("rd", it % 2)])
                        ph.dve(lambda e, rd=rd, r0=r0: e.reciprocal(out=rd[r0:r0 + 64, :], in_=rd[r0:r0 + 64, :]), reads=[("rd", it % 2)], writes=[("rd", it % 2)])
                    else:
                        ph.dve(lambda e, po=po, rd=rd, r0=r0: e.reciprocal(out=rd[r0:r0 + 64, :], in_=PS[po][r0:r0 + 64, 128:256]),
                               reads=[("ps", po)], writes=[("rd", it % 2)])
                    ph.dve(lambda e, po=po, rd=rd, r0=r0, t=t: e.tensor_tensor(out=OT[r0:r0 + 64, t * 128:(t + 1) * 128], in0=PS[po][r0:r0 + 64, 0:128],
                                                                           in1=rd[r0:r0 + 64, :], op=ALU.mult),
                           reads=[("ps", po), ("rd", it % 2)], writes=[("ot", e_, t)])

                LAG = ATT_LAG
                for i in range(len(iters) + LAG):
                    if i < len(iters):
                        stage_a(i, *iters[i])
                    if i >= LAG:
                        stage_b(i - LAG, *iters[i - LAG])
                ph.dma("sp", YCAT_d[m, :, 0:nq], OT[:, 0:nq], reads=["ot"])
                ph.emit()
                if l == 0 and m == 0:
                    dump("OT0", OT[:], (128, T))

        MF = CF[:, 256:384]
        MB = CF[:, 384:512]
        SFm = CF[:, 512:640]
        SBm = CF[:, 640:768]
        SST_d = nc.dram_tensor("sst_scr", [2, NT, 128, 512], BF16, kind="Internal").ap()

        def scan_phase(P, groups, XS, BTOK, BT, CT, A4, DT4, const_decay, post_fn, out_tiles, pre_fn=None, H=8):
            HP = H * P
            nyb = HP // 512
            NTA = 1 if const_decay else NT
            ti = (lambda t: 0) if const_decay else (lambda t: t)
            oes = ExitStack()
            ESC = oes.enter_context(SBT("esc", [128, NTA, 2, H], F32))
            with ExitStack() as pes:
                ph = Phase(ctx, "scan")
                al = lambda name, shape, dtype: pes.enter_context(SBT(name, shape, dtype))
                PS = [pes.enter_context(PST("sps%d" % i, [128, 512], F32)) for i in range(8)]
                psi = [0]

                def nb():
                    psi[0] = (psi[0] + 1) % 8
                    return psi[0]
                if pre_fn is not None:
                    pre_fn(ph, al, PS, nb)
                CUMS = al("cums", [128, NTA, 3, 2 * H], F32)
                EW = al("ew", [128, NTA, 2, H], F32)
                ETOT = al("etot", [128, NTA, 2, H], F32)
                S = [al("st%d" % d, [128, 512], F32) for d in range(2)]
                STMP = al("stmp", [128, 512], F32)
                SBF = [al("sbf%d" % i, [128, 512], BF16) for i in range(3)]
                XW = [al("xw%d" % i, [128, HP], BF16) for i in range(2)]
                for t in range(NTA):
                    b = nb()
                    for ci, lm in enumerate((MF, MB, ONESF)):
                        ph.pe(lambda e, b=b, ci=ci, lm=lm, t=t: e.matmul(PS[b][:, ci * 2 * H:(ci + 1) * 2 * H], lm, A4[:, t].rearrange("p d h -> p (d h)"),
                                                                         start=True, stop=True), reads=["A4", "CF"], writes=[("ps", b)])
                    ph.act(lambda e, b=b, t=t: e.copy(out=CUMS[:, t].rearrange("p c n -> p (c n)"), in_=PS[b][:, 0:6 * H]), reads=[("ps", b)], writes=[("cums", t)])
                ph.act(lambda e: e.activation(out=ETOT[:].rearrange("p t d h -> p t (d h)"), in_=CUMS[:, :, 2, :], func=AF.Exp), reads=["cums"], writes=["etot"])
                for d in range(2):
                    ph.act(lambda e, d=d: e.activation(out=ESC[:, :, d, :], in_=CUMS[:, :, d, d * H:(d + 1) * H], func=AF.Exp), reads=["cums"], writes=[("esc", d)])
                    ph.dve(lambda e, d=d: e.tensor_tensor(out=EW[:, :, d, :], in0=CUMS[:, :, 2, d * H:(d + 1) * H], in1=CUMS[:, :, d, d * H:(d + 1) * H], op=ALU.subtract),
                           reads=["cums"], writes=[("ew", d)])
                    ph.act(lambda e, d=d: e.activation(out=EW[:, :, d, :], in_=EW[:, :, d, :], func=AF.Exp), reads=[("ew", d)], writes=[("ew", d)])
                    if DT4 is not None:
                        ph.dve(lambda e, d=d: e.tensor_tensor(out=EW[:, :, d, :], in0=EW[:, :, d, :], in1=DT4[:, :, d, :], op=ALU.mult),
                               reads=[("ew", d), "DT4"], writes=[("ew", d)])
                order = {0: [16, 17] + list(range(16)), 1: [17, 16] + list(range(15, -1, -1))}
                si = 0
                for d in range(2):
                    ph.dve(lambda e, d=d: e.memset(S[d][:], 0.0), writes=[("st", d)])
                for step in range(NT):
                    for d in range(2):
                        t = order[d][step]
                        sbf = SBF[si % 3]
                        xw = XW[si % 2]
                        ph.act(lambda e, sbf=sbf, d=d: e.copy(out=sbf[:], in_=S[d][:]), reads=[("st", d)], writes=[("sbf", si % 3)])
                        ph.dma("sp", SST_d[d, t], sbf[:], reads=[("sbf", si % 3)], writes=[("sst", d, t)])
                        if step < NT - 1:
                            ph.any2(lambda e, xw=xw, t=t, d=d: e.tensor_tensor(out=xw[:].rearrange("p (h q) -> p h q", h=H), in0=XS[:, t].rearrange("p (h q) -> p h q", h=H),
                                                                          in1=EW[:, ti(t), d, :].unsqueeze(2).broadcast_to([128, H, P]), op=ALU.mult),
                                    reads=["XS", ("ew", d)], writes=[("xw", si % 2)])
                            bks = [nb() for _ in range(nyb)]
                            for gi, g in enumerate(groups):
                                pc0 = g["heads"][0] * P
                                pcn = len(g["heads"]) * P
                                ph.pe(lambda e, g=g, pc0=pc0, pcn=pcn, xw=xw, t=t, bks=bks: e.matmul(
                                    PS[bks[pc0 // 512]][:, pc0 % 512:pc0 % 512 + pcn], BTOK[:, t, g["chunk"] * 128:(g["chunk"] + 1) * 128], xw[:, pc0:pc0 + pcn], start=True, stop=True),
                                    reads=["BTOK", ("xw", si % 2)], writes=[("ps", bks[pc0 // 512])])
                            for gi, g in enumerate(groups):
                                r0, nr, nh = g["row0"], g["nrows"], len(g["heads"])
                                h0 = g["heads"][0]
                                pc0 = h0 * P
                                pcn = nh * P
                                sc0 = g["scol0"]
                                ph.pool(lambda e, r0=r0, nr=nr, nh=nh, h0=h0, sc0=sc0, pcn=pcn, d=d, t=t: e.tensor_tensor(
                                    out=STMP[r0:r0 + nr, sc0:sc0 + pcn].rearrange("p (h q) -> p h q", h=nh), in0=S[d][r0:r0 + nr, sc0:sc0 + pcn].rearrange("p (h q) -> p h q", h=nh),
                                    in1=ETOT[r0:r0 + nr, ti(t), d, h0:h0 + nh].unsqueeze(2).broadcast_to([nr, nh, P]), op=ALU.mult),
                                    reads=[("st", d), "etot", ("sbf", si % 3)], writes=[("stmp", gi)])
                                ph.dve(lambda e, r0=r0, nr=nr, sc0=sc0, pc0=pc0, pcn=pcn, d=d, bks=bks: e.tensor_tensor(
                                    out=S[d][r0:r0 + nr, sc0:sc0 + pcn], in0=PS[bks[pc0 // 512]][r0:r0 + nr, pc0 % 512:pc0 % 512 + pcn], in1=STMP[r0:r0 + nr, sc0:sc0 + pcn], op=ALU.add),
                                    reads=[("stmp", gi), ("ps", bks[pc0 // 512])], writes=[("st", d, gi)])
                        si += 1
                ph.emit()
            with ExitStack() as pes:
                ph = Phase(ctx, "scan2")
                al = lambda name, shape, dtype: pes.enter_context(SBT(name, shape, dtype))
                PS = [pes.enter_context(PST("tps%d" % i, [128, 512], F32)) for i in range(7)]
                PBT = pes.enter_context(PST("tpb", [128, 1024], BF16))
                psi = [0]

                def nb():
                    psi[0] = (psi[0] + 1) % 7
                    return psi[0]
                ng = len(groups)
                GM = [[al("gm%d_%d" % (tp, d), [128, ng, 128], F32) for d in range(2)] for tp in range(2)]
                RH1 = al("rh", [128, H, 128], F32)
                RHS = [RH1, RH1]
                EXPD = [al("expd%d" % d, [128, H, 128], F32) for d in range(2)]
                XD = [al("xd%d" % i, [128, HP], BF16) for i in range(4)] if DT4 is not None else None
                MP = [al("mp%d" % i, [128, H, 128], BF16) for i in range(4)]
                SW = max(g["scol0"] + len(g["heads"]) * P for g in groups)
                SIN = [al("sin%d" % i, [128, SW], BF16) for i in range(4)]
                YT1 = al("yt", [128, HP], F32)
                YT = [YT1, YT1]
                Y = [al("y%d" % i, [128, HP], F32) for i in range(2)]
                post_state = post_fn("init", ph, al, PS, nb, PBT)
                if dbg_d and "SST" in dbg_d:
                    sf = al("dbgsst", [128, 512], F32)
                    ph.dma("sp", SIN[0][:], SST_d[0, 17], writes=[("sin", 0)])
                    ph.dve(lambda e: e.tensor_copy(out=sf[:], in_=SIN[0][:]), reads=[("sin", 0)], writes=["sf"])
                    ph.dma("sp", dbg_d["SST"], sf[:], reads=["sf"])
                masks = (MF, MB)
                u1 = (SFm, SBm)

                def build_expd(t, d):
                    RH = RHS[d]
                    ph.any2(lambda e, t=t, d=d, RH=RH: e.tensor_tensor(out=RH[:], in0=masks[d].unsqueeze(1).broadcast_to([128, H, 128]),
                                                                       in1=A4[:, t, d, :].unsqueeze(2).broadcast_to([128, H, 128]), op=ALU.mult),
                            reads=["A4", "CF"], writes=["rh"])
                    for hh in range(H // 4):
                        b = nb()
                        ph.pe(lambda e, b=b, hh=hh, d=d, RH=RH: e.matmul(PS[b][:], u1[d], RH[:, hh * 4:(hh + 1) * 4, :].rearrange("p h i -> p (h i)"), start=True, stop=True),
                              reads=["rh", "CF"], writes=[("ps", b)])
                        ph.act(lambda e, b=b, hh=hh, d=d: e.activation(out=EXPD[d][:, hh * 4:(hh + 1) * 4, :].rearrange("p h i -> p (h i)"), in_=PS[b][:], func=AF.Exp),
                               reads=[("ps", b)], writes=[("expd", d, hh)])
                if const_decay:
                    for d in range(2):
                        build_expd(0, d)
                it = 0
                def p2_stage_a(ti_, t):
                    tsl = slice(t * 128, (t + 1) * 128)
                    tp = ti_ % 2
                    gbanks = []
                    for gi, g in enumerate(groups):
                        if not gbanks or groups[gbanks[-1][1]]["row0"] != g["row0"] or gbanks[-1][2] == 4:
                            gbanks.append([nb(), gi, 0])
                        bk, g0, n = gbanks[-1]
                        r0, nr, ch = g["row0"], g["nrows"], g["chunk"]
                        ph.pe(lambda e, bk=bk, n=n, r0=r0, nr=nr, ch=ch, tsl=tsl: e.matmul(PS[bk][:, n * 128:(n + 1) * 128], BT[r0:r0 + nr, ch, tsl], CT[r0:r0 + nr, ch, tsl],
                                                                                       start=True, stop=True), reads=["BT", "CT"], writes=[("ps", bk)])
                        gbanks[-1][2] += 1
                    for d in range(2):
                        for (bk, g0, n) in gbanks:
                            ph.dve(lambda e, d=d, bk=bk, g0=g0, n=n, tp=tp: e.tensor_tensor(out=GM[tp][d][:, g0:g0 + n, :], in0=PS[bk][:, 0:n * 128].rearrange("p (g i) -> p g i", g=n),
                                                                                        in1=masks[d].unsqueeze(1).broadcast_to([128, n, 128]), op=ALU.mult),
                                   reads=[("ps", bk), "CF"], writes=[("gm", tp, d, g0)])
                    for d in range(2):
                        slot = tp * 2 + d
                        mp = MP[slot]
                        sin = SIN[slot]
                        ph.dma("sp", sin[:], SST_d[d, t, :, 0:SW], writes=[("sin", slot)])
                        if not const_decay:
                            build_expd(t, d)
                        gmb = GM[tp][d][:] if ng == H else GM[tp][d][:, 0:1, :].broadcast_to([128, H, 128])
                        if DT4 is not None:
                            xd = XD[slot]
                            ph.any2(lambda e, d=d, t=t, xd=xd: e.tensor_tensor(out=xd[:].rearrange("p (h q) -> p h q", h=H), in0=XS[:, t].rearrange("p (h q) -> p h q", h=H),
                                                                              in1=DT4[:, t, d, :].unsqueeze(2).broadcast_to([128, H, P]), op=ALU.mult),
                                    reads=["XS", "DT4"], writes=[("xd", slot)])
                        ph.any2(lambda e, mp=mp, gmb=gmb, d=d: e.tensor_tensor(out=mp[:], in0=EXPD[d][:], in1=gmb, op=ALU.mult),
                                reads=[("expd", d), ("gm", tp, d)], writes=[("mp", slot)])

                def p2_stage_b(ti_, t):
                    tsl = slice(t * 128, (t + 1) * 128)
                    tp = ti_ % 2
                    for d in range(2):
                        slot = tp * 2 + d
                        mp = MP[slot]
                        sin = SIN[slot]
                        yd = [nb() for _ in range(nyb)]
                        for h in range(H):
                            xrhs = XD[slot][:, h * P:(h + 1) * P] if DT4 is not None else XS[:, t, h * P:(h + 1) * P]
                            ph.pe(lambda e, h=h, mp=mp, yd=yd, xrhs=xrhs: e.matmul(PS[yd[h * P // 512]][:, (h * P) % 512:(h * P) % 512 + P], mp[:, h, :], xrhs, start=True, stop=True),
                                  reads=[("mp", slot), "XS", ("xd", slot)], writes=[("ps", yd[h * P // 512])])
                        ybanks = []
                        for gi, g in enumerate(groups):
                            r0, nr, ch = g["row0"], g["nrows"], g["chunk"]
                            pc0 = g["heads"][0] * P
                            pcn = len(g["heads"]) * P
                            sc0 = g["scol0"]
                            if not ybanks or ybanks[-1][5] != r0 or ybanks[-1][2] + pcn > 512:
                                ybanks.append([nb(), pc0, 0, g["heads"][0], 0, r0])
                            bk, used = ybanks[-1][0], ybanks[-1][2]
                            ph.pe(lambda e, r0=r0, nr=nr, ch=ch, pcn=pcn, sc0=sc0, sin=sin, bk=bk, used=used, tsl=tsl: e.matmul(
                                PS[bk][:, used:used + pcn], CT[r0:r0 + nr, ch, tsl], sin[r0:r0 + nr, sc0:sc0 + pcn], start=True, stop=True),
                                reads=["CT", ("sin", slot)], writes=[("ps", bk)])
                            ybanks[-1][2] += pcn
                            ybanks[-1][4] += len(g["heads"])
                        yacc = Y[0] if d == 0 else Y[1]
                        yt = YT[d]
                        for (bk, c0, cn, h0, nh, _) in ybanks:
                            ph.dve(lambda e, bk=bk, c0=c0, cn=cn, h0=h0, nh=nh, t=t, d=d, yt=yt: e.tensor_tensor(
                                out=yt[:, c0:c0 + cn].rearrange("p (h q) -> p h q", h=nh), in0=PS[bk][:, 0:cn].rearrange("p (h q) -> p h q", h=nh),
                                in1=ESC[:, ti(t), d, h0:h0 + nh].unsqueeze(2).broadcast_to([128, nh, P]), op=ALU.mult),
                                reads=[("ps", bk), ("esc", d)], writes=[("yt", c0)])
                        for q in range(nyb):
                            csl = slice(q * 512, (q + 1) * 512)
                            ph.dve(lambda e, q=q, yd=yd, csl=csl, yacc=yacc, yt=yt: e.tensor_tensor(out=yacc[:, csl], in0=PS[yd[q]][:], in1=yt[:, csl], op=ALU.add),
                                   reads=[("ps", yd[q]), "yt"], writes=[("y", d, q)])
                    if not (dbg_d and "Yall" in dbg_d):
                        ph.pool(lambda e: e.tensor_tensor(out=Y[0][:], in0=Y[0][:], in1=Y[1][:], op=ALU.add), reads=[("y", 0), ("y", 1)], writes=[("y", 0)])
                    if dbg_d and "Yall" in dbg_d and HP == 512:
                        ph.dma("sp", dbg_d["Yall"][:, t * 512:(t + 1) * 512], Y[0][:], reads=[("y", 0)])
                        ph.dma("sp", dbg_d["Y1all"][:, t * 512:(t + 1) * 512], Y[1][:], reads=[("y", 1)])
                    post_fn("tile", ph, al, PS, nb, post_state, t, Y[0])

                otl = list(out_tiles)
                for i in range(len(otl) + 1):
                    if i < len(otl):
                        p2_stage_a(i, otl[i])
                    if i >= 1:
                        p2_stage_b(i - 1, otl[i - 1])
                post_fn("fini", ph, al, PS, nb, post_state)
                ph.emit()
            oes.close()

        def ssd_group(g):
            with ExitStack() as ges:
                gal = lambda name, shape, dtype: ges.enter_context(SBT(name, shape, dtype))
                XS = gal("xs", [128, NT, 512], BF16)
                BTOK = gal("btok", [128, NT, 128], BF16)
                BT = gal("bt", [128, 1, T], BF16)
                CT = gal("ct", [128, 1, T], BF16)
                DT4 = gal("dt4", [128, NT, 2, 8], F32)
                A4 = gal("a4", [128, NT, 2, 8], F32)
                WZ = gal("wz", [128, KC, 512], BF16)
                Wr = ev_w_in_d.rearrange("(k p) n -> p k n", p=128)
                with ExitStack() as pes:
                    ph = Phase(ctx, "ssdproj")
                    al = lambda name, shape, dtype: pes.enter_context(SBT(name, shape, dtype))
                    PS = [pes.enter_context(PST("bps%d" % i, [128, 512], F32)) for i in range(6)]
                    PB = [pes.enter_context(PST("bpb%d" % i, [128, 1024], BF16)) for i in range(2)]
                    psi = [0]

                    def nb():
                        psi[0] = (psi[0] + 1) % 6
                        return psi[0]
                    WS = [al("wsl%d" % i, [128, KC, 128], BF16) for i in range(2)]
                    WDT = al("wdt", [128, KC, 16], BF16)
                    DBA = al("dba", [128, 2, 2, 8], F32)
                    XPAD = al("xpad", [128, 2320], F32)
                    ACC = al("acc", [128, T], F32)
                    XST = [al("xst%d" % i, [128, T], BF16) for i in range(2)]
                    ph.dma("pool", WZ[:], Wr[:, :, 1536 + g * 512:1536 + (g + 1) * 512], writes=["wz"])
                    for d in range(2):
                        ph.dma("pool", WDT[:, :, d * 8:(d + 1) * 8], Wr[:, :, 4096 + d * 16 + g * 8:4096 + d * 16 + g * 8 + 8], writes=[("wdt", d)])
                        for w in range(2):
                            ph.dma("sp", DBA[:, w, d, :], dtba_d[w:w + 1, d * 16 + g * 8:d * 16 + g * 8 + 8].broadcast_to([128, 8]), writes=[("dba", w, d)])
                    ph.pool(lambda e: e.memset(XPAD[:], 0.0), writes=["xpad"])
                    b = nb()
                    for t in range(NT):
                        for k in range(KC):
                            ph.pe(lambda e, b=b, t=t, k=k: e.matmul(PS[b][:, t * 16:(t + 1) * 16], HT[:, k, t * 128:(t + 1) * 128], WDT[:, k, :], start=(k == 0), stop=(k == KC - 1)),
                                  reads=["wdt", "HT"], writes=[("ps", b)])
                    dt3 = DT4[:].rearrange("p t d h -> p t (d h)")
                    ph.dve(lambda e, b=b: e.tensor_tensor(out=dt3, in0=PS[b][:, 0:NT * 16].rearrange("p (t n) -> p t n", t=NT),
                                                          in1=DBA[:, 0].rearrange("p d h -> p (d h)").unsqueeze(1).broadcast_to([128, NT, 16]), op=ALU.add),
                           reads=[("ps", b), "dba"], writes=["DT4"])
                    ph.act(lambda e: e.activation(out=dt3, in_=dt3, func=AF.Exp), reads=["DT4"], writes=["DT4"])
                    ph.act(lambda e: e.activation(out=dt3, in_=dt3, func=AF.Ln, bias=1.0), reads=["DT4"], writes=["DT4"])
                    ph.act(lambda e: e.activation(out=DBA[:, 1], in_=DBA[:, 1], func=AF.Exp), reads=["dba"], writes=["dba"])
                    ph.dve(lambda e: e.scalar_tensor_tensor(out=A4[:].rearrange("p t d h -> p t (d h)"), in0=dt3, scalar=-1.0,
                                                            in1=DBA[:, 1].rearrange("p d h -> p (d h)").unsqueeze(1).broadcast_to([128, NT, 16]), op0=ALU.mult, op1=ALU.mult),
                           reads=["DT4", "dba"], writes=["A4"])
                    chunks = [4 * g + i for i in range(4)] + [8 + g, 10 + g]
                    for ci, c in enumerate(chunks):
                        ws = WS[ci % 2]
                        ph.dma("pool", ws[:], Wr[:, :, 2560 + c * 128:2560 + (c + 1) * 128], writes=[("wsl", ci % 2)])
                        for (t0, tn) in BLKS:
                            b = nb()
                            for k in range(KC):
                                ph.pe(lambda e, b=b, k=k, ws=ws, t0=t0, tn=tn: e.matmul(PS[b][:, :tn], ws[:, k, :], HT[:, k, t0:t0 + tn], start=(k == 0), stop=(k == KC - 1)),
                                      reads=[("wsl", ci % 2), "HT"], writes=[("ps", b)])
                            o0 = 2 + t0 if t0 < L else 2054 + (t0 - L)
                            ph.act(lambda e, b=b, o0=o0, tn=tn: e.copy(out=XPAD[:, o0:o0 + tn], in_=PS[b][:, :tn]), reads=[("ps", b)], writes=[("xpad", t0)])
                        eng = "dve"
                        for (o0, a0, n) in ((2, 0, L), (2054, L, LC)):
                            wcol = lambda k, c=c: VT[:, VR["conv_w"] + k * 12 + c:VR["conv_w"] + k * 12 + c + 1]
                            bcol = VT[:, VR["conv_b"] + c:VR["conv_b"] + c + 1]
                            ph.op(eng, lambda e, o0=o0, a0=a0, n=n, wcol=wcol, bcol=bcol: e.tensor_scalar(out=ACC[:, a0:a0 + n], in0=XPAD[:, o0 - 2:o0 - 2 + n], scalar1=wcol(0), scalar2=bcol,
                                                                                                    op0=ALU.mult, op1=ALU.add), reads=["xpad", "VT"], writes=[("acc", a0)])
                            for k in range(1, 5):
                                ph.op(eng, lambda e, o0=o0, a0=a0, n=n, k=k, wcol=wcol: e.scalar_tensor_tensor(out=ACC[:, a0:a0 + n], in0=XPAD[:, o0 - 2 + k:o0 - 2 + k + n], scalar=wcol(k),
                                                                                                      in1=ACC[:, a0:a0 + n], op0=ALU.mult, op1=ALU.add),
                                      reads=["xpad", ("acc", a0)], writes=[("acc", a0)])
                        if ci < 4:
                            dst, dkey = XST[ci % 2][:], ("xst", ci % 2)
                        elif ci == 4:
                            dst, dkey = BT[:, 0, :], "BT"
                        else:
                            dst, dkey = CT[:, 0, :], "CT"
                        ph.act(lambda e, dst=dst: e.activation(out=dst, in_=ACC[:], func=AF.Silu), reads=["acc"], writes=[dkey])
                        if ci <= 4:
                            for t8 in range(0, NT, 8):
                                n8 = min(8, NT - t8)
                                pb = (t8 // 8 + ci) % 2
                                for tt in range(n8):
                                    t = t8 + tt
                                    ph.pe(lambda e, pb=pb, tt=tt, t=t, dst=dst: e.transpose(PB[pb][:, tt * 128:(tt + 1) * 128], dst[:, t * 128:(t + 1) * 128], IDB),
                                          reads=[dkey, "CB"], writes=[("pb", pb)])
                                if ci < 4:
                                    o = XS[:, t8:t8 + n8, ci * 128:(ci + 1) * 128]
                                    okey = ("XS", ci, t8)
                                else:
                                    o = BTOK[:, t8:t8 + n8, :]
                                    okey = ("BTOK", t8)
                                ph.act(lambda e, pb=pb, n8=n8, o=o: e.copy(out=o, in_=PB[pb][:, 0:n8 * 128].rearrange("p (a n) -> p a n", a=n8)),
                                       reads=[("pb", pb)], writes=[okey])
                    ph.emit()
                if g == 0:
                    dump("XS0", XS[:].rearrange("p t n -> p (t n)"), (128, NT * 512))
                    dump("A40", A4[:].rearrange("p t d h -> p (t d h)"), (128, NT * 16))
                    dump("DT40", DT4[:].rearrange("p t d h -> p (t d h)"), (128, NT * 16))
                    dump("CT0", CT[:, 0, :], (128, T))
                    dump("BTOK0", BTOK[:].rearrange("p t n -> p (t n)"), (128, NT * 128))

                def post(stage, ph, al, PS, nb, st=None, t=None, Yt=None):
                    if stage == "init":
                        pbt = st
                        st = {}
                        st["dsk"] = al("dsk", [128, 8], F32)
                        st["gng"] = al("gng", [128, 512], F32)
                        st["sz"] = al("sz", [128, 512], F32)
                        st["yz"] = al("yz", [128, 512], F32)
                        st["sq"] = st["sz"]
                        st["ssq"] = al("ssq", [128, 4], F32)
                        st["yn"] = al("yn", [128, 512], BF16)
                        st["stg"] = [al("stg%d" % i, [128, 4, 128], BF16) for i in range(2)]
                        st["pb"] = pbt
                        ph.dma("sp", st["dsk"][:], ssmd_d[0:1, g * 8:(g + 1) * 8].broadcast_to([128, 8]), writes=["dsk"])
                        ph.dma("sp", st["gng"][:], ssmg_d[0:1, g * 512:(g + 1) * 512].broadcast_to([128, 512]), writes=["gng"])
                        return st
                    if stage == "fini":
                        return
                    dsk, gng, sz, yz, sq, ssq, yn = st["dsk"], st["gng"], st["sz"], st["yz"], st["sq"], st["ssq"], st["yn"]
                    b = nb()
                    for k in range(KC):
                        ph.pe(lambda e, b=b, k=k: e.matmul(PS[b][:], HT[:, k, t * 128:(t + 1) * 128], WZ[:, k, :], start=(k == 0), stop=(k == KC - 1)),
                              reads=["HT", "wz"], writes=[("ps", b)])
                    ph.act(lambda e, b=b: e.activation(out=sz[:], in_=PS[b][:], func=AF.Silu), reads=[("ps", b)], writes=["sz"])
                    ph.dve(lambda e: e.tensor_tensor(out=yz[:].rearrange("p (h q) -> p h q", h=8), in0=XS[:, t].rearrange("p (h q) -> p h q", h=8),
                                                     in1=dsk[:].unsqueeze(2).broadcast_to([128, 8, 64]), op=ALU.mult), reads=["XS", "dsk"], writes=["yz"])
                    ph.dve(lambda e: e.tensor_tensor(out=yz[:], in0=yz[:], in1=Yt[:], op=ALU.add), reads=["yz", ("y", 0)], writes=["yz"])
                    ph.dve(lambda e: e.tensor_tensor(out=yz[:], in0=yz[:], in1=sz[:], op=ALU.mult), reads=["yz", "sz"], writes=["yz"])
                    ph.act(lambda e: e.activation(out=sq[:], in_=yz[:], func=AF.Square, accum_out=ssq[:, 0:1]), reads=["yz"], writes=["sz", "ssq"])
                    ph.dve(lambda e: e.tensor_scalar(out=ssq[:, 1:2], in0=ssq[:, 0:1], scalar1=1.0 / 512, scalar2=EPS, op0=ALU.mult, op1=ALU.add), reads=["ssq"], writes=["ssq"])
                    ph.act(lambda e: e.sqrt(out=ssq[:, 2:3], in_=ssq[:, 1:2]), reads=["ssq"], writes=["ssq"])
                    ph.dve(lambda e: e.reciprocal(out=ssq[:, 3:4], in_=ssq[:, 2:3]), reads=["ssq"], writes=["ssq"])
                    ph.dve(lambda e: e.scalar_tensor_tensor(out=yn[:], in0=yz[:], scalar=ssq[:, 3:4], in1=gng[:], op0=ALU.mult, op1=ALU.mult),
                           reads=["yz", "ssq", "gng"], writes=["yn"])
                    pb = st["pb"]
                    stg = st["stg"][t % 2]
                    for cl in range(4):
                        ph.pe(lambda e, cl=cl: e.transpose(pb[:, cl * 128:(cl + 1) * 128], yn[:, cl * 128:(cl + 1) * 128], IDB), reads=["yn", "CB"], writes=["pbt"])
                    ph.act(lambda e, stg=stg: e.copy(out=stg[:], in_=pb[:, 0:512].rearrange("p (a n) -> p a n", a=4)), reads=["pbt"], writes=[("stg", t % 2)])
                    ph.dma("sp", YCAT_d[4 + 4 * g:8 + 4 * g, :, t * 128:(t + 1) * 128].rearrange("c p t -> p c t"), stg[:], reads=[("stg", t % 2)])

                pes_pb = [None]

                def pre(ph, al, PS, nb):
                    pass
                groups = [dict(chunk=0, row0=0, nrows=128, heads=list(range(8)), scol0=0, sncols=512)]
                scan_phase(64, groups, XS, BTOK, BT, CT, A4, DT4, False, post, list(range(NT)))

        def out_proj(l):
            W_d = ev_w_out_d if l == 0 else od_w_out_d
            with ExitStack() as pes:
                ph = Phase(ctx, "oproj")
                al = lambda name, shape, dtype: pes.enter_context(SBT(name, shape, dtype))
                PS = [pes.enter_context(PST("ops%d" % i, [128, 512], F32)) for i in range(8)]
                WO = al("wo", [128, 12, D], BF16)
                YB = [al("yb%d" % i, [128, 12, 512], BF16) for i in range(2)]
                for c in range(0, 12, 4):
                    ph.dma("pool", WO[:, c:c + 4, :], W_d.rearrange("(c p) n -> p c n", p=128)[:, c:c + 4, :], writes=[("wo", c)])
                pi = 0
                for bi_, (t0, tn) in enumerate(BLKS):
                    if l == 1 and t0 >= L:
                        continue
                    j = 0 if t0 < L else 1
                    yb = YB[bi_ % 2]
                    ph.dma("sp", yb[:, :, :tn], YCAT_d[:, :, t0:t0 + tn].rearrange("c p t -> p c t"), writes=[("yb", bi_ % 2)])
                    for dc in range(KC):
                        b = pi % 8
                        pi += 1
                        for c in range(12):
                            ph.pe(lambda e, b=b, c=c, dc=dc, yb=yb, tn=tn: e.matmul(PS[b][:, :tn], WO[:, c, dc * 128:(dc + 1) * 128], yb[:, c, :tn], start=(c == 0), stop=(c == 11)),
                                  reads=["wo", ("yb", bi_ % 2)], writes=[("ps", b)])
                        ph.dve(lambda e, b=b, dc=dc, t0=t0, tn=tn, j=j: e.scalar_tensor_tensor(out=XT[:, dc, t0:t0 + tn], in0=PS[b][:, :tn], scalar=AB[:, l, j, 2, dc:dc + 1],
                                                                                            in1=XT[:, dc, t0:t0 + tn], op0=ALU.mult, op1=ALU.add),
                               reads=[("ps", b)], writes=[("XT", dc, bi_)])
                ph.emit()

        THIRDS = [[(0, 512), (512, 256)], [(768, 512), (1280, 256)], [(1536, 512), (2048, 256)]]

        def ffn(l, GT=None):
            moe = (l == 1)
            nfc = 28 if moe else 22
            nexp = NEXPERTS if moe else 1
            with ExitStack() as pes:
                ph = Phase(ctx, "ffn")
                al = lambda name, shape, dtype: pes.enter_context(SBT(name, shape, dtype))
                PS = [pes.enter_context(PST("fps%d" % i, [128, 512], F32)) for i in range(8)]
                psi = [0]

                def nb():
                    psi[0] = (psi[0] + 1) % 8
                    return psi[0]
                tgroups = [[(0, 512), (512, 512)], [(1024, 512), (1536, 512)]] if moe else THIRDS
                gmax = 1024 if moe else 768
                fchunks = [list(range(0, 14)), list(range(14, 28))] if moe else [list(range(22))]
                nfl = len(fchunks[0])
                ACTT = al("actt", [128, nfl, gmax], BF16)
                W13 = [al("w13_%d" % i, [128, 2, KC, 128], BF16) for i in range(3)]
                W2S = [al("w2s_%d" % i, [128, nfl, 128], BF16) for i in range(2)]
                SIL = [al("sil%d" % i, [128, 512], F32) for i in range(2)]
                if moe:
                    HG = al("hg", [128, KC, gmax], BF16)
                wi = 0
                w2i = 0
                si = 0
                for th, blks in enumerate(tgroups):
                    tb = blks[0][0]
                    for ex in range(nexp):
                        if moe:
                            w1_d, w3_d, w2_d = moe_w1_d[ex], moe_w3_d[ex], moe_w2_d[ex]
                            for (t0, tn) in blks:
                                b = nb()
                                ph.pe(lambda e, b=b, ex=ex, t0=t0, tn=tn: e.matmul(PS[b][:, :tn], SEL[:, ex * 128:(ex + 1) * 128], GT[:, t0:t0 + tn], start=True, stop=True),
                                      reads=["GT", "SEL"], writes=[("ps", b)])
                                for k in range(KC):
                                    ph.dve(lambda e, b=b, k=k, t0=t0, tn=tn, tb=tb: e.tensor_tensor(out=HG[:, k, t0 - tb:t0 - tb + tn], in0=HT[:, k, t0:t0 + tn], in1=PS[b][:, :tn], op=ALU.mult),
                                           reads=[("ps", b), "HT"], writes=[("hg", k, t0)])
                        else:
                            w1_d, w3_d, w2_d = ffn_w1_d, ffn_w3_d, ffn_w2_d
                        w1r = w1_d.rearrange("(k p) n -> p k n", p=128)
                        w3r = w3_d.rearrange("(k p) n -> p k n", p=128)
                        w2r = w2_d.rearrange("(f p) n -> p f n", p=128)
                        for fcs in fchunks:
                            for fi, fc in enumerate(fcs):
                                w = W13[wi % 3]
                                ph.dma("pool", w[:, 0], w1r[:, :, fc * 128:(fc + 1) * 128], writes=[("w13", wi % 3, 0)])
                                ph.dma("pool", w[:, 1], w3r[:, :, fc * 128:(fc + 1) * 128], writes=[("w13", wi % 3, 1)])
                                for (t0, tn) in blks:
                                    b1, b3 = nb(), nb()
                                    for k in range(KC):
                                        ph.pe(lambda e, b1=b1, k=k, w=w, t0=t0, tn=tn: e.matmul(PS[b1][:, :tn], w[:, 0, k, :], HT[:, k, t0:t0 + tn], start=(k == 0), stop=(k == KC - 1)),
                                              reads=[("w13", wi % 3, 0), "HT"], writes=[("ps", b1)])
                                    for k in range(KC):
                                        rhs = HG[:, k, t0 - tb:t0 - tb + tn] if moe else HT[:, k, t0:t0 + tn]
                                        ph.pe(lambda e, b3=b3, k=k, w=w, rhs=rhs, tn=tn: e.matmul(PS[b3][:, :tn], w[:, 1, k, :], rhs, start=(k == 0), stop=(k == KC - 1)),
                                              reads=[("w13", wi % 3, 1), "HT", "hg"], writes=[("ps", b3)])
                                    sil = SIL[si % 2]
                                    ph.act(lambda e, b1=b1, sil=sil, tn=tn: e.activation(out=sil[:, :tn], in_=PS[b1][:, :tn], func=AF.Silu), reads=[("ps", b1)], writes=[("sil", si % 2)])
                                    ph.dve(lambda e, b3=b3, sil=sil, fi=fi, t0=t0, tn=tn, tb=tb: e.tensor_tensor(out=ACTT[:, fi, t0 - tb:t0 - tb + tn], in0=PS[b3][:, :tn], in1=sil[:, :tn], op=ALU.mult),
                                           reads=[("ps", b3), ("sil", si % 2)], writes=[("actt", fi, t0)])
                                    si += 1
                                wi += 1
                            nf = len(fcs)
                            for dc in range(KC):
                                w2 = W2S[w2i % 2]
                                ph.dma("pool", w2[:, 0:nf, :], w2r[:, fcs[0]:fcs[0] + nf, dc * 128:(dc + 1) * 128], writes=[("w2s", w2i % 2)])
                                for (t0, tn) in blks:
                                    j = 0 if t0 < L else 1
                                    b = nb()
                                    for fi in range(nf):
                                        ph.pe(lambda e, b=b, fi=fi, w2=w2, t0=t0, tn=tn, tb=tb, nf=nf: e.matmul(PS[b][:, :tn], w2[:, fi, :], ACTT[:, fi, t0 - tb:t0 - tb + tn], start=(fi == 0), stop=(fi == nf - 1)),
                                              reads=[("w2s", w2i % 2), "actt"], writes=[("ps", b)])
                                    ph.dve(lambda e, b=b, dc=dc, t0=t0, tn=tn, j=j: e.scalar_tensor_tensor(out=XT[:, dc, t0:t0 + tn], in0=PS[b][:, :tn], scalar=AB[:, l, j, 5, dc:dc + 1],
                                                                                                        in1=XT[:, dc, t0:t0 + tn], op0=ALU.mult, op1=ALU.add),
                                           reads=[("ps", b)], writes=[("XT", dc, t0)])
                                w2i += 1
                ph.emit()

        def final_out():
            with ExitStack() as pes:
                ph = Phase(ctx, "fin")
                al = lambda name, shape, dtype: pes.enter_context(SBT(name, shape, dtype))
                PS = [pes.enter_context(PST("zps%d" % i, [128, 512], F32)) for i in range(8)]
                SQ = [al("fsq%d" % i, [128, 512], BF16) for i in range(3)]
                RS = [al("frs%d" % i, [128, 512], F32) for i in range(2)]
                XN = [al("fxn%d" % i, [128, KC, 512], F32) for i in range(2)]
                OTK = [al("fot%d" % i, [128, D], F32) for i in range(3)]
                qi = 0
                oi = 0
                pi = 0
                g0 = VR["final_g"]
                for bi_, (t0, tn) in enumerate(BLKS[:4]):
                    pb = pi % 8
                    pi += 1
                    for k in range(KC):
                        sq = SQ[qi % 3]
                        ph.act(lambda e, sq=sq, k=k, t0=t0: e.activation(out=sq[:], in_=XT[:, k, t0:t0 + 512], func=AF.Square), reads=["XT"], writes=[("sq", qi % 3)])
                        ph.pe(lambda e, sq=sq, k=k, pb=pb: e.matmul(PS[pb][:], ONESB, sq[:], start=(k == 0), stop=(k == KC - 1)), reads=[("sq", qi % 3)], writes=[("ps", pb)])
                        qi += 1
                    rs = RS[bi_ % 2]
                    xn = XN[bi_ % 2]
                    ph.dve(lambda e, rs=rs, pb=pb: e.tensor_scalar(out=rs[:], in0=PS[pb][:], scalar1=1.0 / D, scalar2=EPS, op0=ALU.mult, op1=ALU.add), reads=[("ps", pb)], writes=[("rs", bi_ % 2)])
                    ph.act(lambda e, rs=rs: e.sqrt(out=rs[:], in_=rs[:]), reads=[("rs", bi_ % 2)], writes=[("rs", bi_ % 2)])
                    ph.dve(lambda e, rs=rs: e.reciprocal(out=rs[:], in_=rs[:]), reads=[("rs", bi_ % 2)], writes=[("rs", bi_ % 2)])
                    for k in range(KC):
                        ph.dve(lambda e, k=k, t0=t0, rs=rs, xn=xn: e.scalar_tensor_tensor(out=xn[:, k, :], in0=XT[:, k, t0:t0 + 512], scalar=VT[:, g0 + k:g0 + k + 1], in1=rs[:], op0=ALU.mult, op1=ALU.mult),
                               reads=["XT", ("rs", bi_ % 2)], writes=[("xn", bi_ % 2, k)])
                    for tt in range(4):
                        otk = OTK[oi % 3]
                        for half in range(2):
                            pb = pi % 8
                            pi += 1
                            for kk in range(4):
                                k = half * 4 + kk
                                ph.pe(lambda e, pb=pb, kk=kk, k=k, tt=tt, xn=xn: e.transpose(PS[pb][:, kk * 128:(kk + 1) * 128], xn[:, k, tt * 128:(tt + 1) * 128], IDF),
                                      reads=[("xn", bi_ % 2), "CF"], writes=[("ps", pb)])
                            if half == 0:
                                ph.act(lambda e, pb=pb, otk=otk: e.copy(out=otk[:, 0:512], in_=PS[pb][:]), reads=[("ps", pb)], writes=[("otk", oi % 3, 0)])
                            else:
                                ph.dve(lambda e, pb=pb, otk=otk: e.tensor_copy(out=otk[:, 512:1024], in_=PS[pb][:]), reads=[("ps", pb)], writes=[("otk", oi % 3, 1)])
                        ph.dma("sp", out_d[t0 + tt * 128:t0 + (tt + 1) * 128, :], otk[:], reads=[("otk", oi % 3)])
                        oi += 1
                ph.emit()

        RM = CB[:, 768:896]

        def rope(ph, X, key, nb, PS, al):
            sid = 0
            store = ph.__dict__.setdefault("_rope_store", {})
            if sid not in store:
                COS = al("cos", [128, L], F32)
                SIN = al("sin", [128, L], F32)
                T1 = [al("rt1_%d" % i, [128, 512], F32) for i in range(2)]
                T2 = [al("rt2_%d" % i, [128, 512], F32) for i in range(2)]
                ph.dma("sp", COS[:], rope_d[0], writes=["cos"])
                ph.dma("sp", SIN[:], rope_d[1], writes=["sin"])
                store[sid] = (COS, SIN, T1, T2, [0])
            COS, SIN, T1, T2, cnt = store[sid]
            for (t0, tn) in BLKS[:4]:
                b = nb()
                i = cnt[0] % 2
                cnt[0] += 1
                ph.pe(lambda e, b=b, t0=t0: e.matmul(PS[b][:], RM, X[:, t0:t0 + 512], start=True, stop=True), reads=[key, "CB"], writes=[("ps", b)])
                ph.dve(lambda e, i=i, t0=t0: e.tensor_tensor(out=T1[i][:], in0=X[:, t0:t0 + 512], in1=COS[:, t0:t0 + 512], op=ALU.mult), reads=[key, "cos"], writes=[("rt1", i)])
                ph.dve(lambda e, i=i, b=b, t0=t0: e.tensor_tensor(out=T2[i][:], in0=PS[b][:], in1=SIN[:, t0:t0 + 512], op=ALU.mult), reads=[("ps", b), "sin"], writes=[("rt2", i)])
                ph.pool(lambda e, i=i, t0=t0: e.tensor_tensor(out=X[:, t0:t0 + 512], in0=T1[i][:], in1=T2[i][:], op=ALU.add), reads=[("rt1", i), ("rt2", i)], writes=[(key, "r", t0) if isinstance(key, str) else key])

        def ret_half(hh):
            Wr = od_w_in_d.rearrange("(k p) n -> p k n", p=128)
            with ExitStack() as ges:
                gal = lambda name, shape, dtype: ges.enter_context(SBT(name, shape, dtype))
                XS = gal("rxs", [128, NT, 512], BF16)
                BTOK = gal("rbtok", [128, NT, 256], BF16)
                BT = gal("rbt", [128, 2, T], BF16)
                CT = gal("rct", [128, 2, T], BF16)
                A4 = gal("ra4", [128, 1, 2, 4], F32)
                WG = gal("rwg", [128, KC, 512], BF16)
                with ExitStack() as pes:
                    ph = Phase(ctx, "retproj")
                    al = lambda name, shape, dtype: pes.enter_context(SBT(name, shape, dtype))
                    PS = [pes.enter_context(PST("rps%d" % i, [128, 512], F32)) for i in range(6)]
                    PB = [pes.enter_context(PST("rpb%d" % i, [128, 1024], BF16)) for i in range(2)]
                    psi = [0]

                    def nb():
                        psi[0] = (psi[0] + 1) % 6
                        return psi[0]
                    WS = [al("rws%d" % i, [128, KC, 128], BF16) for i in range(2)]
                    WV = al("rwv", [128, KC, 512], BF16)
                    for hs in range(4):
                        hl = RPERM[hs]
                        ph.dma("pool", WG[:, :, hs * 128:(hs + 1) * 128], Wr[:, :, 2816 + (4 * hh + hl) * 128:2816 + (4 * hh + hl + 1) * 128], writes=[("wg", hs)])
                        ph.dma("pool", WV[:, :, hs * 128:(hs + 1) * 128], Wr[:, :, 1792 + (4 * hh + hl) * 128:1792 + (4 * hh + hl + 1) * 128], writes=[("wv", hs)])
                        for d in range(2):
                            ph.dma("sp", A4[:, 0, d, hs:hs + 1], retld_d[d:d + 1, 4 * hh + hl:4 * hh + hl + 1].broadcast_to([128, 1]), writes=[("a4", d, hs)])
                    a4f = A4[:].rearrange("p a d h -> p (a d h)")
                    ph.act(lambda e: e.activation(out=a4f, in_=a4f, func=AF.Exp), reads=["a4"], writes=["a4"])
                    ph.act(lambda e: e.activation(out=a4f, in_=a4f, func=AF.Ln, scale=-1.0, bias=1.0), reads=["a4"], writes=["a4"])
                    for t in range(NT):
                        b = nb()
                        for k in range(KC):
                            ph.pe(lambda e, b=b, k=k, t=t: e.matmul(PS[b][:], HT[:, k, t * 128:(t + 1) * 128], WV[:, k, :], start=(k == 0), stop=(k == KC - 1)),
                                  reads=["HT", "wv"], writes=[("ps", b)])
                        if t % 2 == 0:
                            ph.act(lambda e, b=b, t=t: e.copy(out=XS[:, t, :], in_=PS[b][:]), reads=[("ps", b)], writes=[("XS", t)])
                        else:
                            ph.dve(lambda e, b=b, t=t: e.tensor_copy(out=XS[:, t, :], in_=PS[b][:]), reads=[("ps", b)], writes=[("XS", t)])
                    wi = 0
                    for (dst, c0, scale, nm) in ((CT, 768, 1.0, "CT"), (BT, 1280, 0.125, "BT")):
                        for c in range(2):
                            ws = WS[wi % 2]
                            ph.dma("pool", ws[:], Wr[:, :, c0 + (2 * hh + c) * 128:c0 + (2 * hh + c + 1) * 128], writes=[("rws", wi % 2)])
                            for (t0, tn) in BLKS:
                                b = nb()
                                for k in range(KC):
                                    ph.pe(lambda e, b=b, k=k, ws=ws, t0=t0, tn=tn: e.matmul(PS[b][:, :tn], ws[:, k, :], HT[:, k, t0:t0 + tn], start=(k == 0), stop=(k == KC - 1)),
                                          reads=[("rws", wi % 2), "HT"], writes=[("ps", b)])
                                ph.act(lambda e, b=b, dst=dst, c=c, t0=t0, tn=tn, scale=scale: e.activation(out=dst[:, c, t0:t0 + tn], in_=PS[b][:, :tn], func=AF.Copy, scale=scale),
                                       reads=[("ps", b)], writes=[(nm, c, "p", t0)])
                            rope(ph, dst[:, c, :], (nm, c), nb, PS, al)
                            if nm == "BT":
                                for t8 in range(0, NT, 8):
                                    n8 = min(8, NT - t8)
                                    pb = (t8 // 8 + c) % 2
                                    for tt in range(n8):
                                        t = t8 + tt
                                        ph.pe(lambda e, pb=pb, tt=tt, t=t, c=c: e.transpose(PB[pb][:, tt * 128:(tt + 1) * 128], BT[:, c, t * 128:(t + 1) * 128], IDB),
                                              reads=[("BT", c), "CB"], writes=[("pb", pb)])
                                    ph.act(lambda e, pb=pb, n8=n8, t8=t8, c=c: e.copy(out=BTOK[:, t8:t8 + n8, c * 128:(c + 1) * 128], in_=PB[pb][:, 0:n8 * 128].rearrange("p (a n) -> p a n", a=n8)),
                                           reads=[("pb", pb)], writes=[("BTOK", c, t8)])
                            wi += 1
                    ph.emit()
                if hh == 0:
                    dump("RXS", XS[:].rearrange("p t n -> p (t n)"), (128, NT * 512))
                    dump("RCT", CT[:].rearrange("p c t -> p (c t)"), (128, 2 * T))
                    dump("RBTOK", BTOK[:].rearrange("p t n -> p (t n)"), (128, NT * 256))
                    dump("RA4", A4[:].rearrange("p a d h -> p (a d h)"), (128, 8))

                def post(stage, ph, al, PS, nb, st=None, t=None, Yt=None):
                    if stage == "init":
                        pbt = st
                        st = {"pb": pbt}
                        st["gng"] = al("rgng", [128, 512], F32)
                        st["gnb"] = al("rgnb", [128, 512], F32)
                        st["sg"] = al("rsg", [128, 512], F32)
                        st["yc"] = al("ryc", [128, 512], F32)
                        st["stat"] = al("rstat", [128, 4, 4], F32)
                        st["yn"] = al("ryn", [128, 512], BF16)
                        st["stg"] = [al("rstg%d" % i, [128, 4, 128], BF16) for i in range(2)]
                        for hs in range(4):
                            c0 = (4 * hh + RPERM[hs]) * 128
                            ph.dma("sp", st["gng"][:, hs * 128:(hs + 1) * 128], retg_d[0:1, c0:c0 + 128].broadcast_to([128, 128]), writes=[("gng", hs)])
                            ph.dma("sp", st["gnb"][:, hs * 128:(hs + 1) * 128], retb_d[0:1, c0:c0 + 128].broadcast_to([128, 128]), writes=[("gnb", hs)])
                        return st
                    if stage == "fini":
                        return
                    gng, gnb, sg, yc, stat, yn = st["gng"], st["gnb"], st["sg"], st["yc"], st["stat"], st["yn"]
                    b = nb()
                    for k in range(KC):
                        ph.pe(lambda e, b=b, k=k: e.matmul(PS[b][:], HT[:, k, t * 128:(t + 1) * 128], WG[:, k, :], start=(k == 0), stop=(k == KC - 1)),
                              reads=["HT", "wg"], writes=[("ps", b)])
                    ph.act(lambda e, b=b: e.activation(out=sg[:], in_=PS[b][:], func=AF.Silu), reads=[("ps", b)], writes=["sg"])
                    y3 = Yt[:].rearrange("p (h q) -> p h q", h=4)
                    yc3 = yc[:].rearrange("p (h q) -> p h q", h=4)
                    ph.dve(lambda e: e.reduce_sum(out=stat[:, 0, :], in_=y3, axis=AX.X), reads=[("y", 0)], writes=[("stat", 0)])
                    ph.dve(lambda e: e.tensor_scalar(out=stat[:, 1, :], in0=stat[:, 0, :], scalar1=-1.0 / 128, scalar2=None, op0=ALU.mult), reads=[("stat", 0)], writes=[("stat", 1)])
                    ph.dve(lambda e: e.tensor_tensor(out=yc3, in0=y3, in1=stat[:, 1, :].unsqueeze(2).broadcast_to([128, 4, 128]), op=ALU.add), reads=[("y", 0), ("stat", 1)], writes=["yc"])
                    for hq in range(4):
                        ph.act(lambda e, hq=hq: e.activation(out=yn[:, hq * 128:(hq + 1) * 128], in_=yc[:, hq * 128:(hq + 1) * 128], func=AF.Square, accum_out=stat[:, 2, hq:hq + 1]),
                               reads=["yc"], writes=["yn", ("stat", 2, hq)])
                    ph.dve(lambda e: e.tensor_scalar(out=stat[:, 2, :], in0=stat[:, 2, :], scalar1=1.0 / 128, scalar2=EPS, op0=ALU.mult, op1=ALU.add), reads=[("stat", 2)], writes=[("stat", 2)])
                    ph.act(lambda e: e.sqrt(out=stat[:, 2, :], in_=stat[:, 2, :]), reads=[("stat", 2)], writes=[("stat", 2)])
                    ph.dve(lambda e: e.reciprocal(out=stat[:, 3, :], in_=stat[:, 2, :]), reads=[("stat", 2)], writes=[("stat", 3)])
                    ph.dve(lambda e: e.tensor_tensor(out=yc3, in0=yc3, in1=stat[:, 3, :].unsqueeze(2).broadcast_to([128, 4, 128]), op=ALU.mult), reads=["yc", ("stat", 3)], writes=["yc"])
                    ph.pool(lambda e: e.tensor_tensor(out=yc[:], in0=yc[:], in1=gng[:], op=ALU.mult), reads=["yc", "gng"], writes=["yc"])
                    ph.pool(lambda e: e.tensor_tensor(out=yc[:], in0=yc[:], in1=gnb[:], op=ALU.add), reads=["yc", "gnb"], writes=["yc"])
                    ph.dve(lambda e: e.tensor_tensor(out=yn[:], in0=yc[:], in1=sg[:], op=ALU.mult), reads=["yc", "sg"], writes=["yn"])
                    pb = st["pb"]
                    stg = st["stg"][t % 2]
                    for cl in range(4):
                        ph.pe(lambda e, cl=cl: e.transpose(pb[:, RPERM[cl] * 128:(RPERM[cl] + 1) * 128], yn[:, cl * 128:(cl + 1) * 128], IDB), reads=["yn", "CB"], writes=["pbt"])
                    ph.act(lambda e, stg=stg: e.copy(out=stg[:], in_=pb[:, 0:512].rearrange("p (a n) -> p a n", a=4)), reads=["pbt"], writes=[("stg", t % 2)])
                    ph.dma("sp", YCAT_d[4 + 4 * hh:8 + 4 * hh, :, t * 128:(t + 1) * 128].rearrange("c p t -> p c t"), stg[:], reads=[("stg", t % 2)])

                groups = [dict(chunk=hs % 2, row0=(hs // 2) * 64, nrows=64, heads=[hs], scol0=(hs % 2) * 128, sncols=128) for hs in range(4)]
                if RET_SCAN:
                    scan_phase(128, groups, XS, BTOK, BT, CT, A4, None, True, post, list(range(16)), H=4)

        def moe_gate(LG, GT):
            with ExitStack() as pes:
                ph = Phase(ctx, "gate")
                al = lambda name, shape, dtype: pes.enter_context(SBT(name, shape, dtype))
                PS = [pes.enter_context(PST("gps%d" % i, [128, 512], F32)) for i in range(2)]
                M1 = al("m1", [128, NT], F32)
                M2 = al("m2", [128, NT], F32)
                EQ = al("eq", [128, NT, 8], F32)
                L2 = al("l2", [128, NT, 8], F32)
                EXg = al("exg", [128, NT, 8], F32)
                DEN = al("den", [128, NT], F32)
                bc = lambda a: a[:].unsqueeze(2).broadcast_to([128, NT, 8])
                ph.dve(lambda e: e.reduce_max(out=M1[:], in_=LG[:], axis=AX.X), reads=["LG"], writes=["m1"])
                ph.dve(lambda e: e.tensor_tensor(out=EQ[:], in0=LG[:], in1=bc(M1), op=ALU.is_equal), reads=["LG", "m1"], writes=["eq"])
                ph.dve(lambda e: e.scalar_tensor_tensor(out=L2[:], in0=EQ[:], scalar=-1e30, in1=LG[:], op0=ALU.mult, op1=ALU.add), reads=["eq", "LG"], writes=["l2"])
                ph.dve(lambda e: e.reduce_max(out=M2[:], in_=L2[:], axis=AX.X), reads=["l2"], writes=["m2"])
                ph.dve(lambda e: e.tensor_tensor(out=EQ[:], in0=LG[:], in1=bc(M2), op=ALU.is_ge), reads=["LG", "m2"], writes=["eq"])
                ph.dve(lambda e: e.tensor_tensor(out=L2[:], in0=LG[:], in1=bc(M1), op=ALU.subtract), reads=["LG", "m1"], writes=["l2"])
                ph.act(lambda e: e.activation(out=EXg[:], in_=L2[:], func=AF.Exp), reads=["l2"], writes=["exg"])
                ph.dve(lambda e: e.tensor_tensor(out=EXg[:], in0=EXg[:], in1=EQ[:], op=ALU.mult), reads=["exg", "eq"], writes=["exg"])
                ph.dve(lambda e: e.reduce_sum(out=DEN[:], in_=EXg[:], axis=AX.X), reads=["exg"], writes=["den"])
                ph.dve(lambda e: e.reciprocal(out=DEN[:], in_=DEN[:]), reads=["den"], writes=["den"])
                ph.dve(lambda e: e.tensor_tensor(out=EXg[:], in0=EXg[:], in1=bc(DEN), op=ALU.mult), reads=["exg", "den"], writes=["exg"])
                for t4 in range(0, NT, 4):
                    n4 = min(4, NT - t4)
                    b = (t4 // 4) % 2
                    for tt in range(n4):
                        ph.pe(lambda e, b=b, tt=tt, t4=t4: e.transpose(PS[b][0:8, tt * 128:(tt + 1) * 128], EXg[:, t4 + tt, :], IDF), reads=["exg", "CF"], writes=[("ps", b)])
                    ph.act(lambda e, b=b, t4=t4, n4=n4: e.copy(out=GT[:, t4 * 128:(t4 + n4) * 128], in_=PS[b][0:8, 0:n4 * 128]), reads=[("ps", b)], writes=[("GT", t4)])
                ph.emit()

        if not SKIP_L0:
            norm_mod(0, 1)
            for m in range(NPAIRS):
                attention_pair(0, m)
            for g in range(NGROUPS):
                ssd_group(g)
        if STOP_AFTER >= 1 and not SKIP_L0:
            out_proj(0)
            norm_mod(0, 2)
            ffn(0)
        if dbg_d and "XT1" in dbg_d:
            ph = Phase(ctx, "dxt1")
            ph.dma("sp", dbg_d["XT1"].rearrange("p (k t) -> p k t", k=KC), XT[:])
            ph.emit()
        if STOP_AFTER >= 2:
            norm_mod(1, 1)
            for m in range(NPAIRS):
                attention_pair(1, m)
            for hh in range(2 if RUN_RET else 0):
                ret_half(hh)
        if STOP_AFTER >= 3:
            SEL = sb("SEL", [8, 1024], F32)
            LGT = sb("LGT", [128, NT, 8], F32)
            GTT = sb("GTT", [8, T], F32)
            ph = Phase(ctx, "ldsel")
            ph.dma("sp", SEL[:], sel_d[:, :], writes=["SEL"])
            ph.emit()
            out_proj(1)
            norm_mod(1, 2, LG=LGT)
            moe_gate(LGT, GTT)
            dump("GT", GTT[:], (8, T))
        if dbg_d and "XT2" in dbg_d:
            ph = Phase(ctx, "dxt2")
            ph.dma("sp", dbg_d["XT2"].rearrange("p (k t) -> p k t", k=KC), XT[:])
            ph.emit()
        if STOP_AFTER >= 4:
            ffn(1, GT=GTT)
            final_out()
        if dbg_d and "YC" in dbg_d:
            with ExitStack() as des:
                ph = Phase(ctx, "dumpyc")
                yb = des.enter_context(SBT("ycb", [128, T], BF16))
                yf = des.enter_context(SBT("ycf", [128, T], F32))
                for c in range(12):
                    ph.dma("sp", yb[:], YCAT_d[c], writes=["yb"])
                    ph.dve(lambda e: e.tensor_copy(out=yf[:], in_=yb[:]), reads=["yb"], writes=["yf"])
                    ph.dma("sp", dbg_d["YC"][:, c * T:(c + 1) * T], yf[:], reads=["yf"])
                ph.emit()
    return nc


def make_consts():
    c = np.zeros((128, 1024), np.float32)
    c[:, 0:128] = np.eye(128, dtype=np.float32)
    c[:, 128:256] = 1.0
    p = np.arange(128)[:, None]
    i = np.arange(128)[None, :]
    c[:, 256:384] = (p <= i)
    c[:, 384:512] = (p >= i)
    c[:, 512:640] = (p > i)
    c[:, 640:768] = (p < i)
    for f in range(128):
        if f % 64 < 32:
            c[f + 32, 768 + f] = -1.0
        else:
            c[f - 32, 768 + f] = 1.0
    return c


def kernel(**inputs):
    dbg = inputs.pop("_dbg", None)
    inp = {k: np.asarray(v) for k, v in inputs.items()}
    nc = build_program(dbg)
    cst = make_consts()
    nab = na_bias_table(inp["na_rpb"][0])
    swab = swa_bias_table()
    tpos = np.arange(L)
    inv = (10000.0 ** (-np.arange(16, dtype=np.float32) / 16)).astype(np.float32)
    ang = np.concatenate([(tpos // 64).astype(np.float32)[:, None] * inv, (tpos % 64).astype(np.float32)[:, None] * inv], axis=-1)
    fidx = np.arange(128) % 32
    rope_tab = np.stack([np.cos(ang)[:, fidx].T, np.sin(ang)[:, fidx].T], 0).astype(np.float32)
    sel = np.zeros((8, 1024), np.float32)
    for e in range(8):
        sel[e, e * 128:(e + 1) * 128] = 1.0
    in_maps = []
    for b in range(8):
        vecs = np.zeros((256, 128), np.float32)

        def put(nm, arr):
            a = np.ascontiguousarray(arr, dtype=np.float32).reshape(-1, 128)
            vecs[VR[nm]:VR[nm] + a.shape[0]] = a
        put("c", inp["c"][b])
        put("c_ctx", inp["c_ctx"])
        put("ada_b0", inp["ada_b"][0])
        put("ada_b1", inp["ada_b"][1])
        put("g_attn0", inp["norm_attn_g"][0])
        put("g_attn1", inp["norm_attn_g"][1])
        put("g_ffn0", inp["norm_ffn_g"][0])
        put("g_ffn1", inp["norm_ffn_g"][1])
        put("final_g", inp["final_g"])
        put("conv_w", inp["ssm_conv_w"][0].reshape(5, 1536))
        put("conv_b", inp["ssm_conv_b"][0])
        put("ssm_g", inp["ssm_norm_g"][0])
        in_maps.append({
            "x": np.ascontiguousarray(inp["x"][b]),
            "ctx": np.ascontiguousarray(inp["ctx"][b]),
            "vecs": vecs,
            "cst": cst,
            "ada_w": inp["ada_w"],
            "ev_w_in": inp["ev_w_in"][0], "od_w_in": inp["od_w_in"][0], "nab": nab, "swab": swab,
            "sink": inp["swa_sink"],
            "ev_w_out": inp["ev_w_out"][0], "od_w_out": inp["od_w_out"][0],
            "ffn_w1": inp["ffn_w1"][0], "ffn_w3": inp["ffn_w3"][0], "ffn_w2": inp["ffn_w2"][0],
            "sel": sel, "rope": rope_tab, "retld": inp["ret_log_decay"][0], "retg": inp["ret_gn_g"], "retb": inp["ret_gn_b"],
            "router": inp["moe_router"][0],
            "dtba": np.stack([inp["ssm_dt_bias"][0].reshape(32), inp["ssm_a_log"][0].reshape(32)], 0),
            "ssmd": inp["ssm_d"], "ssmg": inp["ssm_norm_g"],
        })
    if STOP_AFTER >= 4:
        for mp in in_maps:
            mp["moe_w1"] = inp["moe_w1"][0]
            mp["moe_w3"] = inp["moe_w3"][0]
            mp["moe_w2"] = inp["moe_w2"][0]
    res = run_bass_kernel_spmd(nc, in_maps, core_ids=list(range(8)))
    if dbg:
        return res
    out = np.stack([r["out"] for r in res.results], axis=0)
    return out
```

```python
import math
from contextlib import ExitStack
import numpy as np
import concourse.bass as bass
import concourse.mybir as mybir
from concourse.bass_utils import run_bass_kernel_spmd

F32 = mybir.dt.float32
BF16 = mybir.dt.bfloat16
AF = mybir.ActivationFunctionType
ALU = mybir.AluOpType
AX = mybir.AxisListType

ENGS = ("pe", "act", "dve", "pool", "sp")
NDMA_SEMS = 24


class Ctx:
    def __init__(self, nc, es):
        self.nc = nc
        self.eng_sem = {e: es.enter_context(nc.semaphore("sem_" + e)) for e in ENGS if e != "sp"}
        self.eng_cnt = {e: 0 for e in self.eng_sem}
        self.dma_sems = [es.enter_context(nc.semaphore("dsem%d" % i)) for i in range(NDMA_SEMS)]
        self.dma_cnt = [0] * NDMA_SEMS
        self.dma_rr = 0
        self.eng_obj = {"pe": nc.tensor, "act": nc.scalar, "dve": nc.vector, "pool": nc.gpsimd, "sp": nc.sync}


class Phase:
    def __init__(self, ctx, name="ph"):
        self.ctx = ctx
        self.name = name
        self.ops = []
        self.state = {}
        self.rr = 0

    def _st(self, key):
        if isinstance(key, tuple):
            nm, sub = key[0], tuple(key[1:])
        else:
            nm, sub = key, ()
        d = self.state.setdefault(nm, {})
        return d, sub

    @staticmethod
    def _overlap(a, b):
        n = min(len(a), len(b))
        return a[:n] == b[:n]

    def op(self, eng, fn, reads=(), writes=(), dma=False, pe_acc=False):
        oid = len(self.ops)
        deps = set()
        for key in reads:
            d, sub = self._st(key)
            for s2, st in d.items():
                if self._overlap(sub, s2) and st["w"] is not None:
                    deps.add(st["w"])
            d.setdefault(sub, {"w": None, "r": []})["r"].append(oid)
        for key in writes:
            d, sub = self._st(key)
            for s2 in list(d.keys()):
                if self._overlap(sub, s2):
                    st = d[s2]
                    if st["w"] is not None:
                        deps.add(st["w"])
                    deps.update(st["r"])
                    if len(s2) > len(sub):
                        del d[s2]
            st = d.setdefault(sub, {"w": None, "r": []})
            st["w"] = oid
            st["r"] = []
        deps.discard(oid)
        o = {"eng": eng, "fn": fn, "deps": deps, "dma": dma, "pe_acc": pe_acc}
        if dma:
            c = self.ctx
            k = c.dma_rr
            c.dma_rr = (c.dma_rr + 1) % NDMA_SEMS
            prev = getattr(self, "_dma_prev", {}).get(k)
            if prev is not None:
                deps.add(prev)
            self.__dict__.setdefault("_dma_prev", {})[k] = oid
            c.dma_cnt[k] += 16
            o["dsem"] = k
            o["dval"] = c.dma_cnt[k]
        self.ops.append(o)
        return oid

    def pe(self, fn, reads=(), writes=(), acc=False):
        return self.op("pe", fn, reads, writes, pe_acc=acc)

    def act(self, fn, reads=(), writes=()):
        return self.op("act", fn, reads, writes)

    def dve(self, fn, reads=(), writes=()):
        return self.op("dve", fn, reads, writes)

    def pool(self, fn, reads=(), writes=()):
        return self.op("pool", fn, reads, writes)

    def any2(self, fn, reads=(), writes=()):
        self.rr += 1
        return self.op("dve" if self.rr % 2 else "pool", fn, reads, writes)

    def dma(self, q, out, in_, reads=(), writes=()):
        return self.op(q, lambda e: e.dma_start(out=out, in_=in_), reads, writes, dma=True)

    def emit(self):
        c = self.ctx
        ops = self.ops
        for o in ops:
            best = {}
            pd = []
            for d in o["deps"]:
                po = ops[d]
                if po["dma"]:
                    pd.append(d)
                    continue
                if po["eng"] == "pe" and o["eng"] == "pe" and not o["dma"]:
                    continue
                if d > best.get(po["eng"], -1):
                    best[po["eng"]] = d
            o["deps"] = set(pd) | set(best.values())
        needed = set()
        for o in ops:
            for d in o["deps"]:
                po = ops[d]
                if po["dma"]:
                    continue
                needed.add(d)
        last_of = {}
        for i, o in enumerate(ops):
            if not o["dma"]:
                last_of[o["eng"]] = i
        for i in last_of.values():
            needed.add(i)
        for i, o in enumerate(ops):
            if o["dma"]:
                continue
            if i in needed:
                c.eng_cnt[o["eng"]] += 1
                o["inc"] = True
            o["cnt"] = c.eng_cnt[o["eng"]] if i in needed else None
        per_eng = {e: [] for e in ENGS}
        waited = {e: {} for e in ENGS}
        for i, o in enumerate(ops):
            w = {}
            for d in o["deps"]:
                po = ops[d]
                if po["dma"]:
                    key = ("d", po["dsem"])
                    val = po["dval"]
                else:
                    if po["eng"] == "pe" and o["eng"] == "pe" and not o["dma"]:
                        continue
                    key = ("e", po["eng"])
                    val = po["cnt"]
                if val > w.get(key, 0):
                    w[key] = val
            wl = []
            for key, val in w.items():
                if waited[o["eng"]].get(key, 0) >= val:
                    continue
                waited[o["eng"]][key] = val
                wl.append((key, val))
            o["waits"] = wl
            per_eng[o["eng"]].append(o)
        fin = []
        for e, i in last_of.items():
            fin.append((("e", e), ops[i]["cnt"]))
        for k in range(NDMA_SEMS):
            if c.dma_cnt[k] > 0:
                fin.append((("d", k), c.dma_cnt[k]))

        def semof(key):
            return c.eng_sem[key[1]] if key[0] == "e" else c.dma_sems[key[1]]

        def run(engname):
            def body(eng):
                for o in per_eng[engname]:
                    for key, val in o["waits"]:
                        eng.wait_ge(semof(key), val)
                    ins = o["fn"](eng)
                    if o["dma"]:
                        ins.then_inc(c.dma_sems[o["dsem"]], 16)
                    elif o.get("inc"):
                        ins.then_inc(c.eng_sem[o["eng"]], 1)
                if engname == "sp":
                    for key, val in fin:
                        eng.wait_ge(semof(key), val)
            return body

        with c.nc.Block() as block:
            block.tensor(run("pe"))
            block.scalar(run("act"))
            block.vector(run("dve"))
            block.gpsimd(run("pool"))
            block.sync(run("sp"))
        self.ops = []
        self.state = {}
        self._dma_prev = {}


D = 1024
L = 2048
LC = 256
T = L + LC
NT = T // 128
KC = D // 128
EPS = 1e-6
BLKS = [(0, 512), (512, 512), (1024, 512), (1536, 512), (2048, 256)]

VR = {}
_r = 0
for _nm, _n in (("c", 8), ("c_ctx", 8), ("ada_b0", 48), ("ada_b1", 48), ("g_attn0", 8), ("g_attn1", 8),
                ("g_ffn0", 8), ("g_ffn1", 8), ("final_g", 8), ("conv_w", 60), ("conv_b", 12), ("ssm_g", 8)):
    VR[_nm] = _r
    _r += _n
NVR = _r

NPAIRS = 4
NGROUPS = 2
ATT_LAG = 2
RPERM = [0, 2, 1, 3]
NEXPERTS = 8
STOP_AFTER = 99
RUN_RET = True
RET_SCAN = True
DBG_NOPOST = False
DBG_NOP2 = False
DBG_SKIP = set()
SKIP_L0 = False


def _na_tiles():
    out = []
    for t in range(16):
        qr = np.arange(t * 128, (t + 1) * 128) // 64
        r0 = np.clip(qr - 4, 0, 24)
        out.append(list(range(int(r0.min()) // 2, (int(r0.max()) + 7) // 2 + 1)))
    return out


NA_KT = _na_tiles()


def na_bias_table(rpb):
    out = np.full((8, 16, 128, 640), -30000.0, np.float32)
    for t in range(16):
        qpos = np.arange(t * 128, (t + 1) * 128)
        qr, qc = qpos // 64, qpos % 64
        r0 = np.clip(qr - 4, 0, 24)
        c0 = np.clip(qc - 8, 0, 48)
        for j, kt in enumerate(NA_KT[t]):
            kpos = np.arange(kt * 128, (kt + 1) * 128)
            kr, kc = kpos // 64, kpos % 64
            ok = ((kr[:, None] >= r0[None, :]) & (kr[:, None] < r0[None, :] + 8)
                  & (kc[:, None] >= c0[None, :]) & (kc[:, None] < c0[None, :] + 16))
            dr = np.clip(kr[:, None] - qr[None, :] + 7, 0, 14)
            dc = np.clip(kc[:, None] - qc[None, :] + 15, 0, 30)
            vals = rpb[:, dr, dc]
            out[:, t, :, j * 128:(j + 1) * 128] = np.where(ok[None], vals, np.float32(-30000.0))
    return out


def swa_bias_table():
    out = np.full((16, 128, 384), -30000.0, np.float32)
    for t in range(16):
        kts = [kt for kt in (t - 1, t, t + 1) if 0 <= kt < 16]
        qpos = np.arange(t * 128, (t + 1) * 128)
        for j, kt in enumerate(kts):
            kpos = np.arange(kt * 128, (kt + 1) * 128)
            ok = np.abs(kpos[:, None] - qpos[None, :]) <= 128
            out[t, :, j * 128:(j + 1) * 128] = np.where(ok, np.float32(0.0), np.float32(-30000.0))
    return out


def build_program(dbg=None):
    nc = bass.Bass("TRN2", target_bir_lowering=False)
    _uid = [0]

    def SBT(name, shape, dtype):
        _uid[0] += 1
        return nc.sbuf_tensor("%s_%d" % (name, _uid[0]), shape, dtype)

    def PST(name, shape, dtype):
        _uid[0] += 1
        return nc.psum_tensor("%s_%d" % (name, _uid[0]), shape, dtype)
    dt = nc.dram_tensor
    x_d = dt("x", [L, D], F32, kind="ExternalInput").ap()
    ctx_d = dt("ctx", [LC, D], F32, kind="ExternalInput").ap()
    vecs_d = dt("vecs", [256, 128], F32, kind="ExternalInput").ap()
    cst_d = dt("cst", [128, 1024], F32, kind="ExternalInput").ap()
    ada_w_d = dt("ada_w", [2, D, 6 * D], F32, kind="ExternalInput").ap()
    out_d = dt("out", [L, D], F32, kind="ExternalOutput").ap()
    ev_w_in_d = dt("ev_w_in", [D, 4128], F32, kind="ExternalInput").ap()
    od_w_in_d = dt("od_w_in", [D, 3840], F32, kind="ExternalInput").ap()
    nab_d = dt("nab", [8, 16, 128, 640], F32, kind="ExternalInput").ap()
    swab_d = dt("swab", [16, 128, 384], F32, kind="ExternalInput").ap()
    sink_d = dt("sink", [1, 8], F32, kind="ExternalInput").ap()
    dtba_d = dt("dtba", [2, 32], F32, kind="ExternalInput").ap()
    ev_w_out_d = dt("ev_w_out", [1536, D], F32, kind="ExternalInput").ap()
    od_w_out_d = dt("od_w_out", [1536, D], F32, kind="ExternalInput").ap()
    ffn_w1_d = dt("ffn_w1", [D, 2816], F32, kind="ExternalInput").ap()
    ffn_w3_d = dt("ffn_w3", [D, 2816], F32, kind="ExternalInput").ap()
    ffn_w2_d = dt("ffn_w2", [2816, D], F32, kind="ExternalInput").ap()
    if STOP_AFTER >= 4:
        moe_w1_t = dt("moe_w1", [8, D, 3584], F32, kind="ExternalInput").ap()
        moe_w3_t = dt("moe_w3", [8, D, 3584], F32, kind="ExternalInput").ap()
        moe_w2_t = dt("moe_w2", [8, 3584, D], F32, kind="ExternalInput").ap()
        moe_w1_d = [moe_w1_t[e] for e in range(8)]
        moe_w3_d = [moe_w3_t[e] for e in range(8)]
        moe_w2_d = [moe_w2_t[e] for e in range(8)]
    sel_d = dt("sel", [8, 1024], F32, kind="ExternalInput").ap()
    rope_d = dt("rope", [2, 128, L], F32, kind="ExternalInput").ap()
    retld_d = dt("retld", [2, 8], F32, kind="ExternalInput").ap()
    retg_d = dt("retg", [1, 1024], F32, kind="ExternalInput").ap()
    retb_d = dt("retb", [1, 1024], F32, kind="ExternalInput").ap()
    router_d = dt("router", [D, 8], F32, kind="ExternalInput").ap()
    ssmd_d = dt("ssmd", [1, 16], F32, kind="ExternalInput").ap()
    ssmg_d = dt("ssmg", [1, 1024], F32, kind="ExternalInput").ap()
    dbg_d = None
    if dbg:
        dbg_d = {k: dt("dbg_" + k, list(shp), F32, kind="ExternalOutput").ap() for k, shp in dbg.items()}

    with ExitStack() as es:
        ctx = Ctx(nc, es)
        sb = lambda name, shape, dtype: es.enter_context(SBT(name, shape, dtype))
        XT = sb("XT", [128, KC, T], F32)
        HT = sb("HT", [128, KC, T], BF16)
        VT = sb("VT", [128, 256], F32)
        CF = sb("CF", [128, 1024], F32)
        CB = sb("CB", [128, 1024], BF16)
        MOD = sb("MOD", [128, 2, 2, 48], F32)
        AB = sb("AB", [128, 2, 2, 6, 8], F32)
        IDF = CF[:, 0:128]
        ONESF = CF[:, 128:256]
        IDB = CB[:, 0:128]
        ONESB = CB[:, 128:256]

        with ExitStack() as ps_es:
            ph = Phase(ctx, "p0")
            PS = [ps_es.enter_context(PST("ps%d" % i, [128, 512], F32)) for i in range(8)]
            XIN = [ps_es.enter_context(SBT("xin%d" % i, [128, D], F32)) for i in range(3)]
            VIN = ps_es.enter_context(SBT("vin", [128, 2, 128], F32))
            SC = ps_es.enter_context(SBT("sc", [128, KC, 2], BF16))
            AW = [ps_es.enter_context(SBT("aw%d" % i, [128, KC, 768], BF16)) for i in range(2)]
            ph.dma("sp", CF[:], cst_d[:, :], writes=["CF"])
            ph.dma("pool", CB[:], cst_d[:, :], writes=["CB"])
            ph.dma("sp", VIN[:], vecs_d.rearrange("(a p) n -> p a n", p=128), writes=["VIN"])
            for a in range(2):
                ph.pe(lambda e, a=a: e.transpose(PS[0][:, a * 128:(a + 1) * 128], VIN[:, a, :], IDF),
                      reads=["CF", "VIN"], writes=[("ps", 0)])
            ph.dve(lambda e: e.tensor_copy(out=VT[:], in_=PS[0][:, 0:256]), reads=[("ps", 0)], writes=["VT"])
            pi = 1
            for t in range(NT):
                xin = XIN[t % 3]
                src = x_d[t * 128:(t + 1) * 128, :] if t < 16 else ctx_d[(t - 16) * 128:(t - 15) * 128, :]
                ph.dma("sp", xin[:], src, writes=[("xin", t % 3)])
                for half in range(2):
                    b = 1 + (pi % 7)
                    pi += 1
                    for kk in range(4):
                        k = half * 4 + kk
                        ph.pe(lambda e, b=b, kk=kk, k=k, xin=xin: e.transpose(
                            PS[b][:, kk * 128:(kk + 1) * 128], xin[:, k * 128:(k + 1) * 128], IDF),
                            reads=["CF", ("xin", t % 3)], writes=[("ps", b)])
                    dst = XT[:, half * 4:half * 4 + 4, t * 128:(t + 1) * 128]
                    srcp = PS[b][:].rearrange("p (a n) -> p a n", a=4)
                    if (t + half) % 2 == 0:
                        ph.dve(lambda e, dst=dst, srcp=srcp: e.tensor_copy(out=dst, in_=srcp),
                               reads=[("ps", b)], writes=[("XT", t)])
                    else:
                        ph.act(lambda e, dst=dst, srcp=srcp: e.copy(out=dst, in_=srcp),
                               reads=[("ps", b)], writes=[("XT", t)])
            for j, nm in enumerate(("c", "c_ctx")):
                ph.act(lambda e, j=j, nm=nm: e.activation(out=SC[:, :, j], in_=VT[:, VR[nm]:VR[nm] + 8], func=AF.Silu),
                       reads=["VT"], writes=["SC"])
            si = 0
            for l in range(2):
                for s in range(8):
                    aw = AW[si % 2]
                    ph.dma("pool", aw[:], ada_w_d[l, :, s * 768:(s + 1) * 768].rearrange("(k p) n -> p k n", p=128),
                           writes=[("aw", si % 2)])
                    for c6 in range(6):
                        cc = s * 6 + c6
                        for k in range(KC):
                            ph.pe(lambda e, l=l, cc=cc, k=k, c6=c6, aw=aw: e.matmul(
                                PS[0][:, l * 96 + cc * 2:l * 96 + cc * 2 + 2], aw[:, k, c6 * 128:(c6 + 1) * 128],
                                SC[:, k, :], start=(k == 0), stop=(k == KC - 1)),
                                reads=[("aw", si % 2), "SC"], writes=[("ps", 0)])
                    si += 1
            for l in range(2):
                for j in range(2):
                    r0 = VR["ada_b%d" % l]
                    ph.dve(lambda e, l=l, j=j, r0=r0: e.tensor_tensor(
                        out=MOD[:, l, j, :], in0=PS[0][:, l * 96:(l + 1) * 96].rearrange("p (c j) -> p c j", j=2)[:, :, j],
                        in1=VT[:, r0:r0 + 48], op=ALU.add), reads=[("ps", 0), "VT"], writes=["MOD"])
                    for (ai, sci, gname) in ((0, 1, "g_attn%d" % l), (3, 4, "g_ffn%d" % l)):
                        g0 = VR[gname]
                        ph.dve(lambda e, l=l, j=j, ai=ai, sci=sci, g0=g0: e.scalar_tensor_tensor(
                            out=AB[:, l, j, ai, :], in0=MOD[:, l, j, sci * 8:(sci + 1) * 8], scalar=1.0,
                            in1=VT[:, g0:g0 + 8], op0=ALU.add, op1=ALU.mult), reads=["MOD", "VT"], writes=["AB"])
                    for (bi, shi) in ((1, 0), (2, 2), (4, 3), (5, 5)):
                        ph.dve(lambda e, l=l, j=j, bi=bi, shi=shi: e.tensor_copy(
                            out=AB[:, l, j, bi, :], in_=MOD[:, l, j, shi * 8:(shi + 1) * 8]), reads=["MOD"], writes=["AB"])
            ph.emit()

        def norm_mod(l, which, LG=None):
            ai, bi = (0, 1) if which == 1 else (3, 4)
            with ExitStack() as pes:
                ph = Phase(ctx, "nm")
                PS = [pes.enter_context(PST("nps%d" % i, [128, 512], F32)) for i in range(4)]
                SQ = [pes.enter_context(SBT("sq%d" % i, [128, 512], BF16)) for i in range(3)]
                RS = [pes.enter_context(SBT("rs%d" % i, [128, 512], F32)) for i in range(2)]
                TMP = [pes.enter_context(SBT("tmp%d" % i, [128, 512], F32)) for i in range(3)]
                if LG is not None:
                    PSR = [pes.enter_context(PST("npr%d" % i, [128, 512], F32)) for i in range(2)]
                    H2F = pes.enter_context(SBT("h2f", [128, KC, 512], F32))
                    RWF = pes.enter_context(SBT("rwf", [128, KC, 8], F32))
                    ph.dma("sp", RWF[:], router_d.rearrange("(k p) n -> p k n", p=128), writes=["rwf"])
                cnt = {"qi": 0, "ti": 0}

                def nm_a(bi_, t0, tn):
                    pb = bi_ % 4
                    for k in range(KC):
                        qi = cnt["qi"]
                        sq = SQ[qi % 3]
                        ph.act(lambda e, sq=sq, k=k, t0=t0, tn=tn: e.activation(out=sq[:, :tn], in_=XT[:, k, t0:t0 + tn], func=AF.Square),
                               reads=[], writes=[("sq", qi % 3)])
                        ph.pe(lambda e, sq=sq, k=k, pb=pb, tn=tn: e.matmul(PS[pb][:, :tn], ONESB, sq[:, :tn], start=(k == 0), stop=(k == KC - 1)),
                              reads=[("sq", qi % 3)], writes=[("nps", pb)])
                        cnt["qi"] += 1
                    rs = RS[bi_ % 2]
                    ph.dve(lambda e, rs=rs, pb=pb, tn=tn: e.tensor_scalar(out=rs[:, :tn], in0=PS[pb][:, :tn], scalar1=1.0 / D, scalar2=EPS,
                                                                          op0=ALU.mult, op1=ALU.add), reads=[("nps", pb)], writes=[("rs", bi_ % 2)])
                    ph.act(lambda e, rs=rs, tn=tn: e.sqrt(out=rs[:, :tn], in_=rs[:, :tn]),
                           reads=[("rs", bi_ % 2)], writes=[("rs", bi_ % 2)])
                    ph.dve(lambda e, rs=rs, tn=tn: e.reciprocal(out=rs[:, :tn], in_=rs[:, :tn]),
                           reads=[("rs", bi_ % 2)], writes=[("rs", bi_ % 2)])

                def nm_b(bi_, t0, tn):
                    j = 0 if t0 < L else 1
                    rs = RS[bi_ % 2]
                    for k in range(KC):
                        ti = cnt["ti"]
                        tmp = TMP[ti % 3]
                        ph.any2(lambda e, tmp=tmp, k=k, t0=t0, tn=tn, rs=rs: e.tensor_tensor(out=tmp[:, :tn], in0=XT[:, k, t0:t0 + tn], in1=rs[:, :tn], op=ALU.mult),
                                reads=[("rs", bi_ % 2)], writes=[("tmp", ti % 3)])
                        ph.act(lambda e, tmp=tmp, k=k, t0=t0, tn=tn, j=j: e.activation(out=HT[:, k, t0:t0 + tn], in_=tmp[:, :tn], func=AF.Identity,
                                                                                      scale=AB[:, l, j, ai, k:k + 1], bias=AB[:, l, j, bi, k:k + 1]),
                               reads=[("tmp", ti % 3)], writes=[("HT", k, bi_)])
                        if LG is not None:
                            ph.act(lambda e, tmp=tmp, k=k, tn=tn, j=j: e.activation(out=H2F[:, k, :tn], in_=tmp[:, :tn], func=AF.Identity,
                                                                                scale=AB[:, l, j, ai, k:k + 1], bias=AB[:, l, j, bi, k:k + 1]),
                                   reads=[("tmp", ti % 3)], writes=[("h2f", k)])
                        cnt["ti"] += 1
                    if LG is not None:
                        pr = bi_ % 2
                        for tt in range(tn // 128):
                            for k in range(KC):
                                ph.pe(lambda e, pr=pr, tt=tt, k=k: e.matmul(PSR[pr][:, tt * 8:(tt + 1) * 8], H2F[:, k, tt * 128:(tt + 1) * 128], RWF[:, k, :], start=(k == 0), stop=(k == KC - 1)),
                                      reads=["h2f", "rwf"], writes=[("npr", pr)])
                        ntl = tn // 128
                        ph.dve(lambda e, pr=pr, t0=t0, ntl=ntl: e.tensor_copy(out=LG[:, t0 // 128:t0 // 128 + ntl, :], in_=PSR[pr][:, 0:ntl * 8].rearrange("p (a n) -> p a n", a=ntl)),
                               reads=[("npr", pr)], writes=[("LG", t0)])

                for i in range(len(BLKS) + 1):
                    if i < len(BLKS):
                        nm_a(i, *BLKS[i])
                    if i >= 1:
                        nm_b(i - 1, *BLKS[i - 1])
                ph.emit()

        YCAT_d = nc.dram_tensor("ycat_scr", [12, 128, T], BF16, kind="Internal").ap()

        def dump(name, src_ap, shape2, reads=()):
            if not dbg_d or name not in dbg_d:
                return
            with ExitStack() as des:
                ph = Phase(ctx, "dump")
                f = des.enter_context(SBT("dbgf_" + name, list(shape2), F32))
                ph.dve(lambda e: e.tensor_copy(out=f[:], in_=src_ap), writes=["f"])
                ph.dma("sp", dbg_d[name], f[:], reads=["f"])
                ph.emit()

        def attention_pair(l, m):
            W_d = ev_w_in_d if l == 0 else od_w_in_d
            with ExitStack() as pes:
                ph = Phase(ctx, "att")
                al = lambda name, shape, dtype: pes.enter_context(SBT(name, shape, dtype))
                PS = [pes.enter_context(PST("aps%d" % i, [128, 512], F32)) for i in range(8)]
                psi = [0]

                def nb():
                    psi[0] = (psi[0] + 1) % 8
                    return psi[0]
                WS = al("ws", [128, KC, 384], BF16)
                QT = al("qt", [128, T], BF16)
                KT = al("kt", [128, T], BF16)
                VK = al("vk", [128, NT, 128], BF16)
                OT = al("ot", [128, T], BF16)
                BI = [al("bi%d" % i, [128, 640], F32) for i in range(3)]
                SS = [al("ss%d" % i, [128, 640], F32) for i in range(2)]
                PT = [al("pt%d" % i, [128, 896], BF16) for i in range(4)]
                RD = [al("rd%d" % i, [128, 128], F32) for i in range(2)]
                Wr = W_d.rearrange("(k p) n -> p k n", p=128)
                if l == 0:
                    cols = [(m * 128, 128), (512 + m * 128, 128), (1024 + m * 128, 128)]
                else:
                    kv = m // 2
                    cols = [(m * 128, 128), (512 + kv * 64, 64), (512 + kv * 64, 64), (640 + kv * 64, 64), (640 + kv * 64, 64)]
                off = 0
                for (c0, cn) in cols:
                    ph.dma("pool", WS[:, :, off:off + cn], Wr[:, :, c0:c0 + cn], writes=[("ws", off)])
                    off += cn
                wsr = [("ws", o) for o in (0, 64, 128, 192, 256, 320)]
                nq = T if l == 0 else L
                for (dst, wo, lim, nm) in ((QT, 0, nq, "qt"), (KT, 128, T, "kt")):
                    for (t0, tn) in BLKS:
                        if t0 >= lim:
                            continue
                        b = nb()
                        for k in range(KC):
                            ph.pe(lambda e, b=b, k=k, wo=wo, t0=t0, tn=tn: e.matmul(PS[b][:, :tn], WS[:, k, wo:wo + 128], HT[:, k, t0:t0 + tn],
                                                                                 start=(k == 0), stop=(k == KC - 1)),
                                  reads=wsr + ["HT"], writes=[("ps", b)])
                        ph.act(lambda e, b=b, dst=dst, t0=t0, tn=tn: e.copy(out=dst[:, t0:t0 + tn], in_=PS[b][:, :tn]),
                               reads=[("ps", b)], writes=[(nm, t0)])
                for t4 in range(0, NT, 4):
                    b = nb()
                    n4 = min(4, NT - t4)
                    for tt in range(n4):
                        t = t4 + tt
                        for k in range(KC):
                            ph.pe(lambda e, b=b, k=k, t=t, tt=tt: e.matmul(PS[b][:, tt * 128:(tt + 1) * 128], HT[:, k, t * 128:(t + 1) * 128], WS[:, k, 256:384],
                                                                         start=(k == 0), stop=(k == KC - 1)),
                                  reads=wsr + ["HT"], writes=[("ps", b)])
                    ph.dve(lambda e, b=b, t4=t4, n4=n4: e.tensor_copy(out=VK[:, t4:t4 + n4, :], in_=PS[b][:, :n4 * 128].rearrange("p (a n) -> p a n", a=n4)),
                           reads=[("ps", b)], writes=[("vk", t4)])
                if l == 1:
                    rope(ph, QT, "qt", nb, PS, al)
                    rope(ph, KT, "kt", nb, PS, al)
                    ES = al("es", [128, 8], F32)
                    ph.dma("sp", ES[:], sink_d[0:1, :].broadcast_to([128, 8]), writes=["es"])
                    ph.act(lambda e: e.activation(out=ES[:], in_=ES[:], func=AF.Exp), reads=["es"], writes=["es"])
                qtiles = list(range(NT)) if l == 0 else list(range(16))
                iters = [(e_, t) for e_ in range(2) for t in qtiles]
                info = {}

                def stage_a(it, e_, t):
                    r0 = 64 * e_
                    h = 2 * m + e_
                    if t >= 16:
                        kts, nbias = [16, 17], 0
                    elif l == 0:
                        kts, nbias = NA_KT[t] + [16, 17], len(NA_KT[t])
                    else:
                        kts = [kt for kt in (t - 1, t, t + 1) if 0 <= kt < 16]
                        nbias = len(kts)
                        kts = kts + [16, 17]
                    nk = len(kts)
                    info[it] = (kts, nk)
                    bi = BI[it % 3]
                    ss = SS[it % 2]
                    pt = PT[it % 4]
                    if nbias:
                        src = nab_d[h, t, :, 0:nbias * 128] if l == 0 else swab_d[t, :, 0:nbias * 128]
                        ph.dma("sp", bi[:, 0:nbias * 128], src, writes=[("bi", it % 3)])
                    banks = [nb(), nb()]
                    for j, kt in enumerate(kts):
                        b = banks[j // 4]
                        ph.pe(lambda e, b=b, j=j, kt=kt, t=t, r0=r0: e.matmul(PS[b][:, (j % 4) * 128:(j % 4 + 1) * 128], KT[r0:r0 + 64, kt * 128:(kt + 1) * 128],
                                                                          QT[r0:r0 + 64, t * 128:(t + 1) * 128], start=True, stop=True),
                              reads=["kt", "qt"], writes=[("ps", b)])
                    for bk in range(2):
                        j0, j1 = bk * 4, min(nk, bk * 4 + 4)
                        if j0 >= j1:
                            continue
                        b = banks[bk]
                        jb = min(j1, max(j0, nbias))
                        if jb > j0:
                            ph.dve(lambda e, b=b, j0=j0, jb=jb, ss=ss, bi=bi: e.scalar_tensor_tensor(
                                out=ss[:, j0 * 128:jb * 128], in0=PS[b][:, (j0 % 4) * 128:(j0 % 4) * 128 + (jb - j0) * 128], scalar=0.125,
                                in1=bi[:, j0 * 128:jb * 128], op0=ALU.mult, op1=ALU.add),
                                reads=[("ps", b), ("bi", it % 3)], writes=[("ss", it % 2, bk)])
                            ph.act(lambda e, j0=j0, jb=jb, ss=ss, pt=pt: e.activation(out=pt[:, j0 * 128:jb * 128], in_=ss[:, j0 * 128:jb * 128], func=AF.Exp),
                                   reads=[("ss", it % 2, bk)], writes=[("pt", it % 4)])
                        if j1 > jb:
                            ph.act(lambda e, b=b, jb=jb, j1=j1, pt=pt: e.activation(out=pt[:, jb * 128:j1 * 128], in_=PS[b][:, (jb % 4) * 128:(jb % 4) * 128 + (j1 - jb) * 128],
                                                                                 func=AF.Exp, scale=0.125),
                                   reads=[("ps", b)], writes=[("pt", it % 4)])

                def stage_b(it, e_, t):
                    r0 = 64 * e_
                    h = 2 * m + e_
                    kts, nk = info[it]
                    pt = PT[it % 4]
                    rd = RD[it % 2]
                    po = nb()
                    for j, kt in enumerate(kts):
                        ph.pe(lambda e, po=po, j=j, kt=kt, pt=pt, nk=nk: e.matmul(PS[po][:, 0:128], VK[:, kt, :], pt[:, j * 128:(j + 1) * 128],
                                                                              start=(j == 0), stop=(j == nk - 1)),
                              reads=["vk", ("pt", it % 4)], writes=[("ps", po)])
                    for j, kt in enumerate(kts):
                        ph.pe(lambda e, po=po, j=j, pt=pt, nk=nk: e.matmul(PS[po][:, 128:256], ONESB, pt[:, j * 128:(j + 1) * 128],
                                                                       start=(j == 0), stop=(j == nk - 1)),
                              reads=[("pt", it % 4)], writes=[("ps", po)])
                    if l == 1:
                        ph.dve(lambda e, po=po, rd=rd, r0=r0, h=h: e.tensor_scalar(out=rd[r0:r0 + 64, :], in0=PS[po][r0:r0 + 64, 128:256], scalar1=ES[r0:r0 + 64, h:h + 1],
                                                                                scalar2=None, op0=ALU.add), reads=[("ps", po), "es"], writes=[("rd", it % 2)])
                        ph.dve(lambda e, rd=rd, r0=r0: e.reciprocal(out=rd[r0:r0 + 64, :], in_=rd[r0:r0 + 64, :]), reads=[("rd", it % 2)], writes=[("rd", it % 2)])
                    else:
                        ph.dve(lambda e, po=po, rd=rd, r0=r0: e.reciprocal(out=rd[r0:r0 + 64, :], in_=PS[po][r0:r0 + 64, 128:256]),
                               reads=[("ps", po)], writes=[("rd", it % 2)])
                    ph.dve(lambda e, po=po, rd=rd, r0=r0, t=t: e.tensor_tensor(out=OT[r0:r0 + 64, t * 128:(t + 1) * 128], in0=PS[po][r0:r0 + 64, 0:128],
                                                                           in1=rd[r0:r0 + 64, :], op=ALU.mult),
                           reads=[("ps", po), ("rd", it % 2)], writes=[("ot", e_, t)])

                LAG = ATT_LAG
                for i in range(len(iters) + LAG):
                    if i < len(iters):
                        stage_a(i, *iters[i])
                    if i >= LAG:
                        stage_b(i - LAG, *iters[i - LAG])
                ph.dma("sp", YCAT_d[m, :, 0:nq], OT[:, 0:nq], reads=["ot"])
                ph.emit()
                if l == 0 and m == 0:
                    dump("OT0", OT[:], (128, T))

        MF = CF[:, 256:384]
        MB = CF[:, 384:512]
        SFm = CF[:, 512:640]
        SBm = CF[:, 640:768]
        SST_d = nc.dram_tensor("sst_scr", [2, NT, 128, 512], BF16, kind="Internal").ap()

        def scan_phase(P, groups, XS, BTOK, BT, CT, A4, DT4, const_decay, post_fn, out_tiles, pre_fn=None, H=8):
            HP = H * P
            nyb = HP // 512
            NTA = 1 if const_decay else NT
            ti = (lambda t: 0) if const_decay else (lambda t: t)
            oes = ExitStack()
            ESC = oes.enter_context(SBT("esc", [128, NTA, 2, H], F32))
            with ExitStack() as pes:
                ph = Phase(ctx, "scan")
                al = lambda name, shape, dtype: pes.enter_context(SBT(name, shape, dtype))
                PS = [pes.enter_context(PST("sps%d" % i, [128, 512], F32)) for i in range(8)]
                psi = [0]

                def nb():
                    psi[0] = (psi[0] + 1) % 8
                    return psi[0]
                if pre_fn is not None:
                    pre_fn(ph, al, PS, nb)
                CUMS = al("cums", [128, NTA, 3, 2 * H], F32)
                EW = al("ew", [128, NTA, 2, H], F32)
                ETOT = al("etot", [128, NTA, 2, H], F32)
                S = [al("st%d" % d, [128, 512], F32) for d in range(2)]
                STMP = al("stmp", [128, 512], F32)
                SBF = [al("sbf%d" % i, [128, 512], BF16) for i in range(3)]
                XW = [al("xw%d" % i, [128, HP], BF16) for i in range(2)]
                for t in range(NTA):
                    b = nb()
                    for ci, lm in enumerate((MF, MB, ONESF)):
                        ph.pe(lambda e, b=b, ci=ci, lm=lm, t=t: e.matmul(PS[b][:, ci * 2 * H:(ci + 1) * 2 * H], lm, A4[:, t].rearrange("p d h -> p (d h)"),
                                                                         start=True, stop=True), reads=["A4", "CF"], writes=[("ps", b)])
                    ph.act(lambda e, b=b, t=t: e.copy(out=CUMS[:, t].rearrange("p c n -> p (c n)"), in_=PS[b][:, 0:6 * H]), reads=[("ps", b)], writes=[("cums", t)])
                ph.act(lambda e: e.activation(out=ETOT[:].rearrange("p t d h -> p t (d h)"), in_=CUMS[:, :, 2, :], func=AF.Exp), reads=["cums"], writes=["etot"])
                for d in range(2):
                    ph.act(lambda e, d=d: e.activation(out=ESC[:, :, d, :], in_=CUMS[:, :, d, d * H:(d + 1) * H], func=AF.Exp), reads=["cums"], writes=[("esc", d)])
                    ph.dve(lambda e, d=d: e.tensor_tensor(out=EW[:, :, d, :], in0=CUMS[:, :, 2, d * H:(d + 1) * H], in1=CUMS[:, :, d, d * H:(d + 1) * H], op=ALU.subtract),
                           reads=["cums"], writes=[("ew", d)])
                    ph.act(lambda e, d=d: e.activation(out=EW[:, :, d, :], in_=EW[:, :, d, :], func=AF.Exp), reads=[("ew", d)], writes=[("ew", d)])
                    if DT4 is not None:
                        ph.dve(lambda e, d=d: e.tensor_tensor(out=EW[:, :, d, :], in0=EW[:, :, d, :], in1=DT4[:, :, d, :], op=ALU.mult),
                               reads=[("ew", d), "DT4"], writes=[("ew", d)])
                order = {0: [16, 17] + list(range(16)), 1: [17, 16] + list(range(15, -1, -1))}
                si = 0
                for d in range(2):
                    ph.dve(lambda e, d=d: e.memset(S[d][:], 0.0), writes=[("st", d)])
                for step in range(NT):
                    for d in range(2):
                        t = order[d][step]
                        sbf = SBF[si % 3]
                        xw = XW[si % 2]
                        ph.act(lambda e, sbf=sbf, d=d: e.copy(out=sbf[:], in_=S[d][:]), reads=[("st", d)], writes=[("sbf", si % 3)])
                        ph.dma("sp", SST_d[d, t], sbf[:], reads=[("sbf", si % 3)], writes=[("sst", d, t)])
                        if step < NT - 1:
                            ph.any2(lambda e, xw=xw, t=t, d=d: e.tensor_tensor(out=xw[:].rearrange("p (h q) -> p h q", h=H), in0=XS[:, t].rearrange("p (h q) -> p h q", h=H),
                                                                          in1=EW[:, ti(t), d, :].unsqueeze(2).broadcast_to([128, H, P]), op=ALU.mult),
                                    reads=["XS", ("ew", d)], writes=[("xw", si % 2)])
                            bks = [nb() for _ in range(nyb)]
                            for gi, g in enumerate(groups):
                                pc0 = g["heads"][0] * P
                                pcn = len(g["heads"]) * P
                                ph.pe(lambda e, g=g, pc0=pc0, pcn=pcn, xw=xw, t=t, bks=bks: e.matmul(
                                    PS[bks[pc0 // 512]][:, pc0 % 512:pc0 % 512 + pcn], BTOK[:, t, g["chunk"] * 128:(g["chunk"] + 1) * 128], xw[:, pc0:pc0 + pcn], start=True, stop=True),
                                    reads=["BTOK", ("xw", si % 2)], writes=[("ps", bks[pc0 // 512])])
                            for gi, g in enumerate(groups):
                                r0, nr, nh = g["row0"], g["nrows"], len(g["heads"])
                                h0 = g["heads"][0]
                                pc0 = h0 * P
                                pcn = nh * P
                                sc0 = g["scol0"]
                                ph.dve(lambda e, r0=r0, nr=nr, nh=nh, h0=h0, sc0=sc0, pcn=pcn, d=d, t=t: e.tensor_tensor(
                                    out=STMP[r0:r0 + nr, sc0:sc0 + pcn].rearrange("p (h q) -> p h q", h=nh), in0=S[d][r0:r0 + nr, sc0:sc0 + pcn].rearrange("p (h q) -> p h q", h=nh),
                                    in1=ETOT[r0:r0 + nr, ti(t), d, h0:h0 + nh].unsqueeze(2).broadcast_to([nr, nh, P]), op=ALU.mult),
                                    reads=[("st", d), "etot", ("sbf", si % 3)], writes=[("stmp", gi)])
                                ph.dve(lambda e, r0=r0, nr=nr, sc0=sc0, pc0=pc0, pcn=pcn, d=d, bks=bks: e.tensor_tensor(
                                    out=S[d][r0:r0 + nr, sc0:sc0 + pcn], in0=PS[bks[pc0 // 512]][r0:r0 + nr, pc0 % 512:pc0 % 512 + pcn], in1=STMP[r0:r0 + nr, sc0:sc0 + pcn], op=ALU.add),
                                    reads=[("stmp", gi), ("ps", bks[pc0 // 512])], writes=[("st", d, gi)])
                        si += 1
                ph.emit()
            with ExitStack() as pes:
                ph = Phase(ctx, "scan2")
                al = lambda name, shape, dtype: pes.enter_context(SBT(name, shape, dtype))
                PS = [pes.enter_context(PST("tps%d" % i, [128, 512], F32)) for i in range(7)]
                PBT = pes.enter_context(PST("tpb", [128, 1024], BF16))
                psi = [0]

                def nb():
                    psi[0] = (psi[0] + 1) % 7
                    return psi[0]
                ng = len(groups)
                GM = [[al("gm%d_%d" % (tp, d), [128, ng, 128], F32) for d in range(2)] for tp in range(2)]
                RH1 = al("rh", [128, H, 128], F32)
                RHS = [RH1, RH1]
                EXPD = [al("expd%d" % d, [128, H, 128], F32) for d in range(2)]
                XD = [al("xd%d" % i, [128, HP], BF16) for i in range(4)] if DT4 is not None else None
                MP = [al("mp%d" % i, [128, H, 128], BF16) for i in range(4)]
                SW = max(g["scol0"] + len(g["heads"]) * P for g in groups)
                SIN = [al("sin%d" % i, [128, SW], BF16) for i in range(4)]
                YT1 = al("yt", [128, HP], F32)
                YT = [YT1, YT1]
                Y = [al("y%d" % i, [128, HP], F32) for i in range(2)]
                post_state = post_fn("init", ph, al, PS, nb, PBT)
                if dbg_d and "SST" in dbg_d:
                    sf = al("dbgsst", [128, 512], F32)
                    ph.dma("sp", SIN[0][:], SST_d[0, 17], writes=[("sin", 0)])
                    ph.dve(lambda e: e.tensor_copy(out=sf[:], in_=SIN[0][:]), reads=[("sin", 0)], writes=["sf"])
                    ph.dma("sp", dbg_d["SST"], sf[:], reads=["sf"])
                masks = (MF, MB)
                u1 = (SFm, SBm)

                def build_expd(t, d):
                    RH = RHS[d]
                    ph.any2(lambda e, t=t, d=d, RH=RH: e.tensor_tensor(out=RH[:], in0=masks[d].unsqueeze(1).broadcast_to([128, H, 128]),
                                                                       in1=A4[:, t, d, :].unsqueeze(2).broadcast_to([128, H, 128]), op=ALU.mult),
                            reads=["A4", "CF"], writes=["rh"])
                    for hh in range(H // 4):
                        b = nb()
                        ph.pe(lambda e, b=b, hh=hh, d=d, RH=RH: e.matmul(PS[b][:], u1[d], RH[:, hh * 4:(hh + 1) * 4, :].rearrange("p h i -> p (h i)"), start=True, stop=True),
                              reads=["rh", "CF"], writes=[("ps", b)])
                        ph.act(lambda e, b=b, hh=hh, d=d: e.activation(out=EXPD[d][:, hh * 4:(hh + 1) * 4, :].rearrange("p h i -> p (h i)"), in_=PS[b][:], func=AF.Exp),
                               reads=[("ps", b)], writes=[("expd", d, hh)])
                if const_decay:
                    for d in range(2):
                        build_expd(0, d)
                it = 0
                def p2_stage_a(ti_, t):
                    tsl = slice(t * 128, (t + 1) * 128)
                    tp = ti_ % 2
                    gbanks = []
                    for gi, g in enumerate(groups):
                        if not gbanks or groups[gbanks[-1][1]]["row0"] != g["row0"] or gbanks[-1][2] == 4:
                            gbanks.append([nb(), gi, 0])
                        bk, g0, n = gbanks[-1]
                        r0, nr, ch = g["row0"], g["nrows"], g["chunk"]
                        ph.pe(lambda e, bk=bk, n=n, r0=r0, nr=nr, ch=ch, tsl=tsl: e.matmul(PS[bk][:, n * 128:(n + 1) * 128], BT[r0:r0 + nr, ch, tsl], CT[r0:r0 + nr, ch, tsl],
                                                                                       start=True, stop=True), reads=["BT", "CT"], writes=[("ps", bk)])
                        gbanks[-1][2] += 1
                    for d in range(2):
                        for (bk, g0, n) in gbanks:
                            ph.dve(lambda e, d=d, bk=bk, g0=g0, n=n, tp=tp: e.tensor_tensor(out=GM[tp][d][:, g0:g0 + n, :], in0=PS[bk][:, 0:n * 128].rearrange("p (g i) -> p g i", g=n),
                                                                                        in1=masks[d].unsqueeze(1).broadcast_to([128, n, 128]), op=ALU.mult),
                                   reads=[("ps", bk), "CF"], writes=[("gm", tp, d, g0)])
                    for d in range(2):
                        slot = tp * 2 + d
                        mp = MP[slot]
                        sin = SIN[slot]
                        ph.dma("sp", sin[:], SST_d[d, t, :, 0:SW], writes=[("sin", slot)])
                        if not const_decay:
                            build_expd(t, d)
                        gmb = GM[tp][d][:] if ng == H else GM[tp][d][:, 0:1, :].broadcast_to([128, H, 128])
                        if DT4 is not None:
                            xd = XD[slot]
                            ph.any2(lambda e, d=d, t=t, xd=xd: e.tensor_tensor(out=xd[:].rearrange("p (h q) -> p h q", h=H), in0=XS[:, t].rearrange("p (h q) -> p h q", h=H),
                                                                              in1=DT4[:, t, d, :].unsqueeze(2).broadcast_to([128, H, P]), op=ALU.mult),
                                    reads=["XS", "DT4"], writes=[("xd", slot)])
                        ph.any2(lambda e, mp=mp, gmb=gmb, d=d: e.tensor_tensor(out=mp[:], in0=EXPD[d][:], in1=gmb, op=ALU.mult),
                                reads=[("expd", d), ("gm", tp, d)], writes=[("mp", slot)])

                def p2_stage_b(ti_, t):
                    tsl = slice(t * 128, (t + 1) * 128)
                    tp = ti_ % 2
                    for d in range(2):
                        slot = tp * 2 + d
                        mp = MP[slot]
                        sin = SIN[slot]
                        yd = [nb() for _ in range(nyb)]
                        for h in range(H):
                            xrhs = XD[slot][:, h * P:(h + 1) * P] if DT4 is not None else XS[:, t, h * P:(h + 1) * P]
                            ph.pe(lambda e, h=h, mp=mp, yd=yd, xrhs=xrhs: e.matmul(PS[yd[h * P // 512]][:, (h * P) % 512:(h * P) % 512 + P], mp[:, h, :], xrhs, start=True, stop=True),
                                  reads=[("mp", slot), "XS", ("xd", slot)], writes=[("ps", yd[h * P // 512])])
                        ybanks = []
                        for gi, g in enumerate(groups):
                            r0, nr, ch = g["row0"], g["nrows"], g["chunk"]
                            pc0 = g["heads"][0] * P
                            pcn = len(g["heads"]) * P
                            sc0 = g["scol0"]
                            if not ybanks or ybanks[-1][5] != r0 or ybanks[-1][2] + pcn > 512:
                                ybanks.append([nb(), pc0, 0, g["heads"][0], 0, r0])
                            bk, used = ybanks[-1][0], ybanks[-1][2]
                            ph.pe(lambda e, r0=r0, nr=nr, ch=ch, pcn=pcn, sc0=sc0, sin=sin, bk=bk, used=used, tsl=tsl: e.matmul(
                                PS[bk][:, used:used + pcn], CT[r0:r0 + nr, ch, tsl], sin[r0:r0 + nr, sc0:sc0 + pcn], start=True, stop=True),
                                reads=["CT", ("sin", slot)], writes=[("ps", bk)])
                            ybanks[-1][2] += pcn
                            ybanks[-1][4] += len(g["heads"])
                        yacc = Y[0] if d == 0 else Y[1]
                        yt = YT[d]
                        for (bk, c0, cn, h0, nh, _) in ybanks:
                            ph.dve(lambda e, bk=bk, c0=c0, cn=cn, h0=h0, nh=nh, t=t, d=d, yt=yt: e.tensor_tensor(
                                out=yt[:, c0:c0 + cn].rearrange("p (h q) -> p h q", h=nh), in0=PS[bk][:, 0:cn].rearrange("p (h q) -> p h q", h=nh),
                                in1=ESC[:, ti(t), d, h0:h0 + nh].unsqueeze(2).broadcast_to([128, nh, P]), op=ALU.mult),
                                reads=[("ps", bk), ("esc", d)], writes=[("yt", c0)])
                        for q in range(nyb):
                            csl = slice(q * 512, (q + 1) * 512)
                            ph.dve(lambda e, q=q, yd=yd, csl=csl, yacc=yacc, yt=yt: e.tensor_tensor(out=yacc[:, csl], in0=PS[yd[q]][:], in1=yt[:, csl], op=ALU.add),
                                   reads=[("ps", yd[q]), "yt"], writes=[("y", d, q)])
                    if not (dbg_d and "Yall" in dbg_d):
                        ph.pool(lambda e: e.tensor_tensor(out=Y[0][:], in0=Y[0][:], in1=Y[1][:], op=ALU.add), reads=[("y", 0), ("y", 1)], writes=[("y", 0)])
                    if dbg_d and "Yall" in dbg_d and HP == 512:
                        ph.dma("sp", dbg_d["Yall"][:, t * 512:(t + 1) * 512], Y[0][:], reads=[("y", 0)])
                        ph.dma("sp", dbg_d["Y1all"][:, t * 512:(t + 1) * 512], Y[1][:], reads=[("y", 1)])
                    post_fn("tile", ph, al, PS, nb, post_state, t, Y[0])

                otl = list(out_tiles)
                for i in range(len(otl) + 1):
                    if i < len(otl):
                        p2_stage_a(i, otl[i])
                    if i >= 1:
                        p2_stage_b(i - 1, otl[i - 1])
                post_fn("fini", ph, al, PS, nb, post_state)
                ph.emit()
            oes.close()

        def ssd_group(g):
            with ExitStack() as ges:
                gal = lambda name, shape, dtype: ges.enter_context(SBT(name, shape, dtype))
                XS = gal("xs", [128, NT, 512], BF16)
                BTOK = gal("btok", [128, NT, 128], BF16)
                BT = gal("bt", [128, 1, T], BF16)
                CT = gal("ct", [128, 1, T], BF16)
                DT4 = gal("dt4", [128, NT, 2, 8], F32)
                A4 = gal("a4", [128, NT, 2, 8], F32)
                WZ = gal("wz", [128, KC, 512], BF16)
                Wr = ev_w_in_d.rearrange("(k p) n -> p k n", p=128)
                with ExitStack() as pes:
                    ph = Phase(ctx, "ssdproj")
                    al = lambda name, shape, dtype: pes.enter_context(SBT(name, shape, dtype))
                    PS = [pes.enter_context(PST("bps%d" % i, [128, 512], F32)) for i in range(6)]
                    PB = [pes.enter_context(PST("bpb%d" % i, [128, 1024], BF16)) for i in range(2)]
                    psi = [0]

                    def nb():
                        psi[0] = (psi[0] + 1) % 6
                        return psi[0]
                    WS = [al("wsl%d" % i, [128, KC, 128], BF16) for i in range(2)]
                    WDT = al("wdt", [128, KC, 16], BF16)
                    DBA = al("dba", [128, 2, 2, 8], F32)
                    XPAD = al("xpad", [128, 2320], F32)
                    ACC = al("acc", [128, T], F32)
                    XST = [al("xst%d" % i, [128, T], BF16) for i in range(2)]
                    ph.dma("pool", WZ[:], Wr[:, :, 1536 + g * 512:1536 + (g + 1) * 512], writes=["wz"])
                    for d in range(2):
                        ph.dma("pool", WDT[:, :, d * 8:(d + 1) * 8], Wr[:, :, 4096 + d * 16 + g * 8:4096 + d * 16 + g * 8 + 8], writes=[("wdt", d)])
                        for w in range(2):
                            ph.dma("sp", DBA[:, w, d, :], dtba_d[w:w + 1, d * 16 + g * 8:d * 16 + g * 8 + 8].broadcast_to([128, 8]), writes=[("dba", w, d)])
                    ph.pool(lambda e: e.memset(XPAD[:], 0.0), writes=["xpad"])
                    b = nb()
                    for t in range(NT):
                        for k in range(KC):
                            ph.pe(lambda e, b=b, t=t, k=k: e.matmul(PS[b][:, t * 16:(t + 1) * 16], HT[:, k, t * 128:(t + 1) * 128], WDT[:, k, :], start=(k == 0), stop=(k == KC - 1)),
                                  reads=["wdt", "HT"], writes=[("ps", b)])
                    dt3 = DT4[:].rearrange("p t d h -> p t (d h)")
                    ph.dve(lambda e, b=b: e.tensor_tensor(out=dt3, in0=PS[b][:, 0:NT * 16].rearrange("p (t n) -> p t n", t=NT),
                                                          in1=DBA[:, 0].rearrange("p d h -> p (d h)").unsqueeze(1).broadcast_to([128, NT, 16]), op=ALU.add),
                           reads=[("ps", b), "dba"], writes=["DT4"])
                    ph.act(lambda e: e.activation(out=dt3, in_=dt3, func=AF.Exp), reads=["DT4"], writes=["DT4"])
                    ph.act(lambda e: e.activation(out=dt3, in_=dt3, func=AF.Ln, bias=1.0), reads=["DT4"], writes=["DT4"])
                    ph.act(lambda e: e.activation(out=DBA[:, 1], in_=DBA[:, 1], func=AF.Exp), reads=["dba"], writes=["dba"])
                    ph.dve(lambda e: e.scalar_tensor_tensor(out=A4[:].rearrange("p t d h -> p t (d h)"), in0=dt3, scalar=-1.0,
                                                            in1=DBA[:, 1].rearrange("p d h -> p (d h)").unsqueeze(1).broadcast_to([128, NT, 16]), op0=ALU.mult, op1=ALU.mult),
                           reads=["DT4", "dba"], writes=["A4"])
                    chunks = [4 * g + i for i in range(4)] + [8 + g, 10 + g]
                    for ci, c in enumerate(chunks):
                        ws = WS[ci % 2]
                        ph.dma("pool", ws[:], Wr[:, :, 2560 + c * 128:2560 + (c + 1) * 128], writes=[("wsl", ci % 2)])
                        for (t0, tn) in BLKS:
                            b = nb()
                            for k in range(KC):
                                ph.pe(lambda e, b=b, k=k, ws=ws, t0=t0, tn=tn: e.matmul(PS[b][:, :tn], ws[:, k, :], HT[:, k, t0:t0 + tn], start=(k == 0), stop=(k == KC - 1)),
                                      reads=[("wsl", ci % 2), "HT"], writes=[("ps", b)])
                            o0 = 2 + t0 if t0 < L else 2054 + (t0 - L)
                            ph.act(lambda e, b=b, o0=o0, tn=tn: e.copy(out=XPAD[:, o0:o0 + tn], in_=PS[b][:, :tn]), reads=[("ps", b)], writes=[("xpad", t0)])
                        eng = "dve"
                        for (o0, a0, n) in ((2, 0, L), (2054, L, LC)):
                            wcol = lambda k, c=c: VT[:, VR["conv_w"] + k * 12 + c:VR["conv_w"] + k * 12 + c + 1]
                            bcol = VT[:, VR["conv_b"] + c:VR["conv_b"] + c + 1]
                            ph.op(eng, lambda e, o0=o0, a0=a0, n=n, wcol=wcol, bcol=bcol: e.tensor_scalar(out=ACC[:, a0:a0 + n], in0=XPAD[:, o0 - 2:o0 - 2 + n], scalar1=wcol(0), scalar2=bcol,
                                                                                                    op0=ALU.mult, op1=ALU.add), reads=["xpad", "VT"], writes=[("acc", a0)])
                            for k in range(1, 5):
                                ph.op(eng, lambda e, o0=o0, a0=a0, n=n, k=k, wcol=wcol: e.scalar_tensor_tensor(out=ACC[:, a0:a0 + n], in0=XPAD[:, o0 - 2 + k:o0 - 2 + k + n], scalar=wcol(k),
                                                                                                      in1=ACC[:, a0:a0 + n], op0=ALU.mult, op1=ALU.add),
                                      reads=["xpad", ("acc", a0)], writes=[("acc", a0)])
                        if ci < 4:
                            dst, dkey = XST[ci % 2][:], ("xst", ci % 2)
                        elif ci == 4:
                            dst, dkey = BT[:, 0, :], "BT"
                        else:
                            dst, dkey = CT[:, 0, :], "CT"
                        ph.act(lambda e, dst=dst: e.activation(out=dst, in_=ACC[:], func=AF.Silu), reads=["acc"], writes=[dkey])
                        if ci <= 4:
                            for t8 in range(0, NT, 8):
                                n8 = min(8, NT - t8)
                                pb = (t8 // 8 + ci) % 2
                                for tt in range(n8):
                                    t = t8 + tt
                                    ph.pe(lambda e, pb=pb, tt=tt, t=t, dst=dst: e.transpose(PB[pb][:, tt * 128:(tt + 1) * 128], dst[:, t * 128:(t + 1) * 128], IDB),
                                          reads=[dkey, "CB"], writes=[("pb", pb)])
                                if ci < 4:
                                    o = XS[:, t8:t8 + n8, ci * 128:(ci + 1) * 128]
                                    okey = ("XS", ci, t8)
                                else:
                                    o = BTOK[:, t8:t8 + n8, :]
                                    okey = ("BTOK", t8)
                                ph.act(lambda e, pb=pb, n8=n8, o=o: e.copy(out=o, in_=PB[pb][:, 0:n8 * 128].rearrange("p (a n) -> p a n", a=n8)),
                                       reads=[("pb", pb)], writes=[okey])
                    ph.emit()
                if g == 0:
                    dump("XS0", XS[:].rearrange("p t n -> p (t n)"), (128, NT * 512))
                    dump("A40", A4[:].rearrange("p t d h -> p (t d h)"), (128, NT * 16))
                    dump("DT40", DT4[:].rearrange("p t d h -> p (t d h)"), (128, NT * 16))
                    dump("CT0", CT[:, 0, :], (128, T))
                    dump("BTOK0", BTOK[:].rearrange("p t n -> p (t n)"), (128, NT * 128))

                def post(stage, ph, al, PS, nb, st=None, t=None, Yt=None):
                    if stage == "init":
                        pbt = st
                        st = {}
                        st["dsk"] = al("dsk", [128, 8], F32)
                        st["gng"] = al("gng", [128, 512], F32)
                        st["sz"] = al("sz", [128, 512], F32)
                        st["yz"] = al("yz", [128, 512], F32)
                        st["sq"] = st["sz"]
                        st["ssq"] = al("ssq", [128, 4], F32)
                        st["yn"] = al("yn", [128, 512], BF16)
                        st["stg"] = [al("stg%d" % i, [128, 4, 128], BF16) for i in range(2)]
                        st["pb"] = pbt
                        ph.dma("sp", st["dsk"][:], ssmd_d[0:1, g * 8:(g + 1) * 8].broadcast_to([128, 8]), writes=["dsk"])
                        ph.dma("sp", st["gng"][:], ssmg_d[0:1, g * 512:(g + 1) * 512].broadcast_to([128, 512]), writes=["gng"])
                        return st
                    if stage == "fini":
                        return
                    dsk, gng, sz, yz, sq, ssq, yn = st["dsk"], st["gng"], st["sz"], st["yz"], st["sq"], st["ssq"], st["yn"]
                    b = nb()
                    for k in range(KC):
                        ph.pe(lambda e, b=b, k=k: e.matmul(PS[b][:], HT[:, k, t * 128:(t + 1) * 128], WZ[:, k, :], start=(k == 0), stop=(k == KC - 1)),
                              reads=["HT", "wz"], writes=[("ps", b)])
                    ph.act(lambda e, b=b: e.activation(out=sz[:], in_=PS[b][:], func=AF.Silu), reads=[("ps", b)], writes=["sz"])
                    ph.dve(lambda e: e.tensor_tensor(out=yz[:].rearrange("p (h q) -> p h q", h=8), in0=XS[:, t].rearrange("p (h q) -> p h q", h=8),
                                                     in1=dsk[:].unsqueeze(2).broadcast_to([128, 8, 64]), op=ALU.mult), reads=["XS", "dsk"], writes=["yz"])
                    ph.dve(lambda e: e.tensor_tensor(out=yz[:], in0=yz[:], in1=Yt[:], op=ALU.add), reads=["yz", ("y", 0)], writes=["yz"])
                    ph.dve(lambda e: e.tensor_tensor(out=yz[:], in0=yz[:], in1=sz[:], op=ALU.mult), reads=["yz", "sz"], writes=["yz"])
                    ph.act(lambda e: e.activation(out=sq[:], in_=yz[:], func=AF.Square, accum_out=ssq[:, 0:1]), reads=["yz"], writes=["sz", "ssq"])
                    ph.dve(lambda e: e.tensor_scalar(out=ssq[:, 1:2], in0=ssq[:, 0:1], scalar1=1.0 / 512, scalar2=EPS, op0=ALU.mult, op1=ALU.add), reads=["ssq"], writes=["ssq"])
                    ph.act(lambda e: e.sqrt(out=ssq[:, 2:3], in_=ssq[:, 1:2]), reads=["ssq"], writes=["ssq"])
                    ph.dve(lambda e: e.reciprocal(out=ssq[:, 3:4], in_=ssq[:, 2:3]), reads=["ssq"], writes=["ssq"])
                    ph.dve(lambda e: e.scalar_tensor_tensor(out=yn[:], in0=yz[:], scalar=ssq[:, 3:4], in1=gng[:], op0=ALU.mult, op1=ALU.mult),
                           reads=["yz", "ssq", "gng"], writes=["yn"])
                    pb = st["pb"]
                    stg = st["stg"][t % 2]
                    for cl in range(4):
                        ph.pe(lambda e, cl=cl: e.transpose(pb[:, cl * 128:(cl + 1) * 128], yn[:, cl * 128:(cl + 1) * 128], IDB), reads=["yn", "CB"], writes=["pbt"])
                    ph.act(lambda e, stg=stg: e.copy(out=stg[:], in_=pb[:, 0:512].rearrange("p (a n) -> p a n", a=4)), reads=["pbt"], writes=[("stg", t % 2)])
                    ph.dma("sp", YCAT_d[4 + 4 * g:8 + 4 * g, :, t * 128:(t + 1) * 128].rearrange("c p t -> p c t"), stg[:], reads=[("stg", t % 2)])

                pes_pb = [None]

                def pre(ph, al, PS, nb):
                    pass
                groups = [dict(chunk=0, row0=0, nrows=128, heads=list(range(8)), scol0=0, sncols=512)]
                scan_phase(64, groups, XS, BTOK, BT, CT, A4, DT4, False, post, list(range(NT)))

        def out_proj(l):
            W_d = ev_w_out_d if l == 0 else od_w_out_d
            with ExitStack() as pes:
                ph = Phase(ctx, "oproj")
                al = lambda name, shape, dtype: pes.enter_context(SBT(name, shape, dtype))
                PS = [pes.enter_context(PST("ops%d" % i, [128, 512], F32)) for i in range(8)]
                WO = al("wo", [128, 12, D], BF16)
                YB = [al("yb%d" % i, [128, 12, 512], BF16) for i in range(2)]
                for c in range(0, 12, 4):
                    ph.dma("pool", WO[:, c:c + 4, :], W_d.rearrange("(c p) n -> p c n", p=128)[:, c:c + 4, :], writes=[("wo", c)])
                pi = 0
                for bi_, (t0, tn) in enumerate(BLKS):
                    if l == 1 and t0 >= L:
                        continue
                    j = 0 if t0 < L else 1
                    yb = YB[bi_ % 2]
                    ph.dma("sp", yb[:, :, :tn], YCAT_d[:, :, t0:t0 + tn].rearrange("c p t -> p c t"), writes=[("yb", bi_ % 2)])
                    for dc in range(KC):
                        b = pi % 8
                        pi += 1
                        for c in range(12):
                            ph.pe(lambda e, b=b, c=c, dc=dc, yb=yb, tn=tn: e.matmul(PS[b][:, :tn], WO[:, c, dc * 128:(dc + 1) * 128], yb[:, c, :tn], start=(c == 0), stop=(c == 11)),
                                  reads=["wo", ("yb", bi_ % 2)], writes=[("ps", b)])
                        ph.dve(lambda e, b=b, dc=dc, t0=t0, tn=tn, j=j: e.scalar_tensor_tensor(out=XT[:, dc, t0:t0 + tn], in0=PS[b][:, :tn], scalar=AB[:, l, j, 2, dc:dc + 1],
                                                                                            in1=XT[:, dc, t0:t0 + tn], op0=ALU.mult, op1=ALU.add),
                               reads=[("ps", b)], writes=[("XT", dc, bi_)])
                ph.emit()

        THIRDS = [[(0, 512), (512, 256)], [(768, 512), (1280, 256)], [(1536, 512), (2048, 256)]]

        def ffn(l, GT=None):
            moe = (l == 1)
            nfc = 28 if moe else 22
            nexp = NEXPERTS if moe else 1
            with ExitStack() as pes:
                ph = Phase(ctx, "ffn")
                al = lambda name, shape, dtype: pes.enter_context(SBT(name, shape, dtype))
                PS = [pes.enter_context(PST("fps%d" % i, [128, 512], F32)) for i in range(8)]
                psi = [0]

                def nb():
                    psi[0] = (psi[0] + 1) % 8
                    return psi[0]
                tgroups = [[(0, 512), (512, 512)], [(1024, 512), (1536, 512)]] if moe else THIRDS
                gmax = 1024 if moe else 768
                fchunks = [list(range(0, 14)), list(range(14, 28))] if moe else [list(range(22))]
                nfl = len(fchunks[0])
                ACTT = al("actt", [128, nfl, gmax], BF16)
                W13 = [al("w13_%d" % i, [128, 2, KC, 128], BF16) for i in range(3)]
                W2S = [al("w2s_%d" % i, [128, nfl, 128], BF16) for i in range(2)]
                SIL = [al("sil%d" % i, [128, 512], F32) for i in range(2)]
                if moe:
                    HG = al("hg", [128, KC, gmax], BF16)
                wi = 0
                w2i = 0
                si = 0
                for th, blks in enumerate(tgroups):
                    tb = blks[0][0]
                    for ex in range(nexp):
                        if moe:
                            w1_d, w3_d, w2_d = moe_w1_d[ex], moe_w3_d[ex], moe_w2_d[ex]
                            for (t0, tn) in blks:
                                b = nb()
                                ph.pe(lambda e, b=b, ex=ex, t0=t0, tn=tn: e.matmul(PS[b][:, :tn], SEL[:, ex * 128:(ex + 1) * 128], GT[:, t0:t0 + tn], start=True, stop=True),
                                      reads=["GT", "SEL"], writes=[("ps", b)])
                                for k in range(KC):
                                    ph.dve(lambda e, b=b, k=k, t0=t0, tn=tn, tb=tb: e.tensor_tensor(out=HG[:, k, t0 - tb:t0 - tb + tn], in0=HT[:, k, t0:t0 + tn], in1=PS[b][:, :tn], op=ALU.mult),
                                           reads=[("ps", b), "HT"], writes=[("hg", k, t0)])
                        else:
                            w1_d, w3_d, w2_d = ffn_w1_d, ffn_w3_d, ffn_w2_d
                        w1r = w1_d.rearrange("(k p) n -> p k n", p=128)
                        w3r = w3_d.rearrange("(k p) n -> p k n", p=128)
                        w2r = w2_d.rearrange("(f p) n -> p f n", p=128)
                        for fcs in fchunks:
                            for fi, fc in enumerate(fcs):
                                w = W13[wi % 3]
                                ph.dma("pool", w[:, 0], w1r[:, :, fc * 128:(fc + 1) * 128], writes=[("w13", wi % 3, 0)])
                                ph.dma("pool", w[:, 1], w3r[:, :, fc * 128:(fc + 1) * 128], writes=[("w13", wi % 3, 1)])
                                for (t0, tn) in blks:
                                    b1, b3 = nb(), nb()
                                    for k in range(KC):
                                        ph.pe(lambda e, b1=b1, k=k, w=w, t0=t0, tn=tn: e.matmul(PS[b1][:, :tn], w[:, 0, k, :], HT[:, k, t0:t0 + tn], start=(k == 0), stop=(k == KC - 1)),
                                              reads=[("w13", wi % 3, 0), "HT"], writes=[("ps", b1)])
                                    for k in range(KC):
                                        rhs = HG[:, k, t0 - tb:t0 - tb + tn] if moe else HT[:, k, t0:t0 + tn]
                                        ph.pe(lambda e, b3=b3, k=k, w=w, rhs=rhs, tn=tn: e.matmul(PS[b3][:, :tn], w[:, 1, k, :], rhs, start=(k == 0), stop=(k == KC - 1)),
                                              reads=[("w13", wi % 3, 1), "HT", "hg"], writes=[("ps", b3)])
                                    sil = SIL[si % 2]
                                    ph.act(lambda e, b1=b1, sil=sil, tn=tn: e.activation(out=sil[:, :tn], in_=PS[b1][:, :tn], func=AF.Silu), reads=[("ps", b1)], writes=[("sil", si % 2)])
                                    ph.dve(lambda e, b3=b3, sil=sil, fi=fi, t0=t0, tn=tn, tb=tb: e.tensor_tensor(out=ACTT[:, fi, t0 - tb:t0 - tb + tn], in0=PS[b3][:, :tn], in1=sil[:, :tn], op=ALU.mult),
                                           reads=[("ps", b3), ("sil", si % 2)], writes=[("actt", fi, t0)])
                                    si += 1
                                wi += 1
                            nf = len(fcs)
                            for dc in range(KC):
                                w2 = W2S[w2i % 2]
                                ph.dma("pool", w2[:, 0:nf, :], w2r[:, fcs[0]:fcs[0] + nf, dc * 128:(dc + 1) * 128], writes=[("w2s", w2i % 2)])
                                for (t0, tn) in blks:
                                    j = 0 if t0 < L else 1
                                    b = nb()
                                    for fi in range(nf):
                                        ph.pe(lambda e, b=b, fi=fi, w2=w2, t0=t0, tn=tn, tb=tb, nf=nf: e.matmul(PS[b][:, :tn], w2[:, fi, :], ACTT[:, fi, t0 - tb:t0 - tb + tn], start=(fi == 0), stop=(fi == nf - 1)),
                                              reads=[("w2s", w2i % 2), "actt"], writes=[("ps", b)])
                                    ph.dve(lambda e, b=b, dc=dc, t0=t0, tn=tn, j=j: e.scalar_tensor_tensor(out=XT[:, dc, t0:t0 + tn], in0=PS[b][:, :tn], scalar=AB[:, l, j, 5, dc:dc + 1],
                                                                                                        in1=XT[:, dc, t0:t0 + tn], op0=ALU.mult, op1=ALU.add),
                                           reads=[("ps", b)], writes=[("XT", dc, t0)])
                                w2i += 1
                ph.emit()

        def final_out():
            with ExitStack() as pes:
                ph = Phase(ctx, "fin")
                al = lambda name, shape, dtype: pes.enter_context(SBT(name, shape, dtype))
                PS = [pes.enter_context(PST("zps%d" % i, [128, 512], F32)) for i in range(8)]
                SQ = [al("fsq%d" % i, [128, 512], BF16) for i in range(3)]
                RS = [al("frs%d" % i, [128, 512], F32) for i in range(2)]
                XN = [al("fxn%d" % i, [128, KC, 512], F32) for i in range(2)]
                OTK = [al("fot%d" % i, [128, D], F32) for i in range(3)]
                qi = 0
                oi = 0
                pi = 0
                g0 = VR["final_g"]
                for bi_, (t0, tn) in enumerate(BLKS[:4]):
                    pb = pi % 8
                    pi += 1
                    for k in range(KC):
                        sq = SQ[qi % 3]
                        ph.act(lambda e, sq=sq, k=k, t0=t0: e.activation(out=sq[:], in_=XT[:, k, t0:t0 + 512], func=AF.Square), reads=["XT"], writes=[("sq", qi % 3)])
                        ph.pe(lambda e, sq=sq, k=k, pb=pb: e.matmul(PS[pb][:], ONESB, sq[:], start=(k == 0), stop=(k == KC - 1)), reads=[("sq", qi % 3)], writes=[("ps", pb)])
                        qi += 1
                    rs = RS[bi_ % 2]
                    xn = XN[bi_ % 2]
                    ph.dve(lambda e, rs=rs, pb=pb: e.tensor_scalar(out=rs[:], in0=PS[pb][:], scalar1=1.0 / D, scalar2=EPS, op0=ALU.mult, op1=ALU.add), reads=[("ps", pb)], writes=[("rs", bi_ % 2)])
                    ph.act(lambda e, rs=rs: e.sqrt(out=rs[:], in_=rs[:]), reads=[("rs", bi_ % 2)], writes=[("rs", bi_ % 2)])
                    ph.dve(lambda e, rs=rs: e.reciprocal(out=rs[:], in_=rs[:]), reads=[("rs", bi_ % 2)], writes=[("rs", bi_ % 2)])
                    for k in range(KC):
                        ph.dve(lambda e, k=k, t0=t0, rs=rs, xn=xn: e.scalar_tensor_tensor(out=xn[:, k, :], in0=XT[:, k, t0:t0 + 512], scalar=VT[:, g0 + k:g0 + k + 1], in1=rs[:], op0=ALU.mult, op1=ALU.mult),
                               reads=["XT", ("rs", bi_ % 2)], writes=[("xn", bi_ % 2, k)])
                    for tt in range(4):
                        otk = OTK[oi % 3]
                        for half in range(2):
                            pb = pi % 8
                            pi += 1
                            for kk in range(4):
                                k = half * 4 + kk
                                ph.pe(lambda e, pb=pb, kk=kk, k=k, tt=tt, xn=xn: e.transpose(PS[pb][:, kk * 128:(kk + 1) * 128], xn[:, k, tt * 128:(tt + 1) * 128], IDF),
                                      reads=[("xn", bi_ % 2), "CF"], writes=[("ps", pb)])
                            if half == 0:
                                ph.act(lambda e, pb=pb, otk=otk: e.copy(out=otk[:, 0:512], in_=PS[pb][:]), reads=[("ps", pb)], writes=[("otk", oi % 3, 0)])
                            else:
                                ph.dve(lambda e, pb=pb, otk=otk: e.tensor_copy(out=otk[:, 512:1024], in_=PS[pb][:]), reads=[("ps", pb)], writes=[("otk", oi % 3, 1)])
                        ph.dma("sp", out_d[t0 + tt * 128:t0 + (tt + 1) * 128, :], otk[:], reads=[("otk", oi % 3)])
                        oi += 1
                ph.emit()

        RM = CB[:, 768:896]

        def rope(ph, X, key, nb, PS, al):
            sid = 0
            store = ph.__dict__.setdefault("_rope_store", {})
            if sid not in store:
                COS = al("cos", [128, L], F32)
                SIN = al("sin", [128, L], F32)
                T1 = [al("rt1_%d" % i, [128, 512], F32) for i in range(2)]
                T2 = [al("rt2_%d" % i, [128, 512], F32) for i in range(2)]
                ph.dma("sp", COS[:], rope_d[0], writes=["cos"])
                ph.dma("sp", SIN[:], rope_d[1], writes=["sin"])
                store[sid] = (COS, SIN, T1, T2, [0])
            COS, SIN, T1, T2, cnt = store[sid]
            for (t0, tn) in BLKS[:4]:
                b = nb()
                i = cnt[0] % 2
                cnt[0] += 1
                ph.pe(lambda e, b=b, t0=t0: e.matmul(PS[b][:], RM, X[:, t0:t0 + 512], start=True, stop=True), reads=[key, "CB"], writes=[("ps", b)])
                ph.dve(lambda e, i=i, t0=t0: e.tensor_tensor(out=T1[i][:], in0=X[:, t0:t0 + 512], in1=COS[:, t0:t0 + 512], op=ALU.mult), reads=[key, "cos"], writes=[("rt1", i)])
                ph.dve(lambda e, i=i, b=b, t0=t0: e.tensor_tensor(out=T2[i][:], in0=PS[b][:], in1=SIN[:, t0:t0 + 512], op=ALU.mult), reads=[("ps", b), "sin"], writes=[("rt2", i)])
                ph.pool(lambda e, i=i, t0=t0: e.tensor_tensor(out=X[:, t0:t0 + 512], in0=T1[i][:], in1=T2[i][:], op=ALU.add), reads=[("rt1", i), ("rt2", i)], writes=[(key, "r", t0) if isinstance(key, str) else key])

        def ret_half(hh):
            Wr = od_w_in_d.rearrange("(k p) n -> p k n", p=128)
            with ExitStack() as ges:
                gal = lambda name, shape, dtype: ges.enter_context(SBT(name, shape, dtype))
                XS = gal("rxs", [128, NT, 512], BF16)
                BTOK = gal("rbtok", [128, NT, 256], BF16)
                BT = gal("rbt", [128, 2, T], BF16)
                CT = gal("rct", [128, 2, T], BF16)
                A4 = gal("ra4", [128, 1, 2, 4], F32)
                WG = gal("rwg", [128, KC, 512], BF16)
                with ExitStack() as pes:
                    ph = Phase(ctx, "retproj")
                    al = lambda name, shape, dtype: pes.enter_context(SBT(name, shape, dtype))
                    PS = [pes.enter_context(PST("rps%d" % i, [128, 512], F32)) for i in range(6)]
                    PB = [pes.enter_context(PST("rpb%d" % i, [128, 1024], BF16)) for i in range(2)]
                    psi = [0]

                    def nb():
                        psi[0] = (psi[0] + 1) % 6
                        return psi[0]
                    WS = [al("rws%d" % i, [128, KC, 128], BF16) for i in range(2)]
                    WV = al("rwv", [128, KC, 512], BF16)
                    for hs in range(4):
                        hl = RPERM[hs]
                        ph.dma("pool", WG[:, :, hs * 128:(hs + 1) * 128], Wr[:, :, 2816 + (4 * hh + hl) * 128:2816 + (4 * hh + hl + 1) * 128], writes=[("wg", hs)])
                        ph.dma("pool", WV[:, :, hs * 128:(hs + 1) * 128], Wr[:, :, 1792 + (4 * hh + hl) * 128:1792 + (4 * hh + hl + 1) * 128], writes=[("wv", hs)])
                        for d in range(2):
                            ph.dma("sp", A4[:, 0, d, hs:hs + 1], retld_d[d:d + 1, 4 * hh + hl:4 * hh + hl + 1].broadcast_to([128, 1]), writes=[("a4", d, hs)])
                    a4f = A4[:].rearrange("p a d h -> p (a d h)")
                    ph.act(lambda e: e.activation(out=a4f, in_=a4f, func=AF.Exp), reads=["a4"], writes=["a4"])
                    ph.act(lambda e: e.activation(out=a4f, in_=a4f, func=AF.Ln, scale=-1.0, bias=1.0), reads=["a4"], writes=["a4"])
                    for t in range(NT):
                        b = nb()
                        for k in range(KC):
                            ph.pe(lambda e, b=b, k=k, t=t: e.matmul(PS[b][:], HT[:, k, t * 128:(t + 1) * 128], WV[:, k, :], start=(k == 0), stop=(k == KC - 1)),
                                  reads=["HT", "wv"], writes=[("ps", b)])
                        if t % 2 == 0:
                            ph.act(lambda e, b=b, t=t: e.copy(out=XS[:, t, :], in_=PS[b][:]), reads=[("ps", b)], writes=[("XS", t)])
                        else:
                            ph.dve(lambda e, b=b, t=t: e.tensor_copy(out=XS[:, t, :], in_=PS[b][:]), reads=[("ps", b)], writes=[("XS", t)])
                    wi = 0
                    for (dst, c0, scale, nm) in ((CT, 768, 1.0, "CT"), (BT, 1280, 0.125, "BT")):
                        for c in range(2):
                            ws = WS[wi % 2]
                            ph.dma("pool", ws[:], Wr[:, :, c0 + (2 * hh + c) * 128:c0 + (2 * hh + c + 1) * 128], writes=[("rws", wi % 2)])
                            for (t0, tn) in BLKS:
                                b = nb()
                                for k in range(KC):
                                    ph.pe(lambda e, b=b, k=k, ws=ws, t0=t0, tn=tn: e.matmul(PS[b][:, :tn], ws[:, k, :], HT[:, k, t0:t0 + tn], start=(k == 0), stop=(k == KC - 1)),
                                          reads=[("rws", wi % 2), "HT"], writes=[("ps", b)])
                                ph.act(lambda e, b=b, dst=dst, c=c, t0=t0, tn=tn, scale=scale: e.activation(out=dst[:, c, t0:t0 + tn], in_=PS[b][:, :tn], func=AF.Copy, scale=scale),
                                       reads=[("ps", b)], writes=[(nm, c, "p", t0)])
                            rope(ph, dst[:, c, :], (nm, c), nb, PS, al)
                            if nm == "BT":
                                for t8 in range(0, NT, 8):
                                    n8 = min(8, NT - t8)
                                    pb = (t8 // 8 + c) % 2
                                    for tt in range(n8):
                                        t = t8 + tt
                                        ph.pe(lambda e, pb=pb, tt=tt, t=t, c=c: e.transpose(PB[pb][:, tt * 128:(tt + 1) * 128], BT[:, c, t * 128:(t + 1) * 128], IDB),
                                              reads=[("BT", c), "CB"], writes=[("pb", pb)])
                                    ph.act(lambda e, pb=pb, n8=n8, t8=t8, c=c: e.copy(out=BTOK[:, t8:t8 + n8, c * 128:(c + 1) * 128], in_=PB[pb][:, 0:n8 * 128].rearrange("p (a n) -> p a n", a=n8)),
                                           reads=[("pb", pb)], writes=[("BTOK", c, t8)])
                            wi += 1
                    ph.emit()
                if hh == 0:
                    dump("RXS", XS[:].rearrange("p t n -> p (t n)"), (128, NT * 512))
                    dump("RCT", CT[:].rearrange("p c t -> p (c t)"), (128, 2 * T))
                    dump("RBTOK", BTOK[:].rearrange("p t n -> p (t n)"), (128, NT * 256))
                    dump("RA4", A4[:].rearrange("p a d h -> p (a d h)"), (128, 8))

                def post(stage, ph, al, PS, nb, st=None, t=None, Yt=None):
                    if stage == "init":
                        pbt = st
                        st = {"pb": pbt}
                        st["gng"] = al("rgng", [128, 512], F32)
                        st["gnb"] = al("rgnb", [128, 512], F32)
                        st["sg"] = al("rsg", [128, 512], F32)
                        st["yc"] = al("ryc", [128, 512], F32)
                        st["stat"] = al("rstat", [128, 4, 4], F32)
                        st["yn"] = al("ryn", [128, 512], BF16)
                        st["stg"] = [al("rstg%d" % i, [128, 4, 128], BF16) for i in range(2)]
                        for hs in range(4):
                            c0 = (4 * hh + RPERM[hs]) * 128
                            ph.dma("sp", st["gng"][:, hs * 128:(hs + 1) * 128], retg_d[0:1, c0:c0 + 128].broadcast_to([128, 128]), writes=[("gng", hs)])
                            ph.dma("sp", st["gnb"][:, hs * 128:(hs + 1) * 128], retb_d[0:1, c0:c0 + 128].broadcast_to([128, 128]), writes=[("gnb", hs)])
                        return st
                    if stage == "fini":
                        return
                    gng, gnb, sg, yc, stat, yn = st["gng"], st["gnb"], st["sg"], st["yc"], st["stat"], st["yn"]
                    b = nb()
                    for k in range(KC):
                        ph.pe(lambda e, b=b, k=k: e.matmul(PS[b][:], HT[:, k, t * 128:(t + 1) * 128], WG[:, k, :], start=(k == 0), stop=(k == KC - 1)),
                              reads=["HT", "wg"], writes=[("ps", b)])
                    ph.act(lambda e, b=b: e.activation(out=sg[:], in_=PS[b][:], func=AF.Silu), reads=[("ps", b)], writes=["sg"])
                    y3 = Yt[:].rearrange("p (h q) -> p h q", h=4)
                    yc3 = yc[:].rearrange("p (h q) -> p h q", h=4)
                    ph.dve(lambda e: e.reduce_sum(out=stat[:, 0, :], in_=y3, axis=AX.X), reads=[("y", 0)], writes=[("stat", 0)])
                    ph.dve(lambda e: e.tensor_scalar(out=stat[:, 1, :], in0=stat[:, 0, :], scalar1=-1.0 / 128, scalar2=None, op0=ALU.mult), reads=[("stat", 0)], writes=[("stat", 1)])
                    ph.dve(lambda e: e.tensor_tensor(out=yc3, in0=y3, in1=stat[:, 1, :].unsqueeze(2).broadcast_to([128, 4, 128]), op=ALU.add), reads=[("y", 0), ("stat", 1)], writes=["yc"])
                    for hq in range(4):
                        ph.act(lambda e, hq=hq: e.activation(out=yn[:, hq * 128:(hq + 1) * 128], in_=yc[:, hq * 128:(hq + 1) * 128], func=AF.Square, accum_out=stat[:, 2, hq:hq + 1]),
                               reads=["yc"], writes=["yn", ("stat", 2, hq)])
                    ph.dve(lambda e: e.tensor_scalar(out=stat[:, 2, :], in0=stat[:, 2, :], scalar1=1.0 / 128, scalar2=EPS, op0=ALU.mult, op1=ALU.add), reads=[("stat", 2)], writes=[("stat", 2)])
                    ph.act(lambda e: e.sqrt(out=stat[:, 2, :], in_=stat[:, 2, :]), reads=[("stat", 2)], writes=[("stat", 2)])
                    ph.dve(lambda e: e.reciprocal(out=stat[:, 3, :], in_=stat[:, 2, :]), reads=[("stat", 2)], writes=[("stat", 3)])
                    ph.dve(lambda e: e.tensor_tensor(out=yc3, in0=yc3, in1=stat[:, 3, :].unsqueeze(2).broadcast_to([128, 4, 128]), op=ALU.mult), reads=["yc", ("stat", 3)], writes=["yc"])
                    ph.pool(lambda e: e.tensor_tensor(out=yc[:], in0=yc[:], in1=gng[:], op=ALU.mult), reads=["yc", "gng"], writes=["yc"])
                    ph.pool(lambda e: e.tensor_tensor(out=yc[:], in0=yc[:], in1=gnb[:], op=ALU.add), reads=["yc", "gnb"], writes=["yc"])
                    ph.dve(lambda e: e.tensor_tensor(out=yn[:], in0=yc[:], in1=sg[:], op=ALU.mult), reads=["yc", "sg"], writes=["yn"])
                    pb = st["pb"]
                    stg = st["stg"][t % 2]
                    for cl in range(4):
                        ph.pe(lambda e, cl=cl: e.transpose(pb[:, RPERM[cl] * 128:(RPERM[cl] + 1) * 128], yn[:, cl * 128:(cl + 1) * 128], IDB), reads=["yn", "CB"], writes=["pbt"])
                    ph.act(lambda e, stg=stg: e.copy(out=stg[:], in_=pb[:, 0:512].rearrange("p (a n) -> p a n", a=4)), reads=["pbt"], writes=[("stg", t % 2)])
                    ph.dma("sp", YCAT_d[4 + 4 * hh:8 + 4 * hh, :, t * 128:(t + 1) * 128].rearrange("c p t -> p c t"), stg[:], reads=[("stg", t % 2)])

                groups = [dict(chunk=hs % 2, row0=(hs // 2) * 64, nrows=64, heads=[hs], scol0=(hs % 2) * 128, sncols=128) for hs in range(4)]
                if RET_SCAN:
                    scan_phase(128, groups, XS, BTOK, BT, CT, A4, None, True, post, list(range(16)), H=4)

        def moe_gate(LG, GT):
            with ExitStack() as pes:
                ph = Phase(ctx, "gate")
                al = lambda name, shape, dtype: pes.enter_context(SBT(name, shape, dtype))
                PS = [pes.enter_context(PST("gps%d" % i, [128, 512], F32)) for i in range(2)]
                M1 = al("m1", [128, NT], F32)
                M2 = al("m2", [128, NT], F32)
                EQ = al("eq", [128, NT, 8], F32)
                L2 = al("l2", [128, NT, 8], F32)
                EXg = al("exg", [128, NT, 8], F32)
                DEN = al("den", [128, NT], F32)
                bc = lambda a: a[:].unsqueeze(2).broadcast_to([128, NT, 8])
                ph.dve(lambda e: e.reduce_max(out=M1[:], in_=LG[:], axis=AX.X), reads=["LG"], writes=["m1"])
                ph.dve(lambda e: e.tensor_tensor(out=EQ[:], in0=LG[:], in1=bc(M1), op=ALU.is_equal), reads=["LG", "m1"], writes=["eq"])
                ph.dve(lambda e: e.scalar_tensor_tensor(out=L2[:], in0=EQ[:], scalar=-1e30, in1=LG[:], op0=ALU.mult, op1=ALU.add), reads=["eq", "LG"], writes=["l2"])
                ph.dve(lambda e: e.reduce_max(out=M2[:], in_=L2[:], axis=AX.X), reads=["l2"], writes=["m2"])
                ph.dve(lambda e: e.tensor_tensor(out=EQ[:], in0=LG[:], in1=bc(M2), op=ALU.is_ge), reads=["LG", "m2"], writes=["eq"])
                ph.dve(lambda e: e.tensor_tensor(out=L2[:], in0=LG[:], in1=bc(M1), op=ALU.subtract), reads=["LG", "m1"], writes=["l2"])
                ph.act(lambda e: e.activation(out=EXg[:], in_=L2[:], func=AF.Exp), reads=["l2"], writes=["exg"])
                ph.dve(lambda e: e.tensor_tensor(out=EXg[:], in0=EXg[:], in1=EQ[:], op=ALU.mult), reads=["exg", "eq"], writes=["exg"])
                ph.dve(lambda e: e.reduce_sum(out=DEN[:], in_=EXg[:], axis=AX.X), reads=["exg"], writes=["den"])
                ph.dve(lambda e: e.reciprocal(out=DEN[:], in_=DEN[:]), reads=["den"], writes=["den"])
                ph.dve(lambda e: e.tensor_tensor(out=EXg[:], in0=EXg[:], in1=bc(DEN), op=ALU.mult), reads=["exg", "den"], writes=["exg"])
                for t4 in range(0, NT, 4):
                    n4 = min(4, NT - t4)
                    b = (t4 // 4) % 2
                    for tt in range(n4):
                        ph.pe(lambda e, b=b, tt=tt, t4=t4: e.transpose(PS[b][0:8, tt * 128:(tt + 1) * 128], EXg[:, t4 + tt, :], IDF), reads=["exg", "CF"], writes=[("ps", b)])
                    ph.act(lambda e, b=b, t4=t4, n4=n4: e.copy(out=GT[:, t4 * 128:(t4 + n4) * 128], in_=PS[b][0:8, 0:n4 * 128]), reads=[("ps", b)], writes=[("GT", t4)])
                ph.emit()

        if not SKIP_L0:
            norm_mod(0, 1)
            for m in range(NPAIRS):
                attention_pair(0, m)
            for g in range(NGROUPS):
                ssd_group(g)
        if STOP_AFTER >= 1 and not SKIP_L0:
            out_proj(0)
            norm_mod(0, 2)
            ffn(0)
        if dbg_d and "XT1" in dbg_d:
            ph = Phase(ctx, "dxt1")
            ph.dma("sp", dbg_d["XT1"].rearrange("p (k t) -> p k t", k=KC), XT[:])
            ph.emit()
        if STOP_AFTER >= 2:
            norm_mod(1, 1)
            for m in range(NPAIRS):
                attention_pair(1, m)
            for hh in range(2 if RUN_RET else 0):
                ret_half(hh)
        if STOP_AFTER >= 3:
            SEL = sb("SEL", [8, 1024], F32)
            LGT = sb("LGT", [128, NT, 8], F32)
            GTT = sb("GTT", [8, T], F32)
            ph = Phase(ctx, "ldsel")
            ph.dma("sp", SEL[:], sel_d[:, :], writes=["SEL"])
            ph.emit()
            out_proj(1)
            norm_mod(1, 2, LG=LGT)
            moe_gate(LGT, GTT)
            dump("GT", GTT[:], (8, T))
        if dbg_d and "XT2" in dbg_d:
            ph = Phase(ctx, "dxt2")
            ph.dma("sp", dbg_d["XT2"].rearrange("p (k t) -> p k t", k=KC), XT[:])
            ph.emit()
        if STOP_AFTER >= 4:
            ffn(1, GT=GTT)
            final_out()
        if dbg_d and "YC" in dbg_d:
            with ExitStack() as des:
                ph = Phase(ctx, "dumpyc")
                yb = des.enter_context(SBT("ycb", [128, T], BF16))
                yf = des.enter_context(SBT("ycf", [128, T], F32))
                for c in range(12):
                    ph.dma("sp", yb[:], YCAT_d[c], writes=["yb"])
                    ph.dve(lambda e: e.tensor_copy(out=yf[:], in_=yb[:]), reads=["yb"], writes=["yf"])
                    ph.dma("sp", dbg_d["YC"][:, c * T:(c + 1) * T], yf[:], reads=["yf"])
                ph.emit()
    return nc


def make_consts():
    c = np.zeros((128, 1024), np.float32)
    c[:, 0:128] = np.eye(128, dtype=np.float32)
    c[:, 128:256] = 1.0
    p = np.arange(128)[:, None]
    i = np.arange(128)[None, :]
    c[:, 256:384] = (p <= i)
    c[:, 384:512] = (p >= i)
    c[:, 512:640] = (p > i)
    c[:, 640:768] = (p < i)
    for f in range(128):
        if f % 64 < 32:
            c[f + 32, 768 + f] = -1.0
        else:
            c[f - 32, 768 + f] = 1.0
    return c


def kernel(**inputs):
    dbg = inputs.pop("_dbg", None)
    inp = {k: np.asarray(v) for k, v in inputs.items()}
    nc = build_program(dbg)
    cst = make_consts()
    nab = na_bias_table(inp["na_rpb"][0])
    swab = swa_bias_table()
    tpos = np.arange(L)
    inv = (10000.0 ** (-np.arange(16, dtype=np.float32) / 16)).astype(np.float32)
    ang = np.concatenate([(tpos // 64).astype(np.float32)[:, None] * inv, (tpos % 64).astype(np.float32)[:, None] * inv], axis=-1)
    fidx = np.arange(128) % 32
    rope_tab = np.stack([np.cos(ang)[:, fidx].T, np.sin(ang)[:, fidx].T], 0).astype(np.float32)
    sel = np.zeros((8, 1024), np.float32)
    for e in range(8):
        sel[e, e * 128:(e + 1) * 128] = 1.0
    in_maps = []
    for b in range(8):
        vecs = np.zeros((256, 128), np.float32)

        def put(nm, arr):
            a = np.ascontiguousarray(arr, dtype=np.float32).reshape(-1, 128)
            vecs[VR[nm]:VR[nm] + a.shape[0]] = a
        put("c", inp["c"][b])
        put("c_ctx", inp["c_ctx"])
        put("ada_b0", inp["ada_b"][0])
        put("ada_b1", inp["ada_b"][1])
        put("g_attn0", inp["norm_attn_g"][0])
        put("g_attn1", inp["norm_attn_g"][1])
        put("g_ffn0", inp["norm_ffn_g"][0])
        put("g_ffn1", inp["norm_ffn_g"][1])
        put("final_g", inp["final_g"])
        put("conv_w", inp["ssm_conv_w"][0].reshape(5, 1536))
        put("conv_b", inp["ssm_conv_b"][0])
        put("ssm_g", inp["ssm_norm_g"][0])
        in_maps.append({
            "x": np.ascontiguousarray(inp["x"][b]),
            "ctx": np.ascontiguousarray(inp["ctx"][b]),
            "vecs": vecs,
            "cst": cst,
            "ada_w": inp["ada_w"],
            "ev_w_in": inp["ev_w_in"][0], "od_w_in": inp["od_w_in"][0], "nab": nab, "swab": swab,
            "sink": inp["swa_sink"],
            "ev_w_out": inp["ev_w_out"][0], "od_w_out": inp["od_w_out"][0],
            "ffn_w1": inp["ffn_w1"][0], "ffn_w3": inp["ffn_w3"][0], "ffn_w2": inp["ffn_w2"][0],
            "sel": sel, "rope": rope_tab, "retld": inp["ret_log_decay"][0], "retg": inp["ret_gn_g"], "retb": inp["ret_gn_b"],
            "router": inp["moe_router"][0],
            "dtba": np.stack([inp["ssm_dt_bias"][0].reshape(32), inp["ssm_a_log"][0].reshape(32)], 0),
            "ssmd": inp["ssm_d"], "ssmg": inp["ssm_norm_g"],
        })
    if STOP_AFTER >= 4:
        for mp in in_maps:
            mp["moe_w1"] = inp["moe_w1"][0]
            mp["moe_w3"] = inp["moe_w3"][0]
            mp["moe_w2"] = inp["moe_w2"][0]
    res = run_bass_kernel_spmd(nc, in_maps, core_ids=list(range(8)))
    if dbg:
        return res
    out = np.stack([r["out"] for r in res.results], axis=0)
    return out
```

```python
import math
from contextlib import ExitStack
import numpy as np
import concourse.bass as bass
import concourse.mybir as mybir
from concourse.bass_utils import run_bass_kernel_spmd

F32 = mybir.dt.float32
BF16 = mybir.dt.bfloat16
AF = mybir.ActivationFunctionType
ALU = mybir.AluOpType
AX = mybir.AxisListType

ENGS = ("pe", "act", "dve", "pool", "sp")
NDMA_SEMS = 24


class Ctx:
    def __init__(self, nc, es):
        self.nc = nc
        self.eng_sem = {e: es.enter_context(nc.semaphore("sem_" + e)) for e in ENGS if e != "sp"}
        self.eng_cnt = {e: 0 for e in self.eng_sem}
        self.dma_sems = [es.enter_context(nc.semaphore("dsem%d" % i)) for i in range(NDMA_SEMS)]
        self.dma_cnt = [0] * NDMA_SEMS
        self.dma_rr = 0
        self.dma_rr_sw = 0
        self.eng_obj = {"pe": nc.tensor, "act": nc.scalar, "dve": nc.vector, "pool": nc.gpsimd, "sp": nc.sync}


class Phase:
    def __init__(self, ctx, name="ph"):
        self.ctx = ctx
        self.name = name
        self.ops = []
        self.state = {}
        self.rr = 0

    def _st(self, key):
        if isinstance(key, tuple):
            nm, sub = key[0], tuple(key[1:])
        else:
            nm, sub = key, ()
        d = self.state.setdefault(nm, {})
        return d, sub

    @staticmethod
    def _overlap(a, b):
        n = min(len(a), len(b))
        return a[:n] == b[:n]

    def op(self, eng, fn, reads=(), writes=(), dma=False, pe_acc=False):
        oid = len(self.ops)
        deps = set()
        for key in reads:
            d, sub = self._st(key)
            for s2, st in d.items():
                if self._overlap(sub, s2) and st["w"] is not None:
                    deps.add(st["w"])
            d.setdefault(sub, {"w": None, "r": []})["r"].append(oid)
        for key in writes:
            d, sub = self._st(key)
            for s2 in list(d.keys()):
                if self._overlap(sub, s2):
                    st = d[s2]
                    if st["w"] is not None:
                        deps.add(st["w"])
                    deps.update(st["r"])
                    if len(s2) > len(sub):
                        del d[s2]
            st = d.setdefault(sub, {"w": None, "r": []})
            st["w"] = oid
            st["r"] = []
        deps.discard(oid)
        o = {"eng": eng, "fn": fn, "deps": deps, "dma": dma, "pe_acc": pe_acc}
        if dma:
            c = self.ctx
            half = NDMA_SEMS // 2
            if eng == "pool":
                k = half + c.dma_rr_sw
                c.dma_rr_sw = (c.dma_rr_sw + 1) % half
            else:
                k = c.dma_rr
                c.dma_rr = (c.dma_rr + 1) % half
            prev = getattr(self, "_dma_prev", {}).get(k)
            if prev is not None:
                deps.add(prev)
            self.__dict__.setdefault("_dma_prev", {})[k] = oid
            c.dma_cnt[k] += 16
            o["dsem"] = k
            o["dval"] = c.dma_cnt[k]
        self.ops.append(o)
        return oid

    def pe(self, fn, reads=(), writes=(), acc=False):
        return self.op("pe", fn, reads, writes, pe_acc=acc)

    def act(self, fn, reads=(), writes=()):
        return self.op("act", fn, reads, writes)

    def dve(self, fn, reads=(), writes=()):
        return self.op("dve", fn, reads, writes)

    def pool(self, fn, reads=(), writes=()):
        return self.op("pool", fn, reads, writes)

    def any2(self, fn, reads=(), writes=()):
        self.rr += 1
        return self.op("dve" if self.rr % 2 else "pool", fn, reads, writes)

    def dma(self, q, out, in_, reads=(), writes=()):
        return self.op(q, lambda e: e.dma_start(out=out, in_=in_), reads, writes, dma=True)

    def emit(self):
        c = self.ctx
        ops = self.ops
        for o in ops:
            best = {}
            pd = []
            for d in o["deps"]:
                po = ops[d]
                if po["dma"]:
                    pd.append(d)
                    continue
                if po["eng"] == "pe" and o["eng"] == "pe" and not o["dma"]:
                    continue
                if d > best.get(po["eng"], -1):
                    best[po["eng"]] = d
            o["deps"] = set(pd) | set(best.values())
        needed = set()
        for o in ops:
            for d in o["deps"]:
                po = ops[d]
                if po["dma"]:
                    continue
                needed.add(d)
        last_of = {}
        for i, o in enumerate(ops):
            if not o["dma"]:
                last_of[o["eng"]] = i
        for i in last_of.values():
            needed.add(i)
        for i, o in enumerate(ops):
            if o["dma"]:
                continue
            if i in needed:
                c.eng_cnt[o["eng"]] += 1
                o["inc"] = True
            o["cnt"] = c.eng_cnt[o["eng"]] if i in needed else None
        per_eng = {e: [] for e in ENGS}
        waited = {e: {} for e in ENGS}
        for i, o in enumerate(ops):
            w = {}
            for d in o["deps"]:
                po = ops[d]
                if po["dma"]:
                    key = ("d", po["dsem"])
                    val = po["dval"]
                else:
                    if po["eng"] == "pe" and o["eng"] == "pe" and not o["dma"]:
                        continue
                    key = ("e", po["eng"])
                    val = po["cnt"]
                if val > w.get(key, 0):
                    w[key] = val
            wl = []
            for key, val in w.items():
                if waited[o["eng"]].get(key, 0) >= val:
                    continue
                waited[o["eng"]][key] = val
                wl.append((key, val))
            o["waits"] = wl
            per_eng[o["eng"]].append(o)
        fin = []
        for e, i in last_of.items():
            fin.append((("e", e), ops[i]["cnt"]))
        for k in range(NDMA_SEMS):
            if c.dma_cnt[k] > 0:
                fin.append((("d", k), c.dma_cnt[k]))

        def semof(key):
            return c.eng_sem[key[1]] if key[0] == "e" else c.dma_sems[key[1]]

        def run(engname):
            def body(eng):
                for o in per_eng[engname]:
                    for key, val in o["waits"]:
                        eng.wait_ge(semof(key), val)
                    ins = o["fn"](eng)
                    if o["dma"]:
                        ins.then_inc(c.dma_sems[o["dsem"]], 16)
                    elif o.get("inc"):
                        ins.then_inc(c.eng_sem[o["eng"]], 1)
                if engname == "sp":
                    for key, val in fin:
                        eng.wait_ge(semof(key), val)
            return body

        with c.nc.Block() as block:
            block.tensor(run("pe"))
            block.scalar(run("act"))
            block.vector(run("dve"))
            block.gpsimd(run("pool"))
            block.sync(run("sp"))
        self.ops = []
        self.state = {}
        self._dma_prev = {}


D = 1024
L = 2048
LC = 256
T = L + LC
NT = T // 128
KC = D // 128
EPS = 1e-6
BLKS = [(0, 512), (512, 512), (1024, 512), (1536, 512), (2048, 256)]

VR = {}
_r = 0
for _nm, _n in (("c", 8), ("c_ctx", 8), ("ada_b0", 48), ("ada_b1", 48), ("g_attn0", 8), ("g_attn1", 8),
                ("g_ffn0", 8), ("g_ffn1", 8), ("final_g", 8), ("conv_w", 60), ("conv_b", 12), ("ssm_g", 8)):
    VR[_nm] = _r
    _r += _n
NVR = _r

NPAIRS = 4
NGROUPS = 2
ATT_LAG = 2
RPERM = [0, 2, 1, 3]
NEXPERTS = 8
STOP_AFTER = 99
RUN_RET = True
RET_SCAN = True
DBG_NOPOST = False
DBG_NOP2 = False
DBG_SKIP = set()
SKIP_L0 = False


def _na_tiles():
    out = []
    for t in range(16):
        qr = np.arange(t * 128, (t + 1) * 128) // 64
        r0 = np.clip(qr - 4, 0, 24)
        out.append(list(range(int(r0.min()) // 2, (int(r0.max()) + 7) // 2 + 1)))
    return out


NA_KT = _na_tiles()


def na_bias_table(rpb):
    out = np.full((8, 16, 128, 640), -30000.0, np.float32)
    for t in range(16):
        qpos = np.arange(t * 128, (t + 1) * 128)
        qr, qc = qpos // 64, qpos % 64
        r0 = np.clip(qr - 4, 0, 24)
        c0 = np.clip(qc - 8, 0, 48)
        for j, kt in enumerate(NA_KT[t]):
            kpos = np.arange(kt * 128, (kt + 1) * 128)
            kr, kc = kpos // 64, kpos % 64
            ok = ((kr[:, None] >= r0[None, :]) & (kr[:, None] < r0[None, :] + 8)
                  & (kc[:, None] >= c0[None, :]) & (kc[:, None] < c0[None, :] + 16))
            dr = np.clip(kr[:, None] - qr[None, :] + 7, 0, 14)
            dc = np.clip(kc[:, None] - qc[None, :] + 15, 0, 30)
            vals = rpb[:, dr, dc]
            out[:, t, :, j * 128:(j + 1) * 128] = np.where(ok[None], vals, np.float32(-30000.0))
    return out


def swa_bias_table():
    out = np.full((16, 128, 384), -30000.0, np.float32)
    for t in range(16):
        kts = [kt for kt in (t - 1, t, t + 1) if 0 <= kt < 16]
        qpos = np.arange(t * 128, (t + 1) * 128)
        for j, kt in enumerate(kts):
            kpos = np.arange(kt * 128, (kt + 1) * 128)
            ok = np.abs(kpos[:, None] - qpos[None, :]) <= 128
            out[t, :, j * 128:(j + 1) * 128] = np.where(ok, np.float32(0.0), np.float32(-30000.0))
    return out


def build_program(dbg=None):
    nc = bass.Bass("TRN2", target_bir_lowering=False)
    _uid = [0]

    def SBT(name, shape, dtype):
        _uid[0] += 1
        return nc.sbuf_tensor("%s_%d" % (name, _uid[0]), shape, dtype)

    def PST(name, shape, dtype):
        _uid[0] += 1
        return nc.psum_tensor("%s_%d" % (name, _uid[0]), shape, dtype)
    dt = nc.dram_tensor
    x_d = dt("x", [L, D], F32, kind="ExternalInput").ap()
    ctx_d = dt("ctx", [LC, D], F32, kind="ExternalInput").ap()
    vecs_d = dt("vecs", [256, 128], F32, kind="ExternalInput").ap()
    cst_d = dt("cst", [128, 1024], F32, kind="ExternalInput").ap()
    ada_w_d = dt("ada_w", [2, D, 6 * D], F32, kind="ExternalInput").ap()
    out_d = dt("out", [L, D], F32, kind="ExternalOutput").ap()
    ev_w_in_d = dt("ev_w_in", [D, 4128], F32, kind="ExternalInput").ap()
    od_w_in_d = dt("od_w_in", [D, 3840], F32, kind="ExternalInput").ap()
    nab_d = dt("nab", [8, 16, 128, 640], F32, kind="ExternalInput").ap()
    swab_d = dt("swab", [16, 128, 384], F32, kind="ExternalInput").ap()
    sink_d = dt("sink", [1, 8], F32, kind="ExternalInput").ap()
    dtba_d = dt("dtba", [2, 32], F32, kind="ExternalInput").ap()
    ev_w_out_d = dt("ev_w_out", [1536, D], F32, kind="ExternalInput").ap()
    od_w_out_d = dt("od_w_out", [1536, D], F32, kind="ExternalInput").ap()
    ffn_w1_d = dt("ffn_w1", [D, 2816], F32, kind="ExternalInput").ap()
    ffn_w3_d = dt("ffn_w3", [D, 2816], F32, kind="ExternalInput").ap()
    ffn_w2_d = dt("ffn_w2", [2816, D], F32, kind="ExternalInput").ap()
    if STOP_AFTER >= 4:
        moe_w1_t = dt("moe_w1", [8, D, 3584], F32, kind="ExternalInput").ap()
        moe_w3_t = dt("moe_w3", [8, D, 3584], F32, kind="ExternalInput").ap()
        moe_w2_t = dt("moe_w2", [8, 3584, D], F32, kind="ExternalInput").ap()
        moe_w1_d = [moe_w1_t[e] for e in range(8)]
        moe_w3_d = [moe_w3_t[e] for e in range(8)]
        moe_w2_d = [moe_w2_t[e] for e in range(8)]
    sel_d = dt("sel", [8, 1024], F32, kind="ExternalInput").ap()
    rope_d = dt("rope", [2, 128, L], F32, kind="ExternalInput").ap()
    retld_d = dt("retld", [2, 8], F32, kind="ExternalInput").ap()
    retg_d = dt("retg", [1, 1024], F32, kind="ExternalInput").ap()
    retb_d = dt("retb", [1, 1024], F32, kind="ExternalInput").ap()
    router_d = dt("router", [D, 8], F32, kind="ExternalInput").ap()
    ssmd_d = dt("ssmd", [1, 16], F32, kind="ExternalInput").ap()
    ssmg_d = dt("ssmg", [1, 1024], F32, kind="ExternalInput").ap()
    dbg_d = None
    if dbg:
        dbg_d = {k: dt("dbg_" + k, list(shp), F32, kind="ExternalOutput").ap() for k, shp in dbg.items()}

    with ExitStack() as es:
        ctx = Ctx(nc, es)
        sb = lambda name, shape, dtype: es.enter_context(SBT(name, shape, dtype))
        XT = sb("XT", [128, KC, T], F32)
        HT = sb("HT", [128, KC, T], BF16)
        VT = sb("VT", [128, 256], F32)
        CF = sb("CF", [128, 1024], F32)
        CB = sb("CB", [128, 1024], BF16)
        MOD = sb("MOD", [128, 2, 2, 48], F32)
        AB = sb("AB", [128, 2, 2, 6, 8], F32)
        IDF = CF[:, 0:128]
        ONESF = CF[:, 128:256]
        IDB = CB[:, 0:128]
        ONESB = CB[:, 128:256]

        with ExitStack() as ps_es:
            ph = Phase(ctx, "p0")
            PS = [ps_es.enter_context(PST("ps%d" % i, [128, 512], F32)) for i in range(8)]
            XIN = [ps_es.enter_context(SBT("xin%d" % i, [128, D], F32)) for i in range(3)]
            VIN = ps_es.enter_context(SBT("vin", [128, 2, 128], F32))
            SC = ps_es.enter_context(SBT("sc", [128, KC, 2], BF16))
            AW = [ps_es.enter_context(SBT("aw%d" % i, [128, KC, 768], BF16)) for i in range(2)]
            ph.dma("sp", CF[:], cst_d[:, :], writes=["CF"])
            ph.dma("pool", CB[:], cst_d[:, :], writes=["CB"])
            ph.dma("sp", VIN[:], vecs_d.rearrange("(a p) n -> p a n", p=128), writes=["VIN"])
            for a in range(2):
                ph.pe(lambda e, a=a: e.transpose(PS[0][:, a * 128:(a + 1) * 128], VIN[:, a, :], IDF),
                      reads=["CF", "VIN"], writes=[("ps", 0)])
            ph.dve(lambda e: e.tensor_copy(out=VT[:], in_=PS[0][:, 0:256]), reads=[("ps", 0)], writes=["VT"])
            pi = 1
            for t in range(NT):
                xin = XIN[t % 3]
                src = x_d[t * 128:(t + 1) * 128, :] if t < 16 else ctx_d[(t - 16) * 128:(t - 15) * 128, :]
                ph.dma("sp", xin[:], src, writes=[("xin", t % 3)])
                for half in range(2):
                    b = 1 + (pi % 7)
                    pi += 1
                    for kk in range(4):
                        k = half * 4 + kk
                        ph.pe(lambda e, b=b, kk=kk, k=k, xin=xin: e.transpose(
                            PS[b][:, kk * 128:(kk + 1) * 128], xin[:, k * 128:(k + 1) * 128], IDF),
                            reads=["CF", ("xin", t % 3)], writes=[("ps", b)])
                    dst = XT[:, half * 4:half * 4 + 4, t * 128:(t + 1) * 128]
                    srcp = PS[b][:].rearrange("p (a n) -> p a n", a=4)
                    if (t + half) % 2 == 0:
                        ph.dve(lambda e, dst=dst, srcp=srcp: e.tensor_copy(out=dst, in_=srcp),
                               reads=[("ps", b)], writes=[("XT", t)])
                    else:
                        ph.act(lambda e, dst=dst, srcp=srcp: e.copy(out=dst, in_=srcp),
                               reads=[("ps", b)], writes=[("XT", t)])
            for j, nm in enumerate(("c", "c_ctx")):
                ph.act(lambda e, j=j, nm=nm: e.activation(out=SC[:, :, j], in_=VT[:, VR[nm]:VR[nm] + 8], func=AF.Silu),
                       reads=["VT"], writes=["SC"])
            si = 0
            for l in range(2):
                for s in range(8):
                    aw = AW[si % 2]
                    ph.dma("pool", aw[:], ada_w_d[l, :, s * 768:(s + 1) * 768].rearrange("(k p) n -> p k n", p=128),
                           writes=[("aw", si % 2)])
                    for c6 in range(6):
                        cc = s * 6 + c6
                        for k in range(KC):
                            ph.pe(lambda e, l=l, cc=cc, k=k, c6=c6, aw=aw: e.matmul(
                                PS[0][:, l * 96 + cc * 2:l * 96 + cc * 2 + 2], aw[:, k, c6 * 128:(c6 + 1) * 128],
                                SC[:, k, :], start=(k == 0), stop=(k == KC - 1)),
                                reads=[("aw", si % 2), "SC"], writes=[("ps", 0)])
                    si += 1
            for l in range(2):
                for j in range(2):
                    r0 = VR["ada_b%d" % l]
                    ph.dve(lambda e, l=l, j=j, r0=r0: e.tensor_tensor(
                        out=MOD[:, l, j, :], in0=PS[0][:, l * 96:(l + 1) * 96].rearrange("p (c j) -> p c j", j=2)[:, :, j],
                        in1=VT[:, r0:r0 + 48], op=ALU.add), reads=[("ps", 0), "VT"], writes=["MOD"])
                    for (ai, sci, gname) in ((0, 1, "g_attn%d" % l), (3, 4, "g_ffn%d" % l)):
                        g0 = VR[gname]
                        ph.dve(lambda e, l=l, j=j, ai=ai, sci=sci, g0=g0: e.scalar_tensor_tensor(
                            out=AB[:, l, j, ai, :], in0=MOD[:, l, j, sci * 8:(sci + 1) * 8], scalar=1.0,
                            in1=VT[:, g0:g0 + 8], op0=ALU.add, op1=ALU.mult), reads=["MOD", "VT"], writes=["AB"])
                    for (bi, shi) in ((1, 0), (2, 2), (4, 3), (5, 5)):
                        ph.dve(lambda e, l=l, j=j, bi=bi, shi=shi: e.tensor_copy(
                            out=AB[:, l, j, bi, :], in_=MOD[:, l, j, shi * 8:(shi + 1) * 8]), reads=["MOD"], writes=["AB"])
            ph.emit()

        def norm_mod(l, which, LG=None):
            ai, bi = (0, 1) if which == 1 else (3, 4)
            with ExitStack() as pes:
                ph = Phase(ctx, "nm")
                PS = [pes.enter_context(PST("nps%d" % i, [128, 512], F32)) for i in range(4)]
                SQ = [pes.enter_context(SBT("sq%d" % i, [128, 512], BF16)) for i in range(3)]
                RS = [pes.enter_context(SBT("rs%d" % i, [128, 512], F32)) for i in range(2)]
                TMP = [pes.enter_context(SBT("tmp%d" % i, [128, 512], F32)) for i in range(3)]
                if LG is not None:
                    PSR = [pes.enter_context(PST("npr%d" % i, [128, 512], F32)) for i in range(2)]
                    H2F = pes.enter_context(SBT("h2f", [128, KC, 512], F32))
                    RWF = pes.enter_context(SBT("rwf", [128, KC, 8], F32))
                    ph.dma("sp", RWF[:], router_d.rearrange("(k p) n -> p k n", p=128), writes=["rwf"])
                cnt = {"qi": 0, "ti": 0}

                def nm_a(bi_, t0, tn):
                    pb = bi_ % 4
                    for k in range(KC):
                        qi = cnt["qi"]
                        sq = SQ[qi % 3]
                        ph.act(lambda e, sq=sq, k=k, t0=t0, tn=tn: e.activation(out=sq[:, :tn], in_=XT[:, k, t0:t0 + tn], func=AF.Square),
                               reads=[], writes=[("sq", qi % 3)])
                        ph.pe(lambda e, sq=sq, k=k, pb=pb, tn=tn: e.matmul(PS[pb][:, :tn], ONESB, sq[:, :tn], start=(k == 0), stop=(k == KC - 1)),
                              reads=[("sq", qi % 3)], writes=[("nps", pb)])
                        cnt["qi"] += 1
                    rs = RS[bi_ % 2]
                    ph.dve(lambda e, rs=rs, pb=pb, tn=tn: e.tensor_scalar(out=rs[:, :tn], in0=PS[pb][:, :tn], scalar1=1.0 / D, scalar2=EPS,
                                                                          op0=ALU.mult, op1=ALU.add), reads=[("nps", pb)], writes=[("rs", bi_ % 2)])
                    ph.act(lambda e, rs=rs, tn=tn: e.sqrt(out=rs[:, :tn], in_=rs[:, :tn]),
                           reads=[("rs", bi_ % 2)], writes=[("rs", bi_ % 2)])
                    ph.dve(lambda e, rs=rs, tn=tn: e.reciprocal(out=rs[:, :tn], in_=rs[:, :tn]),
                           reads=[("rs", bi_ % 2)], writes=[("rs", bi_ % 2)])

                def nm_b(bi_, t0, tn):
                    j = 0 if t0 < L else 1
                    rs = RS[bi_ % 2]
                    for k in range(KC):
                        ti = cnt["ti"]
                        tmp = TMP[ti % 3]
                        ph.any2(lambda e, tmp=tmp, k=k, t0=t0, tn=tn, rs=rs: e.tensor_tensor(out=tmp[:, :tn], in0=XT[:, k, t0:t0 + tn], in1=rs[:, :tn], op=ALU.mult),
                                reads=[("rs", bi_ % 2)], writes=[("tmp", ti % 3)])
                        ph.act(lambda e, tmp=tmp, k=k, t0=t0, tn=tn, j=j: e.activation(out=HT[:, k, t0:t0 + tn], in_=tmp[:, :tn], func=AF.Identity,
                                                                                      scale=AB[:, l, j, ai, k:k + 1], bias=AB[:, l, j, bi, k:k + 1]),
                               reads=[("tmp", ti % 3)], writes=[("HT", k, bi_)])
                        if LG is not None:
                            ph.act(lambda e, tmp=tmp, k=k, tn=tn, j=j: e.activation(out=H2F[:, k, :tn], in_=tmp[:, :tn], func=AF.Identity,
                                                                                scale=AB[:, l, j, ai, k:k + 1], bias=AB[:, l, j, bi, k:k + 1]),
                                   reads=[("tmp", ti % 3)], writes=[("h2f", k)])
                        cnt["ti"] += 1
                    if LG is not None:
                        pr = bi_ % 2
                        for tt in range(tn // 128):
                            for k in range(KC):
                                ph.pe(lambda e, pr=pr, tt=tt, k=k: e.matmul(PSR[pr][:, tt * 8:(tt + 1) * 8], H2F[:, k, tt * 128:(tt + 1) * 128], RWF[:, k, :], start=(k == 0), stop=(k == KC - 1)),
                                      reads=["h2f", "rwf"], writes=[("npr", pr)])
                        ntl = tn // 128
                        ph.dve(lambda e, pr=pr, t0=t0, ntl=ntl: e.tensor_copy(out=LG[:, t0 // 128:t0 // 128 + ntl, :], in_=PSR[pr][:, 0:ntl * 8].rearrange("p (a n) -> p a n", a=ntl)),
                               reads=[("npr", pr)], writes=[("LG", t0)])

                for i in range(len(BLKS) + 1):
                    if i < len(BLKS):
                        nm_a(i, *BLKS[i])
                    if i >= 1:
                        nm_b(i - 1, *BLKS[i - 1])
                ph.emit()

        YCAT_d = nc.dram_tensor("ycat_scr", [12, 128, T], BF16, kind="Internal").ap()

        def dump(name, src_ap, shape2, reads=()):
            if not dbg_d or name not in dbg_d:
                return
            with ExitStack() as des:
                ph = Phase(ctx, "dump")
                f = des.enter_context(SBT("dbgf_" + name, list(shape2), F32))
                ph.dve(lambda e: e.tensor_copy(out=f[:], in_=src_ap), writes=["f"])
                ph.dma("sp", dbg_d[name], f[:], reads=["f"])
                ph.emit()

        def attention_pair(l, m):
            W_d = ev_w_in_d if l == 0 else od_w_in_d
            with ExitStack() as pes:
                ph = Phase(ctx, "att")
                al = lambda name, shape, dtype: pes.enter_context(SBT(name, shape, dtype))
                PS = [pes.enter_context(PST("aps%d" % i, [128, 512], F32)) for i in range(8)]
                psi = [0]

                def nb():
                    psi[0] = (psi[0] + 1) % 8
                    return psi[0]
                WS = al("ws", [128, KC, 384], BF16)
                QT = al("qt", [128, T], BF16)
                KT = al("kt", [128, T], BF16)
                VK = al("vk", [128, NT, 128], BF16)
                OT = al("ot", [128, T], BF16)
                BI = [al("bi%d" % i, [128, 640], F32) for i in range(3)]
                SS = [al("ss%d" % i, [128, 640], F32) for i in range(2)]
                PT = [al("pt%d" % i, [128, 896], BF16) for i in range(4)]
                RD = [al("rd%d" % i, [128, 128], F32) for i in range(2)]
                Wr = W_d.rearrange("(k p) n -> p k n", p=128)
                if l == 0:
                    cols = [(m * 128, 128), (512 + m * 128, 128), (1024 + m * 128, 128)]
                else:
                    kv = m // 2
                    cols = [(m * 128, 128), (512 + kv * 64, 64), (512 + kv * 64, 64), (640 + kv * 64, 64), (640 + kv * 64, 64)]
                off = 0
                for (c0, cn) in cols:
                    ph.dma("pool", WS[:, :, off:off + cn], Wr[:, :, c0:c0 + cn], writes=[("ws", off)])
                    off += cn
                wsr = [("ws", o) for o in (0, 64, 128, 192, 256, 320)]
                nq = T if l == 0 else L
                for (dst, wo, lim, nm) in ((QT, 0, nq, "qt"), (KT, 128, T, "kt")):
                    for (t0, tn) in BLKS:
                        if t0 >= lim:
                            continue
                        b = nb()
                        for k in range(KC):
                            ph.pe(lambda e, b=b, k=k, wo=wo, t0=t0, tn=tn: e.matmul(PS[b][:, :tn], WS[:, k, wo:wo + 128], HT[:, k, t0:t0 + tn],
                                                                                 start=(k == 0), stop=(k == KC - 1)),
                                  reads=wsr + ["HT"], writes=[("ps", b)])
                        ph.act(lambda e, b=b, dst=dst, t0=t0, tn=tn: e.copy(out=dst[:, t0:t0 + tn], in_=PS[b][:, :tn]),
                               reads=[("ps", b)], writes=[(nm, t0)])
                for t4 in range(0, NT, 4):
                    b = nb()
                    n4 = min(4, NT - t4)
                    for tt in range(n4):
                        t = t4 + tt
                        for k in range(KC):
                            ph.pe(lambda e, b=b, k=k, t=t, tt=tt: e.matmul(PS[b][:, tt * 128:(tt + 1) * 128], HT[:, k, t * 128:(t + 1) * 128], WS[:, k, 256:384],
                                                                         start=(k == 0), stop=(k == KC - 1)),
                                  reads=wsr + ["HT"], writes=[("ps", b)])
                    ph.dve(lambda e, b=b, t4=t4, n4=n4: e.tensor_copy(out=VK[:, t4:t4 + n4, :], in_=PS[b][:, :n4 * 128].rearrange("p (a n) -> p a n", a=n4)),
                           reads=[("ps", b)], writes=[("vk", t4)])
                if l == 1:
                    rope(ph, QT, "qt", nb, PS, al)
                    rope(ph, KT, "kt", nb, PS, al)
                    ES = al("es", [128, 8], F32)
                    ph.dma("sp", ES[:], sink_d[0:1, :].broadcast_to([128, 8]), writes=["es"])
                    ph.act(lambda e: e.activation(out=ES[:], in_=ES[:], func=AF.Exp), reads=["es"], writes=["es"])
                qtiles = list(range(NT)) if l == 0 else list(range(16))
                iters = [(e_, t) for e_ in range(2) for t in qtiles]
                info = {}

                def stage_a(it, e_, t):
                    r0 = 64 * e_
                    h = 2 * m + e_
                    if t >= 16:
                        kts, nbias = [16, 17], 0
                    elif l == 0:
                        kts, nbias = NA_KT[t] + [16, 17], len(NA_KT[t])
                    else:
                        kts = [kt for kt in (t - 1, t, t + 1) if 0 <= kt < 16]
                        nbias = len(kts)
                        kts = kts + [16, 17]
                    nk = len(kts)
                    info[it] = (kts, nk)
                    bi = BI[it % 3]
                    ss = SS[it % 2]
                    pt = PT[it % 4]
                    if nbias:
                        src = nab_d[h, t, :, 0:nbias * 128] if l == 0 else swab_d[t, :, 0:nbias * 128]
                        ph.dma("sp", bi[:, 0:nbias * 128], src, writes=[("bi", it % 3)])
                    banks = [nb(), nb()]
                    for j, kt in enumerate(kts):
                        b = banks[j // 4]
                        ph.pe(lambda e, b=b, j=j, kt=kt, t=t, r0=r0: e.matmul(PS[b][:, (j % 4) * 128:(j % 4 + 1) * 128], KT[r0:r0 + 64, kt * 128:(kt + 1) * 128],
                                                                          QT[r0:r0 + 64, t * 128:(t + 1) * 128], start=True, stop=True),
                              reads=["kt", "qt"], writes=[("ps", b)])
                    for bk in range(2):
                        j0, j1 = bk * 4, min(nk, bk * 4 + 4)
                        if j0 >= j1:
                            continue
                        b = banks[bk]
                        jb = min(j1, max(j0, nbias))
                        if jb > j0:
                            ph.dve(lambda e, b=b, j0=j0, jb=jb, ss=ss, bi=bi: e.scalar_tensor_tensor(
                                out=ss[:, j0 * 128:jb * 128], in0=PS[b][:, (j0 % 4) * 128:(j0 % 4) * 128 + (jb - j0) * 128], scalar=0.125,
                                in1=bi[:, j0 * 128:jb * 128], op0=ALU.mult, op1=ALU.add),
                                reads=[("ps", b), ("bi", it % 3)], writes=[("ss", it % 2, bk)])
                            ph.act(lambda e, j0=j0, jb=jb, ss=ss, pt=pt: e.activation(out=pt[:, j0 * 128:jb * 128], in_=ss[:, j0 * 128:jb * 128], func=AF.Exp),
                                   reads=[("ss", it % 2, bk)], writes=[("pt", it % 4)])
                        if j1 > jb:
                            ph.act(lambda e, b=b, jb=jb, j1=j1, pt=pt: e.activation(out=pt[:, jb * 128:j1 * 128], in_=PS[b][:, (jb % 4) * 128:(jb % 4) * 128 + (j1 - jb) * 128],
                                                                                 func=AF.Exp, scale=0.125),
                                   reads=[("ps", b)], writes=[("pt", it % 4)])

                def stage_b(it, e_, t):
                    r0 = 64 * e_
                    h = 2 * m + e_
                    kts, nk = info[it]
                    pt = PT[it % 4]
                    rd = RD[it % 2]
                    po = nb()
                    for j, kt in enumerate(kts):
                        ph.pe(lambda e, po=po, j=j, kt=kt, pt=pt, nk=nk: e.matmul(PS[po][:, 0:128], VK[:, kt, :], pt[:, j * 128:(j + 1) * 128],
                                                                              start=(j == 0), stop=(j == nk - 1)),
                              reads=["vk", ("pt", it % 4)], writes=[("ps", po)])
                    for j, kt in enumerate(kts):
                        ph.pe(lambda e, po=po, j=j, pt=pt, nk=nk: e.matmul(PS[po][:, 128:256], ONESB, pt[:, j * 128:(j + 1) * 128],
                                                                       start=(j == 0), stop=(j == nk - 1)),
                              reads=[("pt", it % 4)], writes=[("ps", po)])
                    if l == 1:
                        ph.dve(lambda e, po=po, rd=rd, r0=r0, h=h: e.tensor_scalar(out=rd[r0:r0 + 64, :], in0=PS[po][r0:r0 + 64, 128:256], scalar1=ES[r0:r0 + 64, h:h + 1],
                                                                                scalar2=None, op0=ALU.add), reads=[("ps", po), "es"], writes=[("rd", it % 2)])
                        ph.dve(lambda e, rd=rd, r0=r0: e.reciprocal(out=rd[r0:r0 + 64, :], in_=rd[r0:r0 + 64, :]), reads=[("rd", it % 2)], writes=[("rd", it % 2)])
                    else:
                        ph.dve(lambda e, po=po, rd=rd, r0=r0: e.reciprocal(out=rd[r0:r0 + 64, :], in_=PS[po][r0:r0 + 64, 128:256]),
                               reads=[("ps", po)], writes=[("rd", it % 2)])
                    ph.dve(lambda e, po=po, rd=rd, r0=r0, t=t: e.tensor_tensor(out=OT[r0:r0 + 64, t * 128:(t + 1) * 128], in0=PS[po][r0:r0 + 64, 0:128],
                                                                           in1=rd[r0:r0 + 64, :], op=ALU.mult),
                           reads=[("ps", po), ("rd", it % 2)], writes=[("ot", e_, t)])

                LAG = ATT_LAG
                for i in range(len(iters) + LAG):
                    if i < len(iters):
                        stage_a(i, *iters[i])
                    if i >= LAG:
                        stage_b(i - LAG, *iters[i - LAG])
                ph.dma("sp", YCAT_d[m, :, 0:nq], OT[:, 0:nq], reads=["ot"])
                ph.emit()
                if l == 0 and m == 0:
                    dump("OT0", OT[:], (128, T))

        MF = CF[:, 256:384]
        MB = CF[:, 384:512]
        SFm = CF[:, 512:640]
        SBm = CF[:, 640:768]
        SST_d = nc.dram_tensor("sst_scr", [2, NT, 128, 512], BF16, kind="Internal").ap()

        def scan_phase(P, groups, XS, BTOK, BT, CT, A4, DT4, const_decay, post_fn, out_tiles, pre_fn=None, H=8):
            HP = H * P
            nyb = HP // 512
            NTA = 1 if const_decay else NT
            ti = (lambda t: 0) if const_decay else (lambda t: t)
            oes = ExitStack()
            ESC = oes.enter_context(SBT("esc", [128, NTA, 2, H], F32))
            with ExitStack() as pes:
                ph = Phase(ctx, "scan")
                al = lambda name, shape, dtype: pes.enter_context(SBT(name, shape, dtype))
                PS = [pes.enter_context(PST("sps%d" % i, [128, 512], F32)) for i in range(8)]
                psi = [0]

                def nb():
                    psi[0] = (psi[0] + 1) % 8
                    return psi[0]
                if pre_fn is not None:
                    pre_fn(ph, al, PS, nb)
                CUMS = al("cums", [128, NTA, 3, 2 * H], F32)
                EW = al("ew", [128, NTA, 2, H], F32)
                ETOT = al("etot", [128, NTA, 2, H], F32)
                S = [al("st%d" % d, [128, 512], F32) for d in range(2)]
                STMP = al("stmp", [128, 512], F32)
                SBF = [al("sbf%d" % i, [128, 512], BF16) for i in range(3)]
                XW = [al("xw%d" % i, [128, HP], BF16) for i in range(2)]
                for t in range(NTA):
                    b = nb()
                    for ci, lm in enumerate((MF, MB, ONESF)):
                        ph.pe(lambda e, b=b, ci=ci, lm=lm, t=t: e.matmul(PS[b][:, ci * 2 * H:(ci + 1) * 2 * H], lm, A4[:, t].rearrange("p d h -> p (d h)"),
                                                                         start=True, stop=True), reads=["A4", "CF"], writes=[("ps", b)])
                    ph.act(lambda e, b=b, t=t: e.copy(out=CUMS[:, t].rearrange("p c n -> p (c n)"), in_=PS[b][:, 0:6 * H]), reads=[("ps", b)], writes=[("cums", t)])
                ph.act(lambda e: e.activation(out=ETOT[:].rearrange("p t d h -> p t (d h)"), in_=CUMS[:, :, 2, :], func=AF.Exp), reads=["cums"], writes=["etot"])
                for d in range(2):
                    ph.act(lambda e, d=d: e.activation(out=ESC[:, :, d, :], in_=CUMS[:, :, d, d * H:(d + 1) * H], func=AF.Exp), reads=["cums"], writes=[("esc", d)])
                    ph.dve(lambda e, d=d: e.tensor_tensor(out=EW[:, :, d, :], in0=CUMS[:, :, 2, d * H:(d + 1) * H], in1=CUMS[:, :, d, d * H:(d + 1) * H], op=ALU.subtract),
                           reads=["cums"], writes=[("ew", d)])
                    ph.act(lambda e, d=d: e.activation(out=EW[:, :, d, :], in_=EW[:, :, d, :], func=AF.Exp), reads=[("ew", d)], writes=[("ew", d)])
                    if DT4 is not None:
                        ph.dve(lambda e, d=d: e.tensor_tensor(out=EW[:, :, d, :], in0=EW[:, :, d, :], in1=DT4[:, :, d, :], op=ALU.mult),
                               reads=[("ew", d), "DT4"], writes=[("ew", d)])
                order = {0: [16, 17] + list(range(16)), 1: [17, 16] + list(range(15, -1, -1))}
                si = 0
                for d in range(2):
                    ph.dve(lambda e, d=d: e.memset(S[d][:], 0.0), writes=[("st", d)])
                for step in range(NT):
                    for d in range(2):
                        t = order[d][step]
                        sbf = SBF[si % 3]
                        xw = XW[si % 2]
                        ph.act(lambda e, sbf=sbf, d=d: e.copy(out=sbf[:], in_=S[d][:]), reads=[("st", d)], writes=[("sbf", si % 3)])
                        ph.dma("sp", SST_d[d, t], sbf[:], reads=[("sbf", si % 3)], writes=[("sst", d, t)])
                        if step < NT - 1:
                            ph.any2(lambda e, xw=xw, t=t, d=d: e.tensor_tensor(out=xw[:].rearrange("p (h q) -> p h q", h=H), in0=XS[:, t].rearrange("p (h q) -> p h q", h=H),
                                                                          in1=EW[:, ti(t), d, :].unsqueeze(2).broadcast_to([128, H, P]), op=ALU.mult),
                                    reads=["XS", ("ew", d)], writes=[("xw", si % 2)])
                            bks = [nb() for _ in range(nyb)]
                            for gi, g in enumerate(groups):
                                pc0 = g["heads"][0] * P
                                pcn = len(g["heads"]) * P
                                ph.pe(lambda e, g=g, pc0=pc0, pcn=pcn, xw=xw, t=t, bks=bks: e.matmul(
                                    PS[bks[pc0 // 512]][:, pc0 % 512:pc0 % 512 + pcn], BTOK[:, t, g["chunk"] * 128:(g["chunk"] + 1) * 128], xw[:, pc0:pc0 + pcn], start=True, stop=True),
                                    reads=["BTOK", ("xw", si % 2)], writes=[("ps", bks[pc0 // 512])])
                            for gi, g in enumerate(groups):
                                r0, nr, nh = g["row0"], g["nrows"], len(g["heads"])
                                h0 = g["heads"][0]
                                pc0 = h0 * P
                                pcn = nh * P
                                sc0 = g["scol0"]
                                ph.dve(lambda e, r0=r0, nr=nr, nh=nh, h0=h0, sc0=sc0, pcn=pcn, d=d, t=t: e.tensor_tensor(
                                    out=STMP[r0:r0 + nr, sc0:sc0 + pcn].rearrange("p (h q) -> p h q", h=nh), in0=S[d][r0:r0 + nr, sc0:sc0 + pcn].rearrange("p (h q) -> p h q", h=nh),
                                    in1=ETOT[r0:r0 + nr, ti(t), d, h0:h0 + nh].unsqueeze(2).broadcast_to([nr, nh, P]), op=ALU.mult),
                                    reads=[("st", d), "etot", ("sbf", si % 3)], writes=[("stmp", gi)])
                                ph.dve(lambda e, r0=r0, nr=nr, sc0=sc0, pc0=pc0, pcn=pcn, d=d, bks=bks: e.tensor_tensor(
                                    out=S[d][r0:r0 + nr, sc0:sc0 + pcn], in0=PS[bks[pc0 // 512]][r0:r0 + nr, pc0 % 512:pc0 % 512 + pcn], in1=STMP[r0:r0 + nr, sc0:sc0 + pcn], op=ALU.add),
                                    reads=[("stmp", gi), ("ps", bks[pc0 // 512])], writes=[("st", d, gi)])
                        si += 1
                ph.emit()
            with ExitStack() as pes:
                ph = Phase(ctx, "scan2")
                al = lambda name, shape, dtype: pes.enter_context(SBT(name, shape, dtype))
                PS = [pes.enter_context(PST("tps%d" % i, [128, 512], F32)) for i in range(7)]
                PBT = pes.enter_context(PST("tpb", [128, 1024], BF16))
                psi = [0]

                def nb():
                    psi[0] = (psi[0] + 1) % 7
                    return psi[0]
                ng = len(groups)
                GM = [[al("gm%d_%d" % (tp, d), [128, ng, 128], F32) for d in range(2)] for tp in range(2)]
                RH1 = al("rh", [128, H, 128], F32)
                RHS = [RH1, RH1]
                EXPD = [al("expd%d" % d, [128, H, 128], F32) for d in range(2)]
                XD = [al("xd%d" % i, [128, HP], BF16) for i in range(4)] if DT4 is not None else None
                MP = [al("mp%d" % i, [128, H, 128], BF16) for i in range(4)]
                SW = max(g["scol0"] + len(g["heads"]) * P for g in groups)
                SIN = [al("sin%d" % i, [128, SW], BF16) for i in range(4)]
                YT1 = al("yt", [128, HP], F32)
                YT = [YT1, YT1]
                Y = [al("y%d" % i, [128, HP], F32) for i in range(2)]
                post_state = post_fn("init", ph, al, PS, nb, PBT)
                if dbg_d and "SST" in dbg_d:
                    sf = al("dbgsst", [128, 512], F32)
                    ph.dma("sp", SIN[0][:], SST_d[0, 17], writes=[("sin", 0)])
                    ph.dve(lambda e: e.tensor_copy(out=sf[:], in_=SIN[0][:]), reads=[("sin", 0)], writes=["sf"])
                    ph.dma("sp", dbg_d["SST"], sf[:], reads=["sf"])
                masks = (MF, MB)
                u1 = (SFm, SBm)

                def build_expd(t, d):
                    RH = RHS[d]
                    ph.any2(lambda e, t=t, d=d, RH=RH: e.tensor_tensor(out=RH[:], in0=masks[d].unsqueeze(1).broadcast_to([128, H, 128]),
                                                                       in1=A4[:, t, d, :].unsqueeze(2).broadcast_to([128, H, 128]), op=ALU.mult),
                            reads=["A4", "CF"], writes=["rh"])
                    for hh in range(H // 4):
                        b = nb()
                        ph.pe(lambda e, b=b, hh=hh, d=d, RH=RH: e.matmul(PS[b][:], u1[d], RH[:, hh * 4:(hh + 1) * 4, :].rearrange("p h i -> p (h i)"), start=True, stop=True),
                              reads=["rh", "CF"], writes=[("ps", b)])
                        ph.act(lambda e, b=b, hh=hh, d=d: e.activation(out=EXPD[d][:, hh * 4:(hh + 1) * 4, :].rearrange("p h i -> p (h i)"), in_=PS[b][:], func=AF.Exp),
                               reads=[("ps", b)], writes=[("expd", d, hh)])
                if const_decay:
                    for d in range(2):
                        build_expd(0, d)
                it = 0
                def p2_stage_a(ti_, t):
                    tsl = slice(t * 128, (t + 1) * 128)
                    tp = ti_ % 2
                    gbanks = []
                    for gi, g in enumerate(groups):
                        if not gbanks or groups[gbanks[-1][1]]["row0"] != g["row0"] or gbanks[-1][2] == 4:
                            gbanks.append([nb(), gi, 0])
                        bk, g0, n = gbanks[-1]
                        r0, nr, ch = g["row0"], g["nrows"], g["chunk"]
                        ph.pe(lambda e, bk=bk, n=n, r0=r0, nr=nr, ch=ch, tsl=tsl: e.matmul(PS[bk][:, n * 128:(n + 1) * 128], BT[r0:r0 + nr, ch, tsl], CT[r0:r0 + nr, ch, tsl],
                                                                                       start=True, stop=True), reads=["BT", "CT"], writes=[("ps", bk)])
                        gbanks[-1][2] += 1
                    for d in range(2):
                        for (bk, g0, n) in gbanks:
                            ph.dve(lambda e, d=d, bk=bk, g0=g0, n=n, tp=tp: e.tensor_tensor(out=GM[tp][d][:, g0:g0 + n, :], in0=PS[bk][:, 0:n * 128].rearrange("p (g i) -> p g i", g=n),
                                                                                        in1=masks[d].unsqueeze(1).broadcast_to([128, n, 128]), op=ALU.mult),
                                   reads=[("ps", bk), "CF"], writes=[("gm", tp, d, g0)])
                    for d in range(2):
                        slot = tp * 2 + d
                        mp = MP[slot]
                        sin = SIN[slot]
                        ph.dma("sp", sin[:], SST_d[d, t, :, 0:SW], writes=[("sin", slot)])
                        if not const_decay:
                            build_expd(t, d)
                        gmb = GM[tp][d][:] if ng == H else GM[tp][d][:, 0:1, :].broadcast_to([128, H, 128])
                        if DT4 is not None:
                            xd = XD[slot]
                            ph.any2(lambda e, d=d, t=t, xd=xd: e.tensor_tensor(out=xd[:].rearrange("p (h q) -> p h q", h=H), in0=XS[:, t].rearrange("p (h q) -> p h q", h=H),
                                                                              in1=DT4[:, t, d, :].unsqueeze(2).broadcast_to([128, H, P]), op=ALU.mult),
                                    reads=["XS", "DT4"], writes=[("xd", slot)])
                        ph.any2(lambda e, mp=mp, gmb=gmb, d=d: e.tensor_tensor(out=mp[:], in0=EXPD[d][:], in1=gmb, op=ALU.mult),
                                reads=[("expd", d), ("gm", tp, d)], writes=[("mp", slot)])

                def p2_stage_b(ti_, t):
                    tsl = slice(t * 128, (t + 1) * 128)
                    tp = ti_ % 2
                    for d in range(2):
                        slot = tp * 2 + d
                        mp = MP[slot]
                        sin = SIN[slot]
                        yd = [nb() for _ in range(nyb)]
                        for h in range(H):
                            xrhs = XD[slot][:, h * P:(h + 1) * P] if DT4 is not None else XS[:, t, h * P:(h + 1) * P]
                            ph.pe(lambda e, h=h, mp=mp, yd=yd, xrhs=xrhs: e.matmul(PS[yd[h * P // 512]][:, (h * P) % 512:(h * P) % 512 + P], mp[:, h, :], xrhs, start=True, stop=True),
                                  reads=[("mp", slot), "XS", ("xd", slot)], writes=[("ps", yd[h * P // 512])])
                        ybanks = []
                        for gi, g in enumerate(groups):
                            r0, nr, ch = g["row0"], g["nrows"], g["chunk"]
                            pc0 = g["heads"][0] * P
                            pcn = len(g["heads"]) * P
                            sc0 = g["scol0"]
                            if not ybanks or ybanks[-1][5] != r0 or ybanks[-1][2] + pcn > 512:
                                ybanks.append([nb(), pc0, 0, g["heads"][0], 0, r0])
                            bk, used = ybanks[-1][0], ybanks[-1][2]
                            ph.pe(lambda e, r0=r0, nr=nr, ch=ch, pcn=pcn, sc0=sc0, sin=sin, bk=bk, used=used, tsl=tsl: e.matmul(
                                PS[bk][:, used:used + pcn], CT[r0:r0 + nr, ch, tsl], sin[r0:r0 + nr, sc0:sc0 + pcn], start=True, stop=True),
                                reads=["CT", ("sin", slot)], writes=[("ps", bk)])
                            ybanks[-1][2] += pcn
                            ybanks[-1][4] += len(g["heads"])
                        yacc = Y[0] if d == 0 else Y[1]
                        yt = YT[d]
                        for (bk, c0, cn, h0, nh, _) in ybanks:
                            ph.dve(lambda e, bk=bk, c0=c0, cn=cn, h0=h0, nh=nh, t=t, d=d, yt=yt: e.tensor_tensor(
                                out=yt[:, c0:c0 + cn].rearrange("p (h q) -> p h q", h=nh), in0=PS[bk][:, 0:cn].rearrange("p (h q) -> p h q", h=nh),
                                in1=ESC[:, ti(t), d, h0:h0 + nh].unsqueeze(2).broadcast_to([128, nh, P]), op=ALU.mult),
                                reads=[("ps", bk), ("esc", d)], writes=[("yt", c0)])
                        for q in range(nyb):
                            csl = slice(q * 512, (q + 1) * 512)
                            ph.dve(lambda e, q=q, yd=yd, csl=csl, yacc=yacc, yt=yt: e.tensor_tensor(out=yacc[:, csl], in0=PS[yd[q]][:], in1=yt[:, csl], op=ALU.add),
                                   reads=[("ps", yd[q]), "yt"], writes=[("y", d, q)])
                    if not (dbg_d and "Yall" in dbg_d):
                        ph.pool(lambda e: e.tensor_tensor(out=Y[0][:], in0=Y[0][:], in1=Y[1][:], op=ALU.add), reads=[("y", 0), ("y", 1)], writes=[("y", 0)])
                    if dbg_d and "Yall" in dbg_d and HP == 512:
                        ph.dma("sp", dbg_d["Yall"][:, t * 512:(t + 1) * 512], Y[0][:], reads=[("y", 0)])
                        ph.dma("sp", dbg_d["Y1all"][:, t * 512:(t + 1) * 512], Y[1][:], reads=[("y", 1)])
                    post_fn("tile", ph, al, PS, nb, post_state, t, Y[0])

                otl = list(out_tiles)
                for i in range(len(otl) + 1):
                    if i < len(otl):
                        p2_stage_a(i, otl[i])
                    if i >= 1:
                        p2_stage_b(i - 1, otl[i - 1])
                post_fn("fini", ph, al, PS, nb, post_state)
                ph.emit()
            oes.close()

        def ssd_group(g):
            with ExitStack() as ges:
                gal = lambda name, shape, dtype: ges.enter_context(SBT(name, shape, dtype))
                XS = gal("xs", [128, NT, 512], BF16)
                BTOK = gal("btok", [128, NT, 128], BF16)
                BT = gal("bt", [128, 1, T], BF16)
                CT = gal("ct", [128, 1, T], BF16)
                DT4 = gal("dt4", [128, NT, 2, 8], F32)
                A4 = gal("a4", [128, NT, 2, 8], F32)
                WZ = gal("wz", [128, KC, 512], BF16)
                Wr = ev_w_in_d.rearrange("(k p) n -> p k n", p=128)
                with ExitStack() as pes:
                    ph = Phase(ctx, "ssdproj")
                    al = lambda name, shape, dtype: pes.enter_context(SBT(name, shape, dtype))
                    PS = [pes.enter_context(PST("bps%d" % i, [128, 512], F32)) for i in range(6)]
                    PB = [pes.enter_context(PST("bpb%d" % i, [128, 1024], BF16)) for i in range(2)]
                    psi = [0]

                    def nb():
                        psi[0] = (psi[0] + 1) % 6
                        return psi[0]
                    WS = [al("wsl%d" % i, [128, KC, 128], BF16) for i in range(2)]
                    WDT = al("wdt", [128, KC, 16], BF16)
                    DBA = al("dba", [128, 2, 2, 8], F32)
                    XPAD = al("xpad", [128, 2320], F32)
                    ACC = al("acc", [128, T], F32)
                    XST = [al("xst%d" % i, [128, T], BF16) for i in range(2)]
                    ph.dma("pool", WZ[:], Wr[:, :, 1536 + g * 512:1536 + (g + 1) * 512], writes=["wz"])
                    for d in range(2):
                        ph.dma("pool", WDT[:, :, d * 8:(d + 1) * 8], Wr[:, :, 4096 + d * 16 + g * 8:4096 + d * 16 + g * 8 + 8], writes=[("wdt", d)])
                        for w in range(2):
                            ph.dma("sp", DBA[:, w, d, :], dtba_d[w:w + 1, d * 16 + g * 8:d * 16 + g * 8 + 8].broadcast_to([128, 8]), writes=[("dba", w, d)])
                    ph.pool(lambda e: e.memset(XPAD[:], 0.0), writes=["xpad"])
                    b = nb()
                    for t in range(NT):
                        for k in range(KC):
                            ph.pe(lambda e, b=b, t=t, k=k: e.matmul(PS[b][:, t * 16:(t + 1) * 16], HT[:, k, t * 128:(t + 1) * 128], WDT[:, k, :], start=(k == 0), stop=(k == KC - 1)),
                                  reads=["wdt", "HT"], writes=[("ps", b)])
                    dt3 = DT4[:].rearrange("p t d h -> p t (d h)")
                    ph.dve(lambda e, b=b: e.tensor_tensor(out=dt3, in0=PS[b][:, 0:NT * 16].rearrange("p (t n) -> p t n", t=NT),
                                                          in1=DBA[:, 0].rearrange("p d h -> p (d h)").unsqueeze(1).broadcast_to([128, NT, 16]), op=ALU.add),
                           reads=[("ps", b), "dba"], writes=["DT4"])
                    ph.act(lambda e: e.activation(out=dt3, in_=dt3, func=AF.Exp), reads=["DT4"], writes=["DT4"])
                    ph.act(lambda e: e.activation(out=dt3, in_=dt3, func=AF.Ln, bias=1.0), reads=["DT4"], writes=["DT4"])
                    ph.act(lambda e: e.activation(out=DBA[:, 1], in_=DBA[:, 1], func=AF.Exp), reads=["dba"], writes=["dba"])
                    ph.dve(lambda e: e.scalar_tensor_tensor(out=A4[:].rearrange("p t d h -> p t (d h)"), in0=dt3, scalar=-1.0,
                                                            in1=DBA[:, 1].rearrange("p d h -> p (d h)").unsqueeze(1).broadcast_to([128, NT, 16]), op0=ALU.mult, op1=ALU.mult),
                           reads=["DT4", "dba"], writes=["A4"])
                    chunks = [4 * g + i for i in range(4)] + [8 + g, 10 + g]
                    for ci, c in enumerate(chunks):
                        ws = WS[ci % 2]
                        ph.dma("pool", ws[:], Wr[:, :, 2560 + c * 128:2560 + (c + 1) * 128], writes=[("wsl", ci % 2)])
                        for (t0, tn) in BLKS:
                            b = nb()
                            for k in range(KC):
                                ph.pe(lambda e, b=b, k=k, ws=ws, t0=t0, tn=tn: e.matmul(PS[b][:, :tn], ws[:, k, :], HT[:, k, t0:t0 + tn], start=(k == 0), stop=(k == KC - 1)),
                                      reads=[("wsl", ci % 2), "HT"], writes=[("ps", b)])
                            o0 = 2 + t0 if t0 < L else 2054 + (t0 - L)
                            ph.act(lambda e, b=b, o0=o0, tn=tn: e.copy(out=XPAD[:, o0:o0 + tn], in_=PS[b][:, :tn]), reads=[("ps", b)], writes=[("xpad", t0)])
                        eng = "dve"
                        for (o0, a0, n) in ((2, 0, L), (2054, L, LC)):
                            wcol = lambda k, c=c: VT[:, VR["conv_w"] + k * 12 + c:VR["conv_w"] + k * 12 + c + 1]
                            bcol = VT[:, VR["conv_b"] + c:VR["conv_b"] + c + 1]
                            ph.op(eng, lambda e, o0=o0, a0=a0, n=n, wcol=wcol, bcol=bcol: e.tensor_scalar(out=ACC[:, a0:a0 + n], in0=XPAD[:, o0 - 2:o0 - 2 + n], scalar1=wcol(0), scalar2=bcol,
                                                                                                    op0=ALU.mult, op1=ALU.add), reads=["xpad", "VT"], writes=[("acc", a0)])
                            for k in range(1, 5):
                                ph.op(eng, lambda e, o0=o0, a0=a0, n=n, k=k, wcol=wcol: e.scalar_tensor_tensor(out=ACC[:, a0:a0 + n], in0=XPAD[:, o0 - 2 + k:o0 - 2 + k + n], scalar=wcol(k),
                                                                                                      in1=ACC[:, a0:a0 + n], op0=ALU.mult, op1=ALU.add),
                                      reads=["xpad", ("acc", a0)], writes=[("acc", a0)])
                        if ci < 4:
                            dst, dkey = XST[ci % 2][:], ("xst", ci % 2)
                        elif ci == 4:
                            dst, dkey = BT[:, 0, :], "BT"
                        else:
                            dst, dkey = CT[:, 0, :], "CT"
                        ph.act(lambda e, dst=dst: e.activation(out=dst, in_=ACC[:], func=AF.Silu), reads=["acc"], writes=[dkey])
                        if ci <= 4:
                            for t8 in range(0, NT, 8):
                                n8 = min(8, NT - t8)
                                pb = (t8 // 8 + ci) % 2
                                for tt in range(n8):
                                    t = t8 + tt
                                    ph.pe(lambda e, pb=pb, tt=tt, t=t, dst=dst: e.transpose(PB[pb][:, tt * 128:(tt + 1) * 128], dst[:, t * 128:(t + 1) * 128], IDB),
                                          reads=[dkey, "CB"], writes=[("pb", pb)])
                                if ci < 4:
                                    o = XS[:, t8:t8 + n8, ci * 128:(ci + 1) * 128]
                                    okey = ("XS", ci, t8)
                                else:
                                    o = BTOK[:, t8:t8 + n8, :]
                                    okey = ("BTOK", t8)
                                ph.act(lambda e, pb=pb, n8=n8, o=o: e.copy(out=o, in_=PB[pb][:, 0:n8 * 128].rearrange("p (a n) -> p a n", a=n8)),
                                       reads=[("pb", pb)], writes=[okey])
                    ph.emit()
                if g == 0:
                    dump("XS0", XS[:].rearrange("p t n -> p (t n)"), (128, NT * 512))
                    dump("A40", A4[:].rearrange("p t d h -> p (t d h)"), (128, NT * 16))
                    dump("DT40", DT4[:].rearrange("p t d h -> p (t d h)"), (128, NT * 16))
                    dump("CT0", CT[:, 0, :], (128, T))
                    dump("BTOK0", BTOK[:].rearrange("p t n -> p (t n)"), (128, NT * 128))

                def post(stage, ph, al, PS, nb, st=None, t=None, Yt=None):
                    if stage == "init":
                        pbt = st
                        st = {}
                        st["dsk"] = al("dsk", [128, 8], F32)
                        st["gng"] = al("gng", [128, 512], F32)
                        st["sz"] = al("sz", [128, 512], F32)
                        st["yz"] = al("yz", [128, 512], F32)
                        st["sq"] = st["sz"]
                        st["ssq"] = al("ssq", [128, 4], F32)
                        st["yn"] = al("yn", [128, 512], BF16)
                        st["stg"] = [al("stg%d" % i, [128, 4, 128], BF16) for i in range(2)]
                        st["pb"] = pbt
                        ph.dma("sp", st["dsk"][:], ssmd_d[0:1, g * 8:(g + 1) * 8].broadcast_to([128, 8]), writes=["dsk"])
                        ph.dma("sp", st["gng"][:], ssmg_d[0:1, g * 512:(g + 1) * 512].broadcast_to([128, 512]), writes=["gng"])
                        return st
                    if stage == "fini":
                        return
                    dsk, gng, sz, yz, sq, ssq, yn = st["dsk"], st["gng"], st["sz"], st["yz"], st["sq"], st["ssq"], st["yn"]
                    b = nb()
                    for k in range(KC):
                        ph.pe(lambda e, b=b, k=k: e.matmul(PS[b][:], HT[:, k, t * 128:(t + 1) * 128], WZ[:, k, :], start=(k == 0), stop=(k == KC - 1)),
                              reads=["HT", "wz"], writes=[("ps", b)])
                    ph.act(lambda e, b=b: e.activation(out=sz[:], in_=PS[b][:], func=AF.Silu), reads=[("ps", b)], writes=["sz"])
                    ph.dve(lambda e: e.tensor_tensor(out=yz[:].rearrange("p (h q) -> p h q", h=8), in0=XS[:, t].rearrange("p (h q) -> p h q", h=8),
                                                     in1=dsk[:].unsqueeze(2).broadcast_to([128, 8, 64]), op=ALU.mult), reads=["XS", "dsk"], writes=["yz"])
                    ph.dve(lambda e: e.tensor_tensor(out=yz[:], in0=yz[:], in1=Yt[:], op=ALU.add), reads=["yz", ("y", 0)], writes=["yz"])
                    ph.dve(lambda e: e.tensor_tensor(out=yz[:], in0=yz[:], in1=sz[:], op=ALU.mult), reads=["yz", "sz"], writes=["yz"])
                    ph.act(lambda e: e.activation(out=sq[:], in_=yz[:], func=AF.Square, accum_out=ssq[:, 0:1]), reads=["yz"], writes=["sz", "ssq"])
                    ph.dve(lambda e: e.tensor_scalar(out=ssq[:, 1:2], in0=ssq[:, 0:1], scalar1=1.0 / 512, scalar2=EPS, op0=ALU.mult, op1=ALU.add), reads=["ssq"], writes=["ssq"])
                    ph.act(lambda e: e.sqrt(out=ssq[:, 2:3], in_=ssq[:, 1:2]), reads=["ssq"], writes=["ssq"])
                    ph.dve(lambda e: e.reciprocal(out=ssq[:, 3:4], in_=ssq[:, 2:3]), reads=["ssq"], writes=["ssq"])
                    ph.dve(lambda e: e.scalar_tensor_tensor(out=yn[:], in0=yz[:], scalar=ssq[:, 3:4], in1=gng[:], op0=ALU.mult, op1=ALU.mult),
                           reads=["yz", "ssq", "gng"], writes=["yn"])
                    pb = st["pb"]
                    stg = st["stg"][t % 2]
                    for cl in range(4):
                        ph.pe(lambda e, cl=cl: e.transpose(pb[:, cl * 128:(cl + 1) * 128], yn[:, cl * 128:(cl + 1) * 128], IDB), reads=["yn", "CB"], writes=["pbt"])
                    ph.act(lambda e, stg=stg: e.copy(out=stg[:], in_=pb[:, 0:512].rearrange("p (a n) -> p a n", a=4)), reads=["pbt"], writes=[("stg", t % 2)])
                    ph.dma("sp", YCAT_d[4 + 4 * g:8 + 4 * g, :, t * 128:(t + 1) * 128].rearrange("c p t -> p c t"), stg[:], reads=[("stg", t % 2)])

                pes_pb = [None]

                def pre(ph, al, PS, nb):
                    pass
                groups = [dict(chunk=0, row0=0, nrows=128, heads=list(range(8)), scol0=0, sncols=512)]
                scan_phase(64, groups, XS, BTOK, BT, CT, A4, DT4, False, post, list(range(NT)))

        def out_proj(l):
            W_d = ev_w_out_d if l == 0 else od_w_out_d
            with ExitStack() as pes:
                ph = Phase(ctx, "oproj")
                al = lambda name, shape, dtype: pes.enter_context(SBT(name, shape, dtype))
                PS = [pes.enter_context(PST("ops%d" % i, [128, 512], F32)) for i in range(8)]
                WO = al("wo", [128, 12, D], BF16)
                YB = [al("yb%d" % i, [128, 12, 512], BF16) for i in range(2)]
                for c in range(0, 12, 4):
                    ph.dma("pool", WO[:, c:c + 4, :], W_d.rearrange("(c p) n -> p c n", p=128)[:, c:c + 4, :], writes=[("wo", c)])
                pi = 0
                for bi_, (t0, tn) in enumerate(BLKS):
                    if l == 1 and t0 >= L:
                        continue
                    j = 0 if t0 < L else 1
                    yb = YB[bi_ % 2]
                    ph.dma("sp", yb[:, :, :tn], YCAT_d[:, :, t0:t0 + tn].rearrange("c p t -> p c t"), writes=[("yb", bi_ % 2)])
                    for dc in range(KC):
                        b = pi % 8
                        pi += 1
                        for c in range(12):
                            ph.pe(lambda e, b=b, c=c, dc=dc, yb=yb, tn=tn: e.matmul(PS[b][:, :tn], WO[:, c, dc * 128:(dc + 1) * 128], yb[:, c, :tn], start=(c == 0), stop=(c == 11)),
                                  reads=["wo", ("yb", bi_ % 2)], writes=[("ps", b)])
                        ph.dve(lambda e, b=b, dc=dc, t0=t0, tn=tn, j=j: e.scalar_tensor_tensor(out=XT[:, dc, t0:t0 + tn], in0=PS[b][:, :tn], scalar=AB[:, l, j, 2, dc:dc + 1],
                                                                                            in1=XT[:, dc, t0:t0 + tn], op0=ALU.mult, op1=ALU.add),
                               reads=[("ps", b)], writes=[("XT", dc, bi_)])
                ph.emit()

        THIRDS = [[(0, 512), (512, 256)], [(768, 512), (1280, 256)], [(1536, 512), (2048, 256)]]

        def ffn(l, GT=None):
            moe = (l == 1)
            nfc = 28 if moe else 22
            nexp = NEXPERTS if moe else 1
            with ExitStack() as pes:
                ph = Phase(ctx, "ffn")
                al = lambda name, shape, dtype: pes.enter_context(SBT(name, shape, dtype))
                PS = [pes.enter_context(PST("fps%d" % i, [128, 512], F32)) for i in range(8)]
                psi = [0]

                def nb():
                    psi[0] = (psi[0] + 1) % 8
                    return psi[0]
                tgroups = [[(0, 512), (512, 512)], [(1024, 512), (1536, 512)]] if moe else THIRDS
                gmax = 1024 if moe else 768
                fchunks = [list(range(0, 14)), list(range(14, 28))] if moe else [list(range(22))]
                nfl = len(fchunks[0])
                ACTT = al("actt", [128, nfl, gmax], BF16)
                W13 = [al("w13_%d" % i, [128, 2, KC, 128], BF16) for i in range(3)]
                W2S = [al("w2s_%d" % i, [128, nfl, 128], BF16) for i in range(2)]
                SIL = [al("sil%d" % i, [128, 512], F32) for i in range(2)]
                if moe:
                    HG = al("hg", [128, KC, gmax], BF16)
                wi = 0
                w2i = 0
                si = 0
                for th, blks in enumerate(tgroups):
                    tb = blks[0][0]
                    for ex in range(nexp):
                        if moe:
                            w1_d, w3_d, w2_d = moe_w1_d[ex], moe_w3_d[ex], moe_w2_d[ex]
                            for (t0, tn) in blks:
                                b = nb()
                                ph.pe(lambda e, b=b, ex=ex, t0=t0, tn=tn: e.matmul(PS[b][:, :tn], SEL[:, ex * 128:(ex + 1) * 128], GT[:, t0:t0 + tn], start=True, stop=True),
                                      reads=["GT", "SEL"], writes=[("ps", b)])
                                for k in range(KC):
                                    ph.dve(lambda e, b=b, k=k, t0=t0, tn=tn, tb=tb: e.tensor_tensor(out=HG[:, k, t0 - tb:t0 - tb + tn], in0=HT[:, k, t0:t0 + tn], in1=PS[b][:, :tn], op=ALU.mult),
                                           reads=[("ps", b), "HT"], writes=[("hg", k, t0)])
                        else:
                            w1_d, w3_d, w2_d = ffn_w1_d, ffn_w3_d, ffn_w2_d
                        w1r = w1_d.rearrange("(k p) n -> p k n", p=128)
                        w3r = w3_d.rearrange("(k p) n -> p k n", p=128)
                        w2r = w2_d.rearrange("(f p) n -> p f n", p=128)
                        for fcs in fchunks:
                            for fi, fc in enumerate(fcs):
                                w = W13[wi % 3]
                                ph.dma("pool", w[:, 0], w1r[:, :, fc * 128:(fc + 1) * 128], writes=[("w13", wi % 3, 0)])
                                ph.dma("pool", w[:, 1], w3r[:, :, fc * 128:(fc + 1) * 128], writes=[("w13", wi % 3, 1)])
                                for (t0, tn) in blks:
                                    b1, b3 = nb(), nb()
                                    for k in range(KC):
                                        ph.pe(lambda e, b1=b1, k=k, w=w, t0=t0, tn=tn: e.matmul(PS[b1][:, :tn], w[:, 0, k, :], HT[:, k, t0:t0 + tn], start=(k == 0), stop=(k == KC - 1)),
                                              reads=[("w13", wi % 3, 0), "HT"], writes=[("ps", b1)])
                                    for k in range(KC):
                                        rhs = HG[:, k, t0 - tb:t0 - tb + tn] if moe else HT[:, k, t0:t0 + tn]
                                        ph.pe(lambda e, b3=b3, k=k, w=w, rhs=rhs, tn=tn: e.matmul(PS[b3][:, :tn], w[:, 1, k, :], rhs, start=(k == 0), stop=(k == KC - 1)),
                                              reads=[("w13", wi % 3, 1), "HT", "hg"], writes=[("ps", b3)])
                                    sil = SIL[si % 2]
                                    ph.act(lambda e, b1=b1, sil=sil, tn=tn: e.activation(out=sil[:, :tn], in_=PS[b1][:, :tn], func=AF.Silu), reads=[("ps", b1)], writes=[("sil", si % 2)])
                                    ph.dve(lambda e, b3=b3, sil=sil, fi=fi, t0=t0, tn=tn, tb=tb: e.tensor_tensor(out=ACTT[:, fi, t0 - tb:t0 - tb + tn], in0=PS[b3][:, :tn], in1=sil[:, :tn], op=ALU.mult),
                                           reads=[("ps", b3), ("sil", si % 2)], writes=[("actt", fi, t0)])
                                    si += 1
                                wi += 1
                            nf = len(fcs)
                            for dc in range(KC):
                                w2 = W2S[w2i % 2]
                                ph.dma("pool", w2[:, 0:nf, :], w2r[:, fcs[0]:fcs[0] + nf, dc * 128:(dc + 1) * 128], writes=[("w2s", w2i % 2)])
                                for (t0, tn) in blks:
                                    j = 0 if t0 < L else 1
                                    b = nb()
                                    for fi in range(nf):
                                        ph.pe(lambda e, b=b, fi=fi, w2=w2, t0=t0, tn=tn, tb=tb, nf=nf: e.matmul(PS[b][:, :tn], w2[:, fi, :], ACTT[:, fi, t0 - tb:t0 - tb + tn], start=(fi == 0), stop=(fi == nf - 1)),
                                              reads=[("w2s", w2i % 2), "actt"], writes=[("ps", b)])
                                    ph.dve(lambda e, b=b, dc=dc, t0=t0, tn=tn, j=j: e.scalar_tensor_tensor(out=XT[:, dc, t0:t0 + tn], in0=PS[b][:, :tn], scalar=AB[:, l, j, 5, dc:dc + 1],
                                                                                                        in1=XT[:, dc, t0:t0 + tn], op0=ALU.mult, op1=ALU.add),
                                           reads=[("ps", b)], writes=[("XT", dc, t0)])
                                w2i += 1
                ph.emit()

        def final_out():
            with ExitStack() as pes:
                ph = Phase(ctx, "fin")
                al = lambda name, shape, dtype: pes.enter_context(SBT(name, shape, dtype))
                PS = [pes.enter_context(PST("zps%d" % i, [128, 512], F32)) for i in range(8)]
                SQ = [al("fsq%d" % i, [128, 512], BF16) for i in range(3)]
                RS = [al("frs%d" % i, [128, 512], F32) for i in range(2)]
                XN = [al("fxn%d" % i, [128, KC, 512], F32) for i in range(2)]
                OTK = [al("fot%d" % i, [128, D], F32) for i in range(3)]
                qi = 0
                oi = 0
                pi = 0
                g0 = VR["final_g"]
                for bi_, (t0, tn) in enumerate(BLKS[:4]):
                    pb = pi % 8
                    pi += 1
                    for k in range(KC):
                        sq = SQ[qi % 3]
                        ph.act(lambda e, sq=sq, k=k, t0=t0: e.activation(out=sq[:], in_=XT[:, k, t0:t0 + 512], func=AF.Square), reads=["XT"], writes=[("sq", qi % 3)])
                        ph.pe(lambda e, sq=sq, k=k, pb=pb: e.matmul(PS[pb][:], ONESB, sq[:], start=(k == 0), stop=(k == KC - 1)), reads=[("sq", qi % 3)], writes=[("ps", pb)])
                        qi += 1
                    rs = RS[bi_ % 2]
                    xn = XN[bi_ % 2]
                    ph.dve(lambda e, rs=rs, pb=pb: e.tensor_scalar(out=rs[:], in0=PS[pb][:], scalar1=1.0 / D, scalar2=EPS, op0=ALU.mult, op1=ALU.add), reads=[("ps", pb)], writes=[("rs", bi_ % 2)])
                    ph.act(lambda e, rs=rs: e.sqrt(out=rs[:], in_=rs[:]), reads=[("rs", bi_ % 2)], writes=[("rs", bi_ % 2)])
                    ph.dve(lambda e, rs=rs: e.reciprocal(out=rs[:], in_=rs[:]), reads=[("rs", bi_ % 2)], writes=[("rs", bi_ % 2)])
                    for k in range(KC):
                        ph.dve(lambda e, k=k, t0=t0, rs=rs, xn=xn: e.scalar_tensor_tensor(out=xn[:, k, :], in0=XT[:, k, t0:t0 + 512], scalar=VT[:, g0 + k:g0 + k + 1], in1=rs[:], op0=ALU.mult, op1=ALU.mult),
                               reads=["XT", ("rs", bi_ % 2)], writes=[("xn", bi_ % 2, k)])
                    for tt in range(4):
                        otk = OTK[oi % 3]
                        for half in range(2):
                            pb = pi % 8
                            pi += 1
                            for kk in range(4):
                                k = half * 4 + kk
                                ph.pe(lambda e, pb=pb, kk=kk, k=k, tt=tt, xn=xn: e.transpose(PS[pb][:, kk * 128:(kk + 1) * 128], xn[:, k, tt * 128:(tt + 1) * 128], IDF),
                                      reads=[("xn", bi_ % 2), "CF"], writes=[("ps", pb)])
                            if half == 0:
                                ph.act(lambda e, pb=pb, otk=otk: e.copy(out=otk[:, 0:512], in_=PS[pb][:]), reads=[("ps", pb)], writes=[("otk", oi % 3, 0)])
                            else:
                                ph.dve(lambda e, pb=pb, otk=otk: e.tensor_copy(out=otk[:, 512:1024], in_=PS[pb][:]), reads=[("ps", pb)], writes=[("otk", oi % 3, 1)])
                        ph.dma("sp", out_d[t0 + tt * 128:t0 + (tt + 1) * 128, :], otk[:], reads=[("otk", oi % 3)])
                        oi += 1
                ph.emit()

        RM = CB[:, 768:896]

        def rope(ph, X, key, nb, PS, al):
            sid = 0
            store = ph.__dict__.setdefault("_rope_store", {})
            if sid not in store:
                COS = al("cos", [128, L], F32)
                SIN = al("sin", [128, L], F32)
                T1 = [al("rt1_%d" % i, [128, 512], F32) for i in range(2)]
                T2 = [al("rt2_%d" % i, [128, 512], F32) for i in range(2)]
                ph.dma("sp", COS[:], rope_d[0], writes=["cos"])
                ph.dma("sp", SIN[:], rope_d[1], writes=["sin"])
                store[sid] = (COS, SIN, T1, T2, [0])
            COS, SIN, T1, T2, cnt = store[sid]
            for (t0, tn) in BLKS[:4]:
                b = nb()
                i = cnt[0] % 2
                cnt[0] += 1
                ph.pe(lambda e, b=b, t0=t0: e.matmul(PS[b][:], RM, X[:, t0:t0 + 512], start=True, stop=True), reads=[key, "CB"], writes=[("ps", b)])
                ph.dve(lambda e, i=i, t0=t0: e.tensor_tensor(out=T1[i][:], in0=X[:, t0:t0 + 512], in1=COS[:, t0:t0 + 512], op=ALU.mult), reads=[key, "cos"], writes=[("rt1", i)])
                ph.dve(lambda e, i=i, b=b, t0=t0: e.tensor_tensor(out=T2[i][:], in0=PS[b][:], in1=SIN[:, t0:t0 + 512], op=ALU.mult), reads=[("ps", b), "sin"], writes=[("rt2", i)])
                ph.pool(lambda e, i=i, t0=t0: e.tensor_tensor(out=X[:, t0:t0 + 512], in0=T1[i][:], in1=T2[i][:], op=ALU.add), reads=[("rt1", i), ("rt2", i)], writes=[(key, "r", t0) if isinstance(key, str) else key])

        def ret_half(hh):
            Wr = od_w_in_d.rearrange("(k p) n -> p k n", p=128)
            with ExitStack() as ges:
                gal = lambda name, shape, dtype: ges.enter_context(SBT(name, shape, dtype))
                XS = gal("rxs", [128, NT, 512], BF16)
                BTOK = gal("rbtok", [128, NT, 256], BF16)
                BT = gal("rbt", [128, 2, T], BF16)
                CT = gal("rct", [128, 2, T], BF16)
                A4 = gal("ra4", [128, 1, 2, 4], F32)
                WG = gal("rwg", [128, KC, 512], BF16)
                with ExitStack() as pes:
                    ph = Phase(ctx, "retproj")
                    al = lambda name, shape, dtype: pes.enter_context(SBT(name, shape, dtype))
                    PS = [pes.enter_context(PST("rps%d" % i, [128, 512], F32)) for i in range(6)]
                    PB = [pes.enter_context(PST("rpb%d" % i, [128, 1024], BF16)) for i in range(2)]
                    psi = [0]

                    def nb():
                        psi[0] = (psi[0] + 1) % 6
                        return psi[0]
                    WS = [al("rws%d" % i, [128, KC, 128], BF16) for i in range(2)]
                    WV = al("rwv", [128, KC, 512], BF16)
                    for hs in range(4):
                        hl = RPERM[hs]
                        ph.dma("pool", WG[:, :, hs * 128:(hs + 1) * 128], Wr[:, :, 2816 + (4 * hh + hl) * 128:2816 + (4 * hh + hl + 1) * 128], writes=[("wg", hs)])
                        ph.dma("pool", WV[:, :, hs * 128:(hs + 1) * 128], Wr[:, :, 1792 + (4 * hh + hl) * 128:1792 + (4 * hh + hl + 1) * 128], writes=[("wv", hs)])
                        for d in range(2):
                            ph.dma("sp", A4[:, 0, d, hs:hs + 1], retld_d[d:d + 1, 4 * hh + hl:4 * hh + hl + 1].broadcast_to([128, 1]), writes=[("a4", d, hs)])
                    a4f = A4[:].rearrange("p a d h -> p (a d h)")
                    ph.act(lambda e: e.activation(out=a4f, in_=a4f, func=AF.Exp), reads=["a4"], writes=["a4"])
                    ph.act(lambda e: e.activation(out=a4f, in_=a4f, func=AF.Ln, scale=-1.0, bias=1.0), reads=["a4"], writes=["a4"])
                    for t in range(NT):
                        b = nb()
                        for k in range(KC):
                            ph.pe(lambda e, b=b, k=k, t=t: e.matmul(PS[b][:], HT[:, k, t * 128:(t + 1) * 128], WV[:, k, :], start=(k == 0), stop=(k == KC - 1)),
                                  reads=["HT", "wv"], writes=[("ps", b)])
                        if t % 2 == 0:
                            ph.act(lambda e, b=b, t=t: e.copy(out=XS[:, t, :], in_=PS[b][:]), reads=[("ps", b)], writes=[("XS", t)])
                        else:
                            ph.dve(lambda e, b=b, t=t: e.tensor_copy(out=XS[:, t, :], in_=PS[b][:]), reads=[("ps", b)], writes=[("XS", t)])
                    wi = 0
                    for (dst, c0, scale, nm) in ((CT, 768, 1.0, "CT"), (BT, 1280, 0.125, "BT")):
                        for c in range(2):
                            ws = WS[wi % 2]
                            ph.dma("pool", ws[:], Wr[:, :, c0 + (2 * hh + c) * 128:c0 + (2 * hh + c + 1) * 128], writes=[("rws", wi % 2)])
                            for (t0, tn) in BLKS:
                                b = nb()
                                for k in range(KC):
                                    ph.pe(lambda e, b=b, k=k, ws=ws, t0=t0, tn=tn: e.matmul(PS[b][:, :tn], ws[:, k, :], HT[:, k, t0:t0 + tn], start=(k == 0), stop=(k == KC - 1)),
                                          reads=[("rws", wi % 2), "HT"], writes=[("ps", b)])
                                ph.act(lambda e, b=b, dst=dst, c=c, t0=t0, tn=tn, scale=scale: e.activation(out=dst[:, c, t0:t0 + tn], in_=PS[b][:, :tn], func=AF.Copy, scale=scale),
                                       reads=[("ps", b)], writes=[(nm, c, "p", t0)])
                            rope(ph, dst[:, c, :], (nm, c), nb, PS, al)
                            if nm == "BT":
                                for t8 in range(0, NT, 8):
                                    n8 = min(8, NT - t8)
                                    pb = (t8 // 8 + c) % 2
                                    for tt in range(n8):
                                        t = t8 + tt
                                        ph.pe(lambda e, pb=pb, tt=tt, t=t, c=c: e.transpose(PB[pb][:, tt * 128:(tt + 1) * 128], BT[:, c, t * 128:(t + 1) * 128], IDB),
                                              reads=[("BT", c), "CB"], writes=[("pb", pb)])
                                    ph.act(lambda e, pb=pb, n8=n8, t8=t8, c=c: e.copy(out=BTOK[:, t8:t8 + n8, c * 128:(c + 1) * 128], in_=PB[pb][:, 0:n8 * 128].rearrange("p (a n) -> p a n", a=n8)),
                                           reads=[("pb", pb)], writes=[("BTOK", c, t8)])
                            wi += 1
                    ph.emit()
                if hh == 0:
                    dump("RXS", XS[:].rearrange("p t n -> p (t n)"), (128, NT * 512))
                    dump("RCT", CT[:].rearrange("p c t -> p (c t)"), (128, 2 * T))
                    dump("RBTOK", BTOK[:].rearrange("p t n -> p (t n)"), (128, NT * 256))
                    dump("RA4", A4[:].rearrange("p a d h -> p (a d h)"), (128, 8))

                def post(stage, ph, al, PS, nb, st=None, t=None, Yt=None):
                    if stage == "init":
                        pbt = st
                        st = {"pb": pbt}
                        st["gng"] = al("rgng", [128, 512], F32)
                        st["gnb"] = al("rgnb", [128, 512], F32)
                        st["sg"] = al("rsg", [128, 512], F32)
                        st["yc"] = al("ryc", [128, 512], F32)
                        st["stat"] = al("rstat", [128, 4, 4], F32)
                        st["yn"] = al("ryn", [128, 512], BF16)
                        st["stg"] = [al("rstg%d" % i, [128, 4, 128], BF16) for i in range(2)]
                        for hs in range(4):
                            c0 = (4 * hh + RPERM[hs]) * 128
                            ph.dma("sp", st["gng"][:, hs * 128:(hs + 1) * 128], retg_d[0:1, c0:c0 + 128].broadcast_to([128, 128]), writes=[("gng", hs)])
                            ph.dma("sp", st["gnb"][:, hs * 128:(hs + 1) * 128], retb_d[0:1, c0:c0 + 128].broadcast_to([128, 128]), writes=[("gnb", hs)])
                        return st
                    if stage == "fini":
                        return
                    gng, gnb, sg, yc, stat, yn = st["gng"], st["gnb"], st["sg"], st["yc"], st["stat"], st["yn"]
                    b = nb()
                    for k in range(KC):
                        ph.pe(lambda e, b=b, k=k: e.matmul(PS[b][:], HT[:, k, t * 128:(t + 1) * 128], WG[:, k, :], start=(k == 0), stop=(k == KC - 1)),
                              reads=["HT", "wg"], writes=[("ps", b)])
                    ph.act(lambda e, b=b: e.activation(out=sg[:], in_=PS[b][:], func=AF.Silu), reads=[("ps", b)], writes=["sg"])
                    y3 = Yt[:].rearrange("p (h q) -> p h q", h=4)
                    yc3 = yc[:].rearrange("p (h q) -> p h q", h=4)
                    ph.dve(lambda e: e.reduce_sum(out=stat[:, 0, :], in_=y3, axis=AX.X), reads=[("y", 0)], writes=[("stat", 0)])
                    ph.dve(lambda e: e.tensor_scalar(out=stat[:, 1, :], in0=stat[:, 0, :], scalar1=-1.0 / 128, scalar2=None, op0=ALU.mult), reads=[("stat", 0)], writes=[("stat", 1)])
                    ph.dve(lambda e: e.tensor_tensor(out=yc3, in0=y3, in1=stat[:, 1, :].unsqueeze(2).broadcast_to([128, 4, 128]), op=ALU.add), reads=[("y", 0), ("stat", 1)], writes=["yc"])
                    for hq in range(4):
                        ph.act(lambda e, hq=hq: e.activation(out=yn[:, hq * 128:(hq + 1) * 128], in_=yc[:, hq * 128:(hq + 1) * 128], func=AF.Square, accum_out=stat[:, 2, hq:hq + 1]),
                               reads=["yc"], writes=["yn", ("stat", 2, hq)])
                    ph.dve(lambda e: e.tensor_scalar(out=stat[:, 2, :], in0=stat[:, 2, :], scalar1=1.0 / 128, scalar2=EPS, op0=ALU.mult, op1=ALU.add), reads=[("stat", 2)], writes=[("stat", 2)])
                    ph.act(lambda e: e.sqrt(out=stat[:, 2, :], in_=stat[:, 2, :]), reads=[("stat", 2)], writes=[("stat", 2)])
                    ph.dve(lambda e: e.reciprocal(out=stat[:, 3, :], in_=stat[:, 2, :]), reads=[("stat", 2)], writes=[("stat", 3)])
                    ph.dve(lambda e: e.tensor_tensor(out=yc3, in0=yc3, in1=stat[:, 3, :].unsqueeze(2).broadcast_to([128, 4, 128]), op=ALU.mult), reads=["yc", ("stat", 3)], writes=["yc"])
                    ph.pool(lambda e: e.tensor_tensor(out=yc[:], in0=yc[:], in1=gng[:], op=ALU.mult), reads=["yc", "gng"], writes=["yc"])
                    ph.pool(lambda e: e.tensor_tensor(out=yc[:], in0=yc[:], in1=gnb[:], op=ALU.add), reads=["yc", "gnb"], writes=["yc"])
                    ph.dve(lambda e: e.tensor_tensor(out=yn[:], in0=yc[:], in1=sg[:], op=ALU.mult), reads=["yc", "sg"], writes=["yn"])
                    pb = st["pb"]
                    stg = st["stg"][t % 2]
                    for cl in range(4):
                        ph.pe(lambda e, cl=cl: e.transpose(pb[:, RPERM[cl] * 128:(RPERM[cl] + 1) * 128], yn[:, cl * 128:(cl + 1) * 128], IDB), reads=["yn", "CB"], writes=["pbt"])
                    ph.act(lambda e, stg=stg: e.copy(out=stg[:], in_=pb[:, 0:512].rearrange("p (a n) -> p a n", a=4)), reads=["pbt"], writes=[("stg", t % 2)])
                    ph.dma("sp", YCAT_d[4 + 4 * hh:8 + 4 * hh, :, t * 128:(t + 1) * 128].rearrange("c p t -> p c t"), stg[:], reads=[("stg", t % 2)])

                groups = [dict(chunk=hs % 2, row0=(hs // 2) * 64, nrows=64, heads=[hs], scol0=(hs % 2) * 128, sncols=128) for hs in range(4)]
                if RET_SCAN:
                    scan_phase(128, groups, XS, BTOK, BT, CT, A4, None, True, post, list(range(16)), H=4)

        def moe_gate(LG, GT):
            with ExitStack() as pes:
                ph = Phase(ctx, "gate")
                al = lambda name, shape, dtype: pes.enter_context(SBT(name, shape, dtype))
                PS = [pes.enter_context(PST("gps%d" % i, [128, 512], F32)) for i in range(2)]
                M1 = al("m1", [128, NT], F32)
                M2 = al("m2", [128, NT], F32)
                EQ = al("eq", [128, NT, 8], F32)
                L2 = al("l2", [128, NT, 8], F32)
                EXg = al("exg", [128, NT, 8], F32)
                DEN = al("den", [128, NT], F32)
                bc = lambda a: a[:].unsqueeze(2).broadcast_to([128, NT, 8])
                ph.dve(lambda e: e.reduce_max(out=M1[:], in_=LG[:], axis=AX.X), reads=["LG"], writes=["m1"])
                ph.dve(lambda e: e.tensor_tensor(out=EQ[:], in0=LG[:], in1=bc(M1), op=ALU.is_equal), reads=["LG", "m1"], writes=["eq"])
                ph.dve(lambda e: e.scalar_tensor_tensor(out=L2[:], in0=EQ[:], scalar=-1e30, in1=LG[:], op0=ALU.mult, op1=ALU.add), reads=["eq", "LG"], writes=["l2"])
                ph.dve(lambda e: e.reduce_max(out=M2[:], in_=L2[:], axis=AX.X), reads=["l2"], writes=["m2"])
                ph.dve(lambda e: e.tensor_tensor(out=EQ[:], in0=LG[:], in1=bc(M2), op=ALU.is_ge), reads=["LG", "m2"], writes=["eq"])
                ph.dve(lambda e: e.tensor_tensor(out=L2[:], in0=LG[:], in1=bc(M1), op=ALU.subtract), reads=["LG", "m1"], writes=["l2"])
                ph.act(lambda e: e.activation(out=EXg[:], in_=L2[:], func=AF.Exp), reads=["l2"], writes=["exg"])
                ph.dve(lambda e: e.tensor_tensor(out=EXg[:], in0=EXg[:], in1=EQ[:], op=ALU.mult), reads=["exg", "eq"], writes=["exg"])
                ph.dve(lambda e: e.reduce_sum(out=DEN[:], in_=EXg[:], axis=AX.X), reads=["exg"], writes=["den"])
                ph.dve(lambda e: e.reciprocal(out=DEN[:], in_=DEN[:]), reads=["den"], writes=["den"])
                ph.dve(lambda e: e.tensor_tensor(out=EXg[:], in0=EXg[:], in1=bc(DEN), op=ALU.mult), reads=["exg", "den"], writes=["exg"])
                for t4 in range(0, NT, 4):
                    n4 = min(4, NT - t4)
                    b = (t4 // 4) % 2
                    for tt in range(n4):
                        ph.pe(lambda e, b=b, tt=tt, t4=t4: e.transpose(PS[b][0:8, tt * 128:(tt + 1) * 128], EXg[:, t4 + tt, :], IDF), reads=["exg", "CF"], writes=[("ps", b)])
                    ph.act(lambda e, b=b, t4=t4, n4=n4: e.copy(out=GT[:, t4 * 128:(t4 + n4) * 128], in_=PS[b][0:8, 0:n4 * 128]), reads=[("ps", b)], writes=[("GT", t4)])
                ph.emit()

        if not SKIP_L0:
            norm_mod(0, 1)
            for m in range(NPAIRS):
                attention_pair(0, m)
            for g in range(NGROUPS):
                ssd_group(g)
        if STOP_AFTER >= 1 and not SKIP_L0:
            out_proj(0)
            norm_mod(0, 2)
            ffn(0)
        if dbg_d and "XT1" in dbg_d:
            ph = Phase(ctx, "dxt1")
            ph.dma("sp", dbg_d["XT1"].rearrange("p (k t) -> p k t", k=KC), XT[:])
            ph.emit()
        if STOP_AFTER >= 2:
            norm_mod(1, 1)
            for m in range(NPAIRS):
                attention_pair(1, m)
            for hh in range(2 if RUN_RET else 0):
                ret_half(hh)
        if STOP_AFTER >= 3:
            SEL = sb("SEL", [8, 1024], F32)
            LGT = sb("LGT", [128, NT, 8], F32)
            GTT = sb("GTT", [8, T], F32)
            ph = Phase(ctx, "ldsel")
            ph.dma("sp", SEL[:], sel_d[:, :], writes=["SEL"])
            ph.emit()
            out_proj(1)
            norm_mod(1, 2, LG=LGT)
            moe_gate(LGT, GTT)
            dump("GT", GTT[:], (8, T))
        if dbg_d and "XT2" in dbg_d:
            ph = Phase(ctx, "dxt2")
            ph.dma("sp", dbg_d["XT2"].rearrange("p (k t) -> p k t", k=KC), XT[:])
            ph.emit()
        if STOP_AFTER >= 4:
            ffn(1, GT=GTT)
            final_out()
        if dbg_d and "YC" in dbg_d:
            with ExitStack() as des:
                ph = Phase(ctx, "dumpyc")
                yb = des.enter_context(SBT("ycb", [128, T], BF16))
                yf = des.enter_context(SBT("ycf", [128, T], F32))
                for c in range(12):
                    ph.dma("sp", yb[:], YCAT_d[c], writes=["yb"])
                    ph.dve(lambda e: e.tensor_copy(out=yf[:], in_=yb[:]), reads=["yb"], writes=["yf"])
                    ph.dma("sp", dbg_d["YC"][:, c * T:(c + 1) * T], yf[:], reads=["yf"])
                ph.emit()
    return nc


def make_consts():
    c = np.zeros((128, 1024), np.float32)
    c[:, 0:128] = np.eye(128, dtype=np.float32)
    c[:, 128:256] = 1.0
    p = np.arange(128)[:, None]
    i = np.arange(128)[None, :]
    c[:, 256:384] = (p <= i)
    c[:, 384:512] = (p >= i)
    c[:, 512:640] = (p > i)
    c[:, 640:768] = (p < i)
    for f in range(128):
        if f % 64 < 32:
            c[f + 32, 768 + f] = -1.0
        else:
            c[f - 32, 768 + f] = 1.0
    return c


def kernel(**inputs):
    dbg = inputs.pop("_dbg", None)
    inp = {k: np.asarray(v) for k, v in inputs.items()}
    nc = build_program(dbg)
    cst = make_consts()
    nab = na_bias_table(inp["na_rpb"][0])
    swab = swa_bias_table()
    tpos = np.arange(L)
    inv = (10000.0 ** (-np.arange(16, dtype=np.float32) / 16)).astype(np.float32)
    ang = np.concatenate([(tpos // 64).astype(np.float32)[:, None] * inv, (tpos % 64).astype(np.float32)[:, None] * inv], axis=-1)
    fidx = np.arange(128) % 32
    rope_tab = np.stack([np.cos(ang)[:, fidx].T, np.sin(ang)[:, fidx].T], 0).astype(np.float32)
    sel = np.zeros((8, 1024), np.float32)
    for e in range(8):
        sel[e, e * 128:(e + 1) * 128] = 1.0
    in_maps = []
    for b in range(8):
        vecs = np.zeros((256, 128), np.float32)

        def put(nm, arr):
            a = np.ascontiguousarray(arr, dtype=np.float32).reshape(-1, 128)
            vecs[VR[nm]:VR[nm] + a.shape[0]] = a
        put("c", inp["c"][b])
        put("c_ctx", inp["c_ctx"])
        put("ada_b0", inp["ada_b"][0])
        put("ada_b1", inp["ada_b"][1])
        put("g_attn0", inp["norm_attn_g"][0])
        put("g_attn1", inp["norm_attn_g"][1])
        put("g_ffn0", inp["norm_ffn_g"][0])
        put("g_ffn1", inp["norm_ffn_g"][1])
        put("final_g", inp["final_g"])
        put("conv_w", inp["ssm_conv_w"][0].reshape(5, 1536))
        put("conv_b", inp["ssm_conv_b"][0])
        put("ssm_g", inp["ssm_norm_g"][0])
        in_maps.append({
            "x": np.ascontiguousarray(inp["x"][b]),
            "ctx": np.ascontiguousarray(inp["ctx"][b]),
            "vecs": vecs,
            "cst": cst,
            "ada_w": inp["ada_w"],
            "ev_w_in": inp["ev_w_in"][0], "od_w_in": inp["od_w_in"][0], "nab": nab, "swab": swab,
            "sink": inp["swa_sink"],
            "ev_w_out": inp["ev_w_out"][0], "od_w_out": inp["od_w_out"][0],
            "ffn_w1": inp["ffn_w1"][0], "ffn_w3": inp["ffn_w3"][0], "ffn_w2": inp["ffn_w2"][0],
            "sel": sel, "rope": rope_tab, "retld": inp["ret_log_decay"][0], "retg": inp["ret_gn_g"], "retb": inp["ret_gn_b"],
            "router": inp["moe_router"][0],
            "dtba": np.stack([inp["ssm_dt_bias"][0].reshape(32), inp["ssm_a_log"][0].reshape(32)], 0),
            "ssmd": inp["ssm_d"], "ssmg": inp["ssm_norm_g"],
        })
    if STOP_AFTER >= 4:
        for mp in in_maps:
            mp["moe_w1"] = inp["moe_w1"][0]
            mp["moe_w3"] = inp["moe_w3"][0]
            mp["moe_w2"] = inp["moe_w2"][0]
    res = run_bass_kernel_spmd(nc, in_maps, core_ids=list(range(8)))
    if dbg:
        return res
    out = np.stack([r["out"] for r in res.results], axis=0)
    return out
```

```python
import math
from contextlib import ExitStack
import numpy as np
import concourse.bass as bass
import concourse.mybir as mybir
from concourse.bass_utils import run_bass_kernel_spmd

F32 = mybir.dt.float32
BF16 = mybir.dt.bfloat16
AF = mybir.ActivationFunctionType
ALU = mybir.AluOpType
AX = mybir.AxisListType

ENGS = ("pe", "act", "dve", "pool", "sp")
NDMA_SEMS = 24


class Ctx:
    def __init__(self, nc, es):
        self.nc = nc
        self.eng_sem = {e: es.enter_context(nc.semaphore("sem_" + e)) for e in ENGS if e != "sp"}
        self.eng_cnt = {e: 0 for e in self.eng_sem}
        self.dma_sems = [es.enter_context(nc.semaphore("dsem%d" % i)) for i in range(NDMA_SEMS)]
        self.dma_cnt = [0] * NDMA_SEMS
        self.dma_rr = 0
        self.eng_obj = {"pe": nc.tensor, "act": nc.scalar, "dve": nc.vector, "pool": nc.gpsimd, "sp": nc.sync}


class Phase:
    def __init__(self, ctx, name="ph"):
        self.ctx = ctx
        self.name = name
        self.ops = []
        self.state = {}
        self.rr = 0

    def _st(self, key):
        if isinstance(key, tuple):
            nm, sub = key[0], tuple(key[1:])
        else:
            nm, sub = key, ()
        d = self.state.setdefault(nm, {})
        return d, sub

    @staticmethod
    def _overlap(a, b):
        n = min(len(a), len(b))
        return a[:n] == b[:n]

    def op(self, eng, fn, reads=(), writes=(), dma=False, pe_acc=False):
        oid = len(self.ops)
        deps = set()
        for key in reads:
            d, sub = self._st(key)
            for s2, st in d.items():
                if self._overlap(sub, s2) and st["w"] is not None:
                    deps.add(st["w"])
            d.setdefault(sub, {"w": None, "r": []})["r"].append(oid)
        for key in writes:
            d, sub = self._st(key)
            for s2 in list(d.keys()):
                if self._overlap(sub, s2):
                    st = d[s2]
                    if st["w"] is not None:
                        deps.add(st["w"])
                    deps.update(st["r"])
                    if len(s2) > len(sub):
                        del d[s2]
            st = d.setdefault(sub, {"w": None, "r": []})
            st["w"] = oid
            st["r"] = []
        deps.discard(oid)
        o = {"eng": eng, "fn": fn, "deps": deps, "dma": dma, "pe_acc": pe_acc}
        if dma:
            c = self.ctx
            k = c.dma_rr
            c.dma_rr = (c.dma_rr + 1) % NDMA_SEMS
            prev = getattr(self, "_dma_prev", {}).get(k)
            if prev is not None:
                deps.add(prev)
            self.__dict__.setdefault("_dma_prev", {})[k] = oid
            c.dma_cnt[k] += 16
            o["dsem"] = k
            o["dval"] = c.dma_cnt[k]
        self.ops.append(o)
        return oid

    def pe(self, fn, reads=(), writes=(), acc=False):
        return self.op("pe", fn, reads, writes, pe_acc=acc)

    def act(self, fn, reads=(), writes=()):
        return self.op("act", fn, reads, writes)

    def dve(self, fn, reads=(), writes=()):
        return self.op("dve", fn, reads, writes)

    def pool(self, fn, reads=(), writes=()):
        return self.op("pool", fn, reads, writes)

    def any2(self, fn, reads=(), writes=()):
        self.rr += 1
        return self.op("dve" if self.rr % 2 else "pool", fn, reads, writes)

    def dma(self, q, out, in_, reads=(), writes=()):
        return self.op(q, lambda e: e.dma_start(out=out, in_=in_), reads, writes, dma=True)

    def emit(self):
        c = self.ctx
        ops = self.ops
        for o in ops:
            best = {}
            pd = []
            for d in o["deps"]:
                po = ops[d]
                if po["dma"]:
                    pd.append(d)
                    continue
                if po["eng"] == "pe" and o["eng"] == "pe" and not o["dma"]:
                    continue
                if d > best.get(po["eng"], -1):
                    best[po["eng"]] = d
            o["deps"] = set(pd) | set(best.values())
        needed = set()
        for o in ops:
            for d in o["deps"]:
                po = ops[d]
                if po["dma"]:
                    continue
                needed.add(d)
        last_of = {}
        for i, o in enumerate(ops):
            if not o["dma"]:
                last_of[o["eng"]] = i
        for i in last_of.values():
            needed.add(i)
        for i, o in enumerate(ops):
            if o["dma"]:
                continue
            if i in needed:
                c.eng_cnt[o["eng"]] += 1
                o["inc"] = True
            o["cnt"] = c.eng_cnt[o["eng"]] if i in needed else None
        per_eng = {e: [] for e in ENGS}
        waited = {e: {} for e in ENGS}
        for i, o in enumerate(ops):
            w = {}
            for d in o["deps"]:
                po = ops[d]
                if po["dma"]:
                    key = ("d", po["dsem"])
                    val = po["dval"]
                else:
                    if po["eng"] == "pe" and o["eng"] == "pe" and not o["dma"]:
                        continue
                    key = ("e", po["eng"])
                    val = po["cnt"]
                if val > w.get(key, 0):
                    w[key] = val
            wl = []
            for key, val in w.items():
                if waited[o["eng"]].get(key, 0) >= val:
                    continue
                waited[o["eng"]][key] = val
                wl.append((key, val))
            o["waits"] = wl
            per_eng[o["eng"]].append(o)
        fin = []
        for e, i in last_of.items():
            fin.append((("e", e), ops[i]["cnt"]))
        for k in range(NDMA_SEMS):
            if c.dma_cnt[k] > 0:
                fin.append((("d", k), c.dma_cnt[k]))

        def semof(key):
            return c.eng_sem[key[1]] if key[0] == "e" else c.dma_sems[key[1]]

        def run(engname):
            def body(eng):
                for o in per_eng[engname]:
                    for key, val in o["waits"]:
                        eng.wait_ge(semof(key), val)
                    ins = o["fn"](eng)
                    if o["dma"]:
                        ins.then_inc(c.dma_sems[o["dsem"]], 16)
                    elif o.get("inc"):
                        ins.then_inc(c.eng_sem[o["eng"]], 1)
                if engname == "sp":
                    for key, val in fin:
                        eng.wait_ge(semof(key), val)
            return body

        with c.nc.Block() as block:
            block.tensor(run("pe"))
            block.scalar(run("act"))
            block.vector(run("dve"))
            block.gpsimd(run("pool"))
            block.sync(run("sp"))
        self.ops = []
        self.state = {}
        self._dma_prev = {}


D = 1024
L = 2048
LC = 256
T = L + LC
NT = T // 128
KC = D // 128
EPS = 1e-6
BLKS = [(0, 512), (512, 512), (1024, 512), (1536, 512), (2048, 256)]

VR = {}
_r = 0
for _nm, _n in (("c", 8), ("c_ctx", 8), ("ada_b0", 48), ("ada_b1", 48), ("g_attn0", 8), ("g_attn1", 8),
                ("g_ffn0", 8), ("g_ffn1", 8), ("final_g", 8), ("conv_w", 60), ("conv_b", 12), ("ssm_g", 8)):
    VR[_nm] = _r
    _r += _n
NVR = _r

NPAIRS = 4
NGROUPS = 2
ATT_LAG = 2
RPERM = [0, 2, 1, 3]
NEXPERTS = 8
STOP_AFTER = 99
RUN_RET = True
RET_SCAN = True
DBG_NOPOST = False
DBG_NOP2 = False
DBG_SKIP = set()
SKIP_L0 = False


def _na_tiles():
    out = []
    for t in range(16):
        qr = np.arange(t * 128, (t + 1) * 128) // 64
        r0 = np.clip(qr - 4, 0, 24)
        out.append(list(range(int(r0.min()) // 2, (int(r0.max()) + 7) // 2 + 1)))
    return out


NA_KT = _na_tiles()


def na_bias_table(rpb):
    out = np.full((8, 16, 128, 640), -30000.0, np.float32)
    for t in range(16):
        qpos = np.arange(t * 128, (t + 1) * 128)
        qr, qc = qpos // 64, qpos % 64
        r0 = np.clip(qr - 4, 0, 24)
        c0 = np.clip(qc - 8, 0, 48)
        for j, kt in enumerate(NA_KT[t]):
            kpos = np.arange(kt * 128, (kt + 1) * 128)
            kr, kc = kpos // 64, kpos % 64
            ok = ((kr[:, None] >= r0[None, :]) & (kr[:, None] < r0[None, :] + 8)
                  & (kc[:, None] >= c0[None, :]) & (kc[:, None] < c0[None, :] + 16))
            dr = np.clip(kr[:, None] - qr[None, :] + 7, 0, 14)
            dc = np.clip(kc[:, None] - qc[None, :] + 15, 0, 30)
            vals = rpb[:, dr, dc]
            out[:, t, :, j * 128:(j + 1) * 128] = np.where(ok[None], vals, np.float32(-30000.0))
    return out


def swa_bias_table():
    out = np.full((16, 128, 384), -30000.0, np.float32)
    for t in range(16):
        kts = [kt for kt in (t - 1, t, t + 1) if 0 <= kt < 16]
        qpos = np.arange(t * 128, (t + 1) * 128)
        for j, kt in enumerate(kts):
            kpos = np.arange(kt * 128, (kt + 1) * 128)
            ok = np.abs(kpos[:, None] - qpos[None, :]) <= 128
            out[t, :, j * 128:(j + 1) * 128] = np.where(ok, np.float32(0.0), np.float32(-30000.0))
    return out


def build_program(dbg=None):
    nc = bass.Bass("TRN2", target_bir_lowering=False)
    _uid = [0]

    def SBT(name, shape, dtype):
        _uid[0] += 1
        return nc.sbuf_tensor("%s_%d" % (name, _uid[0]), shape, dtype)

    def PST(name, shape, dtype):
        _uid[0] += 1
        return nc.psum_tensor("%s_%d" % (name, _uid[0]), shape, dtype)
    dt = nc.dram_tensor
    x_d = dt("x", [L, D], F32, kind="ExternalInput").ap()
    ctx_d = dt("ctx", [LC, D], F32, kind="ExternalInput").ap()
    vecs_d = dt("vecs", [256, 128], F32, kind="ExternalInput").ap()
    cst_d = dt("cst", [128, 1024], F32, kind="ExternalInput").ap()
    ada_w_d = dt("ada_w", [2, D, 6 * D], F32, kind="ExternalInput").ap()
    out_d = dt("out", [L, D], F32, kind="ExternalOutput").ap()
    ev_w_in_d = dt("ev_w_in", [D, 4128], F32, kind="ExternalInput").ap()
    od_w_in_d = dt("od_w_in", [D, 3840], F32, kind="ExternalInput").ap()
    nab_d = dt("nab", [8, 16, 128, 640], F32, kind="ExternalInput").ap()
    swab_d = dt("swab", [16, 128, 384], F32, kind="ExternalInput").ap()
    sink_d = dt("sink", [1, 8], F32, kind="ExternalInput").ap()
    dtba_d = dt("dtba", [2, 32], F32, kind="ExternalInput").ap()
    ev_w_out_d = dt("ev_w_out", [1536, D], F32, kind="ExternalInput").ap()
    od_w_out_d = dt("od_w_out", [1536, D], F32, kind="ExternalInput").ap()
    ffn_w1_d = dt("ffn_w1", [D, 2816], F32, kind="ExternalInput").ap()
    ffn_w3_d = dt("ffn_w3", [D, 2816], F32, kind="ExternalInput").ap()
    ffn_w2_d = dt("ffn_w2", [2816, D], F32, kind="ExternalInput").ap()
    if STOP_AFTER >= 4:
        moe_w1_t = dt("moe_w1", [8, D, 3584], F32, kind="ExternalInput").ap()
        moe_w3_t = dt("moe_w3", [8, D, 3584], F32, kind="ExternalInput").ap()
        moe_w2_t = dt("moe_w2", [8, 3584, D], F32, kind="ExternalInput").ap()
        moe_w1_d = [moe_w1_t[e] for e in range(8)]
        moe_w3_d = [moe_w3_t[e] for e in range(8)]
        moe_w2_d = [moe_w2_t[e] for e in range(8)]
    sel_d = dt("sel", [8, 1024], F32, kind="ExternalInput").ap()
    rope_d = dt("rope", [2, 128, L], F32, kind="ExternalInput").ap()
    retld_d = dt("retld", [2, 8], F32, kind="ExternalInput").ap()
    retg_d = dt("retg", [1, 1024], F32, kind="ExternalInput").ap()
    retb_d = dt("retb", [1, 1024], F32, kind="ExternalInput").ap()
    router_d = dt("router", [D, 8], F32, kind="ExternalInput").ap()
    ssmd_d = dt("ssmd", [1, 16], F32, kind="ExternalInput").ap()
    ssmg_d = dt("ssmg", [1, 1024], F32, kind="ExternalInput").ap()
    dbg_d = None
    if dbg:
        dbg_d = {k: dt("dbg_" + k, list(shp), F32, kind="ExternalOutput").ap() for k, shp in dbg.items()}

    with ExitStack() as es:
        ctx = Ctx(nc, es)
        sb = lambda name, shape, dtype: es.enter_context(SBT(name, shape, dtype))
        XT = sb("XT", [128, KC, T], F32)
        HT = sb("HT", [128, KC, T], BF16)
        VT = sb("VT", [128, 256], F32)
        CF = sb("CF", [128, 1024], F32)
        CB = sb("CB", [128, 1024], BF16)
        MOD = sb("MOD", [128, 2, 2, 48], F32)
        AB = sb("AB", [128, 2, 2, 6, 8], F32)
        IDF = CF[:, 0:128]
        ONESF = CF[:, 128:256]
        IDB = CB[:, 0:128]
        ONESB = CB[:, 128:256]

        with ExitStack() as ps_es:
            ph = Phase(ctx, "p0")
            PS = [ps_es.enter_context(PST("ps%d" % i, [128, 512], F32)) for i in range(8)]
            XIN = [ps_es.enter_context(SBT("xin%d" % i, [128, D], F32)) for i in range(3)]
            VIN = ps_es.enter_context(SBT("vin", [128, 2, 128], F32))
            SC = ps_es.enter_context(SBT("sc", [128, KC, 2], BF16))
            AW = [ps_es.enter_context(SBT("aw%d" % i, [128, KC, 768], BF16)) for i in range(2)]
            ph.dma("sp", CF[:], cst_d[:, :], writes=["CF"])
            ph.dma("pool", CB[:], cst_d[:, :], writes=["CB"])
            ph.dma("sp", VIN[:], vecs_d.rearrange("(a p) n -> p a n", p=128), writes=["VIN"])
            for a in range(2):
                ph.pe(lambda e, a=a: e.transpose(PS[0][:, a * 128:(a + 1) * 128], VIN[:, a, :], IDF),
                      reads=["CF", "VIN"], writes=[("ps", 0)])
            ph.dve(lambda e: e.tensor_copy(out=VT[:], in_=PS[0][:, 0:256]), reads=[("ps", 0)], writes=["VT"])
            pi = 1
            for t in range(NT):
                xin = XIN[t % 3]
                src = x_d[t * 128:(t + 1) * 128, :] if t < 16 else ctx_d[(t - 16) * 128:(t - 15) * 128, :]
                ph.dma("sp", xin[:], src, writes=[("xin", t % 3)])
                for half in range(2):
                    b = 1 + (pi % 7)
                    pi += 1
                    for kk in range(4):
                        k = half * 4 + kk
                        ph.pe(lambda e, b=b, kk=kk, k=k, xin=xin: e.transpose(
                            PS[b][:, kk * 128:(kk + 1) * 128], xin[:, k * 128:(k + 1) * 128], IDF),
                            reads=["CF", ("xin", t % 3)], writes=[("ps", b)])
                    dst = XT[:, half * 4:half * 4 + 4, t * 128:(t + 1) * 128]
                    srcp = PS[b][:].rearrange("p (a n) -> p a n", a=4)
                    if (t + half) % 2 == 0:
                        ph.dve(lambda e, dst=dst, srcp=srcp: e.tensor_copy(out=dst, in_=srcp),
                               reads=[("ps", b)], writes=[("XT", t)])
                    else:
                        ph.act(lambda e, dst=dst, srcp=srcp: e.copy(out=dst, in_=srcp),
                               reads=[("ps", b)], writes=[("XT", t)])
            for j, nm in enumerate(("c", "c_ctx")):
                ph.act(lambda e, j=j, nm=nm: e.activation(out=SC[:, :, j], in_=VT[:, VR[nm]:VR[nm] + 8], func=AF.Silu),
                       reads=["VT"], writes=["SC"])
            si = 0
            for l in range(2):
                for s in range(8):
                    aw = AW[si % 2]
                    ph.dma("pool", aw[:], ada_w_d[l, :, s * 768:(s + 1) * 768].rearrange("(k p) n -> p k n", p=128),
                           writes=[("aw", si % 2)])
                    for c6 in range(6):
                        cc = s * 6 + c6
                        for k in range(KC):
                            ph.pe(lambda e, l=l, cc=cc, k=k, c6=c6, aw=aw: e.matmul(
                                PS[0][:, l * 96 + cc * 2:l * 96 + cc * 2 + 2], aw[:, k, c6 * 128:(c6 + 1) * 128],
                                SC[:, k, :], start=(k == 0), stop=(k == KC - 1)),
                                reads=[("aw", si % 2), "SC"], writes=[("ps", 0)])
                    si += 1
            for l in range(2):
                for j in range(2):
                    r0 = VR["ada_b%d" % l]
                    ph.dve(lambda e, l=l, j=j, r0=r0: e.tensor_tensor(
                        out=MOD[:, l, j, :], in0=PS[0][:, l * 96:(l + 1) * 96].rearrange("p (c j) -> p c j", j=2)[:, :, j],
                        in1=VT[:, r0:r0 + 48], op=ALU.add), reads=[("ps", 0), "VT"], writes=["MOD"])
                    for (ai, sci, gname) in ((0, 1, "g_attn%d" % l), (3, 4, "g_ffn%d" % l)):
                        g0 = VR[gname]
                        ph.dve(lambda e, l=l, j=j, ai=ai, sci=sci, g0=g0: e.scalar_tensor_tensor(
                            out=AB[:, l, j, ai, :], in0=MOD[:, l, j, sci * 8:(sci + 1) * 8], scalar=1.0,
                            in1=VT[:, g0:g0 + 8], op0=ALU.add, op1=ALU.mult), reads=["MOD", "VT"], writes=["AB"])
                    for (bi, shi) in ((1, 0), (2, 2), (4, 3), (5, 5)):
                        ph.dve(lambda e, l=l, j=j, bi=bi, shi=shi: e.tensor_copy(
                            out=AB[:, l, j, bi, :], in_=MOD[:, l, j, shi * 8:(shi + 1) * 8]), reads=["MOD"], writes=["AB"])
            ph.emit()

        def norm_mod(l, which, LG=None):
            ai, bi = (0, 1) if which == 1 else (3, 4)
            with ExitStack() as pes:
                ph = Phase(ctx, "nm")
                PS = [pes.enter_context(PST("nps%d" % i, [128, 512], F32)) for i in range(4)]
                SQ = [pes.enter_context(SBT("sq%d" % i, [128, 512], BF16)) for i in range(3)]
                RS = [pes.enter_context(SBT("rs%d" % i, [128, 512], F32)) for i in range(2)]
                TMP = [pes.enter_context(SBT("tmp%d" % i, [128, 512], F32)) for i in range(3)]
                if LG is not None:
                    PSR = [pes.enter_context(PST("npr%d" % i, [128, 512], F32)) for i in range(2)]
                    H2FB = [pes.enter_context(SBT("h2f%d" % i, [128, KC, 512], F32)) for i in range(2)]
                    RWF = pes.enter_context(SBT("rwf", [128, KC, 8], F32))
                    ph.dma("sp", RWF[:], router_d.rearrange("(k p) n -> p k n", p=128), writes=["rwf"])
                cnt = {"qi": 0, "ti": 0}

                def nm_a(bi_, t0, tn):
                    pb = bi_ % 4
                    for k in range(KC):
                        qi = cnt["qi"]
                        sq = SQ[qi % 3]
                        ph.act(lambda e, sq=sq, k=k, t0=t0, tn=tn: e.activation(out=sq[:, :tn], in_=XT[:, k, t0:t0 + tn], func=AF.Square),
                               reads=[], writes=[("sq", qi % 3)])
                        ph.pe(lambda e, sq=sq, k=k, pb=pb, tn=tn: e.matmul(PS[pb][:, :tn], ONESB, sq[:, :tn], start=(k == 0), stop=(k == KC - 1)),
                              reads=[("sq", qi % 3)], writes=[("nps", pb)])
                        cnt["qi"] += 1
                    rs = RS[bi_ % 2]
                    ph.dve(lambda e, rs=rs, pb=pb, tn=tn: e.tensor_scalar(out=rs[:, :tn], in0=PS[pb][:, :tn], scalar1=1.0 / D, scalar2=EPS,
                                                                          op0=ALU.mult, op1=ALU.add), reads=[("nps", pb)], writes=[("rs", bi_ % 2)])
                    ph.act(lambda e, rs=rs, tn=tn: e.sqrt(out=rs[:, :tn], in_=rs[:, :tn]),
                           reads=[("rs", bi_ % 2)], writes=[("rs", bi_ % 2)])
                    ph.dve(lambda e, rs=rs, tn=tn: e.reciprocal(out=rs[:, :tn], in_=rs[:, :tn]),
                           reads=[("rs", bi_ % 2)], writes=[("rs", bi_ % 2)])

                def nm_b(bi_, t0, tn):
                    j = 0 if t0 < L else 1
                    rs = RS[bi_ % 2]
                    for k in range(KC):
                        ti = cnt["ti"]
                        tmp = TMP[ti % 3]
                        ph.any2(lambda e, tmp=tmp, k=k, t0=t0, tn=tn, rs=rs: e.tensor_tensor(out=tmp[:, :tn], in0=XT[:, k, t0:t0 + tn], in1=rs[:, :tn], op=ALU.mult),
                                reads=[("rs", bi_ % 2)], writes=[("tmp", ti % 3)])
                        ph.act(lambda e, tmp=tmp, k=k, t0=t0, tn=tn, j=j: e.activation(out=HT[:, k, t0:t0 + tn], in_=tmp[:, :tn], func=AF.Identity,
                                                                                      scale=AB[:, l, j, ai, k:k + 1], bias=AB[:, l, j, bi, k:k + 1]),
                               reads=[("tmp", ti % 3)], writes=[("HT", k, bi_)])
                        if LG is not None:
                            H2F = H2FB[bi_ % 2]
                            ph.act(lambda e, tmp=tmp, k=k, tn=tn, j=j, H2F=H2F: e.activation(out=H2F[:, k, :tn], in_=tmp[:, :tn], func=AF.Identity,
                                                                                         scale=AB[:, l, j, ai, k:k + 1], bias=AB[:, l, j, bi, k:k + 1]),
                                   reads=[("tmp", ti % 3)], writes=[("h2f", bi_ % 2, k)])
                        cnt["ti"] += 1
                    if LG is not None:
                        pr = bi_ % 2
                        for tt in range(tn // 128):
                            for k in range(KC):
                                ph.pe(lambda e, pr=pr, tt=tt, k=k, H2F=H2FB[bi_ % 2]: e.matmul(PSR[pr][:, tt * 8:(tt + 1) * 8], H2F[:, k, tt * 128:(tt + 1) * 128], RWF[:, k, :], start=(k == 0), stop=(k == KC - 1)),
                                      reads=[("h2f", bi_ % 2), "rwf"], writes=[("npr", pr)])
                        ntl = tn // 128
                        ph.dve(lambda e, pr=pr, t0=t0, ntl=ntl: e.tensor_copy(out=LG[:, t0 // 128:t0 // 128 + ntl, :], in_=PSR[pr][:, 0:ntl * 8].rearrange("p (a n) -> p a n", a=ntl)),
                               reads=[("npr", pr)], writes=[("LG", t0)])

                for i in range(len(BLKS) + 1):
                    if i < len(BLKS):
                        nm_a(i, *BLKS[i])
                    if i >= 1:
                        nm_b(i - 1, *BLKS[i - 1])
                ph.emit()

        YCAT_d = nc.dram_tensor("ycat_scr", [12, 128, T], BF16, kind="Internal").ap()

        def dump(name, src_ap, shape2, reads=()):
            if not dbg_d or name not in dbg_d:
                return
            with ExitStack() as des:
                ph = Phase(ctx, "dump")
                f = des.enter_context(SBT("dbgf_" + name, list(shape2), F32))
                ph.dve(lambda e: e.tensor_copy(out=f[:], in_=src_ap), writes=["f"])
                ph.dma("sp", dbg_d[name], f[:], reads=["f"])
                ph.emit()

        def attention_pair(l, m):
            W_d = ev_w_in_d if l == 0 else od_w_in_d
            with ExitStack() as pes:
                ph = Phase(ctx, "att")
                al = lambda name, shape, dtype: pes.enter_context(SBT(name, shape, dtype))
                PS = [pes.enter_context(PST("aps%d" % i, [128, 512], F32)) for i in range(8)]
                psi = [0]

                def nb():
                    psi[0] = (psi[0] + 1) % 8
                    return psi[0]
                WS = al("ws", [128, KC, 384], BF16)
                QT = al("qt", [128, T], BF16)
                KT = al("kt", [128, T], BF16)
                VK = al("vk", [128, NT, 128], BF16)
                OT = al("ot", [128, T], BF16)
                BI = [al("bi%d" % i, [128, 640], F32) for i in range(3)]
                SS = [al("ss%d" % i, [128, 640], F32) for i in range(2)]
                PT = [al("pt%d" % i, [128, 896], BF16) for i in range(4)]
                RD = [al("rd%d" % i, [128, 128], F32) for i in range(2)]
                Wr = W_d.rearrange("(k p) n -> p k n", p=128)
                if l == 0:
                    cols = [(m * 128, 128), (512 + m * 128, 128), (1024 + m * 128, 128)]
                else:
                    kv = m // 2
                    cols = [(m * 128, 128), (512 + kv * 64, 64), (512 + kv * 64, 64), (640 + kv * 64, 64), (640 + kv * 64, 64)]
                off = 0
                for (c0, cn) in cols:
                    ph.dma("pool", WS[:, :, off:off + cn], Wr[:, :, c0:c0 + cn], writes=[("ws", off)])
                    off += cn
                wsr = [("ws", o) for o in (0, 64, 128, 192, 256, 320)]
                nq = T if l == 0 else L
                for (dst, wo, lim, nm) in ((QT, 0, nq, "qt"), (KT, 128, T, "kt")):
                    for (t0, tn) in BLKS:
                        if t0 >= lim:
                            continue
                        b = nb()
                        for k in range(KC):
                            ph.pe(lambda e, b=b, k=k, wo=wo, t0=t0, tn=tn: e.matmul(PS[b][:, :tn], WS[:, k, wo:wo + 128], HT[:, k, t0:t0 + tn],
                                                                                 start=(k == 0), stop=(k == KC - 1)),
                                  reads=wsr + ["HT"], writes=[("ps", b)])
                        ph.act(lambda e, b=b, dst=dst, t0=t0, tn=tn: e.copy(out=dst[:, t0:t0 + tn], in_=PS[b][:, :tn]),
                               reads=[("ps", b)], writes=[(nm, t0)])
                for t4 in range(0, NT, 4):
                    b = nb()
                    n4 = min(4, NT - t4)
                    for tt in range(n4):
                        t = t4 + tt
                        for k in range(KC):
                            ph.pe(lambda e, b=b, k=k, t=t, tt=tt: e.matmul(PS[b][:, tt * 128:(tt + 1) * 128], HT[:, k, t * 128:(t + 1) * 128], WS[:, k, 256:384],
                                                                         start=(k == 0), stop=(k == KC - 1)),
                                  reads=wsr + ["HT"], writes=[("ps", b)])
                    ph.dve(lambda e, b=b, t4=t4, n4=n4: e.tensor_copy(out=VK[:, t4:t4 + n4, :], in_=PS[b][:, :n4 * 128].rearrange("p (a n) -> p a n", a=n4)),
                           reads=[("ps", b)], writes=[("vk", t4)])
                if l == 1:
                    rope(ph, QT, "qt", nb, PS, al)
                    rope(ph, KT, "kt", nb, PS, al)
                    ES = al("es", [128, 8], F32)
                    ph.dma("sp", ES[:], sink_d[0:1, :].broadcast_to([128, 8]), writes=["es"])
                    ph.act(lambda e: e.activation(out=ES[:], in_=ES[:], func=AF.Exp), reads=["es"], writes=["es"])
                qtiles = list(range(NT)) if l == 0 else list(range(16))
                iters = [(e_, t) for e_ in range(2) for t in qtiles]
                info = {}

                def stage_a(it, e_, t):
                    r0 = 64 * e_
                    h = 2 * m + e_
                    if t >= 16:
                        kts, nbias = [16, 17], 0
                    elif l == 0:
                        kts, nbias = NA_KT[t] + [16, 17], len(NA_KT[t])
                    else:
                        kts = [kt for kt in (t - 1, t, t + 1) if 0 <= kt < 16]
                        nbias = len(kts)
                        kts = kts + [16, 17]
                    nk = len(kts)
                    info[it] = (kts, nk)
                    bi = BI[it % 3]
                    ss = SS[it % 2]
                    pt = PT[it % 4]
                    if nbias:
                        src = nab_d[h, t, :, 0:nbias * 128] if l == 0 else swab_d[t, :, 0:nbias * 128]
                        ph.dma("sp", bi[:, 0:nbias * 128], src, writes=[("bi", it % 3)])
                    banks = [nb(), nb()]
                    for j, kt in enumerate(kts):
                        b = banks[j // 4]
                        ph.pe(lambda e, b=b, j=j, kt=kt, t=t, r0=r0: e.matmul(PS[b][:, (j % 4) * 128:(j % 4 + 1) * 128], KT[r0:r0 + 64, kt * 128:(kt + 1) * 128],
                                                                          QT[r0:r0 + 64, t * 128:(t + 1) * 128], start=True, stop=True),
                              reads=["kt", "qt"], writes=[("ps", b)])
                    for bk in range(2):
                        j0, j1 = bk * 4, min(nk, bk * 4 + 4)
                        if j0 >= j1:
                            continue
                        b = banks[bk]
                        jb = min(j1, max(j0, nbias))
                        if jb > j0:
                            ph.dve(lambda e, b=b, j0=j0, jb=jb, ss=ss, bi=bi: e.scalar_tensor_tensor(
                                out=ss[:, j0 * 128:jb * 128], in0=PS[b][:, (j0 % 4) * 128:(j0 % 4) * 128 + (jb - j0) * 128], scalar=0.125,
                                in1=bi[:, j0 * 128:jb * 128], op0=ALU.mult, op1=ALU.add),
                                reads=[("ps", b), ("bi", it % 3)], writes=[("ss", it % 2, bk)])
                            ph.act(lambda e, j0=j0, jb=jb, ss=ss, pt=pt: e.activation(out=pt[:, j0 * 128:jb * 128], in_=ss[:, j0 * 128:jb * 128], func=AF.Exp),
                                   reads=[("ss", it % 2, bk)], writes=[("pt", it % 4)])
                        if j1 > jb:
                            ph.act(lambda e, b=b, jb=jb, j1=j1, pt=pt: e.activation(out=pt[:, jb * 128:j1 * 128], in_=PS[b][:, (jb % 4) * 128:(jb % 4) * 128 + (j1 - jb) * 128],
                                                                                 func=AF.Exp, scale=0.125),
                                   reads=[("ps", b)], writes=[("pt", it % 4)])

                def stage_b(it, e_, t):
                    r0 = 64 * e_
                    h = 2 * m + e_
                    kts, nk = info[it]
                    pt = PT[it % 4]
                    rd = RD[it % 2]
                    po = nb()
                    for j, kt in enumerate(kts):
                        ph.pe(lambda e, po=po, j=j, kt=kt, pt=pt, nk=nk: e.matmul(PS[po][:, 0:128], VK[:, kt, :], pt[:, j * 128:(j + 1) * 128],
                                                                              start=(j == 0), stop=(j == nk - 1)),
                              reads=["vk", ("pt", it % 4)], writes=[("ps", po)])
                    for j, kt in enumerate(kts):
                        ph.pe(lambda e, po=po, j=j, pt=pt, nk=nk: e.matmul(PS[po][:, 128:256], ONESB, pt[:, j * 128:(j + 1) * 128],
                                                                       start=(j == 0), stop=(j == nk - 1)),
                              reads=[("pt", it % 4)], writes=[("ps", po)])
                    if l == 1:
                        ph.dve(lambda e, po=po, rd=rd, r0=r0, h=h: e.tensor_scalar(out=rd[r0:r0 + 64, :], in0=PS[po][r0:r0 + 64, 128:256], scalar1=ES[r0:r0 + 64, h:h + 1],
                                                                                scalar2=None, op0=ALU.add), reads=[("ps", po), "es"], writes=[("rd", it % 2)])
                        ph.dve(lambda e, rd=rd, r0=r0: e.reciprocal(out=rd[r0:r0 + 64, :], in_=rd[r0:r0 + 64, :]), reads=[("rd", it % 2)], writes=[("rd", it % 2)])
                    else:
                        ph.dve(lambda e, po=po, rd=rd, r0=r0: e.reciprocal(out=rd[r0:r0 + 64, :], in_=PS[po][r0:r0 + 64, 128:256]),
                               reads=[("ps", po)], writes=[("rd", it % 2)])
                    ph.dve(lambda e, po=po, rd=rd, r0=r0, t=t: e.tensor_tensor(out=OT[r0:r0 + 64, t * 128:(t + 1) * 128], in0=PS[po][r0:r0 + 64, 0:128],
                                                                           in1=rd[r0:r0 + 64, :], op=ALU.mult),
                           reads=[("ps", po), ("rd", it % 2)], writes=[("ot", e_, t)])

                LAG = ATT_LAG
                for i in range(len(iters) + LAG):
                    if i < len(iters):
                        stage_a(i, *iters[i])
                    if i >= LAG:
                        stage_b(i - LAG, *iters[i - LAG])
                ph.dma("sp", YCAT_d[m, :, 0:nq], OT[:, 0:nq], reads=["ot"])
                ph.emit()
                if l == 0 and m == 0:
                    dump("OT0", OT[:], (128, T))

        MF = CF[:, 256:384]
        MB = CF[:, 384:512]
        SFm = CF[:, 512:640]
        SBm = CF[:, 640:768]
        SST_d = nc.dram_tensor("sst_scr", [2, NT, 128, 512], BF16, kind="Internal").ap()

        def scan_phase(P, groups, XS, BTOK, BT, CT, A4, DT4, const_decay, post_fn, out_tiles, pre_fn=None, H=8):
            HP = H * P
            nyb = HP // 512
            NTA = 1 if const_decay else NT
            ti = (lambda t: 0) if const_decay else (lambda t: t)
            oes = ExitStack()
            ESC = oes.enter_context(SBT("esc", [128, NTA, 2, H], F32))
            with ExitStack() as pes:
                ph = Phase(ctx, "scan")
                al = lambda name, shape, dtype: pes.enter_context(SBT(name, shape, dtype))
                PS = [pes.enter_context(PST("sps%d" % i, [128, 512], F32)) for i in range(8)]
                psi = [0]

                def nb():
                    psi[0] = (psi[0] + 1) % 8
                    return psi[0]
                if pre_fn is not None:
                    pre_fn(ph, al, PS, nb)
                CUMS = al("cums", [128, NTA, 3, 2 * H], F32)
                EW = al("ew", [128, NTA, 2, H], F32)
                ETOT = al("etot", [128, NTA, 2, H], F32)
                S = [al("st%d" % d, [128, 512], F32) for d in range(2)]
                STMP = al("stmp", [128, 512], F32)
                SBF = [al("sbf%d" % i, [128, 512], BF16) for i in range(3)]
                XW = [al("xw%d" % i, [128, HP], BF16) for i in range(2)]
                for t in range(NTA):
                    b = nb()
                    for ci, lm in enumerate((MF, MB, ONESF)):
                        ph.pe(lambda e, b=b, ci=ci, lm=lm, t=t: e.matmul(PS[b][:, ci * 2 * H:(ci + 1) * 2 * H], lm, A4[:, t].rearrange("p d h -> p (d h)"),
                                                                         start=True, stop=True), reads=["A4", "CF"], writes=[("ps", b)])
                    ph.act(lambda e, b=b, t=t: e.copy(out=CUMS[:, t].rearrange("p c n -> p (c n)"), in_=PS[b][:, 0:6 * H]), reads=[("ps", b)], writes=[("cums", t)])
                ph.act(lambda e: e.activation(out=ETOT[:].rearrange("p t d h -> p t (d h)"), in_=CUMS[:, :, 2, :], func=AF.Exp), reads=["cums"], writes=["etot"])
                for d in range(2):
                    ph.act(lambda e, d=d: e.activation(out=ESC[:, :, d, :], in_=CUMS[:, :, d, d * H:(d + 1) * H], func=AF.Exp), reads=["cums"], writes=[("esc", d)])
                    ph.dve(lambda e, d=d: e.tensor_tensor(out=EW[:, :, d, :], in0=CUMS[:, :, 2, d * H:(d + 1) * H], in1=CUMS[:, :, d, d * H:(d + 1) * H], op=ALU.subtract),
                           reads=["cums"], writes=[("ew", d)])
                    ph.act(lambda e, d=d: e.activation(out=EW[:, :, d, :], in_=EW[:, :, d, :], func=AF.Exp), reads=[("ew", d)], writes=[("ew", d)])
                    if DT4 is not None:
                        ph.dve(lambda e, d=d: e.tensor_tensor(out=EW[:, :, d, :], in0=EW[:, :, d, :], in1=DT4[:, :, d, :], op=ALU.mult),
                               reads=[("ew", d), "DT4"], writes=[("ew", d)])
                order = {0: [16, 17] + list(range(16)), 1: [17, 16] + list(range(15, -1, -1))}
                si = 0
                for d in range(2):
                    ph.dve(lambda e, d=d: e.memset(S[d][:], 0.0), writes=[("st", d)])
                for step in range(NT):
                    for d in range(2):
                        t = order[d][step]
                        sbf = SBF[si % 3]
                        xw = XW[si % 2]
                        ph.act(lambda e, sbf=sbf, d=d: e.copy(out=sbf[:], in_=S[d][:]), reads=[("st", d)], writes=[("sbf", si % 3)])
                        ph.dma("sp", SST_d[d, t], sbf[:], reads=[("sbf", si % 3)], writes=[("sst", d, t)])
                        if step < NT - 1:
                            ph.any2(lambda e, xw=xw, t=t, d=d: e.tensor_tensor(out=xw[:].rearrange("p (h q) -> p h q", h=H), in0=XS[:, t].rearrange("p (h q) -> p h q", h=H),
                                                                          in1=EW[:, ti(t), d, :].unsqueeze(2).broadcast_to([128, H, P]), op=ALU.mult),
                                    reads=["XS", ("ew", d)], writes=[("xw", si % 2)])
                            bks = [nb() for _ in range(nyb)]
                            for gi, g in enumerate(groups):
                                pc0 = g["heads"][0] * P
                                pcn = len(g["heads"]) * P
                                ph.pe(lambda e, g=g, pc0=pc0, pcn=pcn, xw=xw, t=t, bks=bks: e.matmul(
                                    PS[bks[pc0 // 512]][:, pc0 % 512:pc0 % 512 + pcn], BTOK[:, t, g["chunk"] * 128:(g["chunk"] + 1) * 128], xw[:, pc0:pc0 + pcn], start=True, stop=True),
                                    reads=["BTOK", ("xw", si % 2)], writes=[("ps", bks[pc0 // 512])])
                            upd = []
                            for g in groups:
                                r0, nr, nh = g["row0"], g["nrows"], len(g["heads"])
                                h0 = g["heads"][0]
                                pc0, pcn, sc0 = h0 * P, nh * P, g["scol0"]
                                if upd and upd[-1][0] == r0 and upd[-1][1] == nr and upd[-1][4] + upd[-1][6] == sc0 and upd[-1][5] + upd[-1][6] == pc0 \
                                        and upd[-1][5] // 512 == (pc0 + pcn - 1) // 512 and upd[-1][2] + upd[-1][3] == h0:
                                    upd[-1][3] += nh
                                    upd[-1][6] += pcn
                                else:
                                    upd.append([r0, nr, h0, nh, sc0, pc0, pcn])
                            for ui, (r0, nr, h0, nh, sc0, pc0, pcn) in enumerate(upd):
                                ph.dve(lambda e, r0=r0, nr=nr, nh=nh, h0=h0, sc0=sc0, pcn=pcn, d=d, t=t: e.tensor_tensor(
                                    out=STMP[r0:r0 + nr, sc0:sc0 + pcn].rearrange("p (h q) -> p h q", h=nh), in0=S[d][r0:r0 + nr, sc0:sc0 + pcn].rearrange("p (h q) -> p h q", h=nh),
                                    in1=ETOT[r0:r0 + nr, ti(t), d, h0:h0 + nh].unsqueeze(2).broadcast_to([nr, nh, P]), op=ALU.mult),
                                    reads=[("st", d), "etot", ("sbf", si % 3)], writes=[("stmp", r0, sc0)])
                                ph.dve(lambda e, r0=r0, nr=nr, sc0=sc0, pc0=pc0, pcn=pcn, d=d, bks=bks: e.tensor_tensor(
                                    out=S[d][r0:r0 + nr, sc0:sc0 + pcn], in0=PS[bks[pc0 // 512]][r0:r0 + nr, pc0 % 512:pc0 % 512 + pcn], in1=STMP[r0:r0 + nr, sc0:sc0 + pcn], op=ALU.add),
                                    reads=[("stmp", r0, sc0), ("ps", bks[pc0 // 512])], writes=[("st", d, r0, sc0)])
                        si += 1
                ph.emit()
            with ExitStack() as pes:
                ph = Phase(ctx, "scan2")
                al = lambda name, shape, dtype: pes.enter_context(SBT(name, shape, dtype))
                PS = [pes.enter_context(PST("tps%d" % i, [128, 512], F32)) for i in range(7)]
                PBT = pes.enter_context(PST("tpb", [128, 1024], BF16))
                psi = [0]

                def nb():
                    psi[0] = (psi[0] + 1) % 7
                    return psi[0]
                ng = len(groups)
                GM = [[al("gm%d_%d" % (tp, d), [128, ng, 128], F32) for d in range(2)] for tp in range(2)]
                RH1 = al("rh", [128, H, 128], F32)
                RHS = [RH1, RH1]
                EXPD = [al("expd%d" % d, [128, H, 128], F32) for d in range(2)]
                XD = [al("xd%d" % i, [128, HP], BF16) for i in range(4)] if DT4 is not None else None
                MP = [al("mp%d" % i, [128, H, 128], BF16) for i in range(4)]
                SW = max(g["scol0"] + len(g["heads"]) * P for g in groups)
                SIN = [al("sin%d" % i, [128, SW], BF16) for i in range(4)]
                YT1 = al("yt", [128, HP], F32)
                YT = [YT1, YT1]
                Y = [al("y%d" % i, [128, HP], F32) for i in range(2)]
                post_state = post_fn("init", ph, al, PS, nb, PBT)
                if dbg_d and "SST" in dbg_d:
                    sf = al("dbgsst", [128, 512], F32)
                    ph.dma("sp", SIN[0][:], SST_d[0, 17], writes=[("sin", 0)])
                    ph.dve(lambda e: e.tensor_copy(out=sf[:], in_=SIN[0][:]), reads=[("sin", 0)], writes=["sf"])
                    ph.dma("sp", dbg_d["SST"], sf[:], reads=["sf"])
                masks = (MF, MB)
                u1 = (SFm, SBm)

                def build_expd(t, d):
                    RH = RHS[d]
                    ph.any2(lambda e, t=t, d=d, RH=RH: e.tensor_tensor(out=RH[:], in0=masks[d].unsqueeze(1).broadcast_to([128, H, 128]),
                                                                       in1=A4[:, t, d, :].unsqueeze(2).broadcast_to([128, H, 128]), op=ALU.mult),
                            reads=["A4", "CF"], writes=["rh"])
                    for hh in range(H // 4):
                        b = nb()
                        ph.pe(lambda e, b=b, hh=hh, d=d, RH=RH: e.matmul(PS[b][:], u1[d], RH[:, hh * 4:(hh + 1) * 4, :].rearrange("p h i -> p (h i)"), start=True, stop=True),
                              reads=["rh", "CF"], writes=[("ps", b)])
                        ph.act(lambda e, b=b, hh=hh, d=d: e.activation(out=EXPD[d][:, hh * 4:(hh + 1) * 4, :].rearrange("p h i -> p (h i)"), in_=PS[b][:], func=AF.Exp),
                               reads=[("ps", b)], writes=[("expd", d, hh)])
                if const_decay:
                    for d in range(2):
                        build_expd(0, d)
                it = 0
                def p2_stage_a(ti_, t):
                    tsl = slice(t * 128, (t + 1) * 128)
                    tp = ti_ % 2
                    gbanks = []
                    for gi, g in enumerate(groups):
                        if not gbanks or groups[gbanks[-1][1]]["row0"] != g["row0"] or gbanks[-1][2] == 4:
                            gbanks.append([nb(), gi, 0])
                        bk, g0, n = gbanks[-1]
                        r0, nr, ch = g["row0"], g["nrows"], g["chunk"]
                        ph.pe(lambda e, bk=bk, n=n, r0=r0, nr=nr, ch=ch, tsl=tsl: e.matmul(PS[bk][:, n * 128:(n + 1) * 128], BT[r0:r0 + nr, ch, tsl], CT[r0:r0 + nr, ch, tsl],
                                                                                       start=True, stop=True), reads=["BT", "CT"], writes=[("ps", bk)])
                        gbanks[-1][2] += 1
                    for d in range(2):
                        for (bk, g0, n) in gbanks:
                            ph.dve(lambda e, d=d, bk=bk, g0=g0, n=n, tp=tp: e.tensor_tensor(out=GM[tp][d][:, g0:g0 + n, :], in0=PS[bk][:, 0:n * 128].rearrange("p (g i) -> p g i", g=n),
                                                                                        in1=masks[d].unsqueeze(1).broadcast_to([128, n, 128]), op=ALU.mult),
                                   reads=[("ps", bk), "CF"], writes=[("gm", tp, d, g0)])
                    for d in range(2):
                        slot = tp * 2 + d
                        mp = MP[slot]
                        sin = SIN[slot]
                        ph.dma("sp", sin[:], SST_d[d, t, :, 0:SW], writes=[("sin", slot)])
                        if not const_decay:
                            build_expd(t, d)
                        gmb = GM[tp][d][:] if ng == H else GM[tp][d][:, 0:1, :].broadcast_to([128, H, 128])
                        if DT4 is not None:
                            xd = XD[slot]
                            ph.any2(lambda e, d=d, t=t, xd=xd: e.tensor_tensor(out=xd[:].rearrange("p (h q) -> p h q", h=H), in0=XS[:, t].rearrange("p (h q) -> p h q", h=H),
                                                                              in1=DT4[:, t, d, :].unsqueeze(2).broadcast_to([128, H, P]), op=ALU.mult),
                                    reads=["XS", "DT4"], writes=[("xd", slot)])
                        ph.any2(lambda e, mp=mp, gmb=gmb, d=d: e.tensor_tensor(out=mp[:], in0=EXPD[d][:], in1=gmb, op=ALU.mult),
                                reads=[("expd", d), ("gm", tp, d)], writes=[("mp", slot)])

                def p2_stage_b(ti_, t):
                    tsl = slice(t * 128, (t + 1) * 128)
                    tp = ti_ % 2
                    for d in range(2):
                        slot = tp * 2 + d
                        mp = MP[slot]
                        sin = SIN[slot]
                        yd = [nb() for _ in range(nyb)]
                        for h in range(H):
                            xrhs = XD[slot][:, h * P:(h + 1) * P] if DT4 is not None else XS[:, t, h * P:(h + 1) * P]
                            ph.pe(lambda e, h=h, mp=mp, yd=yd, xrhs=xrhs: e.matmul(PS[yd[h * P // 512]][:, (h * P) % 512:(h * P) % 512 + P], mp[:, h, :], xrhs, start=True, stop=True),
                                  reads=[("mp", slot), "XS", ("xd", slot)], writes=[("ps", yd[h * P // 512])])
                        ybanks = []
                        for gi, g in enumerate(groups):
                            r0, nr, ch = g["row0"], g["nrows"], g["chunk"]
                            pc0 = g["heads"][0] * P
                            pcn = len(g["heads"]) * P
                            sc0 = g["scol0"]
                            if not ybanks or ybanks[-1][5] != r0 or ybanks[-1][2] + pcn > 512:
                                ybanks.append([nb(), pc0, 0, g["heads"][0], 0, r0])
                            bk, used = ybanks[-1][0], ybanks[-1][2]
                            ph.pe(lambda e, r0=r0, nr=nr, ch=ch, pcn=pcn, sc0=sc0, sin=sin, bk=bk, used=used, tsl=tsl: e.matmul(
                                PS[bk][:, used:used + pcn], CT[r0:r0 + nr, ch, tsl], sin[r0:r0 + nr, sc0:sc0 + pcn], start=True, stop=True),
                                reads=["CT", ("sin", slot)], writes=[("ps", bk)])
                            ybanks[-1][2] += pcn
                            ybanks[-1][4] += len(g["heads"])
                        yacc = Y[0] if d == 0 else Y[1]
                        yt = YT[d]
                        for (bk, c0, cn, h0, nh, _) in ybanks:
                            ph.dve(lambda e, bk=bk, c0=c0, cn=cn, h0=h0, nh=nh, t=t, d=d, yt=yt: e.tensor_tensor(
                                out=yt[:, c0:c0 + cn].rearrange("p (h q) -> p h q", h=nh), in0=PS[bk][:, 0:cn].rearrange("p (h q) -> p h q", h=nh),
                                in1=ESC[:, ti(t), d, h0:h0 + nh].unsqueeze(2).broadcast_to([128, nh, P]), op=ALU.mult),
                                reads=[("ps", bk), ("esc", d)], writes=[("yt", c0)])
                        for q in range(nyb):
                            csl = slice(q * 512, (q + 1) * 512)
                            ph.dve(lambda e, q=q, yd=yd, csl=csl, yacc=yacc, yt=yt: e.tensor_tensor(out=yacc[:, csl], in0=PS[yd[q]][:], in1=yt[:, csl], op=ALU.add),
                                   reads=[("ps", yd[q]), "yt"], writes=[("y", d, q)])
                    if not (dbg_d and "Yall" in dbg_d):
                        ph.pool(lambda e: e.tensor_tensor(out=Y[0][:], in0=Y[0][:], in1=Y[1][:], op=ALU.add), reads=[("y", 0), ("y", 1)], writes=[("y", 0)])
                    if dbg_d and "Yall" in dbg_d and HP == 512:
                        ph.dma("sp", dbg_d["Yall"][:, t * 512:(t + 1) * 512], Y[0][:], reads=[("y", 0)])
                        ph.dma("sp", dbg_d["Y1all"][:, t * 512:(t + 1) * 512], Y[1][:], reads=[("y", 1)])
                    post_fn("tile", ph, al, PS, nb, post_state, t, Y[0])

                otl = list(out_tiles)
                for i in range(len(otl) + 1):
                    if i < len(otl):
                        p2_stage_a(i, otl[i])
                    if i >= 1:
                        p2_stage_b(i - 1, otl[i - 1])
                post_fn("fini", ph, al, PS, nb, post_state)
                ph.emit()
            oes.close()

        def ssd_group(g):
            with ExitStack() as ges:
                gal = lambda name, shape, dtype: ges.enter_context(SBT(name, shape, dtype))
                XS = gal("xs", [128, NT, 512], BF16)
                BTOK = gal("btok", [128, NT, 128], BF16)
                BT = gal("bt", [128, 1, T], BF16)
                CT = gal("ct", [128, 1, T], BF16)
                DT4 = gal("dt4", [128, NT, 2, 8], F32)
                A4 = gal("a4", [128, NT, 2, 8], F32)
                WZ = gal("wz", [128, KC, 512], BF16)
                Wr = ev_w_in_d.rearrange("(k p) n -> p k n", p=128)
                with ExitStack() as pes:
                    ph = Phase(ctx, "ssdproj")
                    al = lambda name, shape, dtype: pes.enter_context(SBT(name, shape, dtype))
                    PS = [pes.enter_context(PST("bps%d" % i, [128, 512], F32)) for i in range(6)]
                    PB = [pes.enter_context(PST("bpb%d" % i, [128, 1024], BF16)) for i in range(2)]
                    psi = [0]

                    def nb():
                        psi[0] = (psi[0] + 1) % 6
                        return psi[0]
                    WS = [al("wsl%d" % i, [128, KC, 128], BF16) for i in range(2)]
                    WDT = al("wdt", [128, KC, 16], BF16)
                    DBA = al("dba", [128, 2, 2, 8], F32)
                    XPAD = al("xpad", [128, 2320], F32)
                    ACC = al("acc", [128, T], F32)
                    XST = [al("xst%d" % i, [128, T], BF16) for i in range(2)]
                    ph.dma("pool", WZ[:], Wr[:, :, 1536 + g * 512:1536 + (g + 1) * 512], writes=["wz"])
                    for d in range(2):
                        ph.dma("pool", WDT[:, :, d * 8:(d + 1) * 8], Wr[:, :, 4096 + d * 16 + g * 8:4096 + d * 16 + g * 8 + 8], writes=[("wdt", d)])
                        for w in range(2):
                            ph.dma("sp", DBA[:, w, d, :], dtba_d[w:w + 1, d * 16 + g * 8:d * 16 + g * 8 + 8].broadcast_to([128, 8]), writes=[("dba", w, d)])
                    ph.pool(lambda e: e.memset(XPAD[:], 0.0), writes=["xpad"])
                    b = nb()
                    for t in range(NT):
                        for k in range(KC):
                            ph.pe(lambda e, b=b, t=t, k=k: e.matmul(PS[b][:, t * 16:(t + 1) * 16], HT[:, k, t * 128:(t + 1) * 128], WDT[:, k, :], start=(k == 0), stop=(k == KC - 1)),
                                  reads=["wdt", "HT"], writes=[("ps", b)])
                    dt3 = DT4[:].rearrange("p t d h -> p t (d h)")
                    ph.dve(lambda e, b=b: e.tensor_tensor(out=dt3, in0=PS[b][:, 0:NT * 16].rearrange("p (t n) -> p t n", t=NT),
                                                          in1=DBA[:, 0].rearrange("p d h -> p (d h)").unsqueeze(1).broadcast_to([128, NT, 16]), op=ALU.add),
                           reads=[("ps", b), "dba"], writes=["DT4"])
                    ph.act(lambda e: e.activation(out=dt3, in_=dt3, func=AF.Exp), reads=["DT4"], writes=["DT4"])
                    ph.act(lambda e: e.activation(out=dt3, in_=dt3, func=AF.Ln, bias=1.0), reads=["DT4"], writes=["DT4"])
                    ph.act(lambda e: e.activation(out=DBA[:, 1], in_=DBA[:, 1], func=AF.Exp), reads=["dba"], writes=["dba"])
                    ph.dve(lambda e: e.scalar_tensor_tensor(out=A4[:].rearrange("p t d h -> p t (d h)"), in0=dt3, scalar=-1.0,
                                                            in1=DBA[:, 1].rearrange("p d h -> p (d h)").unsqueeze(1).broadcast_to([128, NT, 16]), op0=ALU.mult, op1=ALU.mult),
                           reads=["DT4", "dba"], writes=["A4"])
                    chunks = [4 * g + i for i in range(4)] + [8 + g, 10 + g]
                    for ci, c in enumerate(chunks):
                        ws = WS[ci % 2]
                        ph.dma("pool", ws[:], Wr[:, :, 2560 + c * 128:2560 + (c + 1) * 128], writes=[("wsl", ci % 2)])
                        for (t0, tn) in BLKS:
                            b = nb()
                            for k in range(KC):
                                ph.pe(lambda e, b=b, k=k, ws=ws, t0=t0, tn=tn: e.matmul(PS[b][:, :tn], ws[:, k, :], HT[:, k, t0:t0 + tn], start=(k == 0), stop=(k == KC - 1)),
                                      reads=[("wsl", ci % 2), "HT"], writes=[("ps", b)])
                            o0 = 2 + t0 if t0 < L else 2054 + (t0 - L)
                            ph.act(lambda e, b=b, o0=o0, tn=tn: e.copy(out=XPAD[:, o0:o0 + tn], in_=PS[b][:, :tn]), reads=[("ps", b)], writes=[("xpad", t0)])
                        eng = "dve"
                        for (o0, a0, n) in ((2, 0, L), (2054, L, LC)):
                            wcol = lambda k, c=c: VT[:, VR["conv_w"] + k * 12 + c:VR["conv_w"] + k * 12 + c + 1]
                            bcol = VT[:, VR["conv_b"] + c:VR["conv_b"] + c + 1]
                            ph.op(eng, lambda e, o0=o0, a0=a0, n=n, wcol=wcol, bcol=bcol: e.tensor_scalar(out=ACC[:, a0:a0 + n], in0=XPAD[:, o0 - 2:o0 - 2 + n], scalar1=wcol(0), scalar2=bcol,
                                                                                                    op0=ALU.mult, op1=ALU.add), reads=["xpad", "VT"], writes=[("acc", a0)])
                            for k in range(1, 5):
                                ph.op(eng, lambda e, o0=o0, a0=a0, n=n, k=k, wcol=wcol: e.scalar_tensor_tensor(out=ACC[:, a0:a0 + n], in0=XPAD[:, o0 - 2 + k:o0 - 2 + k + n], scalar=wcol(k),
                                                                                                      in1=ACC[:, a0:a0 + n], op0=ALU.mult, op1=ALU.add),
                                      reads=["xpad", ("acc", a0)], writes=[("acc", a0)])
                        if ci < 4:
                            dst, dkey = XST[ci % 2][:], ("xst", ci % 2)
                        elif ci == 4:
                            dst, dkey = BT[:, 0, :], "BT"
                        else:
                            dst, dkey = CT[:, 0, :], "CT"
                        ph.act(lambda e, dst=dst: e.activation(out=dst, in_=ACC[:], func=AF.Silu), reads=["acc"], writes=[dkey])
                        if ci <= 4:
                            for t8 in range(0, NT, 8):
                                n8 = min(8, NT - t8)
                                pb = (t8 // 8 + ci) % 2
                                for tt in range(n8):
                                    t = t8 + tt
                                    ph.pe(lambda e, pb=pb, tt=tt, t=t, dst=dst: e.transpose(PB[pb][:, tt * 128:(tt + 1) * 128], dst[:, t * 128:(t + 1) * 128], IDB),
                                          reads=[dkey, "CB"], writes=[("pb", pb)])
                                if ci < 4:
                                    o = XS[:, t8:t8 + n8, ci * 128:(ci + 1) * 128]
                                    okey = ("XS", ci, t8)
                                else:
                                    o = BTOK[:, t8:t8 + n8, :]
                                    okey = ("BTOK", t8)
                                ph.act(lambda e, pb=pb, n8=n8, o=o: e.copy(out=o, in_=PB[pb][:, 0:n8 * 128].rearrange("p (a n) -> p a n", a=n8)),
                                       reads=[("pb", pb)], writes=[okey])
                    ph.emit()
                if g == 0:
                    dump("XS0", XS[:].rearrange("p t n -> p (t n)"), (128, NT * 512))
                    dump("A40", A4[:].rearrange("p t d h -> p (t d h)"), (128, NT * 16))
                    dump("DT40", DT4[:].rearrange("p t d h -> p (t d h)"), (128, NT * 16))
                    dump("CT0", CT[:, 0, :], (128, T))
                    dump("BTOK0", BTOK[:].rearrange("p t n -> p (t n)"), (128, NT * 128))

                def post(stage, ph, al, PS, nb, st=None, t=None, Yt=None):
                    if stage == "init":
                        pbt = st
                        st = {}
                        st["dsk"] = al("dsk", [128, 8], F32)
                        st["gng"] = al("gng", [128, 512], F32)
                        st["sz"] = al("sz", [128, 512], F32)
                        st["yz"] = al("yz", [128, 512], F32)
                        st["sq"] = st["sz"]
                        st["ssq"] = al("ssq", [128, 4], F32)
                        st["yn"] = al("yn", [128, 512], BF16)
                        st["stg"] = [al("stg%d" % i, [128, 4, 128], BF16) for i in range(2)]
                        st["pb"] = pbt
                        ph.dma("sp", st["dsk"][:], ssmd_d[0:1, g * 8:(g + 1) * 8].broadcast_to([128, 8]), writes=["dsk"])
                        ph.dma("sp", st["gng"][:], ssmg_d[0:1, g * 512:(g + 1) * 512].broadcast_to([128, 512]), writes=["gng"])
                        return st
                    if stage == "fini":
                        return
                    dsk, gng, sz, yz, sq, ssq, yn = st["dsk"], st["gng"], st["sz"], st["yz"], st["sq"], st["ssq"], st["yn"]
                    b = nb()
                    for k in range(KC):
                        ph.pe(lambda e, b=b, k=k: e.matmul(PS[b][:], HT[:, k, t * 128:(t + 1) * 128], WZ[:, k, :], start=(k == 0), stop=(k == KC - 1)),
                              reads=["HT", "wz"], writes=[("ps", b)])
                    ph.act(lambda e, b=b: e.activation(out=sz[:], in_=PS[b][:], func=AF.Silu), reads=[("ps", b)], writes=["sz"])
                    ph.dve(lambda e: e.tensor_tensor(out=yz[:].rearrange("p (h q) -> p h q", h=8), in0=XS[:, t].rearrange("p (h q) -> p h q", h=8),
                                                     in1=dsk[:].unsqueeze(2).broadcast_to([128, 8, 64]), op=ALU.mult), reads=["XS", "dsk"], writes=["yz"])
                    ph.dve(lambda e: e.tensor_tensor(out=yz[:], in0=yz[:], in1=Yt[:], op=ALU.add), reads=["yz", ("y", 0)], writes=["yz"])
                    ph.dve(lambda e: e.tensor_tensor(out=yz[:], in0=yz[:], in1=sz[:], op=ALU.mult), reads=["yz", "sz"], writes=["yz"])
                    ph.act(lambda e: e.activation(out=sq[:], in_=yz[:], func=AF.Square, accum_out=ssq[:, 0:1]), reads=["yz"], writes=["sz", "ssq"])
                    ph.dve(lambda e: e.tensor_scalar(out=ssq[:, 1:2], in0=ssq[:, 0:1], scalar1=1.0 / 512, scalar2=EPS, op0=ALU.mult, op1=ALU.add), reads=["ssq"], writes=["ssq"])
                    ph.act(lambda e: e.sqrt(out=ssq[:, 2:3], in_=ssq[:, 1:2]), reads=["ssq"], writes=["ssq"])
                    ph.dve(lambda e: e.reciprocal(out=ssq[:, 3:4], in_=ssq[:, 2:3]), reads=["ssq"], writes=["ssq"])
                    ph.dve(lambda e: e.scalar_tensor_tensor(out=yn[:], in0=yz[:], scalar=ssq[:, 3:4], in1=gng[:], op0=ALU.mult, op1=ALU.mult),
                           reads=["yz", "ssq", "gng"], writes=["yn"])
                    pb = st["pb"]
                    stg = st["stg"][t % 2]
                    for cl in range(4):
                        ph.pe(lambda e, cl=cl: e.transpose(pb[:, cl * 128:(cl + 1) * 128], yn[:, cl * 128:(cl + 1) * 128], IDB), reads=["yn", "CB"], writes=["pbt"])
                    ph.act(lambda e, stg=stg: e.copy(out=stg[:], in_=pb[:, 0:512].rearrange("p (a n) -> p a n", a=4)), reads=["pbt"], writes=[("stg", t % 2)])
                    ph.dma("sp", YCAT_d[4 + 4 * g:8 + 4 * g, :, t * 128:(t + 1) * 128].rearrange("c p t -> p c t"), stg[:], reads=[("stg", t % 2)])

                pes_pb = [None]

                def pre(ph, al, PS, nb):
                    pass
                groups = [dict(chunk=0, row0=0, nrows=128, heads=list(range(8)), scol0=0, sncols=512)]
                scan_phase(64, groups, XS, BTOK, BT, CT, A4, DT4, False, post, list(range(NT)))

        def out_proj(l):
            W_d = ev_w_out_d if l == 0 else od_w_out_d
            with ExitStack() as pes:
                ph = Phase(ctx, "oproj")
                al = lambda name, shape, dtype: pes.enter_context(SBT(name, shape, dtype))
                PS = [pes.enter_context(PST("ops%d" % i, [128, 512], F32)) for i in range(8)]
                WO = al("wo", [128, 12, D], BF16)
                YB = [al("yb%d" % i, [128, 12, 512], BF16) for i in range(2)]
                for c in range(0, 12, 4):
                    ph.dma("pool", WO[:, c:c + 4, :], W_d.rearrange("(c p) n -> p c n", p=128)[:, c:c + 4, :], writes=[("wo", c)])
                pi = 0
                for bi_, (t0, tn) in enumerate(BLKS):
                    if l == 1 and t0 >= L:
                        continue
                    j = 0 if t0 < L else 1
                    yb = YB[bi_ % 2]
                    ph.dma("sp", yb[:, :, :tn], YCAT_d[:, :, t0:t0 + tn].rearrange("c p t -> p c t"), writes=[("yb", bi_ % 2)])
                    for dc in range(KC):
                        b = pi % 8
                        pi += 1
                        for c in range(12):
                            ph.pe(lambda e, b=b, c=c, dc=dc, yb=yb, tn=tn: e.matmul(PS[b][:, :tn], WO[:, c, dc * 128:(dc + 1) * 128], yb[:, c, :tn], start=(c == 0), stop=(c == 11)),
                                  reads=["wo", ("yb", bi_ % 2)], writes=[("ps", b)])
                        ph.dve(lambda e, b=b, dc=dc, t0=t0, tn=tn, j=j: e.scalar_tensor_tensor(out=XT[:, dc, t0:t0 + tn], in0=PS[b][:, :tn], scalar=AB[:, l, j, 2, dc:dc + 1],
                                                                                            in1=XT[:, dc, t0:t0 + tn], op0=ALU.mult, op1=ALU.add),
                               reads=[("ps", b)], writes=[("XT", dc, bi_)])
                ph.emit()

        THIRDS = [[(0, 512), (512, 256)], [(768, 512), (1280, 256)], [(1536, 512), (2048, 256)]]

        def ffn(l, GT=None):
            moe = (l == 1)
            nfc = 28 if moe else 22
            nexp = NEXPERTS if moe else 1
            with ExitStack() as pes:
                ph = Phase(ctx, "ffn")
                al = lambda name, shape, dtype: pes.enter_context(SBT(name, shape, dtype))
                PS = [pes.enter_context(PST("fps%d" % i, [128, 512], F32)) for i in range(8)]
                psi = [0]

                def nb():
                    psi[0] = (psi[0] + 1) % 8
                    return psi[0]
                tgroups = [[(0, 512), (512, 512)], [(1024, 512), (1536, 512)]] if moe else THIRDS
                gmax = 1024 if moe else 768
                fchunks = [list(range(0, 14)), list(range(14, 28))] if moe else [list(range(22))]
                nfl = len(fchunks[0])
                ACTT = al("actt", [128, nfl, gmax], BF16)
                W13 = [al("w13_%d" % i, [128, 2, KC, 128], BF16) for i in range(3)]
                W2S = [al("w2s_%d" % i, [128, nfl, 128], BF16) for i in range(2)]
                SIL = [al("sil%d" % i, [128, 512], F32) for i in range(2)]
                if moe:
                    HG = al("hg", [128, KC, gmax], BF16)
                wi = 0
                w2i = 0
                si = 0
                for th, blks in enumerate(tgroups):
                    tb = blks[0][0]
                    for ex in range(nexp):
                        if moe:
                            w1_d, w3_d, w2_d = moe_w1_d[ex], moe_w3_d[ex], moe_w2_d[ex]
                            for (t0, tn) in blks:
                                b = nb()
                                ph.pe(lambda e, b=b, ex=ex, t0=t0, tn=tn: e.matmul(PS[b][:, :tn], SEL[:, ex * 128:(ex + 1) * 128], GT[:, t0:t0 + tn], start=True, stop=True),
                                      reads=["GT", "SEL"], writes=[("ps", b)])
                                for k in range(KC):
                                    ph.dve(lambda e, b=b, k=k, t0=t0, tn=tn, tb=tb: e.tensor_tensor(out=HG[:, k, t0 - tb:t0 - tb + tn], in0=HT[:, k, t0:t0 + tn], in1=PS[b][:, :tn], op=ALU.mult),
                                           reads=[("ps", b), "HT"], writes=[("hg", k, t0)])
                        else:
                            w1_d, w3_d, w2_d = ffn_w1_d, ffn_w3_d, ffn_w2_d
                        w1r = w1_d.rearrange("(k p) n -> p k n", p=128)
                        w3r = w3_d.rearrange("(k p) n -> p k n", p=128)
                        w2r = w2_d.rearrange("(f p) n -> p f n", p=128)
                        for fcs in fchunks:
                            for fi, fc in enumerate(fcs):
                                w = W13[wi % 3]
                                ph.dma("pool", w[:, 0], w1r[:, :, fc * 128:(fc + 1) * 128], writes=[("w13", wi % 3, 0)])
                                ph.dma("pool", w[:, 1], w3r[:, :, fc * 128:(fc + 1) * 128], writes=[("w13", wi % 3, 1)])
                                for (t0, tn) in blks:
                                    b1, b3 = nb(), nb()
                                    for k in range(KC):
                                        ph.pe(lambda e, b1=b1, k=k, w=w, t0=t0, tn=tn: e.matmul(PS[b1][:, :tn], w[:, 0, k, :], HT[:, k, t0:t0 + tn], start=(k == 0), stop=(k == KC - 1)),
                                              reads=[("w13", wi % 3, 0), "HT"], writes=[("ps", b1)])
                                    for k in range(KC):
                                        rhs = HG[:, k, t0 - tb:t0 - tb + tn] if moe else HT[:, k, t0:t0 + tn]
                                        ph.pe(lambda e, b3=b3, k=k, w=w, rhs=rhs, tn=tn: e.matmul(PS[b3][:, :tn], w[:, 1, k, :], rhs, start=(k == 0), stop=(k == KC - 1)),
                                              reads=[("w13", wi % 3, 1), "HT", "hg"], writes=[("ps", b3)])
                                    sil = SIL[si % 2]
                                    ph.act(lambda e, b1=b1, sil=sil, tn=tn: e.activation(out=sil[:, :tn], in_=PS[b1][:, :tn], func=AF.Silu), reads=[("ps", b1)], writes=[("sil", si % 2)])
                                    ph.dve(lambda e, b3=b3, sil=sil, fi=fi, t0=t0, tn=tn, tb=tb: e.tensor_tensor(out=ACTT[:, fi, t0 - tb:t0 - tb + tn], in0=PS[b3][:, :tn], in1=sil[:, :tn], op=ALU.mult),
                                           reads=[("ps", b3), ("sil", si % 2)], writes=[("actt", fi, t0)])
                                    si += 1
                                wi += 1
                            nf = len(fcs)
                            for dc in range(KC):
                                w2 = W2S[w2i % 2]
                                ph.dma("pool", w2[:, 0:nf, :], w2r[:, fcs[0]:fcs[0] + nf, dc * 128:(dc + 1) * 128], writes=[("w2s", w2i % 2)])
                                for (t0, tn) in blks:
                                    j = 0 if t0 < L else 1
                                    b = nb()
                                    for fi in range(nf):
                                        ph.pe(lambda e, b=b, fi=fi, w2=w2, t0=t0, tn=tn, tb=tb, nf=nf: e.matmul(PS[b][:, :tn], w2[:, fi, :], ACTT[:, fi, t0 - tb:t0 - tb + tn], start=(fi == 0), stop=(fi == nf - 1)),
                                              reads=[("w2s", w2i % 2), "actt"], writes=[("ps", b)])
                                    ph.dve(lambda e, b=b, dc=dc, t0=t0, tn=tn, j=j: e.scalar_tensor_tensor(out=XT[:, dc, t0:t0 + tn], in0=PS[b][:, :tn], scalar=AB[:, l, j, 5, dc:dc + 1],
                                                                                                        in1=XT[:, dc, t0:t0 + tn], op0=ALU.mult, op1=ALU.add),
                                           reads=[("ps", b)], writes=[("XT", dc, t0)])
                                w2i += 1
                ph.emit()

        def final_out():
            with ExitStack() as pes:
                ph = Phase(ctx, "fin")
                al = lambda name, shape, dtype: pes.enter_context(SBT(name, shape, dtype))
                PS = [pes.enter_context(PST("zps%d" % i, [128, 512], F32)) for i in range(8)]
                SQ = [al("fsq%d" % i, [128, 512], BF16) for i in range(3)]
                RS = [al("frs%d" % i, [128, 512], F32) for i in range(2)]
                XN = [al("fxn%d" % i, [128, KC, 512], F32) for i in range(2)]
                OTK = [al("fot%d" % i, [128, D], F32) for i in range(3)]
                qi = 0
                oi = 0
                pi = 0
                g0 = VR["final_g"]
                for bi_, (t0, tn) in enumerate(BLKS[:4]):
                    pb = pi % 8
                    pi += 1
                    for k in range(KC):
                        sq = SQ[qi % 3]
                        ph.act(lambda e, sq=sq, k=k, t0=t0: e.activation(out=sq[:], in_=XT[:, k, t0:t0 + 512], func=AF.Square), reads=["XT"], writes=[("sq", qi % 3)])
                        ph.pe(lambda e, sq=sq, k=k, pb=pb: e.matmul(PS[pb][:], ONESB, sq[:], start=(k == 0), stop=(k == KC - 1)), reads=[("sq", qi % 3)], writes=[("ps", pb)])
                        qi += 1
                    rs = RS[bi_ % 2]
                    xn = XN[bi_ % 2]
                    ph.dve(lambda e, rs=rs, pb=pb: e.tensor_scalar(out=rs[:], in0=PS[pb][:], scalar1=1.0 / D, scalar2=EPS, op0=ALU.mult, op1=ALU.add), reads=[("ps", pb)], writes=[("rs", bi_ % 2)])
                    ph.act(lambda e, rs=rs: e.sqrt(out=rs[:], in_=rs[:]), reads=[("rs", bi_ % 2)], writes=[("rs", bi_ % 2)])
                    ph.dve(lambda e, rs=rs: e.reciprocal(out=rs[:], in_=rs[:]), reads=[("rs", bi_ % 2)], writes=[("rs", bi_ % 2)])
                    for k in range(KC):
                        ph.dve(lambda e, k=k, t0=t0, rs=rs, xn=xn: e.scalar_tensor_tensor(out=xn[:, k, :], in0=XT[:, k, t0:t0 + 512], scalar=VT[:, g0 + k:g0 + k + 1], in1=rs[:], op0=ALU.mult, op1=ALU.mult),
                               reads=["XT", ("rs", bi_ % 2)], writes=[("xn", bi_ % 2, k)])
                    for tt in range(4):
                        otk = OTK[oi % 3]
                        for half in range(2):
                            pb = pi % 8
                            pi += 1
                            for kk in range(4):
                                k = half * 4 + kk
                                ph.pe(lambda e, pb=pb, kk=kk, k=k, tt=tt, xn=xn: e.transpose(PS[pb][:, kk * 128:(kk + 1) * 128], xn[:, k, tt * 128:(tt + 1) * 128], IDF),
                                      reads=[("xn", bi_ % 2), "CF"], writes=[("ps", pb)])
                            if half == 0:
                                ph.act(lambda e, pb=pb, otk=otk: e.copy(out=otk[:, 0:512], in_=PS[pb][:]), reads=[("ps", pb)], writes=[("otk", oi % 3, 0)])
                            else:
                                ph.dve(lambda e, pb=pb, otk=otk: e.tensor_copy(out=otk[:, 512:1024], in_=PS[pb][:]), reads=[("ps", pb)], writes=[("otk", oi % 3, 1)])
                        ph.dma("sp", out_d[t0 + tt * 128:t0 + (tt + 1) * 128, :], otk[:], reads=[("otk", oi % 3)])
                        oi += 1
                ph.emit()

        RM = CB[:, 768:896]

        def rope(ph, X, key, nb, PS, al):
            sid = 0
            store = ph.__dict__.setdefault("_rope_store", {})
            if sid not in store:
                COS = al("cos", [128, L], F32)
                SIN = al("sin", [128, L], F32)
                T1 = [al("rt1_%d" % i, [128, 512], F32) for i in range(2)]
                T2 = [al("rt2_%d" % i, [128, 512], F32) for i in range(2)]
                ph.dma("sp", COS[:], rope_d[0], writes=["cos"])
                ph.dma("sp", SIN[:], rope_d[1], writes=["sin"])
                store[sid] = (COS, SIN, T1, T2, [0])
            COS, SIN, T1, T2, cnt = store[sid]
            for (t0, tn) in BLKS[:4]:
                b = nb()
                i = cnt[0] % 2
                cnt[0] += 1
                ph.pe(lambda e, b=b, t0=t0: e.matmul(PS[b][:], RM, X[:, t0:t0 + 512], start=True, stop=True), reads=[key, "CB"], writes=[("ps", b)])
                ph.dve(lambda e, i=i, t0=t0: e.tensor_tensor(out=T1[i][:], in0=X[:, t0:t0 + 512], in1=COS[:, t0:t0 + 512], op=ALU.mult), reads=[key, "cos"], writes=[("rt1", i)])
                ph.dve(lambda e, i=i, b=b, t0=t0: e.tensor_tensor(out=T2[i][:], in0=PS[b][:], in1=SIN[:, t0:t0 + 512], op=ALU.mult), reads=[("ps", b), "sin"], writes=[("rt2", i)])
                ph.pool(lambda e, i=i, t0=t0: e.tensor_tensor(out=X[:, t0:t0 + 512], in0=T1[i][:], in1=T2[i][:], op=ALU.add), reads=[("rt1", i), ("rt2", i)], writes=[(key, "r", t0) if isinstance(key, str) else key])

        def ret_half(hh):
            Wr = od_w_in_d.rearrange("(k p) n -> p k n", p=128)
            with ExitStack() as ges:
                gal = lambda name, shape, dtype: ges.enter_context(SBT(name, shape, dtype))
                XS = gal("rxs", [128, NT, 512], BF16)
                BTOK = gal("rbtok", [128, NT, 256], BF16)
                BT = gal("rbt", [128, 2, T], BF16)
                CT = gal("rct", [128, 2, T], BF16)
                A4 = gal("ra4", [128, 1, 2, 4], F32)
                WG = gal("rwg", [128, KC, 512], BF16)
                with ExitStack() as pes:
                    ph = Phase(ctx, "retproj")
                    al = lambda name, shape, dtype: pes.enter_context(SBT(name, shape, dtype))
                    PS = [pes.enter_context(PST("rps%d" % i, [128, 512], F32)) for i in range(6)]
                    PB = [pes.enter_context(PST("rpb%d" % i, [128, 1024], BF16)) for i in range(2)]
                    psi = [0]

                    def nb():
                        psi[0] = (psi[0] + 1) % 6
                        return psi[0]
                    WS = [al("rws%d" % i, [128, KC, 128], BF16) for i in range(2)]
                    WV = al("rwv", [128, KC, 512], BF16)
                    for hs in range(4):
                        hl = RPERM[hs]
                        ph.dma("pool", WG[:, :, hs * 128:(hs + 1) * 128], Wr[:, :, 2816 + (4 * hh + hl) * 128:2816 + (4 * hh + hl + 1) * 128], writes=[("wg", hs)])
                        ph.dma("pool", WV[:, :, hs * 128:(hs + 1) * 128], Wr[:, :, 1792 + (4 * hh + hl) * 128:1792 + (4 * hh + hl + 1) * 128], writes=[("wv", hs)])
                        for d in range(2):
                            ph.dma("sp", A4[:, 0, d, hs:hs + 1], retld_d[d:d + 1, 4 * hh + hl:4 * hh + hl + 1].broadcast_to([128, 1]), writes=[("a4", d, hs)])
                    a4f = A4[:].rearrange("p a d h -> p (a d h)")
                    ph.act(lambda e: e.activation(out=a4f, in_=a4f, func=AF.Exp), reads=["a4"], writes=["a4"])
                    ph.act(lambda e: e.activation(out=a4f, in_=a4f, func=AF.Ln, scale=-1.0, bias=1.0), reads=["a4"], writes=["a4"])
                    for t in range(NT):
                        b = nb()
                        for k in range(KC):
                            ph.pe(lambda e, b=b, k=k, t=t: e.matmul(PS[b][:], HT[:, k, t * 128:(t + 1) * 128], WV[:, k, :], start=(k == 0), stop=(k == KC - 1)),
                                  reads=["HT", "wv"], writes=[("ps", b)])
                        if t % 2 == 0:
                            ph.act(lambda e, b=b, t=t: e.copy(out=XS[:, t, :], in_=PS[b][:]), reads=[("ps", b)], writes=[("XS", t)])
                        else:
                            ph.dve(lambda e, b=b, t=t: e.tensor_copy(out=XS[:, t, :], in_=PS[b][:]), reads=[("ps", b)], writes=[("XS", t)])
                    wi = 0
                    for (dst, c0, scale, nm) in ((CT, 768, 1.0, "CT"), (BT, 1280, 0.125, "BT")):
                        for c in range(2):
                            ws = WS[wi % 2]
                            ph.dma("pool", ws[:], Wr[:, :, c0 + (2 * hh + c) * 128:c0 + (2 * hh + c + 1) * 128], writes=[("rws", wi % 2)])
                            for (t0, tn) in BLKS:
                                b = nb()
                                for k in range(KC):
                                    ph.pe(lambda e, b=b, k=k, ws=ws, t0=t0, tn=tn: e.matmul(PS[b][:, :tn], ws[:, k, :], HT[:, k, t0:t0 + tn], start=(k == 0), stop=(k == KC - 1)),
                                          reads=[("rws", wi % 2), "HT"], writes=[("ps", b)])
                                ph.act(lambda e, b=b, dst=dst, c=c, t0=t0, tn=tn, scale=scale: e.activation(out=dst[:, c, t0:t0 + tn], in_=PS[b][:, :tn], func=AF.Copy, scale=scale),
                                       reads=[("ps", b)], writes=[(nm, c, "p", t0)])
                            rope(ph, dst[:, c, :], (nm, c), nb, PS, al)
                            if nm == "BT":
                                for t8 in range(0, NT, 8):
                                    n8 = min(8, NT - t8)
                                    pb = (t8 // 8 + c) % 2
                                    for tt in range(n8):
                                        t = t8 + tt
                                        ph.pe(lambda e, pb=pb, tt=tt, t=t, c=c: e.transpose(PB[pb][:, tt * 128:(tt + 1) * 128], BT[:, c, t * 128:(t + 1) * 128], IDB),
                                              reads=[("BT", c), "CB"], writes=[("pb", pb)])
                                    ph.act(lambda e, pb=pb, n8=n8, t8=t8, c=c: e.copy(out=BTOK[:, t8:t8 + n8, c * 128:(c + 1) * 128], in_=PB[pb][:, 0:n8 * 128].rearrange("p (a n) -> p a n", a=n8)),
                                           reads=[("pb", pb)], writes=[("BTOK", c, t8)])
                            wi += 1
                    ph.emit()
                if hh == 0:
                    dump("RXS", XS[:].rearrange("p t n -> p (t n)"), (128, NT * 512))
                    dump("RCT", CT[:].rearrange("p c t -> p (c t)"), (128, 2 * T))
                    dump("RBTOK", BTOK[:].rearrange("p t n -> p (t n)"), (128, NT * 256))
                    dump("RA4", A4[:].rearrange("p a d h -> p (a d h)"), (128, 8))

                def post(stage, ph, al, PS, nb, st=None, t=None, Yt=None):
                    if stage == "init":
                        pbt = st
                        st = {"pb": pbt}
                        st["gng"] = al("rgng", [128, 512], F32)
                        st["gnb"] = al("rgnb", [128, 512], F32)
                        st["sg"] = al("rsg", [128, 512], F32)
                        st["yc"] = al("ryc", [128, 512], F32)
                        st["stat"] = al("rstat", [128, 4, 4], F32)
                        st["yn"] = al("ryn", [128, 512], BF16)
                        st["stg"] = [al("rstg%d" % i, [128, 4, 128], BF16) for i in range(2)]
                        for hs in range(4):
                            c0 = (4 * hh + RPERM[hs]) * 128
                            ph.dma("sp", st["gng"][:, hs * 128:(hs + 1) * 128], retg_d[0:1, c0:c0 + 128].broadcast_to([128, 128]), writes=[("gng", hs)])
                            ph.dma("sp", st["gnb"][:, hs * 128:(hs + 1) * 128], retb_d[0:1, c0:c0 + 128].broadcast_to([128, 128]), writes=[("gnb", hs)])
                        return st
                    if stage == "fini":
                        return
                    gng, gnb, sg, yc, stat, yn = st["gng"], st["gnb"], st["sg"], st["yc"], st["stat"], st["yn"]
                    b = nb()
                    for k in range(KC):
                        ph.pe(lambda e, b=b, k=k: e.matmul(PS[b][:], HT[:, k, t * 128:(t + 1) * 128], WG[:, k, :], start=(k == 0), stop=(k == KC - 1)),
                              reads=["HT", "wg"], writes=[("ps", b)])
                    ph.act(lambda e, b=b: e.activation(out=sg[:], in_=PS[b][:], func=AF.Silu), reads=[("ps", b)], writes=["sg"])
                    y3 = Yt[:].rearrange("p (h q) -> p h q", h=4)
                    yc3 = yc[:].rearrange("p (h q) -> p h q", h=4)
                    ph.dve(lambda e: e.reduce_sum(out=stat[:, 0, :], in_=y3, axis=AX.X), reads=[("y", 0)], writes=[("stat", 0)])
                    ph.dve(lambda e: e.tensor_scalar(out=stat[:, 1, :], in0=stat[:, 0, :], scalar1=-1.0 / 128, scalar2=None, op0=ALU.mult), reads=[("stat", 0)], writes=[("stat", 1)])
                    ph.dve(lambda e: e.tensor_tensor(out=yc3, in0=y3, in1=stat[:, 1, :].unsqueeze(2).broadcast_to([128, 4, 128]), op=ALU.add), reads=[("y", 0), ("stat", 1)], writes=["yc"])
                    for hq in range(4):
                        ph.act(lambda e, hq=hq: e.activation(out=yn[:, hq * 128:(hq + 1) * 128], in_=yc[:, hq * 128:(hq + 1) * 128], func=AF.Square, accum_out=stat[:, 2, hq:hq + 1]),
                               reads=["yc"], writes=["yn", ("stat", 2, hq)])
                    ph.dve(lambda e: e.tensor_scalar(out=stat[:, 2, :], in0=stat[:, 2, :], scalar1=1.0 / 128, scalar2=EPS, op0=ALU.mult, op1=ALU.add), reads=[("stat", 2)], writes=[("stat", 2)])
                    ph.act(lambda e: e.sqrt(out=stat[:, 2, :], in_=stat[:, 2, :]), reads=[("stat", 2)], writes=[("stat", 2)])
                    ph.dve(lambda e: e.reciprocal(out=stat[:, 3, :], in_=stat[:, 2, :]), reads=[("stat", 2)], writes=[("stat", 3)])
                    ph.dve(lambda e: e.tensor_tensor(out=yc3, in0=yc3, in1=stat[:, 3, :].unsqueeze(2).broadcast_to([128, 4, 128]), op=ALU.mult), reads=["yc", ("stat", 3)], writes=["yc"])
                    ph.pool(lambda e: e.tensor_tensor(out=yc[:], in0=yc[:], in1=gng[:], op=ALU.mult), reads=["yc", "gng"], writes=["yc"])
                    ph.pool(lambda e: e.tensor_tensor(out=yc[:], in0=yc[:], in1=gnb[:], op=ALU.add), reads=["yc", "gnb"], writes=["yc"])
                    ph.dve(lambda e: e.tensor_tensor(out=yn[:], in0=yc[:], in1=sg[:], op=ALU.mult), reads=["yc", "sg"], writes=["yn"])
                    pb = st["pb"]
                    stg = st["stg"][t % 2]
                    for cl in range(4):
                        ph.pe(lambda e, cl=cl: e.transpose(pb[:, RPERM[cl] * 128:(RPERM[cl] + 1) * 128], yn[:, cl * 128:(cl + 1) * 128], IDB), reads=["yn", "CB"], writes=["pbt"])
                    ph.act(lambda e, stg=stg: e.copy(out=stg[:], in_=pb[:, 0:512].rearrange("p (a n) -> p a n", a=4)), reads=["pbt"], writes=[("stg", t % 2)])
                    ph.dma("sp", YCAT_d[4 + 4 * hh:8 + 4 * hh, :, t * 128:(t + 1) * 128].rearrange("c p t -> p c t"), stg[:], reads=[("stg", t % 2)])

                groups = [dict(chunk=hs % 2, row0=(hs // 2) * 64, nrows=64, heads=[hs], scol0=(hs % 2) * 128, sncols=128) for hs in range(4)]
                if RET_SCAN:
                    scan_phase(128, groups, XS, BTOK, BT, CT, A4, None, True, post, list(range(16)), H=4)

        def moe_gate(LG, GT):
            with ExitStack() as pes:
                ph = Phase(ctx, "gate")
                al = lambda name, shape, dtype: pes.enter_context(SBT(name, shape, dtype))
                PS = [pes.enter_context(PST("gps%d" % i, [128, 512], F32)) for i in range(2)]
                M1 = al("m1", [128, NT], F32)
                M2 = al("m2", [128, NT], F32)
                EQ = al("eq", [128, NT, 8], F32)
                L2 = al("l2", [128, NT, 8], F32)
                EXg = al("exg", [128, NT, 8], F32)
                DEN = al("den", [128, NT], F32)
                bc = lambda a: a[:].unsqueeze(2).broadcast_to([128, NT, 8])
                ph.dve(lambda e: e.reduce_max(out=M1[:], in_=LG[:], axis=AX.X), reads=["LG"], writes=["m1"])
                ph.dve(lambda e: e.tensor_tensor(out=EQ[:], in0=LG[:], in1=bc(M1), op=ALU.is_equal), reads=["LG", "m1"], writes=["eq"])
                ph.dve(lambda e: e.scalar_tensor_tensor(out=L2[:], in0=EQ[:], scalar=-1e30, in1=LG[:], op0=ALU.mult, op1=ALU.add), reads=["eq", "LG"], writes=["l2"])
                ph.dve(lambda e: e.reduce_max(out=M2[:], in_=L2[:], axis=AX.X), reads=["l2"], writes=["m2"])
                ph.dve(lambda e: e.tensor_tensor(out=EQ[:], in0=LG[:], in1=bc(M2), op=ALU.is_ge), reads=["LG", "m2"], writes=["eq"])
                ph.dve(lambda e: e.tensor_tensor(out=L2[:], in0=LG[:], in1=bc(M1), op=ALU.subtract), reads=["LG", "m1"], writes=["l2"])
                ph.act(lambda e: e.activation(out=EXg[:], in_=L2[:], func=AF.Exp), reads=["l2"], writes=["exg"])
                ph.dve(lambda e: e.tensor_tensor(out=EXg[:], in0=EXg[:], in1=EQ[:], op=ALU.mult), reads=["exg", "eq"], writes=["exg"])
                ph.dve(lambda e: e.reduce_sum(out=DEN[:], in_=EXg[:], axis=AX.X), reads=["exg"], writes=["den"])
                ph.dve(lambda e: e.reciprocal(out=DEN[:], in_=DEN[:]), reads=["den"], writes=["den"])
                ph.dve(lambda e: e.tensor_tensor(out=EXg[:], in0=EXg[:], in1=bc(DEN), op=ALU.mult), reads=["exg", "den"], writes=["exg"])
                for t4 in range(0, NT, 4):
                    n4 = min(4, NT - t4)
                    b = (t4 // 4) % 2
                    for tt in range(n4):
                        ph.pe(lambda e, b=b, tt=tt, t4=t4: e.transpose(PS[b][0:8, tt * 128:(tt + 1) * 128], EXg[:, t4 + tt, :], IDF), reads=["exg", "CF"], writes=[("ps", b)])
                    ph.act(lambda e, b=b, t4=t4, n4=n4: e.copy(out=GT[:, t4 * 128:(t4 + n4) * 128], in_=PS[b][0:8, 0:n4 * 128]), reads=[("ps", b)], writes=[("GT", t4)])
                ph.emit()

        if not SKIP_L0:
            norm_mod(0, 1)
            for m in range(NPAIRS):
                attention_pair(0, m)
            for g in range(NGROUPS):
                ssd_group(g)
        if STOP_AFTER >= 1 and not SKIP_L0:
            out_proj(0)
            norm_mod(0, 2)
            ffn(0)
        if dbg_d and "XT1" in dbg_d:
            ph = Phase(ctx, "dxt1")
            ph.dma("sp", dbg_d["XT1"].rearrange("p (k t) -> p k t", k=KC), XT[:])
            ph.emit()
        if STOP_AFTER >= 2:
            norm_mod(1, 1)
            for m in range(NPAIRS):
                attention_pair(1, m)
            for hh in range(2 if RUN_RET else 0):
                ret_half(hh)
        if STOP_AFTER >= 3:
            SEL = sb("SEL", [8, 1024], F32)
            LGT = sb("LGT", [128, NT, 8], F32)
            GTT = sb("GTT", [8, T], F32)
            ph = Phase(ctx, "ldsel")
            ph.dma("sp", SEL[:], sel_d[:, :], writes=["SEL"])
            ph.emit()
            out_proj(1)
            norm_mod(1, 2, LG=LGT)
            moe_gate(LGT, GTT)
            dump("GT", GTT[:], (8, T))
        if dbg_d and "XT2" in dbg_d:
            ph = Phase(ctx, "dxt2")
            ph.dma("sp", dbg_d["XT2"].rearrange("p (k t) -> p k t", k=KC), XT[:])
            ph.emit()
        if STOP_AFTER >= 4:
            ffn(1, GT=GTT)
            final_out()
        if dbg_d and "YC" in dbg_d:
            with ExitStack() as des:
                ph = Phase(ctx, "dumpyc")
                yb = des.enter_context(SBT("ycb", [128, T], BF16))
                yf = des.enter_context(SBT("ycf", [128, T], F32))
                for c in range(12):
                    ph.dma("sp", yb[:], YCAT_d[c], writes=["yb"])
                    ph.dve(lambda e: e.tensor_copy(out=yf[:], in_=yb[:]), reads=["yb"], writes=["yf"])
                    ph.dma("sp", dbg_d["YC"][:, c * T:(c + 1) * T], yf[:], reads=["yf"])
                ph.emit()
    return nc


def make_consts():
    c = np.zeros((128, 1024), np.float32)
    c[:, 0:128] = np.eye(128, dtype=np.float32)
    c[:, 128:256] = 1.0
    p = np.arange(128)[:, None]
    i = np.arange(128)[None, :]
    c[:, 256:384] = (p <= i)
    c[:, 384:512] = (p >= i)
    c[:, 512:640] = (p > i)
    c[:, 640:768] = (p < i)
    for f in range(128):
        if f % 64 < 32:
            c[f + 32, 768 + f] = -1.0
        else:
            c[f - 32, 768 + f] = 1.0
    return c


def kernel(**inputs):
    dbg = inputs.pop("_dbg", None)
    inp = {k: np.asarray(v) for k, v in inputs.items()}
    nc = build_program(dbg)
    cst = make_consts()
    nab = na_bias_table(inp["na_rpb"][0])
    swab = swa_bias_table()
    tpos = np.arange(L)
    inv = (10000.0 ** (-np.arange(16, dtype=np.float32) / 16)).astype(np.float32)
    ang = np.concatenate([(tpos // 64).astype(np.float32)[:, None] * inv, (tpos % 64).astype(np.float32)[:, None] * inv], axis=-1)
    fidx = np.arange(128) % 32
    rope_tab = np.stack([np.cos(ang)[:, fidx].T, np.sin(ang)[:, fidx].T], 0).astype(np.float32)
    sel = np.zeros((8, 1024), np.float32)
    for e in range(8):
        sel[e, e * 128:(e + 1) * 128] = 1.0
    in_maps = []
    for b in range(8):
        vecs = np.zeros((256, 128), np.float32)

        def put(nm, arr):
            a = np.ascontiguousarray(arr, dtype=np.float32).reshape(-1, 128)
            vecs[VR[nm]:VR[nm] + a.shape[0]] = a
        put("c", inp["c"][b])
        put("c_ctx", inp["c_ctx"])
        put("ada_b0", inp["ada_b"][0])
        put("ada_b1", inp["ada_b"][1])
        put("g_attn0", inp["norm_attn_g"][0])
        put("g_attn1", inp["norm_attn_g"][1])
        put("g_ffn0", inp["norm_ffn_g"][0])
        put("g_ffn1", inp["norm_ffn_g"][1])
        put("final_g", inp["final_g"])
        put("conv_w", inp["ssm_conv_w"][0].reshape(5, 1536))
        put("conv_b", inp["ssm_conv_b"][0])
        put("ssm_g", inp["ssm_norm_g"][0])
        in_maps.append({
            "x": np.ascontiguousarray(inp["x"][b]),
            "ctx": np.ascontiguousarray(inp["ctx"][b]),
            "vecs": vecs,
            "cst": cst,
            "ada_w": inp["ada_w"],
            "ev_w_in": inp["ev_w_in"][0], "od_w_in": inp["od_w_in"][0], "nab": nab, "swab": swab,
            "sink": inp["swa_sink"],
            "ev_w_out": inp["ev_w_out"][0], "od_w_out": inp["od_w_out"][0],
            "ffn_w1": inp["ffn_w1"][0], "ffn_w3": inp["ffn_w3"][0], "ffn_w2": inp["ffn_w2"][0],
            "sel": sel, "rope": rope_tab, "retld": inp["ret_log_decay"][0], "retg": inp["ret_gn_g"], "retb": inp["ret_gn_b"],
            "router": inp["moe_router"][0],
            "dtba": np.stack([inp["ssm_dt_bias"][0].reshape(32), inp["ssm_a_log"][0].reshape(32)], 0),
            "ssmd": inp["ssm_d"], "ssmg": inp["ssm_norm_g"],
        })
    if STOP_AFTER >= 4:
        for mp in in_maps:
            mp["moe_w1"] = inp["moe_w1"][0]
            mp["moe_w3"] = inp["moe_w3"][0]
            mp["moe_w2"] = inp["moe_w2"][0]
    res = run_bass_kernel_spmd(nc, in_maps, core_ids=list(range(8)))
    if dbg:
        return res
    out = np.stack([r["out"] for r in res.results], axis=0)
    return out
```
